# Optimizing a Trainium2 kernel written in Bass

```python
import jax
import jax.numpy as jnp
from jax import lax
import numpy as np

D_MODEL = 1024
BATCH = 4
SEQ = 4096
DEPTH = 4

N_GROUPS = 4
GROUP = D_MODEL // N_GROUPS
CHUNK = 64

HGRN_HEADS = 4
GLA_HEADS = 4
GLA_KEY = GROUP // (2 * GLA_HEADS)
GLA_GATE_RANK = 16
GLA_GATE_NORM = 16.0
RWKV_HEADS = 4
RWKV_HEAD = GROUP // RWKV_HEADS
RWKV_DECAY_RANK = 64
RWKV_A_RANK = 64
RWKV_V_RANK = 32
RWKV_GATE_RANK = 160
RWKV_LNX_EPS = 64e-5
RET_HEADS = 4
RET_KEY = GROUP // RET_HEADS
ROPE_BASE = 10000.0

HGRN_COLS = (GROUP, GROUP, GROUP, GROUP)
GLA_COLS = (GLA_HEADS * GLA_KEY, GLA_HEADS * GLA_KEY, GROUP, GLA_GATE_RANK, GROUP)
RWKV_COLS = (GROUP, GROUP, GROUP, RWKV_DECAY_RANK, RWKV_A_RANK, RWKV_GATE_RANK)
RET_COLS = (GROUP, GROUP, GROUP, GROUP)
MIXER_COLS = (sum(HGRN_COLS), sum(GLA_COLS), sum(RWKV_COLS), sum(RET_COLS))
IN_COLS = sum(MIXER_COLS)

FFN_DENSE = 2816
N_EXPERTS = 8
TOP_K = 2
FFN_EXPERT = 1408
N_DENSE = (DEPTH + 1) // 2
N_MOE = DEPTH // 2

DN_ALPHA = (2.0 * DEPTH) ** 0.25
DN_BETA = (8.0 * DEPTH) ** -0.25
LN_EPS = 1e-5
F32 = jnp.float32

kernel_name = 'hybrid_hgrn2_gla_rwkv7_retnet_moe'


def _split(p, sizes):
    return jnp.split(p, np.cumsum(sizes)[:-1].tolist(), axis=-1)


def _heads(t, n):
    return t.reshape(t.shape[:-1] + (n, t.shape[-1] // n))


def _merge(t):
    return t.reshape(t.shape[:-2] + (t.shape[-2] * t.shape[-1],))


def _layer_norm(x, g, b):
    x = x.astype(F32)
    mu = jnp.mean(x, -1, keepdims=True)
    var = jnp.mean(jnp.square(x - mu), -1, keepdims=True)
    return (x - mu) * lax.rsqrt(var + LN_EPS) * g + b


def _rms_norm(x, g, eps=1e-6):
    return x * lax.rsqrt(jnp.mean(jnp.square(x), -1, keepdims=True) + eps) * g


def _group_norm(x, eps):
    mu = jnp.mean(x, -1, keepdims=True)
    var = jnp.mean(jnp.square(x - mu), -1, keepdims=True)
    return (x - mu) * lax.rsqrt(var + eps)


def _to_chunks(t):
    b, s, h, d = t.shape
    return t.reshape(b, s // CHUNK, CHUNK, h, d).transpose(1, 0, 3, 2, 4)


def _from_chunks(t):
    n, b, h, c, d = t.shape
    return t.transpose(1, 0, 3, 2, 4).reshape(b, n * c, h, d)


def _chunked_gated_linear(q, k, v, log_f):
    b, _, h, kd = q.shape
    vd = v.shape[-1]
    qc, kc, vc, gc = (_to_chunks(t.astype(F32)) for t in (q, k, v, log_f))
    causal = jnp.tril(jnp.ones((CHUNK, CHUNK), dtype=bool))

    def step(state, inp):
        qi, ki, vi, gi = inp
        cum = jnp.cumsum(gi, axis=2)
        diff = cum[:, :, :, None, :] - cum[:, :, None, :, :]
        decay = jnp.exp(jnp.where(causal[:, :, None], diff, -jnp.inf))
        scores = jnp.einsum('bhik,bhjk,bhijk->bhij', qi, ki, decay)
        out = (jnp.einsum('bhij,bhjv->bhiv', scores, vi)
               + jnp.einsum('bhik,bhkv->bhiv', qi * jnp.exp(cum), state))
        cum_end = cum[:, :, -1:, :]
        state = (jnp.exp(cum_end[:, :, 0, :])[..., None] * state
                 + jnp.einsum('bhjk,bhjv->bhkv', ki * jnp.exp(cum_end - cum), vi))
        return state, out

    _, o = lax.scan(step, jnp.zeros((b, h, kd, vd), F32), (qc, kc, vc, gc))
    return _from_chunks(o)


def _retention_chunkwise(q, k, v, log_gamma):
    b, _, h, kd = q.shape
    vd = v.shape[-1]
    qc, kc, vc = (_to_chunks(t.astype(F32)) for t in (q, k, v))
    idx = jnp.arange(CHUNK, dtype=F32)
    rel = idx[:, None] - idx[None, :]
    intra = jnp.exp(jnp.where(rel >= 0, rel * log_gamma[:, None, None], -jnp.inf))
    q_dec = jnp.exp((idx + 1.0)[None, :] * log_gamma[:, None])
    k_dec = jnp.exp((CHUNK - 1.0 - idx)[None, :] * log_gamma[:, None])
    c_dec = jnp.exp(CHUNK * log_gamma)

    def step(state, inp):
        qi, ki, vi = inp
        scores = jnp.einsum('bhik,bhjk->bhij', qi, ki) * intra
        out = (jnp.einsum('bhij,bhjv->bhiv', scores, vi)
               + jnp.einsum('bhik,bhkv->bhiv', qi * q_dec[..., None], state))
        state = (c_dec[:, None, None] * state
                 + jnp.einsum('bhjk,bhjv->bhkv', ki * k_dec[..., None], vi))
        return state, out

    _, o = lax.scan(step, jnp.zeros((b, h, kd, vd), F32), (qc, kc, vc))
    return _from_chunks(o)


def _rotary(t, positions):
    half = t.shape[-1] // 2
    inv_freq = ROPE_BASE ** (-jnp.arange(half, dtype=F32) / half)
    ang = positions.astype(F32)[:, :, None, None] * inv_freq
    cos, sin = jnp.cos(ang), jnp.sin(ang)
    t1, t2 = t[..., :half], t[..., half:]
    return jnp.concatenate([t1 * cos - t2 * sin, t1 * sin + t2 * cos], axis=-1)


def _rwkv7_scan(r, w, k, v, kk, a):
    b, _, h, n = r.shape
    xs = tuple(jnp.moveaxis(t, 1, 0) for t in (r, w, k, v, kk, a))

    def step(state, inp):
        rt, wt, kt, vt, kkt, at = inp
        sa = jnp.einsum('bhvk,bhk->bhv', state, -kkt)
        state = (state * wt[:, :, None, :]
                 + sa[..., None] * (kkt * at)[:, :, None, :]
                 + vt[..., None] * kt[:, :, None, :])
        return state, jnp.einsum('bhvk,bhk->bhv', state, rt)

    _, o = lax.scan(step, jnp.zeros((b, h, n, n), F32), xs)
    return jnp.moveaxis(o, 0, 1)


def _hgrn2_mixer(p, lb, norm_g):
    q_raw, f_raw, i_raw, g_raw = _split(p, HGRN_COLS)
    f = lb + (1.0 - lb) * jax.nn.sigmoid(f_raw)
    hd = lambda t: _heads(t, HGRN_HEADS)
    o = _chunked_gated_linear(hd(jax.nn.silu(q_raw)), hd(1.0 - f), hd(i_raw), hd(jnp.log(f)))
    o = _rms_norm(o, norm_g) * jax.nn.silu(hd(g_raw))
    return _merge(o)


def _gla_mixer(p, gate_w2, gate_b, norm_g):
    q, k, v, g_lr, r = _split(p, GLA_COLS)
    log_f = jax.nn.log_sigmoid(g_lr @ gate_w2 + gate_b) / GLA_GATE_NORM
    hd = lambda t: _heads(t, GLA_HEADS)
    o = _chunked_gated_linear(hd(q) * GLA_KEY ** -0.5, hd(k), hd(v), hd(log_f))
    o = _rms_norm(o, norm_g) * jax.nn.silu(hd(r))
    return _merge(o)


def _rwkv7_mixer(p, v_first, v_mix, mu, w0, w2, a0, a2, g2, k_k, k_a, r_k, lnx_g, lnx_b):
    p_prev = jnp.pad(p, ((0, 0), (1, 0), (0, 0)))[:, :-1]
    p = p + (p_prev - p) * mu
    r, k, v, w_lr, a_lr, g_lr = _split(p, RWKV_COLS)
    w = -jax.nn.softplus(-(w0 + jnp.tanh(w_lr) @ w2)) - 0.5
    decay = jnp.exp(-jnp.exp(w))
    a = jax.nn.sigmoid(a0 + a_lr @ a2)
    g = jax.nn.sigmoid(g_lr) @ g2
    if v_mix is None:
        v_first = v
    else:
        v0, v1, v2 = v_mix
        v = v + (v_first - v) * jax.nn.sigmoid(v0 + (v @ v1) @ v2)
    hd = lambda t: _heads(t, RWKV_HEADS)
    kk = hd(k * k_k)
    kk = kk / jnp.maximum(jnp.sqrt(jnp.sum(jnp.square(kk), -1, keepdims=True)), 1e-12)
    k = k * (1.0 + (a - 1.0) * k_a)
    rh, kh, vh = hd(r), hd(k), hd(v)
    o = _rwkv7_scan(rh, hd(decay), kh, vh, kk, hd(a))
    o = _group_norm(o, RWKV_LNX_EPS) * hd(lnx_g) + hd(lnx_b)
    o = o + jnp.sum(rh * kh * r_k, -1, keepdims=True) * vh
    return _merge(o) * g, v_first


def _retnet_mixer(p, positions, log_gamma):
    q, k, v, g = _split(p, RET_COLS)
    hd = lambda t: _heads(t, RET_HEADS)
    qh = _rotary(hd(q), positions)
    kh = _rotary(hd(k), positions) * RET_KEY ** -0.5
    o = _group_norm(_retention_chunkwise(qh, kh, hd(v), log_gamma), LN_EPS)
    return jax.nn.silu(g) * _merge(o)


def _swiglu(h, wg, wu, wd):
    return (jax.nn.silu(h @ wg) * (h @ wu)) @ wd


def _moe(h, router, wg, wu, wd):
    b, s, d = h.shape
    hf = h.reshape(b * s, d)
    probs = jax.nn.softmax((hf @ router).astype(F32), axis=-1)
    top_p, top_i = lax.top_k(probs, TOP_K)
    top_p = top_p / jnp.sum(top_p, -1, keepdims=True)
    gates = jnp.sum(jax.nn.one_hot(top_i, N_EXPERTS, dtype=F32) * top_p[..., None], axis=1)
    out = jnp.zeros_like(hf)
    for e in range(N_EXPERTS):
        out = out + gates[:, e:e + 1] * _swiglu(hf, wg[e], wu[e], wd[e])
    return out.reshape(b, s, d)


def setup_inputs(seed: int = 0) -> dict:
    key = jax.random.key(seed)
    ks = iter(jax.random.split(key, 48))

    def nrm(shape, scale):
        return scale * jax.random.normal(next(ks), shape, F32)

    D = D_MODEL
    decay_base = -6.0 + 5.0 * (jnp.arange(GROUP, dtype=F32) / (GROUP - 1)) ** 0.9
    return {
        'x': nrm((BATCH, SEQ, D), 1.0),
        'positions': jnp.broadcast_to(jnp.arange(SEQ, dtype=jnp.int32)[None, :], (BATCH, SEQ)),
        'w_in': nrm((DEPTH, D, IN_COLS), D ** -0.5),
        'w_out': nrm((DEPTH, D, D), DN_BETA * D ** -0.5),
        'ln1_g': 1.0 + nrm((DEPTH, D), 0.01),
        'ln1_b': nrm((DEPTH, D), 0.01),
        'ln2_g': 1.0 + nrm((DEPTH, D), 0.01),
        'ln2_b': nrm((DEPTH, D), 0.01),
        'hgrn_lb_logits': nrm((DEPTH, GROUP), 0.5),
        'hgrn_norm_g': 1.0 + nrm((DEPTH, GROUP // HGRN_HEADS), 0.01),
        'gla_gate_w2': nrm((DEPTH, GLA_GATE_RANK, GLA_HEADS * GLA_KEY), GLA_GATE_RANK ** -0.5),
        'gla_gate_b': nrm((DEPTH, GLA_HEADS * GLA_KEY), 0.1),
        'gla_norm_g': 1.0 + nrm((DEPTH, GROUP // GLA_HEADS), 0.01),
        'rwkv_mu': jax.random.uniform(next(ks), (DEPTH, sum(RWKV_COLS)), F32),
        'rwkv_w0': decay_base[None, :] + nrm((DEPTH, GROUP), 0.1),
        'rwkv_w2': nrm((DEPTH, RWKV_DECAY_RANK, GROUP), 0.5 * RWKV_DECAY_RANK ** -0.5),
        'rwkv_a0': nrm((DEPTH, GROUP), 0.1),
        'rwkv_a2': nrm((DEPTH, RWKV_A_RANK, GROUP), 0.5 * RWKV_A_RANK ** -0.5),
        'rwkv_g2': nrm((DEPTH, RWKV_GATE_RANK, GROUP), RWKV_GATE_RANK ** -0.5),
        'rwkv_k_k': 0.85 + nrm((DEPTH, GROUP), 0.02),
        'rwkv_k_a': 1.0 + nrm((DEPTH, GROUP), 0.02),
        'rwkv_r_k': -0.04 + nrm((DEPTH, RWKV_HEADS, RWKV_HEAD), 0.02),
        'rwkv_lnx_g': 1.0 + nrm((DEPTH, GROUP), 0.01),
        'rwkv_lnx_b': nrm((DEPTH, GROUP), 0.01),
        'rwkv_v0': 1.0 + nrm((DEPTH - 1, GROUP), 0.1),
        'rwkv_v1': nrm((DEPTH - 1, GROUP, RWKV_V_RANK), GROUP ** -0.5),
        'rwkv_v2': nrm((DEPTH - 1, RWKV_V_RANK, GROUP), 0.5 * RWKV_V_RANK ** -0.5),
        'ffn_w_gate': nrm((N_DENSE, D, FFN_DENSE), D ** -0.5),
        'ffn_w_up': nrm((N_DENSE, D, FFN_DENSE), D ** -0.5),
        'ffn_w_down': nrm((N_DENSE, FFN_DENSE, D), DN_BETA * FFN_DENSE ** -0.5),
        'moe_router': nrm((N_MOE, D, N_EXPERTS), D ** -0.5),
        'moe_w_gate': nrm((N_MOE, N_EXPERTS, D, FFN_EXPERT), D ** -0.5),
        'moe_w_up': nrm((N_MOE, N_EXPERTS, D, FFN_EXPERT), D ** -0.5),
        'moe_w_down': nrm((N_MOE, N_EXPERTS, FFN_EXPERT, D), DN_BETA * FFN_EXPERT ** -0.5),
    }


def reference(x, positions, w_in, w_out, ln1_g, ln1_b, ln2_g, ln2_b, hgrn_lb_logits, hgrn_norm_g,
              gla_gate_w2, gla_gate_b, gla_norm_g, rwkv_mu, rwkv_w0, rwkv_w2, rwkv_a0, rwkv_a2,
              rwkv_g2, rwkv_k_k, rwkv_k_a, rwkv_r_k, rwkv_lnx_g, rwkv_lnx_b, rwkv_v0, rwkv_v1,
              rwkv_v2, ffn_w_gate, ffn_w_up, ffn_w_down, moe_router, moe_w_gate, moe_w_up,
              moe_w_down):
    out_dtype = x.dtype
    h = x.astype(F32)
    lb_all = jnp.cumsum(jax.nn.softmax(hgrn_lb_logits.astype(F32), axis=0), axis=0)
    lb_all = lb_all - lb_all[:1]
    log_gamma = jnp.log(1.0 - 2.0 ** (-5.0 - jnp.arange(RET_HEADS, dtype=F32)))
    v_first = None
    for l in range(DEPTH):
        p = jnp.einsum('btd,dc->btc', h, w_in[l]).astype(F32)
        pa, pb, pc, pd = _split(p, MIXER_COLS)
        ya = _hgrn2_mixer(pa, lb_all[l], hgrn_norm_g[l])
        yb = _gla_mixer(pb, gla_gate_w2[l], gla_gate_b[l], gla_norm_g[l])
        v_mix = None if l == 0 else (rwkv_v0[l - 1], rwkv_v1[l - 1], rwkv_v2[l - 1])
        yc, v_first = _rwkv7_mixer(pc, v_first, v_mix, rwkv_mu[l], rwkv_w0[l], rwkv_w2[l],
                                   rwkv_a0[l], rwkv_a2[l], rwkv_g2[l], rwkv_k_k[l], rwkv_k_a[l],
                                   rwkv_r_k[l], rwkv_lnx_g[l], rwkv_lnx_b[l])
        yd = _retnet_mixer(pd, positions, log_gamma)
        mix = jnp.einsum('btc,cd->btd', jnp.concatenate([ya, yb, yc, yd], axis=-1), w_out[l])
        h = _layer_norm(DN_ALPHA * h + mix, ln1_g[l], ln1_b[l])
        if l % 2 == 0:
            i = l // 2
            f = _swiglu(h, ffn_w_gate[i], ffn_w_up[i], ffn_w_down[i])
        else:
            i = l // 2
            f = _moe(h, moe_router[i], moe_w_gate[i], moe_w_up[i], moe_w_down[i])
        h = _layer_norm(DN_ALPHA * h + f, ln2_g[l], ln2_b[l])
    return h.astype(out_dtype)
```

```python
import numpy as np
import ml_dtypes
import concourse.bass as bass
import concourse.mybir as mybir
from concourse.bass_utils import run_bass_kernel_spmd

F32 = mybir.dt.float32
BF16 = mybir.dt.bfloat16
I32 = mybir.dt.int32
AF = mybir.ActivationFunctionType
ALU = mybir.AluOpType
AX = mybir.AxisListType

D = 1024
NL = 4
INC = 3888
FFD = 2816
NE = 8
FFE = 1408
ALPHA = (2.0 * NL) ** 0.25
C = 64


class V:
    __slots__ = ("tt", "ap")

    def __init__(self, tt, ap):
        self.tt = tt
        self.ap = ap

    def __getitem__(self, k):
        return V(self.tt, self.ap[k])

    def re(self, s, **kw):
        return V(self.tt, self.ap.rearrange(s, **kw))

    def bc(self, shape):
        return V(self.tt, self.ap.to_broadcast(list(shape)))


class TT:
    __slots__ = ("t", "name", "w", "r", "al", "pe_row")

    def __init__(self, t, name):
        self.t = t
        self.name = name
        self.w = None
        self.r = []
        self.al = []
        self.pe_row = None

    def __getitem__(self, k):
        return V(self, self.t[k])


class Ctx:
    NDMA = 24

    def __init__(self, nc, same=True):
        self.nc = nc
        self.same = same
        self.engs = {"pe": nc.tensor, "dve": nc.vector, "act": nc.scalar, "pool": nc.gpsimd, "sp": nc.sync}
        self.sem = {}
        self.cnt = {}
        self.waited = {}
        self._cms = []
        for k in self.engs:
            cm = nc.semaphore("s_" + k)
            self.sem[k] = cm.__enter__()
            self._cms.append(cm)
            self.cnt[k] = 0
            self.waited[k] = {}
        self.dsem = []
        self.dcnt = []
        for i in range(self.NDMA):
            cm = nc.semaphore("d_%d" % i)
            self.dsem.append(cm.__enter__())
            self._cms.append(cm)
            self.dcnt.append(0)
        self.dnext = {"sp": 0, "pool": 0, "act": 0}
        self.drange = {"sp": (0, 14), "pool": (14, 22), "act": (22, 24)}
        self.ntile = 0
        self.ninst = 0

    def sb(self, shape, dt, name=None):
        self.ntile += 1
        name = name or "t%d" % self.ntile
        cm = self.nc.sbuf_tensor(name, list(shape), dt)
        t = cm.__enter__()
        self._cms.append(cm)
        return TT(t, name)

    def ps(self, shape, dt, name=None):
        self.ntile += 1
        name = name or "p%d" % self.ntile
        cm = self.nc.psum_tensor(name, list(shape), dt)
        t = cm.__enter__()
        self._cms.append(cm)
        return TT(t, name)

    def _semof(self, key):
        if key in self.sem:
            return self.sem[key]
        return self.dsem[int(key[1:])]

    def _deps(self, eng, reads, writes):
        need = {}

        def add(dep):
            if dep is None:
                return
            k, v = dep
            if need.get(k, 0) < v:
                need[k] = v
        for t in reads:
            add(t.w)
            for a in t.al:
                add(a.w)
        for t in writes:
            add(t.w)
            for d in t.r:
                add(d)
            for a in t.al:
                add(a.w)
                for d in a.r:
                    add(d)
        h = self.engs[eng]
        for k, v in need.items():
            if k == eng and (not self.same or eng == "pe"):
                continue
            if self.waited[eng].get(k, 0) >= v:
                continue
            h.wait_ge(self._semof(k), v)
            self.waited[eng][k] = v
            self.ninst += 1

    def _mark(self, me, reads, writes):
        for t in reads:
            if len(t.r) > 64:
                mx = {}
                for (k, v) in t.r:
                    if mx.get(k, 0) < v:
                        mx[k] = v
                t.r = list(mx.items())
            t.r.append(me)
        for t in writes:
            t.w = me
            t.r = []

    def op(self, eng, fn, reads=(), writes=()):
        reads = [x.tt if isinstance(x, V) else x for x in reads if x is not None]
        writes = [x.tt if isinstance(x, V) else x for x in writes if x is not None]
        self._deps(eng, reads, writes)
        ins = fn()
        self.cnt[eng] += 1
        ins.then_inc(self.sem[eng], 1)
        self._mark((eng, self.cnt[eng]), reads, writes)
        self.ninst += 1
        return ins

    def dma(self, q, out, in_, **kw):
        reads = [in_.tt] if isinstance(in_, V) else []
        writes = [out.tt] if isinstance(out, V) else []
        lo, hi = self.drange[q]
        i = lo + self.dnext[q]
        self.dnext[q] = (self.dnext[q] + 1) % (hi - lo)
        key = "d%d" % i
        h = self.engs[q]
        if self.dcnt[i] > 0 and self.waited[q].get(key, 0) < self.dcnt[i]:
            h.wait_ge(self.dsem[i], self.dcnt[i])
            self.waited[q][key] = self.dcnt[i]
        self._deps(q, reads, writes)
        oa = out.ap if isinstance(out, V) else out
        ia = in_.ap if isinstance(in_, V) else in_
        ins = h.dma_start(out=oa, in_=ia, **kw)
        self.dcnt[i] += 16
        ins.then_inc(self.dsem[i], 16)
        self._mark((key, self.dcnt[i]), reads, writes)
        self.ninst += 1
        return ins

    def _pe_row_guard(self, out, lhsT):
        row = (int(lhsT.ap.start_partition()), int(lhsT.ap.partition_size()))
        t = out.tt
        if t.pe_row is not None and t.pe_row != row and t.w is not None and t.w[0] == "pe":
            v = t.w[1]
            if self.waited["pe"].get("pe", 0) < v:
                self.nc.tensor.wait_ge(self.sem["pe"], v)
                self.waited["pe"]["pe"] = v
                self.ninst += 1
        t.pe_row = row

    def mm(self, out, lhsT, rhs, start=True, stop=True, sg=False):
        nc = self.nc
        self._pe_row_guard(out, lhsT)
        return self.op("pe", lambda: nc.tensor.matmul(out.ap, lhsT=lhsT.ap, rhs=rhs.ap, start=start, stop=stop,
                                                      skip_group_check=sg),
                       reads=[lhsT, rhs], writes=[out])

    def tr(self, out, in_, ident):
        nc = self.nc
        self._pe_row_guard(out, in_)
        return self.op("pe", lambda: nc.tensor.transpose(out.ap, in_.ap, ident.ap), reads=[in_, ident], writes=[out])

    def act(self, out, in_, func, bias=None, scale=None, eng="act"):
        nc = self.nc
        kw = {}
        rd = [in_]
        if bias is not None:
            if isinstance(bias, V):
                kw["bias"] = bias.ap
                rd.append(bias)
            else:
                kw["bias"] = bias
        if scale is not None:
            if isinstance(scale, V):
                kw["scale"] = scale.ap
                rd.append(scale)
            else:
                kw["scale"] = scale
        if func == AF.Copy and isinstance(scale, V):
            func = AF.Identity
        return self.op("act", lambda: nc.scalar.activation(out.ap, in_.ap, func, **kw), reads=rd, writes=[out])

    def tt(self, out, in0, in1, op, eng="dve"):
        h = self.engs[eng]
        return self.op(eng, lambda: h.tensor_tensor(out.ap, in0.ap, in1.ap, op), reads=[in0, in1], writes=[out])

    def ts(self, out, in0, s1, op0, s2=None, op1=None, eng="dve"):
        h = self.engs[eng]
        rd = [in0]
        a1 = s1
        if isinstance(s1, V):
            rd.append(s1)
            a1 = s1.ap
        a2 = s2
        if isinstance(s2, V):
            rd.append(s2)
            a2 = s2.ap
        if op1 is None:
            return self.op(eng, lambda: h.tensor_scalar(out.ap, in0.ap, a1, None, op0), reads=rd, writes=[out])
        return self.op(eng, lambda: h.tensor_scalar(out.ap, in0.ap, a1, a2, op0, op1), reads=rd, writes=[out])

    def stt(self, out, in0, scalar, in1, op0, op1):
        nc = self.nc
        rd = [in0, in1]
        a = scalar
        if isinstance(scalar, V):
            rd.append(scalar)
            a = scalar.ap
        return self.op("dve", lambda: nc.vector.scalar_tensor_tensor(out.ap, in0.ap, a, in1.ap, op0, op1),
                       reads=rd, writes=[out])

    def copy(self, out, in_, eng="dve"):
        nc = self.nc
        if eng == "act":
            return self.op("act", lambda: nc.scalar.copy(out.ap, in_.ap), reads=[in_], writes=[out])
        h = self.engs[eng]
        return self.op(eng, lambda: h.tensor_copy(out.ap, in_.ap), reads=[in_], writes=[out])

    def memset(self, out, val, eng="dve"):
        h = self.engs[eng]
        return self.op(eng, lambda: h.memset(out.ap, val), reads=[], writes=[out])

    def scan(self, out, d0, d1, init, op0, op1):
        nc = self.nc
        rd = [d0, d1]
        a = init
        if isinstance(init, V):
            rd.append(init)
            a = init.ap
        return self.op("dve", lambda: nc.vector.tensor_tensor_scan(out.ap, d0.ap, d1.ap, a, op0, op1),
                       reads=rd, writes=[out])

    def reduce(self, out, in_, op, axis=AX.X):
        nc = self.nc
        return self.op("dve", lambda: nc.vector.tensor_reduce(out.ap, in_.ap, axis, op), reads=[in_], writes=[out])

    def recip(self, out, in_):
        nc = self.nc
        return self.op("dve", lambda: nc.vector.reciprocal(out.ap, in_.ap), reads=[in_], writes=[out])

    def wait_all(self, eng, tts):
        self._deps(eng, tts, ())


class Arena:
    def __init__(self, c, nbytes, name):
        self.tt = c.sb([128, nbytes // 4], F32, name)
        self.views = []
        self.nbytes = nbytes

    def view(self, off, shape, dt, name):
        esz = 4 if dt in (F32, I32) else 2
        n = esz
        for s in shape[1:]:
            n *= s
        assert off % 4 == 0 and n % 4 == 0 and off + n <= self.nbytes, (name, off, n, self.nbytes)
        ap = self.tt.t[:, off // 4:(off + n) // 4]
        if dt != F32:
            ap = ap.bitcast(dt)
        if len(shape) == 3:
            ap = ap.rearrange("p (a b) -> p a b", a=shape[1])
        elif len(shape) == 4:
            ap = ap.rearrange("p (a b c) -> p a b c", a=shape[1], b=shape[2])
        if shape[0] < 128:
            ap = ap[0:shape[0]]
        t = TT(ap, name)
        for (v, lo, hi) in self.views:
            if lo < off + n and off < hi:
                t.al.append(v)
                v.al.append(t)
        self.views.append((t, off, off + n))
        return t


class Ring:
    def __init__(self, tiles):
        self.tiles = tiles
        self.i = 0

    def next(self):
        t = self.tiles[self.i]
        self.i = (self.i + 1) % len(self.tiles)
        return t


def _consts(TB):
    p = np.arange(128)
    f = {}
    j = p[:, None]
    i = p[None, :]
    same = (j // C) == (i // C)
    f["mask_incl"] = (same & (j <= i)).astype(np.float32)
    f["mask_strict"] = (same & (j < i)).astype(np.float32)
    f["mask_strictT"] = (same & (j > i)).astype(np.float32)
    f["ident"] = np.eye(128, dtype=np.float32)
    f["blockones"] = same.astype(np.float32)
    f["identZ"] = ((p[:, None] % 64) == np.arange(64)[None, :]).astype(np.float32)
    hs = np.zeros((128, 2), np.float32)
    hs[:64, 0] = 1.0
    hs[64:, 1] = 1.0
    f["headsel"] = hs
    t = np.arange(TB)
    f["reset"] = np.broadcast_to((t % C != 0).astype(np.float32)[None, :], (128, TB)).copy()
    half = 32
    inv = (10000.0 ** (-np.arange(half, dtype=np.float32) / half)).astype(np.float32)
    d = p % 64
    f["invfreq"] = inv[d % 32][:, None].astype(np.float32)
    f["sinsign"] = np.where(d < 32, 1.0, -1.0)[:, None].astype(np.float32)
    lg = np.log(1.0 - 2.0 ** (-5.0 - np.arange(4, dtype=np.float64)))
    idx = np.arange(C, dtype=np.float64)
    req = np.zeros((128, 2, C), np.float32)
    rek = np.zeros((128, 2, C), np.float32)
    rdec = np.zeros((128, 2), np.float32)
    for tl in range(2):
        for pp in range(128):
            h = tl * 2 + pp // 64
            req[pp, tl] = np.exp((idx + 1.0) * lg[h])
            rek[pp, tl] = np.exp(-(idx + 1.0) * lg[h]) * (64.0 ** -0.5)
            rdec[pp, tl] = np.exp(C * lg[h])
    f["ret_eq"] = req.reshape(128, 2 * C)
    f["ret_ek"] = rek.reshape(128, 2 * C)
    f["ret_dec"] = rdec
    names = list(f.keys())
    offs = {}
    o = 0
    for n in names:
        offs[n] = (o, f[n].shape[1])
        o += f[n].shape[1]
    arr = np.concatenate([f[n] for n in names], axis=1).astype(np.float32)
    return arr, offs


def build(T, L=NL, TB=512, mixers=(1, 1, 1, 1), ffn=True, same=True):
    assert T % TB == 0 and TB % 128 == 0
    NT = TB // 128
    NBLK = T // TB
    NCH = TB // C
    nc = bass.Bass("TRN2", target_bir_lowering=False)
    c = Ctx(nc, same=same)
    carr, coff = _consts(TB)

    def din(name, shape, dt=F32):
        return nc.dram_tensor(name, list(shape), dt, kind="ExternalInput").ap()

    x_d = din("x", [T, D])
    pos_d = din("positions", [1, T], I32)
    w_in_d = din("w_in", [NL, D, INC])
    w_out_d = din("w_out", [NL, D, D])
    ln_d = {k: din(k, [NL, D]) for k in ("ln1_g", "ln1_b", "ln2_g", "ln2_b")}
    lb_d = din("hgrn_lb_logits", [NL, 256])
    hng_d = din("hgrn_norm_g", [NL, 64])
    gw2_d = din("gla_gate_w2", [NL, 16, 128])
    gb_d = din("gla_gate_b", [NL, 128])
    gng_d = din("gla_norm_g", [NL, 64])
    mu_d = din("rwkv_mu", [NL, 1056])
    w0_d = din("rwkv_w0", [NL, 256])
    w2_d = din("rwkv_w2", [NL, 64, 256])
    a0_d = din("rwkv_a0", [NL, 256])
    a2_d = din("rwkv_a2", [NL, 64, 256])
    g2_d = din("rwkv_g2", [NL, 160, 256])
    kk_d = din("rwkv_k_k", [NL, 256])
    ka_d = din("rwkv_k_a", [NL, 256])
    rk_d = din("rwkv_r_k", [NL, 256])
    lxg_d = din("rwkv_lnx_g", [NL, 256])
    lxb_d = din("rwkv_lnx_b", [NL, 256])
    v0_d = din("rwkv_v0", [NL - 1, 256])
    v1_d = din("rwkv_v1", [NL - 1, 256, 32])
    v2_d = din("rwkv_v2", [NL - 1, 32, 256])
    fg_d = din("ffn_w_gate", [2, D, FFD])
    fu_d = din("ffn_w_up", [2, D, FFD])
    fd_d = din("ffn_w_down", [2, FFD, D])
    mr_d = din("moe_router", [2, D, NE])
    mg_d = din("moe_w_gate", [2, NE, D, FFE])
    mu2_d = din("moe_w_up", [2, NE, D, FFE])
    md_d = din("moe_w_down", [2, NE, FFE, D])
    cst_d = din("consts", list(carr.shape))
    out_d = nc.dram_tensor("out", [T, D], F32, kind="ExternalOutput").ap()

    cst = c.sb([128, carr.shape[1]], F32, "cst")
    c.dma("sp", cst[:], cst_d)

    def K(name):
        o, n = coff[name]
        return cst[:, o:o + n]
    ident_bf = c.sb([128, 128], BF16, "identbf")
    c.copy(ident_bf[:], K("ident"))
    mask_bf = c.sb([128, 128], BF16, "maskbf")
    c.copy(mask_bf[:], K("mask_incl"))

    def pp_tile(dram, n, name, nl=NL):
        t = c.sb([128, nl, n], F32, name)
        for l in range(nl):
            for k in range(n):
                c.dma("sp", t[:, l, k:k + 1], dram[l:l + 1, k * 128:(k + 1) * 128].rearrange("o p -> p o"))
        return t

    lbl = pp_tile(lb_d, 2, "lbl")
    w0 = pp_tile(w0_d, 2, "w0")
    a0 = pp_tile(a0_d, 2, "a0")
    kkp = pp_tile(kk_d, 2, "kkp")
    kap = pp_tile(ka_d, 2, "kap")
    rkp = pp_tile(rk_d, 2, "rkp")
    v0p = pp_tile(v0_d, 2, "v0p", nl=NL - 1)
    mup = c.sb([128, NL, 9], F32, "mup")
    c.memset(mup[:], 0.0)
    for l in range(NL):
        for k in range(8):
            c.dma("sp", mup[:, l, k:k + 1], mu_d[l:l + 1, k * 128:(k + 1) * 128].rearrange("o p -> p o"))
        c.dma("sp", mup[0:32, l, 8:9], mu_d[l:l + 1, 1024:1056].rearrange("o p -> p o"))
    omu = c.sb([128, NL, 9], F32, "omu")
    c.ts(omu[:], mup[:], -1.0, ALU.mult, 1.0, ALU.add)
    gb2 = c.sb([64, NL, 2], F32, "gb2")
    for l in range(NL):
        for k in range(2):
            c.dma("sp", gb2[:, l, k:k + 1], gb_d[l:l + 1, k * 64:(k + 1) * 64].rearrange("o p -> p o"))
    ngb2 = c.sb([64, NL, 2], F32, "ngb2")
    c.ts(ngb2[:], gb2[:], -1.0, ALU.mult)
    nw0 = c.sb([128, NL, 2], F32, "nw0")
    c.ts(nw0[:], w0[:], -1.0, ALU.mult)
    lbe = c.sb([128, NL, 2], F32, "lbe")
    c.act(lbe[:], lbl[:], AF.Exp)
    lbs = c.sb([128, 2], F32, "lbs")
    c.tt(lbs[:], lbe[:, 0, :], lbe[:, 1, :], ALU.add)
    for l in range(2, NL):
        c.tt(lbs[:], lbs[:], lbe[:, l, :], ALU.add)
    c.recip(lbs[:], lbs[:])
    lb = c.sb([128, NL, 2], F32, "lb")
    c.memset(lb[:], 0.0)
    for l in range(1, NL):
        c.tt(lb[:, l, :], lbe[:, l, :], lbs[:], ALU.mult)
        c.tt(lb[:, l, :], lb[:, l, :], lb[:, l - 1, :], ALU.add)
    olb = c.sb([128, NL, 2], F32, "olb")
    c.ts(olb[:], lb[:], -1.0, ALU.mult, 1.0, ALU.add)
    nolb = c.sb([128, NL, 2], F32, "nolb")
    c.ts(nolb[:], olb[:], -1.0, ALU.mult)
    epsc = c.sb([128, 4], F32, "epsc")
    c.memset(epsc[:, 0:1], 1e-5)
    c.memset(epsc[:, 1:2], 1e-6)
    c.memset(epsc[:, 2:3], 64e-5)
    c.memset(epsc[:, 3:4], 1.0)
    mrs = c.sb([128, 2, 8, NE], F32, "mrs")
    for i in range(2):
        c.dma("sp", mrs[:, i], mr_d[i].rearrange("(k p) e -> p k e", p=128))
    mrs_bf = c.sb([128, 2, 8, NE], BF16, "mrsbf")
    c.copy(mrs_bf[:], mrs[:])

    rb_small = c.sb([128, 640], F32, "rbsmall")
    lnrow_g = c.sb([128, D], F32, "lnrow_g")
    lnrow_b = c.sb([128, D], F32, "lnrow_b")
    gw2 = c.sb([16, 128], F32, "gw2")
    w2s = c.sb([64, 256], F32, "w2s")
    a2s = c.sb([128, 256], F32, "a2s")
    g2a = c.sb([128, 256], F32, "g2a")
    g2b = c.sb([32, 256], F32, "g2b")
    v1s = c.sb([128, 2, 32], F32, "v1s")
    v2s = c.sb([32, 256], F32, "v2s")

    def load_layer_params(l):
        c.dma("sp", rb_small[:, 0:64], hng_d[l:l + 1, :].partition_broadcast(128))
        c.dma("sp", rb_small[:, 64:128], gng_d[l:l + 1, :].partition_broadcast(128))
        c.dma("sp", rb_small[:, 128:384], lxg_d[l:l + 1, :].partition_broadcast(128))
        c.dma("sp", rb_small[:, 384:640], lxb_d[l:l + 1, :].partition_broadcast(128))
        c.dma("sp", gw2[:], gw2_d[l])
        c.dma("sp", w2s[:], w2_d[l])
        c.dma("sp", a2s[64:128, :], a2_d[l])
        c.dma("sp", g2a[:], g2_d[l, 0:128, :])
        c.dma("sp", g2b[:], g2_d[l, 128:160, :])
        if l > 0:
            c.dma("sp", v1s[:], v1_d[l - 1].rearrange("(c p) n -> p c n", p=128))
            c.dma("sp", v2s[:], v2_d[l - 1])

    SLOTN = 8 * 512
    NSLOT = 4
    wslots = [c.sb([128, SLOTN], BF16, "wslot%d" % i) for i in range(NSLOT)]
    accb = Ring([c.ps([128, 512], F32, "acc%d" % i) for i in range(4)])
    trb = Ring([c.ps([128, 512], F32, "trb%d" % i) for i in range(4)])

    hT = c.sb([128, 8, TB], BF16, "hT")
    htok = c.sb([128, NT, D], F32, "htok")
    ytok = c.sb([128, NT, D], BF16, "ytok")
    P = [c.sb([128, TB], F32, "P%d" % i) for i in range(12)]
    posf = c.sb([128, TB], F32, "posf")
    qk = [c.sb([128, TB], BF16, "qk%d" % i) for i in range(4)]
    vt = c.sb([128, NT, 256], BF16, "vt")
    gt = c.sb([128, NT, 256], BF16, "gt")
    decs = [c.sb([128, NCH], F32, "dec%d" % i) for i in range(2)]
    dec_rt = [c.sb([128, NCH], F32, "decrt%d" % i) for i in range(2)]
    for i in range(2):
        c.copy(dec_rt[i][:], K("ret_dec")[:, i:i + 1].bc([128, NCH]))
    cosT = c.sb([128, TB], F32, "cosT")
    sinT = c.sb([128, TB], F32, "sinT")
    o_sb = c.sb([128, 256], F32, "o_sb")
    sq_sb = c.sb([128, 256], F32, "sq_sb")
    stat_ln = c.sb([128, 16], F32, "stat_ln")
    stat_mx = c.sb([128, 16], F32, "stat_mx")
    stat_moe = c.sb([128, 40], F32, "stat_moe")
    vfirst = c.sb([128, 2, TB], F32, "vfirst")
    rw_carry = c.sb([128, L, 9], F32, "rwcarry")
    rw_bonus = c.sb([128, NT, 4], F32, "rw_bonus")
    rw_dec = c.sb([128, NCH], F32, "rwdec")
    gates = c.sb([128, NT, NE], F32, "gates")

    def mk_state(W, name):
        s = [c.sb([128, W], F32, "%s_f%d" % (name, l)) for l in range(L)]
        sb_ = [c.sb([128, W], BF16, "%s_b%d" % (name, l)) for l in range(L)]
        return s, sb_
    st_hg = [mk_state(128, "hg%d" % t) for t in range(2)]
    st_rt = [mk_state(128, "rt%d" % t) for t in range(2)]
    st_gl = [mk_state(128, "gl%d" % t) for t in range(2)]
    zst = [[c.sb([128, 64], F32, "z%d_%d" % (l, pt)) for pt in range(2)] for l in range(L)]

    RW_BYTES = 5 * 4 * TB + 2 * 4 * (TB + 1) + 2 * 4 * TB + 2 * 4 * TB + 8 * TB + NT * 1024 + NT * 512 + NT * 1024 \
        + 4 * 1024 + 8 * 512 + 3 * 512 + 6 * 256 + 2 * 512
    FF_BYTES = 22 * TB * 2 + NT * D * 4 + 8 * TB * 2
    ar_ = Arena(c, max(RW_BYTES, FF_BYTES) + 64, "arena")
    off = [0]

    def av(shape, dt, name):
        esz = 4 if dt == F32 else 2
        n = esz
        for s in shape[1:]:
            n *= s
        v = ar_.view(off[0], shape, dt, name)
        off[0] += n
        return v
    rw_sh = [av([128, TB], F32, "rwsh%d" % i) for i in range(5)]
    rw_raw = Ring([av([128, TB + 1], F32, "rwraw%d" % i) for i in range(2)])
    th_t = av([128, TB], F32, "th")
    lvs_t = av([128, TB], F32, "lvs")
    rw_kt = av([128, TB], F32, "rw_kt")
    rw_bt = av([128, TB], F32, "rw_bt")
    rw_ar = av([128, 2, TB], F32, "rw_ar")
    rw_vtok = av([128, NT, 256], F32, "rw_vtok")
    rw_gtok = av([128, NT, 256], BF16, "rw_gtok")
    o_acc = av([128, NT, 256], F32, "o_acc")
    a_ring = Ring([av([128, 256], F32, "aring%d" % i) for i in range(4)])
    m_ring = Ring([av([128, 128], F32, "mring%d" % i) for i in range(8)])
    tok_ring = Ring([av([128, 128], F32, "tokr%d" % i) for i in range(3)])
    u_ring = Ring([av([128, 64], F32, "ur%d" % i) for i in range(6)])
    TTt = [av([128, 128], F32, "TTa"), av([128, 128], F32, "TTb")]
    off[0] = 0
    mT = av([128, 22, TB], BF16, "mT")
    facc = av([128, NT, D], F32, "facc")
    yT = av([128, 8, TB], BF16, "yT")

    out_tt = TT(None, "out_dram")

    pieces = []
    pst = {"issued": 0, "taken": 0}
    free_slots = list(range(NSLOT))
    slot_of = {}

    class Piece:
        __slots__ = ("v", "slot")

    def plan_piece(dap, nk, ncols):
        assert nk * ncols <= SLOTN
        pieces.append((dap, nk, ncols))

    def pump():
        while free_slots and pst["issued"] < len(pieces):
            i = pst["issued"]
            dap, nk, ncols = pieces[i]
            s = free_slots.pop(0)
            dst = wslots[s][:, 0:nk * ncols].re("p (k c) -> p k c", k=nk)
            c.dma("pool", dst, dap)
            p_ = Piece()
            p_.v = dst
            p_.slot = s
            slot_of[i] = p_
            pst["issued"] += 1

    def take_piece():
        pump()
        i = pst["taken"]
        assert i in slot_of, ("weight ring deadlock", i)
        pst["taken"] += 1
        return slot_of.pop(i)

    def release(p_):
        free_slots.append(p_.slot)
        pump()

    def rows_piece(w2d, r0, nk, c0, ncols):
        return w2d[r0 * 128:(r0 + nk) * 128, c0:c0 + ncols].rearrange("(k p) c -> p k c", p=128)

    WIN_PIECES = [(0, 512), (512, 512), (1024, 512), (1536, 272), (1808, 512), (2320, 512), (2832, 32),
                  (2864, 512), (3376, 512)]
    GU_COLS_D = [(0, 512), (512, 512), (1024, 512), (1536, 512), (2048, 512), (2560, 256)]
    GU_COLS_E = [(0, 512), (512, 512), (1024, 384)]

    def plan_layer(l):
        for (c0, n) in WIN_PIECES:
            plan_piece(rows_piece(w_in_d[l], 0, 8, c0, n), 8, n)
        for hf in range(2):
            plan_piece(rows_piece(w_out_d[l], 0, 8, hf * 512, 512), 8, 512)
        if not ffn:
            return
        i = l // 2
        if l % 2 == 0:
            for (c0, n) in GU_COLS_D:
                plan_piece(rows_piece(fg_d[i], 0, 8, c0, n), 8, n)
                plan_piece(rows_piece(fu_d[i], 0, 8, c0, n), 8, n)
            for hf in range(2):
                for (r0, nk) in ((0, 8), (8, 8), (16, 6)):
                    plan_piece(rows_piece(fd_d[i], r0, nk, hf * 512, 512), nk, 512)
        else:
            for e in range(NE):
                for (c0, n) in GU_COLS_E:
                    plan_piece(rows_piece(mg_d[i, e], 0, 8, c0, n), 8, n)
                    plan_piece(rows_piece(mu2_d[i, e], 0, 8, c0, n), 8, n)
                for hf in range(2):
                    for (r0, nk) in ((0, 8), (8, 3)):
                        plan_piece(rows_piece(md_d[i, e], r0, nk, hf * 512, 512), nk, 512)

    for b in range(NBLK):
        for l in range(L):
            plan_layer(l)

    def proj_F(wp, cols, ncols, rhs=None):
        bank = trb.next()
        out = bank[0:ncols, 0:TB]
        for k in range(8):
            c.mm(out, wp.v[:, k, cols:cols + ncols], hT[:, k, :], start=(k == 0), stop=(k == 7))
        return out

    def proj_T(wp, cols, ncols, tile):
        bank = trb.next()
        out = bank[:, 0:ncols]
        for k in range(8):
            c.mm(out, hT[:, k, tile * 128:(tile + 1) * 128], wp.v[:, k, cols:cols + ncols],
                 start=(k == 0), stop=(k == 7))
        return out

    def cumdecay(g, sgn_scale, cum, eq, ek):
        c.scan(cum, K("reset")[:, 0:TB], g, 0.0, ALU.mult, ALU.add)
        c.act(eq, cum, AF.Exp, scale=sgn_scale)
        c.act(ek, cum, AF.Exp, scale=-sgn_scale)

    def chunk_end(v):
        return v.re("p (n c) -> p n c", c=C)[:, :, C - 1]

    def rstd_of(dst, src, eps_col):
        c.act(dst, src, AF.Sqrt, bias=epsc[:, eps_col:eps_col + 1])
        c.recip(dst, dst)

    sc_ring = Ring([c.sb([128, 128], BF16, "sc%d" % i) for i in range(4)])
    kt_ring = Ring([c.sb([128, 128], BF16, "kt%d" % i) for i in range(2)])
    md_ring = Ring([c.sb([128, 128], F32, "md%d" % i) for i in range(2)])

    def gla_tile(l, t, qT, kT, dcs, states, KD, nh_tile):
        tok = slice(t * 128, (t + 1) * 128)
        o_bank = accb.next()
        o_ps = o_bank[:, 0:256]
        first = [True]
        W = nh_tile * 64
        NP = nh_tile * KD
        for pt in range(len(qT)):
            S, Sb = states[pt][0][l][0:NP, :], states[pt][1][l][0:NP, :]
            ktp = trb.next()
            ktv = V(ktp, ktp.t[:, 0:NP // 2].bitcast(BF16))
            c.tr(ktv, kT[pt][:, tok], ident_bf[0:NP, 0:NP])
            ktok = kt_ring.next()[:, 0:NP]
            c.copy(ktok, ktv, eng="act")
            scs = []
            for hh in range(nh_tile):
                pr = slice(hh * KD, (hh + 1) * KD)
                sp_ = trb.next()
                c.mm(sp_[:, 0:128], kT[pt][pr, tok], qT[pt][pr, tok])
                sc = sc_ring.next()[:]
                c.tt(sc, sp_[:, 0:128], mask_bf[:], ALU.mult)
                scs.append(sc)
            for hh in range(nh_tile):
                hg = pt * nh_tile + hh
                c.mm(o_ps[:, hg * 64:(hg + 1) * 64], scs[hh], vt[:, t, hg * 64:(hg + 1) * 64],
                     start=first[0], stop=False, sg=True)
                first[0] = False
            for ch in range(2):
                cg = t * 2 + ch
                rows = slice(ch * 64, (ch + 1) * 64)
                ctok = slice(t * 128 + ch * 64, t * 128 + (ch + 1) * 64)
                for hh in range(nh_tile):
                    hg = pt * nh_tile + hh
                    pr = slice(hh * KD, (hh + 1) * KD)
                    c.mm(o_ps[rows, hg * 64:(hg + 1) * 64], qT[pt][pr, ctok], Sb[pr, hh * 64:(hh + 1) * 64],
                         start=False, stop=True, sg=True)
                mp = trb.next()
                c.mm(mp[0:NP, 0:W], ktok[rows, :], vt[rows, t, pt * W:(pt + 1) * W])
                md = md_ring.next()[0:NP, 0:W]
                c.act(md, mp[0:NP, 0:W], AF.Copy, scale=dcs[pt][:, cg:cg + 1])
                c.stt(S, S, dcs[pt][:, cg:cg + 1], md, ALU.mult, ALU.add)
                c.copy(Sb, S, eng="act")
        return o_ps

    def finish_simple(l, t, o_ps, mix_idx, kind, gam_off):
        o = o_sb[:]
        c.copy(o, o_ps, eng="act")
        o3 = o.re("p (h v) -> p h v", h=4)
        sq = sq_sb[:]
        msum = stat_mx[:, 0:4]
        ssum = stat_mx[:, 4:8]
        rstd = stat_mx[:, 8:12]
        if kind == "gn":
            c.reduce(msum, o3, ALU.add)
            c.ts(msum, msum, 1.0 / 64.0, ALU.mult)
            c.tt(o3, o3, msum.re("p (h o) -> p h o", o=1).bc([128, 4, 64]), ALU.subtract)
        c.act(sq, o, AF.Square)
        c.reduce(ssum, sq.re("p (h v) -> p h v", h=4), ALU.add)
        c.ts(ssum, ssum, 1.0 / 64.0, ALU.mult)
        rstd_of(rstd, ssum, 1 if kind == "rms" else 0)
        c.tt(o3, o3, rstd.re("p (h o) -> p h o", o=1).bc([128, 4, 64]), ALU.mult)
        if gam_off is not None:
            gm = rb_small[:, gam_off:gam_off + 64]
            c.tt(o3, o3, gm.re("p (o v) -> p o v", o=1).bc([128, 4, 64]), ALU.mult)
        c.tt(ytok[:, t, mix_idx * 256:(mix_idx + 1) * 256], o, gt[:, t, :], ALU.mult)

    def hgrn(l, wp0, wp1):
        for pt in range(2):
            qps = proj_F(wp0, pt * 128, 128)
            qs = P[0][:]
            c.act(qs, qps, AF.Silu)
            fps = proj_F(wp0, 256 + pt * 128, 128)
            s = P[1][:]
            c.act(s, fps, AF.Sigmoid)
            f = P[2][:]
            c.ts(f, s, olb[:, l, pt:pt + 1], ALU.mult, lb[:, l, pt:pt + 1], ALU.add)
            k = P[3][:]
            c.ts(k, s, nolb[:, l, pt:pt + 1], ALU.mult, olb[:, l, pt:pt + 1], ALU.add)
            g = P[4][:]
            c.act(g, f, AF.Ln)
            cum, eq, ek = P[5][:], P[6][:], P[7][:]
            cumdecay(g, 1.0, cum, eq, ek)
            c.tt(qk[0 + pt][:], qs, eq, ALU.mult)
            c.tt(qk[2 + pt][:], k, ek, ALU.mult)
            c.copy(decs[pt][:], chunk_end(eq))
        for t in range(NT):
            ps_ = proj_T(wp1, 0, 512, t)
            c.copy(vt[:, t, :], ps_[:, 0:256], eng="act")
            c.act(gt[:, t, :], ps_[:, 256:512], AF.Silu)
        release(wp0)
        release(wp1)
        import os
        if os.environ.get("DBG_NOCORE"):
            return
        for t in range(NT):
            o_ps = gla_tile(l, t, [qk[0][:], qk[1][:]], [qk[2][:], qk[3][:]], [decs[0][:], decs[1][:]],
                            st_hg, 64, 2)
            if os.environ.get("DBG_NOFIN"):
                continue
            finish_simple(l, t, o_ps, 0, "rms", 0)

    def gla(l, wp2, wp3):
        gps = proj_F(wp3, 0, 16)
        glr = P[2]
        c.copy(glr[0:16, :], gps, eng="act")
        for pt in range(2):
            qps = proj_F(wp2, pt * 64, 64)
            qs = P[0][0:64, :]
            c.act(qs, qps, AF.Copy, scale=32.0 ** -0.5)
            kps = proj_F(wp2, 128 + pt * 64, 64)
            ks = P[1][0:64, :]
            c.copy(ks, kps, eng="act")
            bank = trb.next()
            zps = bank[0:64, 0:TB]
            c.mm(zps, gw2[:, pt * 64:(pt + 1) * 64], glr[0:16, :])
            e = P[3][0:64, :]
            c.act(e, zps, AF.Exp, bias=ngb2[:, l, pt:pt + 1], scale=-1.0)
            sp_ = P[4][0:64, :]
            c.act(sp_, e, AF.Ln, bias=epsc[0:64, 3:4])
            cum, eq, ek = P[5][0:64, :], P[6][0:64, :], P[7][0:64, :]
            c.scan(cum, K("reset")[0:64, 0:TB], sp_, 0.0, ALU.mult, ALU.add)
            c.act(eq, cum, AF.Exp, scale=-1.0 / 16.0)
            c.act(ek, cum, AF.Exp, scale=1.0 / 16.0)
            c.tt(qk[0 + pt][0:64, :], qs, eq, ALU.mult)
            c.tt(qk[2 + pt][0:64, :], ks, ek, ALU.mult)
            c.copy(decs[pt][0:64, :], chunk_end(eq))
        for t in range(NT):
            ps_ = proj_T(wp2, 256, 256, t)
            c.copy(vt[:, t, :], ps_, eng="act")
            ps2 = proj_T(wp3, 16, 256, t)
            c.act(gt[:, t, :], ps2, AF.Silu)
        release(wp2)
        release(wp3)
        for t in range(NT):
            o_ps = gla_tile(l, t, [qk[0][0:64, :], qk[1][0:64, :]], [qk[2][0:64, :], qk[3][0:64, :]],
                            [decs[0][0:64, :], decs[1][0:64, :]], st_gl, 32, 2)
            finish_simple(l, t, o_ps, 1, "rms", 64)

    def rope_tables(b):
        posi = V(P[0], P[0].t[:].bitcast(I32))
        c.dma("sp", posi, pos_d[0:1, b * TB:(b + 1) * TB].partition_broadcast(128))
        c.copy(posf[:], posi)
        ang = P[1][:]
        c.ts(ang, posf[:], K("invfreq"), ALU.mult)
        kq = P[2][:]
        c.ts(kq, ang, float(1.0 / (2.0 * np.pi)), ALU.mult, 12582912.0, ALU.add)
        c.ts(kq, kq, -12582912.0, ALU.add)
        r = P[3][:]
        C1 = 6.28125
        C2 = float(2.0 * np.pi - 6.28125)
        c.stt(r, kq, -C1, ang, ALU.mult, ALU.add)
        c.stt(r, kq, -C2, r, ALU.mult, ALU.add)
        c.ts(r, r, 3.14159, ALU.min, -3.14159, ALU.max)
        c.act(sinT[:], r, AF.Sin)
        ab = P[4][:]
        c.ts(ab, r, -1.0, ALU.mult)
        c.tt(ab, ab, r, ALU.max)
        c.ts(ab, ab, -1.0, ALU.mult, float(np.pi / 2.0), ALU.add)
        c.act(cosT[:], ab, AF.Sin)
        c.ts(sinT[:], sinT[:], K("sinsign"), ALU.mult)

    def ret(l, wp6, wp7):
        for which in range(2):
            for pt in range(2):
                ps_ = proj_F(wp6, which * 256 + pt * 128, 128)
                xs = P[0][:]
                c.copy(xs, ps_, eng="act")
                a = P[1][:]
                c.tt(a, xs, cosT[:], ALU.mult)
                bsw = P[2][:]
                for hh in range(2):
                    lo = slice(hh * 64, hh * 64 + 32)
                    hi = slice(hh * 64 + 32, hh * 64 + 64)
                    c.tt(bsw[lo, :], xs[hi, :], sinT[hi, :], ALU.mult)
                    c.tt(bsw[hi, :], xs[lo, :], sinT[lo, :], ALU.mult)
                c.tt(a, a, bsw, ALU.add)
                tab = K("ret_eq" if which == 0 else "ret_ek")[:, pt * C:(pt + 1) * C]
                dst = qk[which * 2 + pt]
                c.tt(dst[:].re("p (n c) -> p n c", c=C), a.re("p (n c) -> p n c", c=C),
                     tab.re("p (o c) -> p o c", o=1).bc([128, NCH, C]), ALU.mult)
        for t in range(NT):
            ps_ = proj_T(wp7, 0, 512, t)
            c.copy(vt[:, t, :], ps_[:, 0:256], eng="act")
            c.act(gt[:, t, :], ps_[:, 256:512], AF.Silu)
        release(wp6)
        release(wp7)
        for t in range(NT):
            o_ps = gla_tile(l, t, [qk[0][:], qk[1][:]], [qk[2][:], qk[3][:]], [dec_rt[0][:], dec_rt[1][:]],
                            st_rt, 64, 2)
            finish_simple(l, t, o_ps, 3, "gn", None)

    def rw_shift(l, b, i, wp, c0, n, dst):
        ps_ = proj_F(wp, c0, n)
        raw = rw_raw.next()
        if b == 0:
            c.memset(raw[0:n, 0:1], 0.0)
        else:
            c.copy(raw[0:n, 0:1], rw_carry[0:n, l, i:i + 1])
        c.copy(raw[0:n, 1:TB + 1], ps_, eng="act")
        c.copy(rw_carry[0:n, l, i:i + 1], raw[0:n, TB:TB + 1])
        tm = P[11]
        c.act(tm[0:n, :], raw[0:n, 1:TB + 1], AF.Copy, scale=omu[0:n, l, i:i + 1])
        c.stt(dst[0:n, :], raw[0:n, 0:TB], mup[0:n, l, i:i + 1], tm[0:n, :], ALU.mult, ALU.add)

    def rwkv(l, b, wp4, wp5, wp5b):
        vS = [rw_sh[0], rw_sh[1]]
        waS, rS, kS = rw_sh[2], rw_sh[3], rw_sh[4]
        rw_shift(l, b, 6, wp5, 256, 128, waS)
        g0S, g1S = P[0], P[1]
        rw_shift(l, b, 7, wp5, 384, 128, g0S)
        rw_shift(l, b, 8, wp5b, 0, 32, g1S)
        rw_shift(l, b, 4, wp5, 0, 128, vS[0])
        rw_shift(l, b, 5, wp5, 128, 128, vS[1])
        release(wp5)
        release(wp5b)
        c.act(th_t[0:64, :], waS[0:64, :], AF.Tanh)
        c.act(g0S[:], g0S[:], AF.Sigmoid)
        c.act(g1S[0:32, :], g1S[0:32, :], AF.Sigmoid)
        for t in range(NT):
            bank = trb.next()
            c.mm(bank[:, 0:256], g0S[:, t * 128:(t + 1) * 128], g2a[:], start=True, stop=False)
            c.mm(bank[:, 0:256], g1S[0:32, t * 128:(t + 1) * 128], g2b[:], start=False, stop=True)
            c.copy(rw_gtok[:, t, :], bank[:, 0:256], eng="act")
        if l > 0:
            bank = trb.next()
            lv = bank[0:32, 0:TB]
            for k in range(2):
                c.mm(lv, v1s[:, k, :], vS[k][:], start=(k == 0), stop=(k == 1))
            c.copy(lvs_t[0:32, :], lv, eng="act")
        for pt in range(2):
            if l == 0:
                c.copy(vfirst[:, pt, :], vS[pt][:])
            else:
                bank = trb.next()
                c.mm(bank[:, 0:TB], v2s[:, pt * 128:(pt + 1) * 128], lvs_t[0:32, :])
                sgm = P[2][:]
                c.act(sgm, bank[:, 0:TB], AF.Sigmoid, bias=v0p[:, l - 1, pt:pt + 1])
                dv = P[3][:]
                c.tt(dv, vfirst[:, pt, :], vS[pt][:], ALU.subtract)
                c.tt(dv, dv, sgm, ALU.mult)
                c.tt(vS[pt][:], vS[pt][:], dv, ALU.add)
            for t in range(NT):
                bank = trb.next()
                c.tr(bank[:, 0:128], vS[pt][:, t * 128:(t + 1) * 128], K("ident"))
                c.copy(rw_vtok[:, t, pt * 128:(pt + 1) * 128], bank[:, 0:128], eng="act")
        for pt in range(2):
            rw_shift(l, b, 0 + pt, wp4, pt * 128, 128, rS)
            rw_shift(l, b, 2 + pt, wp4, 256 + pt * 128, 128, kS)
            if pt == 1:
                release(wp4)
            bank = trb.next()
            c.mm(bank[:, 0:TB], w2s[:, pt * 128:(pt + 1) * 128], th_t[0:64, :])
            t1 = P[2][:]
            c.act(t1, bank[:, 0:TB], AF.Exp, bias=nw0[:, l, pt:pt + 1], scale=-1.0)
            c.act(t1, t1, AF.Ln, bias=epsc[:, 3:4])
            c.ts(t1, t1, -1.0, ALU.mult, -0.5, ALU.add)
            c.act(t1, t1, AF.Exp)
            g = P[3][:]
            c.ts(g, t1, -1.0, ALU.mult)
            cum, eq, ek = P[4][:], P[5][:], P[6][:]
            cumdecay(g, 1.0, cum, eq, ek)
            c.copy(rw_dec[:], chunk_end(eq))
            bank = trb.next()
            c.mm(bank[:, 0:TB], a2s[64:128, pt * 128:(pt + 1) * 128], waS[64:128, :])
            ag = P[7][:]
            c.act(ag, bank[:, 0:TB], AF.Sigmoid, bias=a0[:, l, pt:pt + 1])
            kk = P[8][:]
            c.ts(kk, kS[:], kkp[:, l, pt:pt + 1], ALU.mult)
            nrm = P[9][:]
            c.act(nrm, kk, AF.Square)
            bank = trb.next()
            c.mm(bank[:, 0:TB], K("blockones"), nrm)
            c.act(nrm, bank[:, 0:TB], AF.Sqrt)
            c.ts(nrm, nrm, 1e-12, ALU.max)
            c.act(nrm, nrm, AF.Ln)
            c.act(nrm, nrm, AF.Exp, scale=-1.0)
            c.tt(kk, kk, nrm, ALU.mult)
            fk = P[9][:]
            c.ts(fk, ag, -1.0, ALU.add, kap[:, l, pt:pt + 1], ALU.mult)
            c.ts(fk, fk, 1.0, ALU.add)
            km = P[10][:]
            c.tt(km, kS[:], fk, ALU.mult)
            bo = P[2][:]
            c.stt(bo, rS[:], rkp[:, l, pt:pt + 1], km, ALU.mult, ALU.mult)
            for t in range(NT):
                bank = trb.next()
                c.mm(bank[:, 0:2], bo[:, t * 128:(t + 1) * 128], K("headsel"))
                c.copy(rw_bonus[:, t, pt * 2:pt * 2 + 2], bank[:, 0:2], eng="act")
            c.tt(rw_ar[:, 1, :], rS[:], eq, ALU.mult)
            c.tt(rw_kt[:], km, ek, ALU.mult)
            bb = P[9][:]
            c.tt(bb, kk, ag, ALU.mult)
            c.tt(rw_bt[:], bb, ek, ALU.mult)
            ex = P[9][:]
            c.tt(ex, cum, g, ALU.subtract)
            c.act(ex, ex, AF.Exp)
            c.stt(rw_ar[:, 0, :], kk, -1.0, ex, ALU.mult, ALU.mult)
            for t in range(NT):
                for hh in range(2):
                    rwkv_core(l, t, pt, hh)
        for t in range(NT):
            rwkv_finish(l, t)

    def rwkv_core(l, t, pt, hh):
        h = pt * 2 + hh
        tok = slice(t * 128, (t + 1) * 128)
        pr = slice(hh * 64, hh * 64 + 64)
        ar = rw_ar[pr, :, tok]
        kt = rw_kt[pr, tok]
        bt = rw_bt[pr, tok]
        at = rw_ar[pr, 0, tok]
        rt = rw_ar[pr, 1, tok]
        Z = zst[l][pt]
        vtk = rw_vtok[:, t, h * 64:(h + 1) * 64]
        ocol = slice(h * 64, (h + 1) * 64)
        b1 = trb.next()
        c.mm(b1[:, 0:256].re("p (a n) -> p a n", a=2), kt, ar)
        Ak = a_ring.next()[:]
        c.tt(Ak[:, 0:128], b1[:, 0:128], K("mask_strict"), ALU.mult)
        c.tt(Ak[:, 128:256], b1[:, 128:256], K("mask_incl"), ALU.mult)
        b2 = trb.next()
        c.mm(b2[:, 0:256].re("p (a n) -> p a n", a=2), bt, ar)
        Ab = a_ring.next()[:]
        c.tt(Ab[:, 0:128], b2[:, 0:128], K("mask_strict"), ALU.mult)
        c.tt(Ab[:, 128:256], b2[:, 128:256], K("mask_incl"), ALU.mult)
        b3 = trb.next()
        c.mm(b3[:, 0:128], at, bt)
        Pm = m_ring.next()[:]
        c.tt(Pm, b3[:, 0:128], K("mask_strictT"), ALU.mult)
        Q = Ab[:, 0:128]
        Tc = TTt[0][:]
        c.tt(Tc, Q, K("ident"), ALU.add)
        cur = 0
        for i in range(1, 6):
            bq = None
            if i < 5:
                bq = trb.next()
                c.mm(bq[:, 0:128], Pm, Q)
            bp = trb.next()
            c.mm(bp[:, 0:128], Q, Pm)
            Pn = m_ring.next()[:]
            c.copy(Pn, bp[:, 0:128], eng="act")
            if i < 5:
                Qn = m_ring.next()[:]
                c.copy(Qn, bq[:, 0:128])
                Q = Qn
            Pm = Pn
            bt_ = trb.next()
            c.mm(bt_[:, 0:128], Pm, Tc)
            Tn = TTt[1 - cur][:]
            c.tt(Tn, Tc, bt_[:, 0:128], ALU.add)
            Tc = Tn
            cur = 1 - cur
        bk = trb.next()
        c.tr(bk[:, 0:64], kt, K("ident")[pr, pr])
        c.tr(bk[:, 64:128], bt, K("ident")[pr, pr])
        kb_tok = tok_ring.next()[:]
        c.copy(kb_tok, bk[:, 0:128], eng="act")
        gb_ = accb.next()
        c.mm(gb_[:, 0:64], Ak[:, 0:128], vtk, start=True, stop=False, sg=True)
        utok = u_ring.next()[:]
        for ch in range(2):
            rows = slice(ch * 64, (ch + 1) * 64)
            cg = t * 2 + ch
            c.mm(gb_[rows, 0:64], at[:, rows], Z[pr, :], start=False, stop=True, sg=True)
            rhs_u = u_ring.next()[:]
            c.copy(rhs_u[rows, :], gb_[rows, 0:64])
            ub = trb.next()
            c.mm(ub[rows, 0:64], Tc[rows, rows], rhs_u[rows, :])
            c.copy(utok[rows, :], ub[rows, 0:64], eng="act")
            ob = trb.next()
            c.mm(ob[rows, 0:64], rt[:, rows], Z[pr, :])
            c.copy(o_acc[rows, t, ocol], ob[rows, 0:64], eng="act")
            zb = trb.next()
            c.mm(zb[pr, 0:64], K("identZ")[pr, :], Z[pr, :], start=True, stop=False, sg=True)
            c.mm(zb[pr, 0:64], kb_tok[rows, 0:64], vtk[rows, :], start=False, stop=False, sg=True)
            c.mm(zb[pr, 0:64], kb_tok[rows, 64:128], utok[rows, :], start=False, stop=True, sg=True)
            c.act(Z[pr, :], zb[pr, 0:64], AF.Copy, scale=rw_dec[pr, cg:cg + 1])
        ob2 = trb.next()
        c.mm(ob2[:, 0:64], Ak[:, 128:256], vtk, start=True, stop=False)
        c.mm(ob2[:, 0:64], Ab[:, 128:256], utok, start=False, stop=True)
        c.tt(o_acc[:, t, ocol], o_acc[:, t, ocol], ob2[:, 0:64], ALU.add)

    def rwkv_finish(l, t):
        o = o_sb[:]
        c.copy(o, o_acc[:, t, :])
        o3 = o.re("p (h v) -> p h v", h=4)
        msum = stat_mx[:, 0:4]
        ssum = stat_mx[:, 4:8]
        rstd = stat_mx[:, 8:12]
        c.reduce(msum, o3, ALU.add)
        c.ts(msum, msum, 1.0 / 64.0, ALU.mult)
        c.tt(o3, o3, msum.re("p (h o) -> p h o", o=1).bc([128, 4, 64]), ALU.subtract)
        sq = sq_sb[:]
        c.act(sq, o, AF.Square)
        c.reduce(ssum, sq.re("p (h v) -> p h v", h=4), ALU.add)
        c.ts(ssum, ssum, 1.0 / 64.0, ALU.mult)
        rstd_of(rstd, ssum, 2)
        c.tt(o3, o3, rstd.re("p (h o) -> p h o", o=1).bc([128, 4, 64]), ALU.mult)
        c.tt(o, o, rb_small[:, 128:384], ALU.mult)
        c.tt(o, o, rb_small[:, 384:640], ALU.add)
        c.tt(sq.re("p (h v) -> p h v", h=4), rw_vtok[:, t, :].re("p (h v) -> p h v", h=4),
             rw_bonus[:, t, :].re("p (h o) -> p h o", o=1).bc([128, 4, 64]), ALU.mult)
        c.tt(o, o, sq, ALU.add)
        c.tt(ytok[:, t, 512:768], o, rw_gtok[:, t, :], ALU.mult)

    def to_hT(t):
        for hf in range(2):
            bank = trb.next()
            for k4 in range(4):
                k = hf * 4 + k4
                c.tr(bank[:, k4 * 128:(k4 + 1) * 128], htok[:, t, k * 128:(k + 1) * 128], K("ident"))
            c.copy(hT[:, hf * 4:(hf + 1) * 4, t * 128:(t + 1) * 128],
                   bank[:, 0:512].re("p (k n) -> p k n", k=4), eng="act")

    def layer_norm(l, which):
        c.dma("sp", lnrow_g[:], ln_d[which + "_g"][l:l + 1, :].partition_broadcast(128))
        c.dma("sp", lnrow_b[:], ln_d[which + "_b"][l:l + 1, :].partition_broadcast(128))
        for t in range(NT):
            z = htok[:, t, :]
            st6 = stat_ln[:, 0:12]
            for hf in range(2):
                zz = z[:, hf * 512:(hf + 1) * 512]
                dst = stat_ln[:, hf * 6:(hf + 1) * 6]
                c.op("dve", lambda zz=zz, dst=dst: nc.vector.bn_stats(dst.ap, zz.ap), reads=[zz], writes=[dst])
            mv = stat_ln[:, 12:14]
            c.op("dve", lambda: nc.vector.bn_aggr(mv.ap, st6.ap), reads=[st6], writes=[mv])
            rs = stat_ln[:, 14:15]
            rstd_of(rs, stat_ln[:, 13:14], 0)
            c.ts(z, z, stat_ln[:, 12:13], ALU.subtract, rs, ALU.mult)
            c.tt(z, z, lnrow_g[:], ALU.mult)
            c.tt(z, z, lnrow_b[:], ALU.add)
            to_hT(t)

    def residual_from_bank(t, hf, bank_v):
        z = htok[:, t, hf * 512:(hf + 1) * 512]
        c.stt(z, z, ALPHA, bank_v, ALU.mult, ALU.add)

    def gate_up(cols_list):
        ft = 0
        for (c0, n) in cols_list:
            wg = take_piece()
            wu = take_piece()
            for j in range(n // 128):
                gps = proj_F(wg, j * 128, 128)
                ups = proj_F(wu, j * 128, 128)
                sl = P[ft % 3][:]
                c.act(sl, gps, AF.Silu)
                c.tt(mT[:, ft, :], sl, ups, ALU.mult)
                ft += 1
            release(wg)
            release(wu)
        return ft

    def down_proj(nft, row_groups, consume):
        for hf in range(2):
            accs = [accb.next() for _ in range(NT)]
            for (r0, nk) in row_groups:
                wd = take_piece()
                for kk_ in range(nk):
                    ft = r0 + kk_
                    for t in range(NT):
                        c.mm(accs[t][:, 0:512], mT[:, ft, t * 128:(t + 1) * 128], wd.v[:, kk_, :],
                             start=(ft == 0), stop=(ft == nft - 1))
                release(wd)
            for t in range(NT):
                consume(t, hf, accs[t][:, 0:512])

    def ffn_dense(l):
        gate_up(GU_COLS_D)
        down_proj(22, ((0, 8), (8, 8), (16, 6)), residual_from_bank)

    def moe(l):
        i = l // 2
        for t in range(NT):
            bank = trb.next()
            lg = bank[:, 0:NE]
            for k in range(8):
                c.mm(lg, hT[:, k, t * 128:(t + 1) * 128], mrs_bf[:, i, k, :], start=(k == 0), stop=(k == 7))
            lgs = stat_moe[:, 0:8]
            c.copy(lgs, lg)
            m8 = stat_moe[:, 8:16]
            c.op("dve", lambda: nc.vector.max(m8.ap, lgs.ap), reads=[lgs], writes=[m8])
            nm1 = stat_moe[:, 32:33]
            c.ts(nm1, m8[:, 0:1], -1.0, ALU.mult)
            ex = stat_moe[:, 16:24]
            c.act(ex, lgs, AF.Exp, bias=nm1)
            sel = stat_moe[:, 24:32]
            c.ts(sel, lgs, m8[:, 1:2], ALU.is_ge)
            c.tt(ex, ex, sel, ALU.mult)
            ssum = stat_moe[:, 33:34]
            c.reduce(ssum, ex, ALU.add)
            c.recip(ssum, ssum)
            c.ts(gates[:, t, :], ex, ssum, ALU.mult)
        c.memset(facc[:], 0.0)
        for e in range(NE):
            gate_up(GU_COLS_E)

            def cons(t, hf, bank_v, e=e):
                fa = facc[:, t, hf * 512:(hf + 1) * 512]
                c.stt(fa, bank_v, gates[:, t, e:e + 1], fa, ALU.mult, ALU.add)
            down_proj(11, ((0, 8), (8, 3)), cons)
        for t in range(NT):
            z = htok[:, t, :]
            c.stt(z, z, ALPHA, facc[:, t, :], ALU.mult, ALU.add)

    for l in range(L):
        for pt in range(2):
            for st in (st_hg, st_rt, st_gl):
                c.memset(st[pt][0][l][:], 0.0)
                c.memset(st[pt][1][l][:], 0.0)
            c.memset(zst[l][pt][:], 0.0)
    c.memset(ytok[:], 0.0)

    for b in range(NBLK):
        for t in range(NT):
            r0 = b * TB + t * 128
            c.dma("sp", htok[:, t, :], x_d[r0:r0 + 128, :])
            to_hT(t)
        if mixers[3]:
            rope_tables(b)
        for l in range(L):
            load_layer_params(l)
            wp0, wp1 = take_piece(), take_piece()
            if mixers[0]:
                hgrn(l, wp0, wp1)
            else:
                release(wp0)
                release(wp1)
            wp2, wp3 = take_piece(), take_piece()
            if mixers[1]:
                gla(l, wp2, wp3)
            else:
                release(wp2)
                release(wp3)
            wp4, wp5, wp5b = take_piece(), take_piece(), take_piece()
            if mixers[2]:
                rwkv(l, b, wp4, wp5, wp5b)
            else:
                release(wp4)
                release(wp5)
                release(wp5b)
            wp6, wp7 = take_piece(), take_piece()
            if mixers[3]:
                ret(l, wp6, wp7)
            else:
                release(wp6)
                release(wp7)
            for t in range(NT):
                for hf in range(2):
                    bank = trb.next()
                    bv = V(bank, bank.t[:, 0:256].bitcast(BF16))
                    for k4 in range(4):
                        k = hf * 4 + k4
                        c.tr(bv[:, k4 * 128:(k4 + 1) * 128], ytok[:, t, k * 128:(k + 1) * 128], ident_bf[:])
                    c.copy(yT[:, hf * 4:(hf + 1) * 4, t * 128:(t + 1) * 128],
                           bv.re("p (k n) -> p k n", k=4), eng="act")
            wo = [take_piece(), take_piece()]
            for t in range(NT):
                for hf in range(2):
                    bank = trb.next()
                    for k in range(8):
                        c.mm(bank[:, 0:512], yT[:, k, t * 128:(t + 1) * 128], wo[hf].v[:, k, :],
                             start=(k == 0), stop=(k == 7))
                    residual_from_bank(t, hf, bank[:, 0:512])
            release(wo[0])
            release(wo[1])
            layer_norm(l, "ln1")
            if ffn:
                if l % 2 == 0:
                    ffn_dense(l)
                else:
                    moe(l)
                layer_norm(l, "ln2")
        for t in range(NT):
            r0 = b * TB + t * 128
            c.dma("sp", V(out_tt, out_d[r0:r0 + 128, :]), htok[:, t, :])
    c.wait_all("sp", [out_tt])
    assert pst["taken"] == len(pieces), (pst, len(pieces))
    return nc, carr, c


_CACHE = {}

NAMES = ["w_in", "w_out", "ln1_g", "ln1_b", "ln2_g", "ln2_b", "hgrn_lb_logits", "hgrn_norm_g", "gla_gate_w2",
         "gla_gate_b", "gla_norm_g", "rwkv_mu", "rwkv_w0", "rwkv_w2", "rwkv_a0", "rwkv_a2", "rwkv_g2", "rwkv_k_k",
         "rwkv_k_a", "rwkv_r_k", "rwkv_lnx_g", "rwkv_lnx_b", "rwkv_v0", "rwkv_v1", "rwkv_v2", "ffn_w_gate",
         "ffn_w_up", "ffn_w_down", "moe_router", "moe_w_gate", "moe_w_up", "moe_w_down"]


def run(inputs, T, L=NL, TB=512, **kw):
    x = np.asarray(inputs["x"], dtype=np.float32)
    B = x.shape[0]
    key = (T, L, TB, tuple(sorted(kw.items())))
    if key not in _CACHE:
        _CACHE[key] = build(T, L, TB, **kw)
    nc, carr, c = _CACHE[key]
    shared = {}
    for n in NAMES:
        a = np.ascontiguousarray(np.asarray(inputs[n], dtype=np.float32))
        if n == "rwkv_r_k":
            a = a.reshape(NL, 256)
        shared[n] = a
    shared["consts"] = carr
    pos = np.asarray(inputs["positions"]).astype(np.int32)
    in_maps = []
    for b in range(B):
        m = dict(shared)
        m["x"] = np.ascontiguousarray(x[b, :T])
        m["positions"] = np.ascontiguousarray(pos[b:b + 1, :T])
        in_maps.append(m)
    res = run_bass_kernel_spmd(nc, in_maps, core_ids=list(range(B)))
    return np.stack([np.asarray(r["out"]) for r in res.results], axis=0)


def kernel(**inputs):
    x = np.asarray(inputs["x"])
    B, S, _ = x.shape
    out = run(inputs, S)
    return out.astype(x.dtype)
```

```python
import numpy as np
import ml_dtypes
import concourse.bass as bass
import concourse.mybir as mybir
from concourse.bass_utils import run_bass_kernel_spmd

F32 = mybir.dt.float32
BF16 = mybir.dt.bfloat16
I32 = mybir.dt.int32
AF = mybir.ActivationFunctionType
ALU = mybir.AluOpType
AX = mybir.AxisListType

D = 1024
NL = 4
INC = 3888
FFD = 2816
NE = 8
FFE = 1408
ALPHA = (2.0 * NL) ** 0.25
C = 64


class V:
    __slots__ = ("tt", "ap")

    def __init__(self, tt, ap):
        self.tt = tt
        self.ap = ap

    def __getitem__(self, k):
        return V(self.tt, self.ap[k])

    def re(self, s, **kw):
        return V(self.tt, self.ap.rearrange(s, **kw))

    def bc(self, shape):
        return V(self.tt, self.ap.to_broadcast(list(shape)))


class TT:
    __slots__ = ("t", "name", "w", "r", "al", "pe_row")

    def __init__(self, t, name):
        self.t = t
        self.name = name
        self.w = None
        self.r = []
        self.al = []
        self.pe_row = None

    def __getitem__(self, k):
        return V(self, self.t[k])


class Ctx:
    NDMA = 24

    def __init__(self, nc, same=True):
        self.nc = nc
        self.same = same
        self.engs = {"pe": nc.tensor, "dve": nc.vector, "act": nc.scalar, "pool": nc.gpsimd, "sp": nc.sync}
        self.sem = {}
        self.cnt = {}
        self.waited = {}
        self._cms = []
        for k in self.engs:
            cm = nc.semaphore("s_" + k)
            self.sem[k] = cm.__enter__()
            self._cms.append(cm)
            self.cnt[k] = 0
            self.waited[k] = {}
        self.dsem = []
        self.dcnt = []
        for i in range(self.NDMA):
            cm = nc.semaphore("d_%d" % i)
            self.dsem.append(cm.__enter__())
            self._cms.append(cm)
            self.dcnt.append(0)
        self.dnext = {"sp": 0, "pool": 0, "act": 0}
        self.drange = {"sp": (0, 14), "pool": (14, 22), "act": (22, 24)}
        self.ntile = 0
        self.ninst = 0

    def sb(self, shape, dt, name=None):
        self.ntile += 1
        name = name or "t%d" % self.ntile
        cm = self.nc.sbuf_tensor(name, list(shape), dt)
        t = cm.__enter__()
        self._cms.append(cm)
        return TT(t, name)

    def ps(self, shape, dt, name=None):
        self.ntile += 1
        name = name or "p%d" % self.ntile
        cm = self.nc.psum_tensor(name, list(shape), dt)
        t = cm.__enter__()
        self._cms.append(cm)
        return TT(t, name)

    def _semof(self, key):
        if key in self.sem:
            return self.sem[key]
        return self.dsem[int(key[1:])]

    def _deps(self, eng, reads, writes):
        need = {}

        def add(dep):
            if dep is None:
                return
            k, v = dep
            if need.get(k, 0) < v:
                need[k] = v
        for t in reads:
            add(t.w)
            for a in t.al:
                add(a.w)
        for t in writes:
            add(t.w)
            for d in t.r:
                add(d)
            for a in t.al:
                add(a.w)
                for d in a.r:
                    add(d)
        h = self.engs[eng]
        for k, v in need.items():
            if k == eng and (not self.same or eng == "pe"):
                continue
            if self.waited[eng].get(k, 0) >= v:
                continue
            h.wait_ge(self._semof(k), v)
            self.waited[eng][k] = v
            self.ninst += 1

    def _mark(self, me, reads, writes):
        for t in reads:
            if len(t.r) > 64:
                mx = {}
                for (k, v) in t.r:
                    if mx.get(k, 0) < v:
                        mx[k] = v
                t.r = list(mx.items())
            t.r.append(me)
        for t in writes:
            t.w = me
            t.r = []

    def op(self, eng, fn, reads=(), writes=()):
        reads = [x.tt if isinstance(x, V) else x for x in reads if x is not None]
        writes = [x.tt if isinstance(x, V) else x for x in writes if x is not None]
        self._deps(eng, reads, writes)
        ins = fn()
        self.cnt[eng] += 1
        ins.then_inc(self.sem[eng], 1)
        self._mark((eng, self.cnt[eng]), reads, writes)
        self.ninst += 1
        return ins

    def dma(self, q, out, in_, **kw):
        reads = [in_.tt] if isinstance(in_, V) else []
        writes = [out.tt] if isinstance(out, V) else []
        lo, hi = self.drange[q]
        i = lo + self.dnext[q]
        self.dnext[q] = (self.dnext[q] + 1) % (hi - lo)
        key = "d%d" % i
        h = self.engs[q]
        if self.dcnt[i] > 0 and self.waited[q].get(key, 0) < self.dcnt[i]:
            h.wait_ge(self.dsem[i], self.dcnt[i])
            self.waited[q][key] = self.dcnt[i]
        self._deps(q, reads, writes)
        oa = out.ap if isinstance(out, V) else out
        ia = in_.ap if isinstance(in_, V) else in_
        ins = h.dma_start(out=oa, in_=ia, **kw)
        self.dcnt[i] += 16
        ins.then_inc(self.dsem[i], 16)
        self._mark((key, self.dcnt[i]), reads, writes)
        self.ninst += 1
        return ins

    def _pe_row_guard(self, out, lhsT):
        row = (int(lhsT.ap.start_partition()), int(lhsT.ap.partition_size()))
        t = out.tt
        if t.pe_row is not None and t.pe_row != row and t.w is not None and t.w[0] == "pe":
            v = t.w[1]
            if self.waited["pe"].get("pe", 0) < v:
                self.nc.tensor.wait_ge(self.sem["pe"], v)
                self.waited["pe"]["pe"] = v
                self.ninst += 1
        t.pe_row = row

    def mm(self, out, lhsT, rhs, start=True, stop=True, sg=False):
        nc = self.nc
        self._pe_row_guard(out, lhsT)
        return self.op("pe", lambda: nc.tensor.matmul(out.ap, lhsT=lhsT.ap, rhs=rhs.ap, start=start, stop=stop,
                                                      skip_group_check=sg),
                       reads=[lhsT, rhs], writes=[out])

    def tr(self, out, in_, ident):
        nc = self.nc
        self._pe_row_guard(out, in_)
        return self.op("pe", lambda: nc.tensor.transpose(out.ap, in_.ap, ident.ap), reads=[in_, ident], writes=[out])

    def act(self, out, in_, func, bias=None, scale=None, eng="act"):
        nc = self.nc
        kw = {}
        rd = [in_]
        if bias is not None:
            if isinstance(bias, V):
                kw["bias"] = bias.ap
                rd.append(bias)
            else:
                kw["bias"] = bias
        if scale is not None:
            if isinstance(scale, V):
                kw["scale"] = scale.ap
                rd.append(scale)
            else:
                kw["scale"] = scale
        if func == AF.Copy and isinstance(scale, V):
            func = AF.Identity
        return self.op("act", lambda: nc.scalar.activation(out.ap, in_.ap, func, **kw), reads=rd, writes=[out])

    def tt(self, out, in0, in1, op, eng="dve"):
        h = self.engs[eng]
        return self.op(eng, lambda: h.tensor_tensor(out.ap, in0.ap, in1.ap, op), reads=[in0, in1], writes=[out])

    def ts(self, out, in0, s1, op0, s2=None, op1=None, eng="dve"):
        h = self.engs[eng]
        rd = [in0]
        a1 = s1
        if isinstance(s1, V):
            rd.append(s1)
            a1 = s1.ap
        a2 = s2
        if isinstance(s2, V):
            rd.append(s2)
            a2 = s2.ap
        if op1 is None:
            return self.op(eng, lambda: h.tensor_scalar(out.ap, in0.ap, a1, None, op0), reads=rd, writes=[out])
        return self.op(eng, lambda: h.tensor_scalar(out.ap, in0.ap, a1, a2, op0, op1), reads=rd, writes=[out])

    def stt(self, out, in0, scalar, in1, op0, op1):
        nc = self.nc
        rd = [in0, in1]
        a = scalar
        if isinstance(scalar, V):
            rd.append(scalar)
            a = scalar.ap
        return self.op("dve", lambda: nc.vector.scalar_tensor_tensor(out.ap, in0.ap, a, in1.ap, op0, op1),
                       reads=rd, writes=[out])

    def copy(self, out, in_, eng="dve"):
        nc = self.nc
        if eng == "act":
            return self.op("act", lambda: nc.scalar.copy(out.ap, in_.ap), reads=[in_], writes=[out])
        h = self.engs[eng]
        return self.op(eng, lambda: h.tensor_copy(out.ap, in_.ap), reads=[in_], writes=[out])

    def memset(self, out, val, eng="dve"):
        h = self.engs[eng]
        return self.op(eng, lambda: h.memset(out.ap, val), reads=[], writes=[out])

    def scan(self, out, d0, d1, init, op0, op1):
        nc = self.nc
        rd = [d0, d1]
        a = init
        if isinstance(init, V):
            rd.append(init)
            a = init.ap
        return self.op("dve", lambda: nc.vector.tensor_tensor_scan(out.ap, d0.ap, d1.ap, a, op0, op1),
                       reads=rd, writes=[out])

    def reduce(self, out, in_, op, axis=AX.X):
        nc = self.nc
        return self.op("dve", lambda: nc.vector.tensor_reduce(out.ap, in_.ap, axis, op), reads=[in_], writes=[out])

    def recip(self, out, in_):
        nc = self.nc
        return self.op("dve", lambda: nc.vector.reciprocal(out.ap, in_.ap), reads=[in_], writes=[out])

    def wait_all(self, eng, tts):
        self._deps(eng, tts, ())


class Arena:
    def __init__(self, c, nbytes, name):
        self.tt = c.sb([128, nbytes // 4], F32, name)
        self.views = []
        self.nbytes = nbytes

    def view(self, off, shape, dt, name):
        esz = 4 if dt in (F32, I32) else 2
        n = esz
        for s in shape[1:]:
            n *= s
        assert off % 4 == 0 and n % 4 == 0 and off + n <= self.nbytes, (name, off, n, self.nbytes)
        ap = self.tt.t[:, off // 4:(off + n) // 4]
        if dt != F32:
            ap = ap.bitcast(dt)
        if len(shape) == 3:
            ap = ap.rearrange("p (a b) -> p a b", a=shape[1])
        elif len(shape) == 4:
            ap = ap.rearrange("p (a b c) -> p a b c", a=shape[1], b=shape[2])
        if shape[0] < 128:
            ap = ap[0:shape[0]]
        t = TT(ap, name)
        for (v, lo, hi) in self.views:
            if lo < off + n and off < hi:
                t.al.append(v)
                v.al.append(t)
        self.views.append((t, off, off + n))
        return t


class Ring:
    def __init__(self, tiles):
        self.tiles = tiles
        self.i = 0

    def next(self):
        t = self.tiles[self.i]
        self.i = (self.i + 1) % len(self.tiles)
        return t


def _consts(TB):
    p = np.arange(128)
    f = {}
    j = p[:, None]
    i = p[None, :]
    same = (j // C) == (i // C)
    f["mask_incl"] = (same & (j <= i)).astype(np.float32)
    f["mask_strict"] = (same & (j < i)).astype(np.float32)
    f["mask_strictT"] = (same & (j > i)).astype(np.float32)
    f["ident"] = np.eye(128, dtype=np.float32)
    f["blockones"] = same.astype(np.float32)
    f["identZ"] = ((p[:, None] % 64) == np.arange(64)[None, :]).astype(np.float32)
    hs = np.zeros((128, 2), np.float32)
    hs[:64, 0] = 1.0
    hs[64:, 1] = 1.0
    f["headsel"] = hs
    t = np.arange(TB)
    f["reset"] = np.broadcast_to((t % C != 0).astype(np.float32)[None, :], (128, TB)).copy()
    half = 32
    inv = (10000.0 ** (-np.arange(half, dtype=np.float32) / half)).astype(np.float32)
    d = p % 64
    f["invfreq"] = inv[d % 32][:, None].astype(np.float32)
    f["sinsign"] = np.where(d < 32, 1.0, -1.0)[:, None].astype(np.float32)
    lg = np.log(1.0 - 2.0 ** (-5.0 - np.arange(4, dtype=np.float64)))
    idx = np.arange(C, dtype=np.float64)
    req = np.zeros((128, 2, C), np.float32)
    rek = np.zeros((128, 2, C), np.float32)
    rdec = np.zeros((128, 2), np.float32)
    for tl in range(2):
        for pp in range(128):
            h = tl * 2 + pp // 64
            req[pp, tl] = np.exp((idx + 1.0) * lg[h])
            rek[pp, tl] = np.exp(-(idx + 1.0) * lg[h]) * (64.0 ** -0.5)
            rdec[pp, tl] = np.exp(C * lg[h])
    f["ret_eq"] = req.reshape(128, 2 * C)
    f["ret_ek"] = rek.reshape(128, 2 * C)
    f["ret_dec"] = rdec
    names = list(f.keys())
    offs = {}
    o = 0
    for n in names:
        offs[n] = (o, f[n].shape[1])
        o += f[n].shape[1]
    arr = np.concatenate([f[n] for n in names], axis=1).astype(np.float32)
    return arr, offs


def build(T, L=NL, TB=512, mixers=(1, 1, 1, 1), ffn=True, same=True):
    assert T % TB == 0 and TB % 128 == 0
    NT = TB // 128
    NBLK = T // TB
    NCH = TB // C
    nc = bass.Bass("TRN2", target_bir_lowering=False)
    c = Ctx(nc, same=same)
    carr, coff = _consts(TB)

    def din(name, shape, dt=F32):
        return nc.dram_tensor(name, list(shape), dt, kind="ExternalInput").ap()

    x_d = din("x", [T, D])
    pos_d = din("positions", [1, T], I32)
    w_in_d = din("w_in", [NL, D, INC])
    w_out_d = din("w_out", [NL, D, D])
    ln_d = {k: din(k, [NL, D]) for k in ("ln1_g", "ln1_b", "ln2_g", "ln2_b")}
    lb_d = din("hgrn_lb_logits", [NL, 256])
    hng_d = din("hgrn_norm_g", [NL, 64])
    gw2_d = din("gla_gate_w2", [NL, 16, 128])
    gb_d = din("gla_gate_b", [NL, 128])
    gng_d = din("gla_norm_g", [NL, 64])
    mu_d = din("rwkv_mu", [NL, 1056])
    w0_d = din("rwkv_w0", [NL, 256])
    w2_d = din("rwkv_w2", [NL, 64, 256])
    a0_d = din("rwkv_a0", [NL, 256])
    a2_d = din("rwkv_a2", [NL, 64, 256])
    g2_d = din("rwkv_g2", [NL, 160, 256])
    kk_d = din("rwkv_k_k", [NL, 256])
    ka_d = din("rwkv_k_a", [NL, 256])
    rk_d = din("rwkv_r_k", [NL, 256])
    lxg_d = din("rwkv_lnx_g", [NL, 256])
    lxb_d = din("rwkv_lnx_b", [NL, 256])
    v0_d = din("rwkv_v0", [NL - 1, 256])
    v1_d = din("rwkv_v1", [NL - 1, 256, 32])
    v2_d = din("rwkv_v2", [NL - 1, 32, 256])
    fg_d = din("ffn_w_gate", [2, D, FFD])
    fu_d = din("ffn_w_up", [2, D, FFD])
    fd_d = din("ffn_w_down", [2, FFD, D])
    mr_d = din("moe_router", [2, D, NE])
    mg_d = din("moe_w_gate", [2, NE, D, FFE])
    mu2_d = din("moe_w_up", [2, NE, D, FFE])
    md_d = din("moe_w_down", [2, NE, FFE, D])
    cst_d = din("consts", list(carr.shape))
    out_d = nc.dram_tensor("out", [T, D], F32, kind="ExternalOutput").ap()

    cst = c.sb([128, carr.shape[1]], F32, "cst")
    c.dma("sp", cst[:], cst_d)

    def K(name):
        o, n = coff[name]
        return cst[:, o:o + n]
    ident_bf = c.sb([128, 128], BF16, "identbf")
    c.copy(ident_bf[:], K("ident"))
    mask_bf = c.sb([128, 128], BF16, "maskbf")
    c.copy(mask_bf[:], K("mask_incl"))

    def pp_tile(dram, n, name, nl=NL):
        t = c.sb([128, nl, n], F32, name)
        for l in range(nl):
            for k in range(n):
                c.dma("sp", t[:, l, k:k + 1], dram[l:l + 1, k * 128:(k + 1) * 128].rearrange("o p -> p o"))
        return t

    lbl = pp_tile(lb_d, 2, "lbl")
    w0 = pp_tile(w0_d, 2, "w0")
    a0 = pp_tile(a0_d, 2, "a0")
    kkp = pp_tile(kk_d, 2, "kkp")
    kap = pp_tile(ka_d, 2, "kap")
    rkp = pp_tile(rk_d, 2, "rkp")
    v0p = pp_tile(v0_d, 2, "v0p", nl=NL - 1)
    mup = c.sb([128, NL, 9], F32, "mup")
    c.memset(mup[:], 0.0)
    for l in range(NL):
        for k in range(8):
            c.dma("sp", mup[:, l, k:k + 1], mu_d[l:l + 1, k * 128:(k + 1) * 128].rearrange("o p -> p o"))
        c.dma("sp", mup[0:32, l, 8:9], mu_d[l:l + 1, 1024:1056].rearrange("o p -> p o"))
    omu = c.sb([128, NL, 9], F32, "omu")
    c.ts(omu[:], mup[:], -1.0, ALU.mult, 1.0, ALU.add)
    gb2 = c.sb([64, NL, 2], F32, "gb2")
    for l in range(NL):
        for k in range(2):
            c.dma("sp", gb2[:, l, k:k + 1], gb_d[l:l + 1, k * 64:(k + 1) * 64].rearrange("o p -> p o"))
    ngb2 = c.sb([64, NL, 2], F32, "ngb2")
    c.ts(ngb2[:], gb2[:], -1.0, ALU.mult)
    nw0 = c.sb([128, NL, 2], F32, "nw0")
    c.ts(nw0[:], w0[:], -1.0, ALU.mult)
    lbe = c.sb([128, NL, 2], F32, "lbe")
    c.act(lbe[:], lbl[:], AF.Exp)
    lbs = c.sb([128, 2], F32, "lbs")
    c.tt(lbs[:], lbe[:, 0, :], lbe[:, 1, :], ALU.add)
    for l in range(2, NL):
        c.tt(lbs[:], lbs[:], lbe[:, l, :], ALU.add)
    c.recip(lbs[:], lbs[:])
    lb = c.sb([128, NL, 2], F32, "lb")
    c.memset(lb[:], 0.0)
    for l in range(1, NL):
        c.tt(lb[:, l, :], lbe[:, l, :], lbs[:], ALU.mult)
        c.tt(lb[:, l, :], lb[:, l, :], lb[:, l - 1, :], ALU.add)
    olb = c.sb([128, NL, 2], F32, "olb")
    c.ts(olb[:], lb[:], -1.0, ALU.mult, 1.0, ALU.add)
    nolb = c.sb([128, NL, 2], F32, "nolb")
    c.ts(nolb[:], olb[:], -1.0, ALU.mult)
    epsc = c.sb([128, 4], F32, "epsc")
    c.memset(epsc[:, 0:1], 1e-5)
    c.memset(epsc[:, 1:2], 1e-6)
    c.memset(epsc[:, 2:3], 64e-5)
    c.memset(epsc[:, 3:4], 1.0)
    mrs = c.sb([128, 2, 8, NE], F32, "mrs")
    for i in range(2):
        c.dma("sp", mrs[:, i], mr_d[i].rearrange("(k p) e -> p k e", p=128))
    mrs_bf = c.sb([128, 2, 8, NE], BF16, "mrsbf")
    c.copy(mrs_bf[:], mrs[:])

    rb_small = c.sb([128, 640], F32, "rbsmall")
    gw2 = c.sb([16, 128], F32, "gw2")
    w2s = c.sb([64, 256], F32, "w2s")
    a2s = c.sb([128, 256], F32, "a2s")
    g2a = c.sb([128, 256], F32, "g2a")
    g2b = c.sb([32, 256], F32, "g2b")
    v1s = c.sb([128, 2, 32], F32, "v1s")
    v2s = c.sb([32, 256], F32, "v2s")

    def load_layer_params(l):
        c.dma("sp", rb_small[:, 0:64], hng_d[l:l + 1, :].partition_broadcast(128))
        c.dma("sp", rb_small[:, 64:128], gng_d[l:l + 1, :].partition_broadcast(128))
        c.dma("sp", rb_small[:, 128:384], lxg_d[l:l + 1, :].partition_broadcast(128))
        c.dma("sp", rb_small[:, 384:640], lxb_d[l:l + 1, :].partition_broadcast(128))
        c.dma("sp", gw2[:], gw2_d[l])
        c.dma("sp", w2s[:], w2_d[l])
        c.dma("sp", a2s[64:128, :], a2_d[l])
        c.dma("sp", g2a[:], g2_d[l, 0:128, :])
        c.dma("sp", g2b[:], g2_d[l, 128:160, :])
        if l > 0:
            c.dma("sp", v1s[:], v1_d[l - 1].rearrange("(c p) n -> p c n", p=128))
            c.dma("sp", v2s[:], v2_d[l - 1])

    SLOTN = 8 * 512
    NSLOT = 4
    wslots = [c.sb([128, SLOTN], BF16, "wslot%d" % i) for i in range(NSLOT)]
    accb = Ring([c.ps([128, 512], F32, "acc%d" % i) for i in range(4)])
    trb = Ring([c.ps([128, 512], F32, "trb%d" % i) for i in range(4)])

    hT = c.sb([128, 8, TB], BF16, "hT")
    htok = c.sb([128, NT, D], F32, "htok")
    ytok = c.sb([128, NT, D], BF16, "ytok")
    P = [c.sb([128, TB], F32, "P%d" % i) for i in range(11)]
    qk = [c.sb([128, TB], BF16, "qk%d" % i) for i in range(4)]
    vt = c.sb([128, NT, 256], BF16, "vt")
    gt = c.sb([128, NT, 256], BF16, "gt")
    decs = [c.sb([128, NCH], F32, "dec%d" % i) for i in range(2)]
    dec_rt = [c.sb([128, NCH], F32, "decrt%d" % i) for i in range(2)]
    for i in range(2):
        c.copy(dec_rt[i][:], K("ret_dec")[:, i:i + 1].bc([128, NCH]))
    cosT = c.sb([128, TB], F32, "cosT")
    sinT = c.sb([128, TB], F32, "sinT")
    o_sb = c.sb([128, 256], F32, "o_sb")
    sq_sb = c.sb([128, 256], F32, "sq_sb")
    stat_ln = c.sb([128, 16], F32, "stat_ln")
    stat_mx = c.sb([128, 16], F32, "stat_mx")
    stat_moe = c.sb([128, 40], F32, "stat_moe")
    vfirst = c.sb([128, 2, TB], F32, "vfirst")
    rw_carry = c.sb([128, L, 9], F32, "rwcarry")
    rw_bonus = c.sb([128, NT, 4], F32, "rw_bonus")
    rw_dec = [c.sb([128, NCH], F32, "rwdec%d" % i) for i in range(2)]
    gates = c.sb([128, NT, NE], F32, "gates")

    def mk_state(W, name):
        s = [c.sb([128, W], F32, "%s_f%d" % (name, l)) for l in range(L)]
        sb_ = c.sb([128, W], BF16, "%s_b" % name)
        return s, sb_
    st_hg = [mk_state(128, "hg%d" % t) for t in range(2)]
    st_rt = [mk_state(128, "rt%d" % t) for t in range(2)]
    st_gl = [mk_state(128, "gl%d" % t) for t in range(2)]
    zst = [[c.sb([128, 64], F32, "z%d_%d" % (l, pt)) for pt in range(2)] for l in range(L)]

    RW_BYTES = 5 * 4 * TB + 1 * 4 * (TB + 1) + 2 * 4 * TB + 4 * 4 * TB + 16 * TB + NT * 1024 + NT * 512 + NT * 1024 \
        + 8 * 1024 + 8 * 1024 + 4 * 512 + 8 * 256 + 8 * 512
    FF_BYTES = 22 * TB * 2 + NT * D * 4 + 8 * TB * 2
    ar_ = Arena(c, max(RW_BYTES, FF_BYTES) + 64, "arena")
    off = [0]

    def av(shape, dt, name):
        esz = 4 if dt == F32 else 2
        n = esz
        for s in shape[1:]:
            n *= s
        v = ar_.view(off[0], shape, dt, name)
        off[0] += n
        return v
    rw_sh = [av([128, TB], F32, "rwsh%d" % i) for i in range(5)]
    rw_raw = Ring([av([128, TB + 1], F32, "rwraw%d" % i) for i in range(1)])
    th_t = av([128, TB], F32, "th")
    lvs_t = av([128, TB], F32, "lvs")
    rw_kt = [av([128, TB], F32, "rw_kt%d" % i) for i in range(2)]
    rw_bt = [av([128, TB], F32, "rw_bt%d" % i) for i in range(2)]
    rw_ar = [av([128, 2, TB], F32, "rw_ar%d" % i) for i in range(2)]
    rw_vtok = av([128, NT, 256], F32, "rw_vtok")
    rw_gtok = av([128, NT, 256], BF16, "rw_gtok")
    o_acc = av([128, NT, 256], F32, "o_acc")
    a_ring = Ring([av([128, 256], F32, "aring%d" % i) for i in range(8)])
    m_ring = Ring([av([128, 256], F32, "mring%d" % i) for i in range(8)])
    tok_ring = Ring([av([128, 128], F32, "tokr%d" % i) for i in range(4)])
    u_ring = Ring([av([128, 64], F32, "ur%d" % i) for i in range(8)])
    TT_ring = Ring([av([128, 128], F32, "TTr%d" % i) for i in range(8)])
    off[0] = 0
    mT = av([128, 22, TB], BF16, "mT")
    facc = av([128, NT, D], F32, "facc")
    yT = av([128, 8, TB], BF16, "yT")

    out_tt = TT(None, "out_dram")

    pieces = []
    pst = {"issued": 0, "taken": 0}
    free_slots = list(range(NSLOT))
    slot_of = {}

    class Piece:
        __slots__ = ("v", "slot")

    def plan_piece(dap, nk, ncols):
        assert nk * ncols <= SLOTN
        pieces.append((dap, nk, ncols))

    def pump():
        while free_slots and pst["issued"] < len(pieces):
            i = pst["issued"]
            dap, nk, ncols = pieces[i]
            s = free_slots.pop(0)
            dst = wslots[s][:, 0:nk * ncols].re("p (k c) -> p k c", k=nk)
            c.dma("pool", dst, dap)
            p_ = Piece()
            p_.v = dst
            p_.slot = s
            slot_of[i] = p_
            pst["issued"] += 1

    def take_piece():
        pump()
        i = pst["taken"]
        assert i in slot_of, ("weight ring deadlock", i)
        pst["taken"] += 1
        return slot_of.pop(i)

    def release(p_):
        free_slots.append(p_.slot)
        pump()

    def rows_piece(w2d, r0, nk, c0, ncols):
        return w2d[r0 * 128:(r0 + nk) * 128, c0:c0 + ncols].rearrange("(k p) c -> p k c", p=128)

    WIN_PIECES = [(0, 512), (512, 512), (1024, 512), (1536, 272), (1808, 512), (2320, 512), (2832, 32),
                  (2864, 512), (3376, 512)]
    GU_COLS_D = [(0, 512), (512, 512), (1024, 512), (1536, 512), (2048, 512), (2560, 256)]
    GU_COLS_E = [(0, 512), (512, 512), (1024, 384)]

    def plan_layer(l):
        for (c0, n) in WIN_PIECES:
            plan_piece(rows_piece(w_in_d[l], 0, 8, c0, n), 8, n)
        for hf in range(2):
            plan_piece(rows_piece(w_out_d[l], 0, 8, hf * 512, 512), 8, 512)
        if not ffn:
            return
        i = l // 2
        if l % 2 == 0:
            for (c0, n) in GU_COLS_D:
                plan_piece(rows_piece(fg_d[i], 0, 8, c0, n), 8, n)
                plan_piece(rows_piece(fu_d[i], 0, 8, c0, n), 8, n)
            for hf in range(2):
                for (r0, nk) in ((0, 8), (8, 8), (16, 6)):
                    plan_piece(rows_piece(fd_d[i], r0, nk, hf * 512, 512), nk, 512)
        else:
            for e in range(NE):
                for (c0, n) in GU_COLS_E:
                    plan_piece(rows_piece(mg_d[i, e], 0, 8, c0, n), 8, n)
                    plan_piece(rows_piece(mu2_d[i, e], 0, 8, c0, n), 8, n)
                for hf in range(2):
                    for (r0, nk) in ((0, 8), (8, 3)):
                        plan_piece(rows_piece(md_d[i, e], r0, nk, hf * 512, 512), nk, 512)

    for b in range(NBLK):
        for l in range(L):
            plan_layer(l)

    def proj_F(wp, cols, ncols, rhs=None):
        bank = trb.next()
        out = bank[0:ncols, 0:TB]
        for k in range(8):
            c.mm(out, wp.v[:, k, cols:cols + ncols], hT[:, k, :], start=(k == 0), stop=(k == 7))
        return out

    def proj_T(wp, cols, ncols, tile):
        bank = trb.next()
        out = bank[:, 0:ncols]
        for k in range(8):
            c.mm(out, hT[:, k, tile * 128:(tile + 1) * 128], wp.v[:, k, cols:cols + ncols],
                 start=(k == 0), stop=(k == 7))
        return out

    def cumdecay(g, sgn_scale, cum, eq, ek):
        c.scan(cum, K("reset")[:, 0:TB], g, 0.0, ALU.mult, ALU.add)
        c.act(eq, cum, AF.Exp, scale=sgn_scale)
        c.act(ek, cum, AF.Exp, scale=-sgn_scale)

    def chunk_end(v):
        return v.re("p (n c) -> p n c", c=C)[:, :, C - 1]

    def rstd_of(dst, src, eps_col):
        c.act(dst, src, AF.Sqrt, bias=epsc[:, eps_col:eps_col + 1])
        c.recip(dst, dst)

    sc_ring = Ring([c.sb([128, 128], BF16, "sc%d" % i) for i in range(4)])
    kt_ring = Ring([c.sb([128, 128], BF16, "kt%d" % i) for i in range(2)])
    md_ring = Ring([c.sb([128, 128], F32, "md%d" % i) for i in range(2)])

    def gla_tile(l, t, qT, kT, dcs, states, KD, nh_tile):
        tok = slice(t * 128, (t + 1) * 128)
        o_bank = accb.next()
        o_ps = o_bank[:, 0:256]
        first = [True]
        W = nh_tile * 64
        NP = nh_tile * KD
        for pt in range(len(qT)):
            S, Sb = states[pt][0][l][0:NP, :], states[pt][1][0:NP, :]
            if t == 0:
                c.copy(Sb, S, eng="act")
            ktp = trb.next()
            ktv = V(ktp, ktp.t[:, 0:NP // 2].bitcast(BF16))
            c.tr(ktv, kT[pt][:, tok], ident_bf[0:NP, 0:NP])
            ktok = kt_ring.next()[:, 0:NP]
            c.copy(ktok, ktv, eng="act")
            scs = []
            for hh in range(nh_tile):
                pr = slice(hh * KD, (hh + 1) * KD)
                sp_ = trb.next()
                c.mm(sp_[:, 0:128], kT[pt][pr, tok], qT[pt][pr, tok])
                sc = sc_ring.next()[:]
                c.tt(sc, sp_[:, 0:128], mask_bf[:], ALU.mult)
                scs.append(sc)
            for hh in range(nh_tile):
                hg = pt * nh_tile + hh
                c.mm(o_ps[:, hg * 64:(hg + 1) * 64], scs[hh], vt[:, t, hg * 64:(hg + 1) * 64],
                     start=first[0], stop=False, sg=True)
                first[0] = False
            for ch in range(2):
                cg = t * 2 + ch
                rows = slice(ch * 64, (ch + 1) * 64)
                ctok = slice(t * 128 + ch * 64, t * 128 + (ch + 1) * 64)
                for hh in range(nh_tile):
                    hg = pt * nh_tile + hh
                    pr = slice(hh * KD, (hh + 1) * KD)
                    c.mm(o_ps[rows, hg * 64:(hg + 1) * 64], qT[pt][pr, ctok], Sb[pr, hh * 64:(hh + 1) * 64],
                         start=False, stop=True, sg=True)
                mp = trb.next()
                c.mm(mp[0:NP, 0:W], ktok[rows, :], vt[rows, t, pt * W:(pt + 1) * W])
                md = md_ring.next()[0:NP, 0:W]
                c.act(md, mp[0:NP, 0:W], AF.Copy, scale=dcs[pt][:, cg:cg + 1])
                c.stt(S, S, dcs[pt][:, cg:cg + 1], md, ALU.mult, ALU.add)
                c.copy(Sb, S, eng="act")
        return o_ps

    def finish_simple(l, t, o_ps, mix_idx, kind, gam_off):
        o = o_sb[:]
        c.copy(o, o_ps, eng="act")
        o3 = o.re("p (h v) -> p h v", h=4)
        sq = sq_sb[:]
        msum = stat_mx[:, 0:4]
        ssum = stat_mx[:, 4:8]
        rstd = stat_mx[:, 8:12]
        if kind == "gn":
            c.reduce(msum, o3, ALU.add)
            c.ts(msum, msum, 1.0 / 64.0, ALU.mult)
            c.tt(o3, o3, msum.re("p (h o) -> p h o", o=1).bc([128, 4, 64]), ALU.subtract)
        c.act(sq, o, AF.Square)
        c.reduce(ssum, sq.re("p (h v) -> p h v", h=4), ALU.add)
        c.ts(ssum, ssum, 1.0 / 64.0, ALU.mult)
        rstd_of(rstd, ssum, 1 if kind == "rms" else 0)
        c.tt(o3, o3, rstd.re("p (h o) -> p h o", o=1).bc([128, 4, 64]), ALU.mult)
        if gam_off is not None:
            gm = rb_small[:, gam_off:gam_off + 64]
            c.tt(o3, o3, gm.re("p (o v) -> p o v", o=1).bc([128, 4, 64]), ALU.mult)
        c.tt(ytok[:, t, mix_idx * 256:(mix_idx + 1) * 256], o, gt[:, t, :], ALU.mult)

    def hgrn(l, wp0, wp1):
        for pt in range(2):
            qps = proj_F(wp0, pt * 128, 128)
            qs = P[0][:]
            c.act(qs, qps, AF.Silu)
            fps = proj_F(wp0, 256 + pt * 128, 128)
            s = P[1][:]
            c.act(s, fps, AF.Sigmoid)
            f = P[2][:]
            c.ts(f, s, olb[:, l, pt:pt + 1], ALU.mult, lb[:, l, pt:pt + 1], ALU.add)
            k = P[3][:]
            c.ts(k, s, nolb[:, l, pt:pt + 1], ALU.mult, olb[:, l, pt:pt + 1], ALU.add)
            g = P[4][:]
            c.act(g, f, AF.Ln)
            cum, eq, ek = P[5][:], P[6][:], P[7][:]
            cumdecay(g, 1.0, cum, eq, ek)
            c.tt(qk[0 + pt][:], qs, eq, ALU.mult)
            c.tt(qk[2 + pt][:], k, ek, ALU.mult)
            c.copy(decs[pt][:], chunk_end(eq))
        for t in range(NT):
            ps_ = proj_T(wp1, 0, 512, t)
            c.copy(vt[:, t, :], ps_[:, 0:256], eng="act")
            c.act(gt[:, t, :], ps_[:, 256:512], AF.Silu)
        release(wp0)
        release(wp1)
        import os
        if os.environ.get("DBG_NOCORE"):
            return
        for t in range(NT):
            o_ps = gla_tile(l, t, [qk[0][:], qk[1][:]], [qk[2][:], qk[3][:]], [decs[0][:], decs[1][:]],
                            st_hg, 64, 2)
            if os.environ.get("DBG_NOFIN"):
                continue
            finish_simple(l, t, o_ps, 0, "rms", 0)

    def gla(l, wp2, wp3):
        gps = proj_F(wp3, 0, 16)
        glr = P[2]
        c.copy(glr[0:16, :], gps, eng="act")
        for pt in range(2):
            qps = proj_F(wp2, pt * 64, 64)
            qs = P[0][0:64, :]
            c.act(qs, qps, AF.Copy, scale=32.0 ** -0.5)
            kps = proj_F(wp2, 128 + pt * 64, 64)
            ks = P[1][0:64, :]
            c.copy(ks, kps, eng="act")
            bank = trb.next()
            zps = bank[0:64, 0:TB]
            c.mm(zps, gw2[:, pt * 64:(pt + 1) * 64], glr[0:16, :])
            e = P[3][0:64, :]
            c.act(e, zps, AF.Exp, bias=ngb2[:, l, pt:pt + 1], scale=-1.0)
            sp_ = P[4][0:64, :]
            c.act(sp_, e, AF.Ln, bias=epsc[0:64, 3:4])
            cum, eq, ek = P[5][0:64, :], P[6][0:64, :], P[7][0:64, :]
            c.scan(cum, K("reset")[0:64, 0:TB], sp_, 0.0, ALU.mult, ALU.add)
            c.act(eq, cum, AF.Exp, scale=-1.0 / 16.0)
            c.act(ek, cum, AF.Exp, scale=1.0 / 16.0)
            c.tt(qk[0 + pt][0:64, :], qs, eq, ALU.mult)
            c.tt(qk[2 + pt][0:64, :], ks, ek, ALU.mult)
            c.copy(decs[pt][0:64, :], chunk_end(eq))
        for t in range(NT):
            ps_ = proj_T(wp2, 256, 256, t)
            c.copy(vt[:, t, :], ps_, eng="act")
            ps2 = proj_T(wp3, 16, 256, t)
            c.act(gt[:, t, :], ps2, AF.Silu)
        release(wp2)
        release(wp3)
        for t in range(NT):
            o_ps = gla_tile(l, t, [qk[0][0:64, :], qk[1][0:64, :]], [qk[2][0:64, :], qk[3][0:64, :]],
                            [decs[0][0:64, :], decs[1][0:64, :]], st_gl, 32, 2)
            finish_simple(l, t, o_ps, 1, "rms", 64)

    def rope_tables(b):
        posi = V(P[0], P[0].t[:].bitcast(I32))
        c.dma("sp", posi, pos_d[0:1, b * TB:(b + 1) * TB].partition_broadcast(128))
        posf = P[5]
        c.copy(posf[:], posi)
        ang = P[1][:]
        c.ts(ang, posf[:], K("invfreq"), ALU.mult)
        kq = P[2][:]
        c.ts(kq, ang, float(1.0 / (2.0 * np.pi)), ALU.mult, 12582912.0, ALU.add)
        c.ts(kq, kq, -12582912.0, ALU.add)
        r = P[3][:]
        C1 = 6.28125
        C2 = float(2.0 * np.pi - 6.28125)
        c.stt(r, kq, -C1, ang, ALU.mult, ALU.add)
        c.stt(r, kq, -C2, r, ALU.mult, ALU.add)
        c.ts(r, r, 3.14159, ALU.min, -3.14159, ALU.max)
        c.act(sinT[:], r, AF.Sin)
        ab = P[4][:]
        c.ts(ab, r, -1.0, ALU.mult)
        c.tt(ab, ab, r, ALU.max)
        c.ts(ab, ab, -1.0, ALU.mult, float(np.pi / 2.0), ALU.add)
        c.act(cosT[:], ab, AF.Sin)
        c.ts(sinT[:], sinT[:], K("sinsign"), ALU.mult)

    def ret(l, wp6, wp7):
        for which in range(2):
            for pt in range(2):
                ps_ = proj_F(wp6, which * 256 + pt * 128, 128)
                xs = P[0][:]
                c.copy(xs, ps_, eng="act")
                a = P[1][:]
                c.tt(a, xs, cosT[:], ALU.mult)
                bsw = P[2][:]
                for hh in range(2):
                    lo = slice(hh * 64, hh * 64 + 32)
                    hi = slice(hh * 64 + 32, hh * 64 + 64)
                    c.tt(bsw[lo, :], xs[hi, :], sinT[hi, :], ALU.mult)
                    c.tt(bsw[hi, :], xs[lo, :], sinT[lo, :], ALU.mult)
                c.tt(a, a, bsw, ALU.add)
                tab = K("ret_eq" if which == 0 else "ret_ek")[:, pt * C:(pt + 1) * C]
                dst = qk[which * 2 + pt]
                c.tt(dst[:].re("p (n c) -> p n c", c=C), a.re("p (n c) -> p n c", c=C),
                     tab.re("p (o c) -> p o c", o=1).bc([128, NCH, C]), ALU.mult)
        for t in range(NT):
            ps_ = proj_T(wp7, 0, 512, t)
            c.copy(vt[:, t, :], ps_[:, 0:256], eng="act")
            c.act(gt[:, t, :], ps_[:, 256:512], AF.Silu)
        release(wp6)
        release(wp7)
        for t in range(NT):
            o_ps = gla_tile(l, t, [qk[0][:], qk[1][:]], [qk[2][:], qk[3][:]], [dec_rt[0][:], dec_rt[1][:]],
                            st_rt, 64, 2)
            finish_simple(l, t, o_ps, 3, "gn", None)

    def rw_shift(l, b, i, wp, c0, n, dst, tm):
        ps_ = proj_F(wp, c0, n)
        raw = rw_raw.next()
        if b == 0:
            c.memset(raw[0:n, 0:1], 0.0)
        else:
            c.copy(raw[0:n, 0:1], rw_carry[0:n, l, i:i + 1])
        c.copy(raw[0:n, 1:TB + 1], ps_, eng="act")
        c.copy(rw_carry[0:n, l, i:i + 1], raw[0:n, TB:TB + 1])
        c.act(tm[0:n, :], raw[0:n, 1:TB + 1], AF.Copy, scale=omu[0:n, l, i:i + 1])
        c.stt(dst[0:n, :], raw[0:n, 0:TB], mup[0:n, l, i:i + 1], tm[0:n, :], ALU.mult, ALU.add)

    def rwkv(l, b, wp4, wp5, wp5b):
        vS = [rw_sh[0], rw_sh[1]]
        waS, rS, kS = rw_sh[2], rw_sh[3], rw_sh[4]
        rw_shift(l, b, 6, wp5, 256, 128, waS, P[2])
        g0S, g1S = P[0], P[1]
        rw_shift(l, b, 7, wp5, 384, 128, g0S, P[2])
        rw_shift(l, b, 8, wp5b, 0, 32, g1S, P[2])
        rw_shift(l, b, 4, wp5, 0, 128, vS[0], P[2])
        rw_shift(l, b, 5, wp5, 128, 128, vS[1], P[2])
        release(wp5)
        release(wp5b)
        c.act(th_t[0:64, :], waS[0:64, :], AF.Tanh)
        c.act(g0S[:], g0S[:], AF.Sigmoid)
        c.act(g1S[0:32, :], g1S[0:32, :], AF.Sigmoid)
        for t in range(NT):
            bank = trb.next()
            c.mm(bank[:, 0:256], g0S[:, t * 128:(t + 1) * 128], g2a[:], start=True, stop=False)
            c.mm(bank[:, 0:256], g1S[0:32, t * 128:(t + 1) * 128], g2b[:], start=False, stop=True)
            c.copy(rw_gtok[:, t, :], bank[:, 0:256], eng="act")
        if l > 0:
            bank = trb.next()
            lv = bank[0:32, 0:TB]
            for k in range(2):
                c.mm(lv, v1s[:, k, :], vS[k][:], start=(k == 0), stop=(k == 1))
            c.copy(lvs_t[0:32, :], lv, eng="act")
        for pt in range(2):
            if l == 0:
                c.copy(vfirst[:, pt, :], vS[pt][:])
            else:
                bank = trb.next()
                c.mm(bank[:, 0:TB], v2s[:, pt * 128:(pt + 1) * 128], lvs_t[0:32, :])
                sgm = P[2][:]
                c.act(sgm, bank[:, 0:TB], AF.Sigmoid, bias=v0p[:, l - 1, pt:pt + 1])
                dv = P[3][:]
                c.tt(dv, vfirst[:, pt, :], vS[pt][:], ALU.subtract)
                c.tt(dv, dv, sgm, ALU.mult)
                c.tt(vS[pt][:], vS[pt][:], dv, ALU.add)
            for t in range(NT):
                bank = trb.next()
                c.tr(bank[:, 0:128], vS[pt][:, t * 128:(t + 1) * 128], K("ident"))
                c.copy(rw_vtok[:, t, pt * 128:(pt + 1) * 128], bank[:, 0:128], eng="act")
        for pt in range(2):
            rw_shift(l, b, 0 + pt, wp4, pt * 128, 128, rS, P[0])
            rw_shift(l, b, 2 + pt, wp4, 256 + pt * 128, 128, kS, P[0])
            if pt == 1:
                release(wp4)
            bank = trb.next()
            c.mm(bank[:, 0:TB], w2s[:, pt * 128:(pt + 1) * 128], th_t[0:64, :])
            t1 = P[2][:]
            c.act(t1, bank[:, 0:TB], AF.Exp, bias=nw0[:, l, pt:pt + 1], scale=-1.0)
            c.act(t1, t1, AF.Ln, bias=epsc[:, 3:4])
            c.ts(t1, t1, -1.0, ALU.mult, -0.5, ALU.add)
            c.act(t1, t1, AF.Exp)
            g = P[3][:]
            c.ts(g, t1, -1.0, ALU.mult)
            cum, eq, ek = P[4][:], P[5][:], P[6][:]
            cumdecay(g, 1.0, cum, eq, ek)
            c.copy(rw_dec[pt][:], chunk_end(eq))
            bank = trb.next()
            c.mm(bank[:, 0:TB], a2s[64:128, pt * 128:(pt + 1) * 128], waS[64:128, :])
            ag = P[7][:]
            c.act(ag, bank[:, 0:TB], AF.Sigmoid, bias=a0[:, l, pt:pt + 1])
            kk = P[8][:]
            c.ts(kk, kS[:], kkp[:, l, pt:pt + 1], ALU.mult)
            nrm = P[9][:]
            c.act(nrm, kk, AF.Square)
            bank = trb.next()
            c.mm(bank[:, 0:TB], K("blockones"), nrm)
            c.act(nrm, bank[:, 0:TB], AF.Sqrt)
            c.ts(nrm, nrm, 1e-12, ALU.max)
            c.act(nrm, nrm, AF.Ln)
            c.act(nrm, nrm, AF.Exp, scale=-1.0)
            c.tt(kk, kk, nrm, ALU.mult)
            fk = P[9][:]
            c.ts(fk, ag, -1.0, ALU.add, kap[:, l, pt:pt + 1], ALU.mult)
            c.ts(fk, fk, 1.0, ALU.add)
            km = P[10][:]
            c.tt(km, kS[:], fk, ALU.mult)
            bo = P[2][:]
            c.stt(bo, rS[:], rkp[:, l, pt:pt + 1], km, ALU.mult, ALU.mult)
            for t in range(NT):
                bank = trb.next()
                c.mm(bank[:, 0:2], bo[:, t * 128:(t + 1) * 128], K("headsel"))
                c.copy(rw_bonus[:, t, pt * 2:pt * 2 + 2], bank[:, 0:2], eng="act")
            c.tt(rw_ar[pt][:, 1, :], rS[:], eq, ALU.mult)
            c.tt(rw_kt[pt][:], km, ek, ALU.mult)
            bb = P[9][:]
            c.tt(bb, kk, ag, ALU.mult)
            c.tt(rw_bt[pt][:], bb, ek, ALU.mult)
            ex = P[9][:]
            c.tt(ex, cum, g, ALU.subtract)
            c.act(ex, ex, AF.Exp)
            c.stt(rw_ar[pt][:, 0, :], kk, -1.0, ex, ALU.mult, ALU.mult)
        for t in range(NT):
            gens = [rwkv_core(l, t, h // 2, h % 2, h) for h in range(4)]
            live = list(gens)
            while live:
                nxt = []
                for g_ in live:
                    try:
                        next(g_)
                        nxt.append(g_)
                    except StopIteration:
                        pass
                live = nxt
            rwkv_finish(l, t)

    F32R = mybir.dt.float32r

    def R32(v):
        return V(v.tt, v.ap.bitcast(F32R))

    def rwkv_core(l, t, pt, hh, sidx):
        h = pt * 2 + hh
        ev = ("act", "dve") if sidx % 2 == 0 else ("dve", "act")
        tok = slice(t * 128, (t + 1) * 128)
        pr = slice(hh * 64, hh * 64 + 64)
        ar = rw_ar[pt][pr, :, tok]
        kt = rw_kt[pt][pr, tok]
        bt = rw_bt[pt][pr, tok]
        at = rw_ar[pt][pr, 0, tok]
        rt = rw_ar[pt][pr, 1, tok]
        Z = zst[l][pt]
        vtk = rw_vtok[:, t, h * 64:(h + 1) * 64]
        ocol = slice(h * 64, (h + 1) * 64)
        b1 = trb.next()
        c.mm(b1[:, 0:256].re("p (a n) -> p a n", a=2), kt, ar)
        Ak = a_ring.next()[:]
        c.tt(Ak[:, 0:128], b1[:, 0:128], K("mask_strict"), ALU.mult)
        c.tt(Ak[:, 128:256], b1[:, 128:256], K("mask_incl"), ALU.mult)
        b2 = trb.next()
        c.mm(b2[:, 0:256].re("p (a n) -> p a n", a=2), bt, ar)
        Ab = a_ring.next()[:]
        c.tt(Ab[:, 0:128], b2[:, 0:128], K("mask_strict"), ALU.mult)
        c.tt(Ab[:, 128:256], b2[:, 128:256], K("mask_incl"), ALU.mult)
        b3 = trb.next()
        c.mm(b3[:, 0:128], at, bt)
        pq = m_ring.next()[:]
        Pm = pq[:, 0:128]
        c.tt(Pm, b3[:, 0:128], K("mask_strictT"), ALU.mult)
        Q = Ab[:, 0:128]
        Tc = TT_ring.next()[:]
        c.tt(Tc, Q, K("ident"), ALU.add)
        yield
        for i in range(1, 6):
            bpq = trb.next()
            c.mm(bpq[:, 0:128], Q, Pm)
            if i < 5:
                c.mm(bpq[:, 128:256], Pm, Q)
            pq = m_ring.next()[:]
            if i < 5:
                c.copy(pq, bpq[:, 0:256], eng=ev[0])
            else:
                c.copy(pq[:, 0:128], bpq[:, 0:128], eng=ev[0])
            Pm = pq[:, 0:128]
            Q = pq[:, 128:256]
            yield
            bt_ = trb.next()
            c.mm(bt_[:, 0:128], Pm, Tc)
            Tn = TT_ring.next()[:]
            c.tt(Tn, Tc, bt_[:, 0:128], ALU.add)
            Tc = Tn
            yield
        bk = trb.next()
        c.tr(bk[:, 0:64], kt, K("ident")[pr, pr])
        c.tr(bk[:, 64:128], bt, K("ident")[pr, pr])
        kb_tok = tok_ring.next()[:]
        c.copy(kb_tok, bk[:, 0:128], eng=ev[0])
        gb_ = accb.next()
        c.mm(gb_[:, 0:64], Ak[:, 0:128], vtk, start=True, stop=False, sg=True)
        utok = u_ring.next()[:]
        rhs_u = u_ring.next()[:]
        yield
        for ch in range(2):
            rows = slice(ch * 64, (ch + 1) * 64)
            cg = t * 2 + ch
            c.mm(gb_[rows, 0:64], at[:, rows], Z[pr, :], start=False, stop=True, sg=True)
            c.copy(rhs_u[rows, :], gb_[rows, 0:64], eng=ev[1])
            ob = trb.next()
            c.mm(ob[rows, 0:64], rt[:, rows], Z[pr, :])
            c.copy(o_acc[rows, t, ocol], ob[rows, 0:64], eng=ev[0])
            yield
            ub = trb.next()
            c.mm(ub[rows, 0:64], Tc[rows, rows], rhs_u[rows, :])
            c.copy(utok[rows, :], ub[rows, 0:64], eng=ev[0])
            yield
            zb = trb.next()
            c.mm(zb[pr, 0:64], K("identZ")[pr, :], Z[pr, :], start=True, stop=False, sg=True)
            c.mm(zb[pr, 0:64], kb_tok[rows, 0:64], vtk[rows, :], start=False, stop=False, sg=True)
            c.mm(zb[pr, 0:64], kb_tok[rows, 64:128], utok[rows, :], start=False, stop=True, sg=True)
            c.act(Z[pr, :], zb[pr, 0:64], AF.Copy, scale=rw_dec[pt][pr, cg:cg + 1])
            yield
        ob2 = trb.next()
        c.mm(ob2[:, 0:64], Ak[:, 128:256], vtk, start=True, stop=False)
        c.mm(ob2[:, 0:64], Ab[:, 128:256], utok, start=False, stop=True)
        c.tt(o_acc[:, t, ocol], o_acc[:, t, ocol], ob2[:, 0:64], ALU.add)

    def rwkv_finish(l, t):
        o = o_sb[:]
        c.copy(o, o_acc[:, t, :])
        o3 = o.re("p (h v) -> p h v", h=4)
        msum = stat_mx[:, 0:4]
        ssum = stat_mx[:, 4:8]
        rstd = stat_mx[:, 8:12]
        c.reduce(msum, o3, ALU.add)
        c.ts(msum, msum, 1.0 / 64.0, ALU.mult)
        c.tt(o3, o3, msum.re("p (h o) -> p h o", o=1).bc([128, 4, 64]), ALU.subtract)
        sq = sq_sb[:]
        c.act(sq, o, AF.Square)
        c.reduce(ssum, sq.re("p (h v) -> p h v", h=4), ALU.add)
        c.ts(ssum, ssum, 1.0 / 64.0, ALU.mult)
        rstd_of(rstd, ssum, 2)
        c.tt(o3, o3, rstd.re("p (h o) -> p h o", o=1).bc([128, 4, 64]), ALU.mult)
        c.tt(o, o, rb_small[:, 128:384], ALU.mult)
        c.tt(o, o, rb_small[:, 384:640], ALU.add)
        c.tt(sq.re("p (h v) -> p h v", h=4), rw_vtok[:, t, :].re("p (h v) -> p h v", h=4),
             rw_bonus[:, t, :].re("p (h o) -> p h o", o=1).bc([128, 4, 64]), ALU.mult)
        c.tt(o, o, sq, ALU.add)
        c.tt(ytok[:, t, 512:768], o, rw_gtok[:, t, :], ALU.mult)

    def to_hT(t):
        for hf in range(2):
            bank = trb.next()
            for k4 in range(4):
                k = hf * 4 + k4
                c.tr(bank[:, k4 * 128:(k4 + 1) * 128], htok[:, t, k * 128:(k + 1) * 128], K("ident"))
            c.copy(hT[:, hf * 4:(hf + 1) * 4, t * 128:(t + 1) * 128],
                   bank[:, 0:512].re("p (k n) -> p k n", k=4), eng="act")

    def layer_norm(l, which):
        for hf in range(2):
            c.dma("sp", P[4 + hf][:], ln_d[which + "_g"][l:l + 1, hf * 512:(hf + 1) * 512].partition_broadcast(128))
            c.dma("sp", P[6 + hf][:], ln_d[which + "_b"][l:l + 1, hf * 512:(hf + 1) * 512].partition_broadcast(128))
        for t in range(NT):
            z = htok[:, t, :]
            st6 = stat_ln[:, 0:12]
            for hf in range(2):
                zz = z[:, hf * 512:(hf + 1) * 512]
                dst = stat_ln[:, hf * 6:(hf + 1) * 6]
                c.op("dve", lambda zz=zz, dst=dst: nc.vector.bn_stats(dst.ap, zz.ap), reads=[zz], writes=[dst])
            mv = stat_ln[:, 12:14]
            c.op("dve", lambda: nc.vector.bn_aggr(mv.ap, st6.ap), reads=[st6], writes=[mv])
            rs = stat_ln[:, 14:15]
            rstd_of(rs, stat_ln[:, 13:14], 0)
            c.ts(z, z, stat_ln[:, 12:13], ALU.subtract, rs, ALU.mult)
            for hf in range(2):
                zz = z[:, hf * 512:(hf + 1) * 512]
                c.tt(zz, zz, P[4 + hf][:], ALU.mult)
                c.tt(zz, zz, P[6 + hf][:], ALU.add)
            to_hT(t)

    def residual_from_bank(t, hf, bank_v):
        z = htok[:, t, hf * 512:(hf + 1) * 512]
        c.stt(z, z, ALPHA, bank_v, ALU.mult, ALU.add)

    def gate_up(cols_list):
        ft = 0
        for (c0, n) in cols_list:
            wg = take_piece()
            wu = take_piece()
            for j in range(n // 128):
                gps = proj_F(wg, j * 128, 128)
                ups = proj_F(wu, j * 128, 128)
                sl = P[8 + ft % 3][:]
                c.act(sl, gps, AF.Silu)
                c.tt(mT[:, ft, :], sl, ups, ALU.mult)
                ft += 1
            release(wg)
            release(wu)
        return ft

    def down_proj(nft, row_groups, consume):
        for hf in range(2):
            accs = [accb.next() for _ in range(NT)]
            for (r0, nk) in row_groups:
                wd = take_piece()
                for kk_ in range(nk):
                    ft = r0 + kk_
                    for t in range(NT):
                        c.mm(accs[t][:, 0:512], mT[:, ft, t * 128:(t + 1) * 128], wd.v[:, kk_, :],
                             start=(ft == 0), stop=(ft == nft - 1))
                release(wd)
            for t in range(NT):
                consume(t, hf, accs[t][:, 0:512])

    def ffn_dense(l):
        gate_up(GU_COLS_D)
        down_proj(22, ((0, 8), (8, 8), (16, 6)), residual_from_bank)

    def moe(l):
        i = l // 2
        for t in range(NT):
            bank = trb.next()
            lg = bank[:, 0:NE]
            for k in range(8):
                c.mm(lg, hT[:, k, t * 128:(t + 1) * 128], mrs_bf[:, i, k, :], start=(k == 0), stop=(k == 7))
            lgs = stat_moe[:, 0:8]
            c.copy(lgs, lg)
            m8 = stat_moe[:, 8:16]
            c.op("dve", lambda: nc.vector.max(m8.ap, lgs.ap), reads=[lgs], writes=[m8])
            nm1 = stat_moe[:, 32:33]
            c.ts(nm1, m8[:, 0:1], -1.0, ALU.mult)
            ex = stat_moe[:, 16:24]
            c.act(ex, lgs, AF.Exp, bias=nm1)
            sel = stat_moe[:, 24:32]
            c.ts(sel, lgs, m8[:, 1:2], ALU.is_ge)
            c.tt(ex, ex, sel, ALU.mult)
            ssum = stat_moe[:, 33:34]
            c.reduce(ssum, ex, ALU.add)
            c.recip(ssum, ssum)
            c.ts(gates[:, t, :], ex, ssum, ALU.mult)
        c.memset(facc[:], 0.0)
        for e in range(NE):
            gate_up(GU_COLS_E)

            def cons(t, hf, bank_v, e=e):
                fa = facc[:, t, hf * 512:(hf + 1) * 512]
                c.stt(fa, bank_v, gates[:, t, e:e + 1], fa, ALU.mult, ALU.add)
            down_proj(11, ((0, 8), (8, 3)), cons)
        for t in range(NT):
            z = htok[:, t, :]
            c.stt(z, z, ALPHA, facc[:, t, :], ALU.mult, ALU.add)

    for l in range(L):
        for pt in range(2):
            for st in (st_hg, st_rt, st_gl):
                c.memset(st[pt][0][l][:], 0.0)
            c.memset(zst[l][pt][:], 0.0)
    c.memset(ytok[:], 0.0)

    for b in range(NBLK):
        for t in range(NT):
            r0 = b * TB + t * 128
            c.dma("sp", htok[:, t, :], x_d[r0:r0 + 128, :])
            to_hT(t)
        if mixers[3]:
            rope_tables(b)
        for l in range(L):
            load_layer_params(l)
            wp0, wp1 = take_piece(), take_piece()
            if mixers[0]:
                hgrn(l, wp0, wp1)
            else:
                release(wp0)
                release(wp1)
            wp2, wp3 = take_piece(), take_piece()
            if mixers[1]:
                gla(l, wp2, wp3)
            else:
                release(wp2)
                release(wp3)
            wp4, wp5, wp5b = take_piece(), take_piece(), take_piece()
            if mixers[2]:
                rwkv(l, b, wp4, wp5, wp5b)
            else:
                release(wp4)
                release(wp5)
                release(wp5b)
            wp6, wp7 = take_piece(), take_piece()
            if mixers[3]:
                ret(l, wp6, wp7)
            else:
                release(wp6)
                release(wp7)
            for t in range(NT):
                for hf in range(2):
                    bank = trb.next()
                    bv = V(bank, bank.t[:, 0:256].bitcast(BF16))
                    for k4 in range(4):
                        k = hf * 4 + k4
                        c.tr(bv[:, k4 * 128:(k4 + 1) * 128], ytok[:, t, k * 128:(k + 1) * 128], ident_bf[:])
                    c.copy(yT[:, hf * 4:(hf + 1) * 4, t * 128:(t + 1) * 128],
                           bv.re("p (k n) -> p k n", k=4), eng="act")
            wo = [take_piece(), take_piece()]
            for t in range(NT):
                for hf in range(2):
                    bank = trb.next()
                    for k in range(8):
                        c.mm(bank[:, 0:512], yT[:, k, t * 128:(t + 1) * 128], wo[hf].v[:, k, :],
                             start=(k == 0), stop=(k == 7))
                    residual_from_bank(t, hf, bank[:, 0:512])
            release(wo[0])
            release(wo[1])
            layer_norm(l, "ln1")
            if ffn:
                if l % 2 == 0:
                    ffn_dense(l)
                else:
                    moe(l)
                layer_norm(l, "ln2")
        for t in range(NT):
            r0 = b * TB + t * 128
            c.dma("sp", V(out_tt, out_d[r0:r0 + 128, :]), htok[:, t, :])
    c.wait_all("sp", [out_tt])
    assert pst["taken"] == len(pieces), (pst, len(pieces))
    return nc, carr, c


_CACHE = {}

NAMES = ["w_in", "w_out", "ln1_g", "ln1_b", "ln2_g", "ln2_b", "hgrn_lb_logits", "hgrn_norm_g", "gla_gate_w2",
         "gla_gate_b", "gla_norm_g", "rwkv_mu", "rwkv_w0", "rwkv_w2", "rwkv_a0", "rwkv_a2", "rwkv_g2", "rwkv_k_k",
         "rwkv_k_a", "rwkv_r_k", "rwkv_lnx_g", "rwkv_lnx_b", "rwkv_v0", "rwkv_v1", "rwkv_v2", "ffn_w_gate",
         "ffn_w_up", "ffn_w_down", "moe_router", "moe_w_gate", "moe_w_up", "moe_w_down"]


def run(inputs, T, L=NL, TB=512, **kw):
    x = np.asarray(inputs["x"], dtype=np.float32)
    B = x.shape[0]
    key = (T, L, TB, tuple(sorted(kw.items())))
    if key not in _CACHE:
        _CACHE[key] = build(T, L, TB, **kw)
    nc, carr, c = _CACHE[key]
    shared = {}
    for n in NAMES:
        a = np.ascontiguousarray(np.asarray(inputs[n], dtype=np.float32))
        if n == "rwkv_r_k":
            a = a.reshape(NL, 256)
        shared[n] = a
    shared["consts"] = carr
    pos = np.asarray(inputs["positions"]).astype(np.int32)
    in_maps = []
    for b in range(B):
        m = dict(shared)
        m["x"] = np.ascontiguousarray(x[b, :T])
        m["positions"] = np.ascontiguousarray(pos[b:b + 1, :T])
        in_maps.append(m)
    res = run_bass_kernel_spmd(nc, in_maps, core_ids=list(range(B)))
    return np.stack([np.asarray(r["out"]) for r in res.results], axis=0)


def kernel(**inputs):
    x = np.asarray(inputs["x"])
    B, S, _ = x.shape
    out = run(inputs, S)
    return out.astype(x.dtype)
```

```python
import numpy as np
import ml_dtypes
import concourse.bass as bass
import concourse.mybir as mybir
from concourse.bass_utils import run_bass_kernel_spmd

F32 = mybir.dt.float32
BF16 = mybir.dt.bfloat16
I32 = mybir.dt.int32
AF = mybir.ActivationFunctionType
ALU = mybir.AluOpType
AX = mybir.AxisListType

D = 1024
NL = 4
INC = 3888
FFD = 2816
NE = 8
FFE = 1408
ALPHA = (2.0 * NL) ** 0.25
C = 64


class V:
    __slots__ = ("tt", "ap")

    def __init__(self, tt, ap):
        self.tt = tt
        self.ap = ap

    def __getitem__(self, k):
        return V(self.tt, self.ap[k])

    def re(self, s, **kw):
        return V(self.tt, self.ap.rearrange(s, **kw))

    def bc(self, shape):
        return V(self.tt, self.ap.to_broadcast(list(shape)))


class TT:
    __slots__ = ("t", "name", "w", "r", "al", "pe_row")

    def __init__(self, t, name):
        self.t = t
        self.name = name
        self.w = None
        self.r = []
        self.al = []
        self.pe_row = None

    def __getitem__(self, k):
        return V(self, self.t[k])


class Ctx:
    NDMA = 24

    def __init__(self, nc, same=True):
        self.nc = nc
        self.same = same
        self.engs = {"pe": nc.tensor, "dve": nc.vector, "act": nc.scalar, "pool": nc.gpsimd, "sp": nc.sync}
        self.sem = {}
        self.cnt = {}
        self.waited = {}
        self._cms = []
        for k in self.engs:
            cm = nc.semaphore("s_" + k)
            self.sem[k] = cm.__enter__()
            self._cms.append(cm)
            self.cnt[k] = 0
            self.waited[k] = {}
        self.dsem = []
        self.dcnt = []
        for i in range(self.NDMA):
            cm = nc.semaphore("d_%d" % i)
            self.dsem.append(cm.__enter__())
            self._cms.append(cm)
            self.dcnt.append(0)
        self.dnext = {"sp": 0, "pool": 0, "act": 0}
        self.drange = {"sp": (0, 14), "pool": (14, 22), "act": (22, 24)}
        self.ntile = 0
        self.ninst = 0

    def sb(self, shape, dt, name=None):
        self.ntile += 1
        name = name or "t%d" % self.ntile
        cm = self.nc.sbuf_tensor(name, list(shape), dt)
        t = cm.__enter__()
        self._cms.append(cm)
        return TT(t, name)

    def ps(self, shape, dt, name=None):
        self.ntile += 1
        name = name or "p%d" % self.ntile
        cm = self.nc.psum_tensor(name, list(shape), dt)
        t = cm.__enter__()
        self._cms.append(cm)
        return TT(t, name)

    def _semof(self, key):
        if key in self.sem:
            return self.sem[key]
        return self.dsem[int(key[1:])]

    def _deps(self, eng, reads, writes):
        need = {}

        def add(dep):
            if dep is None:
                return
            k, v = dep
            if need.get(k, 0) < v:
                need[k] = v
        for t in reads:
            add(t.w)
            for a in t.al:
                add(a.w)
        for t in writes:
            add(t.w)
            for d in t.r:
                add(d)
            for a in t.al:
                add(a.w)
                for d in a.r:
                    add(d)
        h = self.engs[eng]
        for k, v in need.items():
            if k == eng and (not self.same or eng == "pe"):
                continue
            if self.waited[eng].get(k, 0) >= v:
                continue
            h.wait_ge(self._semof(k), v)
            self.waited[eng][k] = v
            self.ninst += 1

    def _mark(self, me, reads, writes):
        for t in reads:
            if len(t.r) > 64:
                mx = {}
                for (k, v) in t.r:
                    if mx.get(k, 0) < v:
                        mx[k] = v
                t.r = list(mx.items())
            t.r.append(me)
        for t in writes:
            t.w = me
            t.r = []

    def op(self, eng, fn, reads=(), writes=()):
        reads = [x.tt if isinstance(x, V) else x for x in reads if x is not None]
        writes = [x.tt if isinstance(x, V) else x for x in writes if x is not None]
        self._deps(eng, reads, writes)
        ins = fn()
        self.cnt[eng] += 1
        ins.then_inc(self.sem[eng], 1)
        self._mark((eng, self.cnt[eng]), reads, writes)
        self.ninst += 1
        return ins

    def dma(self, q, out, in_, **kw):
        reads = [in_.tt] if isinstance(in_, V) else []
        writes = [out.tt] if isinstance(out, V) else []
        lo, hi = self.drange[q]
        i = lo + self.dnext[q]
        self.dnext[q] = (self.dnext[q] + 1) % (hi - lo)
        key = "d%d" % i
        h = self.engs[q]
        if self.dcnt[i] > 0 and self.waited[q].get(key, 0) < self.dcnt[i]:
            h.wait_ge(self.dsem[i], self.dcnt[i])
            self.waited[q][key] = self.dcnt[i]
        self._deps(q, reads, writes)
        oa = out.ap if isinstance(out, V) else out
        ia = in_.ap if isinstance(in_, V) else in_
        ins = h.dma_start(out=oa, in_=ia, **kw)
        self.dcnt[i] += 16
        ins.then_inc(self.dsem[i], 16)
        self._mark((key, self.dcnt[i]), reads, writes)
        self.ninst += 1
        return ins

    def _pe_row_guard(self, out, lhsT):
        row = (int(lhsT.ap.start_partition()), int(lhsT.ap.partition_size()))
        t = out.tt
        if t.pe_row is not None and t.pe_row != row and t.w is not None and t.w[0] == "pe":
            v = t.w[1]
            if self.waited["pe"].get("pe", 0) < v:
                self.nc.tensor.wait_ge(self.sem["pe"], v)
                self.waited["pe"]["pe"] = v
                self.ninst += 1
        t.pe_row = row

    def mm(self, out, lhsT, rhs, start=True, stop=True, sg=False):
        nc = self.nc
        self._pe_row_guard(out, lhsT)
        return self.op("pe", lambda: nc.tensor.matmul(out.ap, lhsT=lhsT.ap, rhs=rhs.ap, start=start, stop=stop,
                                                      skip_group_check=sg),
                       reads=[lhsT, rhs], writes=[out])

    def tr(self, out, in_, ident):
        nc = self.nc
        self._pe_row_guard(out, in_)
        return self.op("pe", lambda: nc.tensor.transpose(out.ap, in_.ap, ident.ap), reads=[in_, ident], writes=[out])

    def act(self, out, in_, func, bias=None, scale=None, eng="act"):
        nc = self.nc
        kw = {}
        rd = [in_]
        if bias is not None:
            if isinstance(bias, V):
                kw["bias"] = bias.ap
                rd.append(bias)
            else:
                kw["bias"] = bias
        if scale is not None:
            if isinstance(scale, V):
                kw["scale"] = scale.ap
                rd.append(scale)
            else:
                kw["scale"] = scale
        if func == AF.Copy and isinstance(scale, V):
            func = AF.Identity
        return self.op("act", lambda: nc.scalar.activation(out.ap, in_.ap, func, **kw), reads=rd, writes=[out])

    def tt(self, out, in0, in1, op, eng="dve"):
        h = self.engs[eng]
        return self.op(eng, lambda: h.tensor_tensor(out.ap, in0.ap, in1.ap, op), reads=[in0, in1], writes=[out])

    def ts(self, out, in0, s1, op0, s2=None, op1=None, eng="dve"):
        h = self.engs[eng]
        rd = [in0]
        a1 = s1
        if isinstance(s1, V):
            rd.append(s1)
            a1 = s1.ap
        a2 = s2
        if isinstance(s2, V):
            rd.append(s2)
            a2 = s2.ap
        if op1 is None:
            return self.op(eng, lambda: h.tensor_scalar(out.ap, in0.ap, a1, None, op0), reads=rd, writes=[out])
        return self.op(eng, lambda: h.tensor_scalar(out.ap, in0.ap, a1, a2, op0, op1), reads=rd, writes=[out])

    def stt(self, out, in0, scalar, in1, op0, op1):
        nc = self.nc
        rd = [in0, in1]
        a = scalar
        if isinstance(scalar, V):
            rd.append(scalar)
            a = scalar.ap
        return self.op("dve", lambda: nc.vector.scalar_tensor_tensor(out.ap, in0.ap, a, in1.ap, op0, op1),
                       reads=rd, writes=[out])

    def copy(self, out, in_, eng="dve"):
        nc = self.nc
        if eng == "act":
            return self.op("act", lambda: nc.scalar.copy(out.ap, in_.ap), reads=[in_], writes=[out])
        h = self.engs[eng]
        return self.op(eng, lambda: h.tensor_copy(out.ap, in_.ap), reads=[in_], writes=[out])

    def memset(self, out, val, eng="dve"):
        h = self.engs[eng]
        return self.op(eng, lambda: h.memset(out.ap, val), reads=[], writes=[out])

    def scan(self, out, d0, d1, init, op0, op1):
        nc = self.nc
        rd = [d0, d1]
        a = init
        if isinstance(init, V):
            rd.append(init)
            a = init.ap
        return self.op("dve", lambda: nc.vector.tensor_tensor_scan(out.ap, d0.ap, d1.ap, a, op0, op1),
                       reads=rd, writes=[out])

    def reduce(self, out, in_, op, axis=AX.X):
        nc = self.nc
        return self.op("dve", lambda: nc.vector.tensor_reduce(out.ap, in_.ap, axis, op), reads=[in_], writes=[out])

    def recip(self, out, in_):
        nc = self.nc
        return self.op("dve", lambda: nc.vector.reciprocal(out.ap, in_.ap), reads=[in_], writes=[out])

    def wait_all(self, eng, tts):
        self._deps(eng, tts, ())


class Arena:
    def __init__(self, c, nbytes, name):
        self.tt = c.sb([128, nbytes // 4], F32, name)
        self.views = []
        self.nbytes = nbytes

    def view(self, off, shape, dt, name):
        esz = 4 if dt in (F32, I32) else 2
        n = esz
        for s in shape[1:]:
            n *= s
        assert off % 4 == 0 and n % 4 == 0 and off + n <= self.nbytes, (name, off, n, self.nbytes)
        ap = self.tt.t[:, off // 4:(off + n) // 4]
        if dt != F32:
            ap = ap.bitcast(dt)
        if len(shape) == 3:
            ap = ap.rearrange("p (a b) -> p a b", a=shape[1])
        elif len(shape) == 4:
            ap = ap.rearrange("p (a b c) -> p a b c", a=shape[1], b=shape[2])
        if shape[0] < 128:
            ap = ap[0:shape[0]]
        t = TT(ap, name)
        for (v, lo, hi) in self.views:
            if lo < off + n and off < hi:
                t.al.append(v)
                v.al.append(t)
        self.views.append((t, off, off + n))
        return t


class Ring:
    def __init__(self, tiles):
        self.tiles = tiles
        self.i = 0

    def next(self):
        t = self.tiles[self.i]
        self.i = (self.i + 1) % len(self.tiles)
        return t


def _consts(TB):
    p = np.arange(128)
    f = {}
    j = p[:, None]
    i = p[None, :]
    same = (j // C) == (i // C)
    f["mask_incl"] = (same & (j <= i)).astype(np.float32)
    f["mask_strict"] = (same & (j < i)).astype(np.float32)
    f["mask_strictT"] = (same & (j > i)).astype(np.float32)
    f["ident"] = np.eye(128, dtype=np.float32)
    f["blockones"] = same.astype(np.float32)
    f["identZ"] = ((p[:, None] % 64) == np.arange(64)[None, :]).astype(np.float32)
    hs = np.zeros((128, 2), np.float32)
    hs[:64, 0] = 1.0
    hs[64:, 1] = 1.0
    f["headsel"] = hs
    t = np.arange(TB)
    f["reset"] = np.broadcast_to((t % C != 0).astype(np.float32)[None, :], (128, TB)).copy()
    half = 32
    inv = (10000.0 ** (-np.arange(half, dtype=np.float32) / half)).astype(np.float32)
    d = p % 64
    f["invfreq"] = inv[d % 32][:, None].astype(np.float32)
    f["sinsign"] = np.where(d < 32, 1.0, -1.0)[:, None].astype(np.float32)
    lg = np.log(1.0 - 2.0 ** (-5.0 - np.arange(4, dtype=np.float64)))
    idx = np.arange(C, dtype=np.float64)
    req = np.zeros((128, 2, C), np.float32)
    rek = np.zeros((128, 2, C), np.float32)
    rdec = np.zeros((128, 2), np.float32)
    for tl in range(2):
        for pp in range(128):
            h = tl * 2 + pp // 64
            req[pp, tl] = np.exp((idx + 1.0) * lg[h])
            rek[pp, tl] = np.exp(-(idx + 1.0) * lg[h]) * (64.0 ** -0.5)
            rdec[pp, tl] = np.exp(C * lg[h])
    f["ret_eq"] = req.reshape(128, 2 * C)
    f["ret_ek"] = rek.reshape(128, 2 * C)
    f["ret_dec"] = rdec
    names = list(f.keys())
    offs = {}
    o = 0
    for n in names:
        offs[n] = (o, f[n].shape[1])
        o += f[n].shape[1]
    arr = np.concatenate([f[n] for n in names], axis=1).astype(np.float32)
    return arr, offs


def build(T, L=NL, TB=512, mixers=(1, 1, 1, 1), ffn=True, same=True):
    assert T % TB == 0 and TB % 128 == 0
    NT = TB // 128
    NBLK = T // TB
    NCH = TB // C
    nc = bass.Bass("TRN2", target_bir_lowering=False)
    c = Ctx(nc, same=same)
    carr, coff = _consts(TB)

    def din(name, shape, dt=F32):
        return nc.dram_tensor(name, list(shape), dt, kind="ExternalInput").ap()

    x_d = din("x", [T, D])
    pos_d = din("positions", [1, T], I32)
    w_in_d = din("w_in", [NL, D, INC])
    w_out_d = din("w_out", [NL, D, D])
    ln_d = {k: din(k, [NL, D]) for k in ("ln1_g", "ln1_b", "ln2_g", "ln2_b")}
    lb_d = din("hgrn_lb_logits", [NL, 256])
    hng_d = din("hgrn_norm_g", [NL, 64])
    gw2_d = din("gla_gate_w2", [NL, 16, 128])
    gb_d = din("gla_gate_b", [NL, 128])
    gng_d = din("gla_norm_g", [NL, 64])
    mu_d = din("rwkv_mu", [NL, 1056])
    w0_d = din("rwkv_w0", [NL, 256])
    w2_d = din("rwkv_w2", [NL, 64, 256])
    a0_d = din("rwkv_a0", [NL, 256])
    a2_d = din("rwkv_a2", [NL, 64, 256])
    g2_d = din("rwkv_g2", [NL, 160, 256])
    kk_d = din("rwkv_k_k", [NL, 256])
    ka_d = din("rwkv_k_a", [NL, 256])
    rk_d = din("rwkv_r_k", [NL, 256])
    lxg_d = din("rwkv_lnx_g", [NL, 256])
    lxb_d = din("rwkv_lnx_b", [NL, 256])
    v0_d = din("rwkv_v0", [NL - 1, 256])
    v1_d = din("rwkv_v1", [NL - 1, 256, 32])
    v2_d = din("rwkv_v2", [NL - 1, 32, 256])
    fg_d = din("ffn_w_gate", [2, D, FFD])
    fu_d = din("ffn_w_up", [2, D, FFD])
    fd_d = din("ffn_w_down", [2, FFD, D])
    mr_d = din("moe_router", [2, D, NE])
    mg_d = din("moe_w_gate", [2, NE, D, FFE])
    mu2_d = din("moe_w_up", [2, NE, D, FFE])
    md_d = din("moe_w_down", [2, NE, FFE, D])
    cst_d = din("consts", list(carr.shape))
    out_d = nc.dram_tensor("out", [T, D], F32, kind="ExternalOutput").ap()

    cst = c.sb([128, carr.shape[1]], F32, "cst")
    c.dma("sp", cst[:], cst_d)

    def K(name):
        o, n = coff[name]
        return cst[:, o:o + n]
    ident_bf = c.sb([128, 128], BF16, "identbf")
    c.copy(ident_bf[:], K("ident"))
    mask_bf = c.sb([128, 128], BF16, "maskbf")
    c.copy(mask_bf[:], K("mask_incl"))

    def pp_tile(dram, n, name, nl=NL):
        t = c.sb([128, nl, n], F32, name)
        for l in range(nl):
            for k in range(n):
                c.dma("sp", t[:, l, k:k + 1], dram[l:l + 1, k * 128:(k + 1) * 128].rearrange("o p -> p o"))
        return t

    lbl = pp_tile(lb_d, 2, "lbl")
    w0 = pp_tile(w0_d, 2, "w0")
    a0 = pp_tile(a0_d, 2, "a0")
    kkp = pp_tile(kk_d, 2, "kkp")
    kap = pp_tile(ka_d, 2, "kap")
    rkp = pp_tile(rk_d, 2, "rkp")
    v0p = pp_tile(v0_d, 2, "v0p", nl=NL - 1)
    mup = c.sb([128, NL, 9], F32, "mup")
    c.memset(mup[:], 0.0)
    for l in range(NL):
        for k in range(8):
            c.dma("sp", mup[:, l, k:k + 1], mu_d[l:l + 1, k * 128:(k + 1) * 128].rearrange("o p -> p o"))
        c.dma("sp", mup[0:32, l, 8:9], mu_d[l:l + 1, 1024:1056].rearrange("o p -> p o"))
    omu = c.sb([128, NL, 9], F32, "omu")
    c.ts(omu[:], mup[:], -1.0, ALU.mult, 1.0, ALU.add)
    gb2 = c.sb([64, NL, 2], F32, "gb2")
    for l in range(NL):
        for k in range(2):
            c.dma("sp", gb2[:, l, k:k + 1], gb_d[l:l + 1, k * 64:(k + 1) * 64].rearrange("o p -> p o"))
    ngb2 = c.sb([64, NL, 2], F32, "ngb2")
    c.ts(ngb2[:], gb2[:], -1.0, ALU.mult)
    nw0 = c.sb([128, NL, 2], F32, "nw0")
    c.ts(nw0[:], w0[:], -1.0, ALU.mult)
    lbe = c.sb([128, NL, 2], F32, "lbe")
    c.act(lbe[:], lbl[:], AF.Exp)
    lbs = c.sb([128, 2], F32, "lbs")
    c.tt(lbs[:], lbe[:, 0, :], lbe[:, 1, :], ALU.add)
    for l in range(2, NL):
        c.tt(lbs[:], lbs[:], lbe[:, l, :], ALU.add)
    c.recip(lbs[:], lbs[:])
    lb = c.sb([128, NL, 2], F32, "lb")
    c.memset(lb[:], 0.0)
    for l in range(1, NL):
        c.tt(lb[:, l, :], lbe[:, l, :], lbs[:], ALU.mult)
        c.tt(lb[:, l, :], lb[:, l, :], lb[:, l - 1, :], ALU.add)
    olb = c.sb([128, NL, 2], F32, "olb")
    c.ts(olb[:], lb[:], -1.0, ALU.mult, 1.0, ALU.add)
    nolb = c.sb([128, NL, 2], F32, "nolb")
    c.ts(nolb[:], olb[:], -1.0, ALU.mult)
    epsc = c.sb([128, 4], F32, "epsc")
    c.memset(epsc[:, 0:1], 1e-5)
    c.memset(epsc[:, 1:2], 1e-6)
    c.memset(epsc[:, 2:3], 64e-5)
    c.memset(epsc[:, 3:4], 1.0)
    mrs = c.sb([128, 2, 8, NE], F32, "mrs")
    for i in range(2):
        c.dma("sp", mrs[:, i], mr_d[i].rearrange("(k p) e -> p k e", p=128))
    mrs_bf = c.sb([128, 2, 8, NE], BF16, "mrsbf")
    c.copy(mrs_bf[:], mrs[:])

    rb_small = c.sb([128, 640], F32, "rbsmall")
    gw2 = c.sb([16, 128], F32, "gw2")
    w2s = c.sb([64, 256], F32, "w2s")
    a2s = c.sb([128, 256], F32, "a2s")
    g2a = c.sb([128, 256], F32, "g2a")
    g2b = c.sb([32, 256], F32, "g2b")
    v1s = c.sb([128, 2, 32], F32, "v1s")
    v2s = c.sb([32, 256], F32, "v2s")

    def load_layer_params(l):
        c.dma("sp", rb_small[:, 0:64], hng_d[l:l + 1, :].partition_broadcast(128))
        c.dma("sp", rb_small[:, 64:128], gng_d[l:l + 1, :].partition_broadcast(128))
        c.dma("sp", rb_small[:, 128:384], lxg_d[l:l + 1, :].partition_broadcast(128))
        c.dma("sp", rb_small[:, 384:640], lxb_d[l:l + 1, :].partition_broadcast(128))
        c.dma("sp", gw2[:], gw2_d[l])
        c.dma("sp", w2s[:], w2_d[l])
        c.dma("sp", a2s[64:128, :], a2_d[l])
        c.dma("sp", g2a[:], g2_d[l, 0:128, :])
        c.dma("sp", g2b[:], g2_d[l, 128:160, :])
        if l > 0:
            c.dma("sp", v1s[:], v1_d[l - 1].rearrange("(c p) n -> p c n", p=128))
            c.dma("sp", v2s[:], v2_d[l - 1])

    SLOTN = 8 * 512
    NSLOT = 4
    wslots = [c.sb([128, SLOTN], BF16, "wslot%d" % i) for i in range(NSLOT)]
    accb = Ring([c.ps([128, 512], F32, "acc%d" % i) for i in range(4)])
    trb = Ring([c.ps([128, 512], F32, "trb%d" % i) for i in range(4)])

    hT = c.sb([128, 8, TB], BF16, "hT")
    htok = c.sb([128, NT, D], F32, "htok")
    ytok = c.sb([128, NT, D], BF16, "ytok")
    P = [c.sb([128, TB], F32, "P%d" % i) for i in range(11)]
    qk = [c.sb([128, TB], BF16, "qk%d" % i) for i in range(4)]
    vt = c.sb([128, NT, 256], BF16, "vt")
    gt = c.sb([128, NT, 256], BF16, "gt")
    decs = [c.sb([128, NCH], F32, "dec%d" % i) for i in range(2)]
    dec_rt = [c.sb([128, NCH], F32, "decrt%d" % i) for i in range(2)]
    for i in range(2):
        c.copy(dec_rt[i][:], K("ret_dec")[:, i:i + 1].bc([128, NCH]))
    cosT = c.sb([128, TB], F32, "cosT")
    sinT = c.sb([128, TB], F32, "sinT")
    o_sb = c.sb([128, 256], F32, "o_sb")
    sq_sb = c.sb([128, 256], F32, "sq_sb")
    stat_ln = c.sb([128, 16], F32, "stat_ln")
    stat_mx = c.sb([128, 16], F32, "stat_mx")
    stat_moe = c.sb([128, 40], F32, "stat_moe")
    vfirst = c.sb([128, 2, TB], F32, "vfirst")
    rw_carry = c.sb([128, L, 9], F32, "rwcarry")
    rw_bonus = c.sb([128, NT, 4], F32, "rw_bonus")
    rw_dec = [c.sb([128, NCH], F32, "rwdec%d" % i) for i in range(2)]
    gates = c.sb([128, NT, NE], F32, "gates")

    def mk_state(W, name):
        s = [c.sb([128, W], F32, "%s_f%d" % (name, l)) for l in range(L)]
        sb_ = c.sb([128, W], BF16, "%s_b" % name)
        return s, sb_
    st_hg = [mk_state(128, "hg%d" % t) for t in range(2)]
    st_rt = [mk_state(128, "rt%d" % t) for t in range(2)]
    st_gl = [mk_state(128, "gl%d" % t) for t in range(2)]
    zst = [[c.sb([128, 64], F32, "z%d_%d" % (l, pt)) for pt in range(2)] for l in range(L)]

    RW_BYTES = 5 * 4 * TB + 1 * 4 * (TB + 1) + 2 * 4 * TB + 4 * 4 * TB + 16 * TB + NT * 1024 + NT * 512 + NT * 1024 \
        + 8 * 1024 + 8 * 1024 + 4 * 512 + 12 * 256 + 8 * 512
    FF_BYTES = 22 * TB * 2 + NT * D * 4 + 8 * TB * 2
    RW_BYTES = 5 * 4 * TB + 4 * (TB + 1) + 2 * 4 * TB + 4 * 2 * TB + 8 * TB + NT * 512 + 2 * 128 + NT * 512 + NT * 1024 \
        + 8 * 512 + 8 * 512 + 4 * 256 + 8 * 128 + 4 * 256 + 8 * 256
    ar_ = Arena(c, max(RW_BYTES, FF_BYTES) + 64, "arena")
    off = [0]

    def av(shape, dt, name):
        esz = 4 if dt == F32 else 2
        n = esz
        for s in shape[1:]:
            n *= s
        v = ar_.view(off[0], shape, dt, name)
        off[0] += n
        return v
    rw_sh = [av([128, TB], F32, "rwsh%d" % i) for i in range(5)]
    rw_raw = Ring([av([128, TB + 1], F32, "rwraw%d" % i) for i in range(1)])
    th_t = av([128, TB], F32, "th")
    lvs_t = av([128, TB], F32, "lvs")
    rw_kt = [av([128, TB], BF16, "rw_kt%d" % i) for i in range(2)]
    rw_bt = [av([128, TB], BF16, "rw_bt%d" % i) for i in range(2)]
    rw_ar = [av([128, 2, TB], BF16, "rw_ar%d" % i) for i in range(2)]
    rw_vtok = av([128, NT, 256], BF16, "rw_vtok")
    zbf = [av([128, 64], BF16, "zbf%d" % i) for i in range(2)]
    rw_gtok = av([128, NT, 256], BF16, "rw_gtok")
    o_acc = av([128, NT, 256], F32, "o_acc")
    a_ring = Ring([av([128, 256], BF16, "aring%d" % i) for i in range(8)])
    m_ring = Ring([av([128, 256], BF16, "mring%d" % i) for i in range(8)])
    tok_ring = Ring([av([128, 128], BF16, "tokr%d" % i) for i in range(4)])
    u_ring = Ring([av([128, 64], BF16, "ur%d" % i) for i in range(8)])
    g_ring = Ring([av([128, 64], F32, "gr%d" % i) for i in range(4)])
    TT_ring = Ring([av([128, 128], BF16, "TTr%d" % i) for i in range(8)])
    off[0] = 0
    mT = av([128, 22, TB], BF16, "mT")
    facc = av([128, NT, D], F32, "facc")
    yT = av([128, 8, TB], BF16, "yT")

    out_tt = TT(None, "out_dram")

    pieces = []
    pst = {"issued": 0, "taken": 0}
    free_slots = list(range(NSLOT))
    slot_of = {}

    class Piece:
        __slots__ = ("v", "slot")

    def plan_piece(dap, nk, ncols):
        assert nk * ncols <= SLOTN
        pieces.append((dap, nk, ncols))

    def pump():
        while free_slots and pst["issued"] < len(pieces):
            i = pst["issued"]
            dap, nk, ncols = pieces[i]
            s = free_slots.pop(0)
            dst = wslots[s][:, 0:nk * ncols].re("p (k c) -> p k c", k=nk)
            c.dma("pool", dst, dap)
            p_ = Piece()
            p_.v = dst
            p_.slot = s
            slot_of[i] = p_
            pst["issued"] += 1

    def take_piece():
        pump()
        i = pst["taken"]
        assert i in slot_of, ("weight ring deadlock", i)
        pst["taken"] += 1
        return slot_of.pop(i)

    def release(p_):
        free_slots.append(p_.slot)
        pump()

    def rows_piece(w2d, r0, nk, c0, ncols):
        return w2d[r0 * 128:(r0 + nk) * 128, c0:c0 + ncols].rearrange("(k p) c -> p k c", p=128)

    WIN_PIECES = [(1808, 512), (2320, 512), (2832, 32), (0, 512), (512, 512), (1024, 512), (1536, 272),
                  (2864, 512), (3376, 512)]
    GU_COLS_D = [(0, 512), (512, 512), (1024, 512), (1536, 512), (2048, 512), (2560, 256)]
    GU_COLS_E = [(0, 512), (512, 512), (1024, 384)]

    def plan_layer(l):
        for (c0, n) in WIN_PIECES:
            plan_piece(rows_piece(w_in_d[l], 0, 8, c0, n), 8, n)
        for hf in range(2):
            plan_piece(rows_piece(w_out_d[l], 0, 8, hf * 512, 512), 8, 512)
        if not ffn:
            return
        i = l // 2
        if l % 2 == 0:
            for (c0, n) in GU_COLS_D:
                plan_piece(rows_piece(fg_d[i], 0, 8, c0, n), 8, n)
                plan_piece(rows_piece(fu_d[i], 0, 8, c0, n), 8, n)
            for hf in range(2):
                for (r0, nk) in ((0, 8), (8, 8), (16, 6)):
                    plan_piece(rows_piece(fd_d[i], r0, nk, hf * 512, 512), nk, 512)
        else:
            for e in range(NE):
                for (c0, n) in GU_COLS_E:
                    plan_piece(rows_piece(mg_d[i, e], 0, 8, c0, n), 8, n)
                    plan_piece(rows_piece(mu2_d[i, e], 0, 8, c0, n), 8, n)
                for hf in range(2):
                    for (r0, nk) in ((0, 8), (8, 3)):
                        plan_piece(rows_piece(md_d[i, e], r0, nk, hf * 512, 512), nk, 512)

    for b in range(NBLK):
        for l in range(L):
            plan_layer(l)

    def proj_F(wp, cols, ncols, rhs=None):
        bank = trb.next()
        out = bank[0:ncols, 0:TB]
        for k in range(8):
            c.mm(out, wp.v[:, k, cols:cols + ncols], hT[:, k, :], start=(k == 0), stop=(k == 7))
        return out

    def proj_T(wp, cols, ncols, tile):
        bank = trb.next()
        out = bank[:, 0:ncols]
        for k in range(8):
            c.mm(out, hT[:, k, tile * 128:(tile + 1) * 128], wp.v[:, k, cols:cols + ncols],
                 start=(k == 0), stop=(k == 7))
        return out

    def cumdecay(g, sgn_scale, cum, eq, ek):
        c.scan(cum, K("reset")[:, 0:TB], g, 0.0, ALU.mult, ALU.add)
        c.act(eq, cum, AF.Exp, scale=sgn_scale)
        c.act(ek, cum, AF.Exp, scale=-sgn_scale)

    def chunk_end(v):
        return v.re("p (n c) -> p n c", c=C)[:, :, C - 1]

    def rstd_of(dst, src, eps_col):
        c.act(dst, src, AF.Sqrt, bias=epsc[:, eps_col:eps_col + 1])
        c.recip(dst, dst)

    sc_ring = Ring([c.sb([128, 128], BF16, "sc%d" % i) for i in range(4)])
    kt_ring = Ring([c.sb([128, 128], BF16, "kt%d" % i) for i in range(2)])
    md_ring = Ring([c.sb([128, 128], F32, "md%d" % i) for i in range(2)])

    def gla_tile(l, t, qT, kT, dcs, states, KD, nh_tile):
        tok = slice(t * 128, (t + 1) * 128)
        o_bank = accb.next()
        o_ps = o_bank[:, 0:256]
        first = [True]
        W = nh_tile * 64
        NP = nh_tile * KD
        for pt in range(len(qT)):
            S, Sb = states[pt][0][l][0:NP, :], states[pt][1][0:NP, :]
            if t == 0:
                c.copy(Sb, S, eng="act")
            ktp = trb.next()
            ktv = V(ktp, ktp.t[:, 0:NP // 2].bitcast(BF16))
            c.tr(ktv, kT[pt][:, tok], ident_bf[0:NP, 0:NP])
            ktok = kt_ring.next()[:, 0:NP]
            c.copy(ktok, ktv, eng="act")
            scs = []
            for hh in range(nh_tile):
                pr = slice(hh * KD, (hh + 1) * KD)
                sp_ = trb.next()
                c.mm(sp_[:, 0:128], kT[pt][pr, tok], qT[pt][pr, tok])
                sc = sc_ring.next()[:]
                c.tt(sc, sp_[:, 0:128], mask_bf[:], ALU.mult)
                scs.append(sc)
            yield
            for hh in range(nh_tile):
                hg = pt * nh_tile + hh
                c.mm(o_ps[:, hg * 64:(hg + 1) * 64], scs[hh], vt[:, t, hg * 64:(hg + 1) * 64],
                     start=first[0], stop=False, sg=True)
                first[0] = False
            for ch in range(2):
                cg = t * 2 + ch
                rows = slice(ch * 64, (ch + 1) * 64)
                ctok = slice(t * 128 + ch * 64, t * 128 + (ch + 1) * 64)
                for hh in range(nh_tile):
                    hg = pt * nh_tile + hh
                    pr = slice(hh * KD, (hh + 1) * KD)
                    c.mm(o_ps[rows, hg * 64:(hg + 1) * 64], qT[pt][pr, ctok], Sb[pr, hh * 64:(hh + 1) * 64],
                         start=False, stop=True, sg=True)
                mp = trb.next()
                c.mm(mp[0:NP, 0:W], ktok[rows, :], vt[rows, t, pt * W:(pt + 1) * W])
                md = md_ring.next()[0:NP, 0:W]
                c.act(md, mp[0:NP, 0:W], AF.Copy, scale=dcs[pt][:, cg:cg + 1])
                c.stt(S, S, dcs[pt][:, cg:cg + 1], md, ALU.mult, ALU.add)
                c.copy(Sb, S, eng="act")
                yield
        return o_ps

    def finish_simple(l, t, o_ps, mix_idx, kind, gam_off):
        o = o_sb[:]
        c.copy(o, o_ps, eng="act")
        o3 = o.re("p (h v) -> p h v", h=4)
        sq = sq_sb[:]
        msum = stat_mx[:, 0:4]
        ssum = stat_mx[:, 4:8]
        rstd = stat_mx[:, 8:12]
        if kind == "gn":
            c.reduce(msum, o3, ALU.add)
            c.ts(msum, msum, 1.0 / 64.0, ALU.mult)
            c.tt(o3, o3, msum.re("p (h o) -> p h o", o=1).bc([128, 4, 64]), ALU.subtract)
        c.act(sq, o, AF.Square)
        c.reduce(ssum, sq.re("p (h v) -> p h v", h=4), ALU.add)
        c.ts(ssum, ssum, 1.0 / 64.0, ALU.mult)
        rstd_of(rstd, ssum, 1 if kind == "rms" else 0)
        c.tt(o3, o3, rstd.re("p (h o) -> p h o", o=1).bc([128, 4, 64]), ALU.mult)
        if gam_off is not None:
            gm = rb_small[:, gam_off:gam_off + 64]
            c.tt(o3, o3, gm.re("p (o v) -> p o v", o=1).bc([128, 4, 64]), ALU.mult)
        c.tt(ytok[:, t, mix_idx * 256:(mix_idx + 1) * 256], o, gt[:, t, :], ALU.mult)

    def hgrn(l, wp0, wp1):
        for pt in range(2):
            qps = proj_F(wp0, pt * 128, 128)
            qs = P[0][:]
            c.act(qs, qps, AF.Silu)
            fps = proj_F(wp0, 256 + pt * 128, 128)
            s = P[1][:]
            c.act(s, fps, AF.Sigmoid)
            yield
            f = P[2][:]
            c.ts(f, s, olb[:, l, pt:pt + 1], ALU.mult, lb[:, l, pt:pt + 1], ALU.add)
            k = P[3][:]
            c.ts(k, s, nolb[:, l, pt:pt + 1], ALU.mult, olb[:, l, pt:pt + 1], ALU.add)
            g = P[4][:]
            c.act(g, f, AF.Ln)
            cum, eq, ek = P[5][:], P[6][:], P[7][:]
            cumdecay(g, 1.0, cum, eq, ek)
            c.tt(qk[0 + pt][:], qs, eq, ALU.mult)
            c.tt(qk[2 + pt][:], k, ek, ALU.mult)
            c.copy(decs[pt][:], chunk_end(eq))
            yield
        for t in range(NT):
            ps_ = proj_T(wp1, 0, 512, t)
            c.copy(vt[:, t, :], ps_[:, 0:256], eng="act")
            c.act(gt[:, t, :], ps_[:, 256:512], AF.Silu)
            yield
        release(wp0)
        release(wp1)
        for t in range(NT):
            o_ps = yield from gla_tile(l, t, [qk[0][:], qk[1][:]], [qk[2][:], qk[3][:]], [decs[0][:], decs[1][:]],
                                       st_hg, 64, 2)
            finish_simple(l, t, o_ps, 0, "rms", 0)
            yield

    def gla(l, wp2, wp3):
        gps = proj_F(wp3, 0, 16)
        glr = P[2]
        c.copy(glr[0:16, :], gps, eng="act")
        for pt in range(2):
            qps = proj_F(wp2, pt * 64, 64)
            qs = P[0][0:64, :]
            c.act(qs, qps, AF.Copy, scale=32.0 ** -0.5)
            kps = proj_F(wp2, 128 + pt * 64, 64)
            ks = P[1][0:64, :]
            c.copy(ks, kps, eng="act")
            yield
            bank = trb.next()
            zps = bank[0:64, 0:TB]
            c.mm(zps, gw2[:, pt * 64:(pt + 1) * 64], glr[0:16, :])
            e = P[3][0:64, :]
            c.act(e, zps, AF.Exp, bias=ngb2[:, l, pt:pt + 1], scale=-1.0)
            sp_ = P[4][0:64, :]
            c.act(sp_, e, AF.Ln, bias=epsc[0:64, 3:4])
            cum, eq, ek = P[5][0:64, :], P[6][0:64, :], P[7][0:64, :]
            c.scan(cum, K("reset")[0:64, 0:TB], sp_, 0.0, ALU.mult, ALU.add)
            c.act(eq, cum, AF.Exp, scale=-1.0 / 16.0)
            c.act(ek, cum, AF.Exp, scale=1.0 / 16.0)
            c.tt(qk[0 + pt][0:64, :], qs, eq, ALU.mult)
            c.tt(qk[2 + pt][0:64, :], ks, ek, ALU.mult)
            c.copy(decs[pt][0:64, :], chunk_end(eq))
            yield
        for t in range(NT):
            ps_ = proj_T(wp2, 256, 256, t)
            c.copy(vt[:, t, :], ps_, eng="act")
            ps2 = proj_T(wp3, 16, 256, t)
            c.act(gt[:, t, :], ps2, AF.Silu)
            yield
        release(wp2)
        release(wp3)
        for t in range(NT):
            o_ps = yield from gla_tile(l, t, [qk[0][0:64, :], qk[1][0:64, :]], [qk[2][0:64, :], qk[3][0:64, :]],
                                       [decs[0][0:64, :], decs[1][0:64, :]], st_gl, 32, 2)
            finish_simple(l, t, o_ps, 1, "rms", 64)
            yield

    def rope_tables(b):
        posi = V(P[0], P[0].t[:].bitcast(I32))
        c.dma("sp", posi, pos_d[0:1, b * TB:(b + 1) * TB].partition_broadcast(128))
        posf = P[5]
        c.copy(posf[:], posi)
        ang = P[1][:]
        c.ts(ang, posf[:], K("invfreq"), ALU.mult)
        kq = P[2][:]
        c.ts(kq, ang, float(1.0 / (2.0 * np.pi)), ALU.mult, 12582912.0, ALU.add)
        c.ts(kq, kq, -12582912.0, ALU.add)
        r = P[3][:]
        C1 = 6.28125
        C2 = float(2.0 * np.pi - 6.28125)
        c.stt(r, kq, -C1, ang, ALU.mult, ALU.add)
        c.stt(r, kq, -C2, r, ALU.mult, ALU.add)
        c.ts(r, r, 3.14159, ALU.min, -3.14159, ALU.max)
        c.act(sinT[:], r, AF.Sin)
        ab = P[4][:]
        c.ts(ab, r, -1.0, ALU.mult)
        c.tt(ab, ab, r, ALU.max)
        c.ts(ab, ab, -1.0, ALU.mult, float(np.pi / 2.0), ALU.add)
        c.act(cosT[:], ab, AF.Sin)
        c.ts(sinT[:], sinT[:], K("sinsign"), ALU.mult)

    def ret(l, wp6, wp7):
        for which in range(2):
            for pt in range(2):
                ps_ = proj_F(wp6, which * 256 + pt * 128, 128)
                xs = P[0][:]
                c.copy(xs, ps_, eng="act")
                a = P[1][:]
                c.tt(a, xs, cosT[:], ALU.mult)
                bsw = P[2][:]
                for hh in range(2):
                    lo = slice(hh * 64, hh * 64 + 32)
                    hi = slice(hh * 64 + 32, hh * 64 + 64)
                    c.tt(bsw[lo, :], xs[hi, :], sinT[hi, :], ALU.mult)
                    c.tt(bsw[hi, :], xs[lo, :], sinT[lo, :], ALU.mult)
                c.tt(a, a, bsw, ALU.add)
                tab = K("ret_eq" if which == 0 else "ret_ek")[:, pt * C:(pt + 1) * C]
                dst = qk[which * 2 + pt]
                c.tt(dst[:].re("p (n c) -> p n c", c=C), a.re("p (n c) -> p n c", c=C),
                     tab.re("p (o c) -> p o c", o=1).bc([128, NCH, C]), ALU.mult)
                yield
        for t in range(NT):
            ps_ = proj_T(wp7, 0, 512, t)
            c.copy(vt[:, t, :], ps_[:, 0:256], eng="act")
            c.act(gt[:, t, :], ps_[:, 256:512], AF.Silu)
            yield
        release(wp6)
        release(wp7)
        for t in range(NT):
            o_ps = yield from gla_tile(l, t, [qk[0][:], qk[1][:]], [qk[2][:], qk[3][:]],
                                       [dec_rt[0][:], dec_rt[1][:]], st_rt, 64, 2)
            finish_simple(l, t, o_ps, 3, "gn", None)
            yield

    def rw_shift(l, b, i, wp, c0, n, dst, tm):
        ps_ = proj_F(wp, c0, n)
        raw = rw_raw.next()
        if b == 0:
            c.memset(raw[0:n, 0:1], 0.0)
        else:
            c.copy(raw[0:n, 0:1], rw_carry[0:n, l, i:i + 1])
        c.copy(raw[0:n, 1:TB + 1], ps_, eng="act")
        c.copy(rw_carry[0:n, l, i:i + 1], raw[0:n, TB:TB + 1])
        c.act(tm[0:n, :], raw[0:n, 1:TB + 1], AF.Copy, scale=omu[0:n, l, i:i + 1])
        c.stt(dst[0:n, :], raw[0:n, 0:TB], mup[0:n, l, i:i + 1], tm[0:n, :], ALU.mult, ALU.add)

    def rwkv(l, b, wp4, wp5, wp5b):
        vS = [rw_sh[0], rw_sh[1]]
        waS, rS, kS = rw_sh[2], rw_sh[3], rw_sh[4]
        rw_shift(l, b, 6, wp5, 256, 128, waS, P[2])
        g0S, g1S = P[0], P[1]
        rw_shift(l, b, 7, wp5, 384, 128, g0S, P[2])
        rw_shift(l, b, 8, wp5b, 0, 32, g1S, P[2])
        rw_shift(l, b, 4, wp5, 0, 128, vS[0], P[2])
        rw_shift(l, b, 5, wp5, 128, 128, vS[1], P[2])
        release(wp5)
        release(wp5b)
        c.act(th_t[0:64, :], waS[0:64, :], AF.Tanh)
        c.act(g0S[:], g0S[:], AF.Sigmoid)
        c.act(g1S[0:32, :], g1S[0:32, :], AF.Sigmoid)
        for t in range(NT):
            bank = trb.next()
            c.mm(bank[:, 0:256], g0S[:, t * 128:(t + 1) * 128], g2a[:], start=True, stop=False)
            c.mm(bank[:, 0:256], g1S[0:32, t * 128:(t + 1) * 128], g2b[:], start=False, stop=True)
            c.copy(rw_gtok[:, t, :], bank[:, 0:256], eng="act")
        if l > 0:
            bank = trb.next()
            lv = bank[0:32, 0:TB]
            for k in range(2):
                c.mm(lv, v1s[:, k, :], vS[k][:], start=(k == 0), stop=(k == 1))
            c.copy(lvs_t[0:32, :], lv, eng="act")
        for pt in range(2):
            if l == 0:
                c.copy(vfirst[:, pt, :], vS[pt][:])
            else:
                bank = trb.next()
                c.mm(bank[:, 0:TB], v2s[:, pt * 128:(pt + 1) * 128], lvs_t[0:32, :])
                sgm = P[2][:]
                c.act(sgm, bank[:, 0:TB], AF.Sigmoid, bias=v0p[:, l - 1, pt:pt + 1])
                dv = P[3][:]
                c.tt(dv, vfirst[:, pt, :], vS[pt][:], ALU.subtract)
                c.tt(dv, dv, sgm, ALU.mult)
                c.tt(vS[pt][:], vS[pt][:], dv, ALU.add)
            for t in range(NT):
                bank = trb.next()
                c.tr(bank[:, 0:128], vS[pt][:, t * 128:(t + 1) * 128], K("ident"))
                c.copy(rw_vtok[:, t, pt * 128:(pt + 1) * 128], bank[:, 0:128], eng="act")
        for pt in range(2):
            rw_shift(l, b, 0 + pt, wp4, pt * 128, 128, rS, P[0])
            rw_shift(l, b, 2 + pt, wp4, 256 + pt * 128, 128, kS, P[0])
            if pt == 1:
                release(wp4)
            bank = trb.next()
            c.mm(bank[:, 0:TB], w2s[:, pt * 128:(pt + 1) * 128], th_t[0:64, :])
            t1 = P[2][:]
            c.act(t1, bank[:, 0:TB], AF.Exp, bias=nw0[:, l, pt:pt + 1], scale=-1.0)
            c.act(t1, t1, AF.Ln, bias=epsc[:, 3:4])
            c.ts(t1, t1, -1.0, ALU.mult, -0.5, ALU.add)
            c.act(t1, t1, AF.Exp)
            g = P[3][:]
            c.ts(g, t1, -1.0, ALU.mult)
            cum, eq, ek = P[4][:], P[5][:], P[6][:]
            cumdecay(g, 1.0, cum, eq, ek)
            c.copy(rw_dec[pt][:], chunk_end(eq))
            bank = trb.next()
            c.mm(bank[:, 0:TB], a2s[64:128, pt * 128:(pt + 1) * 128], waS[64:128, :])
            ag = P[7][:]
            c.act(ag, bank[:, 0:TB], AF.Sigmoid, bias=a0[:, l, pt:pt + 1])
            kk = P[8][:]
            c.ts(kk, kS[:], kkp[:, l, pt:pt + 1], ALU.mult)
            nrm = P[9][:]
            c.act(nrm, kk, AF.Square)
            bank = trb.next()
            c.mm(bank[:, 0:TB], K("blockones"), nrm)
            c.act(nrm, bank[:, 0:TB], AF.Sqrt)
            c.ts(nrm, nrm, 1e-12, ALU.max)
            c.act(nrm, nrm, AF.Ln)
            c.act(nrm, nrm, AF.Exp, scale=-1.0)
            c.tt(kk, kk, nrm, ALU.mult)
            fk = P[9][:]
            c.ts(fk, ag, -1.0, ALU.add, kap[:, l, pt:pt + 1], ALU.mult)
            c.ts(fk, fk, 1.0, ALU.add)
            km = P[10][:]
            c.tt(km, kS[:], fk, ALU.mult)
            bo = P[2][:]
            c.stt(bo, rS[:], rkp[:, l, pt:pt + 1], km, ALU.mult, ALU.mult)
            for t in range(NT):
                bank = trb.next()
                c.mm(bank[:, 0:2], bo[:, t * 128:(t + 1) * 128], K("headsel"))
                c.copy(rw_bonus[:, t, pt * 2:pt * 2 + 2], bank[:, 0:2], eng="act")
            c.tt(rw_ar[pt][:, 1, :], rS[:], eq, ALU.mult)
            c.tt(rw_kt[pt][:], km, ek, ALU.mult)
            bb = P[9][:]
            c.tt(bb, kk, ag, ALU.mult)
            c.tt(rw_bt[pt][:], bb, ek, ALU.mult)
            ex = P[9][:]
            c.tt(ex, cum, g, ALU.subtract)
            c.act(ex, ex, AF.Exp)
            c.stt(rw_ar[pt][:, 0, :], kk, -1.0, ex, ALU.mult, ALU.mult)


    F32R = mybir.dt.float32r

    def R32(v):
        return V(v.tt, v.ap.bitcast(F32R))

    def rwkv_core(l, t, pt, hh, sidx):
        h = pt * 2 + hh
        ev = ("act", "dve") if sidx % 2 == 0 else ("dve", "act")
        tok = slice(t * 128, (t + 1) * 128)
        pr = slice(hh * 64, hh * 64 + 64)
        ar = rw_ar[pt][pr, :, tok]
        kt = rw_kt[pt][pr, tok]
        bt = rw_bt[pt][pr, tok]
        at = rw_ar[pt][pr, 0, tok]
        rt = rw_ar[pt][pr, 1, tok]
        Z = zst[l][pt]
        Zb = zbf[pt]
        if t == 0:
            c.copy(Zb[pr, :], Z[pr, :], eng=ev[1])
        vtk = rw_vtok[:, t, h * 64:(h + 1) * 64]
        ocol = slice(h * 64, (h + 1) * 64)
        b1 = trb.next()
        c.mm(b1[:, 0:256].re("p (a n) -> p a n", a=2), kt, ar)
        Ak = a_ring.next()[:]
        c.tt(Ak[:, 0:128], b1[:, 0:128], K("mask_strict"), ALU.mult)
        c.tt(Ak[:, 128:256], b1[:, 128:256], K("mask_incl"), ALU.mult)
        b2 = trb.next()
        c.mm(b2[:, 0:256].re("p (a n) -> p a n", a=2), bt, ar)
        Ab = a_ring.next()[:]
        c.tt(Ab[:, 0:128], b2[:, 0:128], K("mask_strict"), ALU.mult)
        c.tt(Ab[:, 128:256], b2[:, 128:256], K("mask_incl"), ALU.mult)
        b3 = trb.next()
        c.mm(b3[:, 0:128], at, bt)
        pq = m_ring.next()[:]
        Pm = pq[:, 0:128]
        c.tt(Pm, b3[:, 0:128], K("mask_strictT"), ALU.mult)
        Q = Ab[:, 0:128]
        Tc = TT_ring.next()[:]
        c.tt(Tc, Q, K("ident"), ALU.add)
        yield
        for i in range(1, 6):
            bpq = trb.next()
            c.mm(bpq[:, 0:128], Q, Pm)
            if i < 5:
                c.mm(bpq[:, 128:256], Pm, Q)
            pq = m_ring.next()[:]
            if i < 5:
                c.copy(pq, bpq[:, 0:256], eng=ev[0])
            else:
                c.copy(pq[:, 0:128], bpq[:, 0:128], eng=ev[0])
            Pm = pq[:, 0:128]
            Q = pq[:, 128:256]
            yield
            bt_ = trb.next()
            c.mm(bt_[:, 0:128], Pm, Tc)
            Tn = TT_ring.next()[:]
            c.tt(Tn, Tc, bt_[:, 0:128], ALU.add)
            Tc = Tn
            yield
        bk = trb.next()
        bkv = V(bk, bk.t[:, 0:64].bitcast(BF16))
        c.tr(bkv[:, 0:64], kt, ident_bf[pr, pr])
        c.tr(bkv[:, 64:128], bt, ident_bf[pr, pr])
        kb_tok = tok_ring.next()[:]
        c.copy(kb_tok, bkv, eng=ev[0])
        gb_ = trb.next()
        c.mm(gb_[:, 0:64], Ak[:, 0:128], vtk)
        utok = u_ring.next()[:]
        rhs_u = u_ring.next()[:]
        G_sb = g_ring.next()[:]
        c.copy(G_sb, gb_[:, 0:64], eng=ev[1])
        yield
        for ch in range(2):
            rows = slice(ch * 64, (ch + 1) * 64)
            cg = t * 2 + ch
            gz = trb.next()
            c.mm(gz[rows, 0:64], at[:, rows], Zb[pr, :])
            c.tt(rhs_u[rows, :], gz[rows, 0:64], G_sb[rows, :], ALU.add)
            ob = trb.next()
            c.mm(ob[rows, 0:64], rt[:, rows], Zb[pr, :])
            c.copy(o_acc[rows, t, ocol], ob[rows, 0:64], eng=ev[0])
            yield
            ub = trb.next()
            c.mm(ub[rows, 0:64], Tc[rows, rows], rhs_u[rows, :])
            c.copy(utok[rows, :], ub[rows, 0:64], eng=ev[0])
            yield
            zb = trb.next()
            c.mm(zb[pr, 0:64], K("identZ")[pr, :], Z[pr, :], start=True, stop=False, sg=True)
            c.mm(zb[pr, 0:64], kb_tok[rows, 0:64], vtk[rows, :], start=False, stop=False, sg=True)
            c.mm(zb[pr, 0:64], kb_tok[rows, 64:128], utok[rows, :], start=False, stop=True, sg=True)
            c.act(Z[pr, :], zb[pr, 0:64], AF.Copy, scale=rw_dec[pt][pr, cg:cg + 1])
            c.copy(Zb[pr, :], Z[pr, :], eng=ev[1])
            yield
        ob2 = trb.next()
        c.mm(ob2[:, 0:64], Ak[:, 128:256], vtk, start=True, stop=False)
        c.mm(ob2[:, 0:64], Ab[:, 128:256], utok, start=False, stop=True)
        c.tt(o_acc[:, t, ocol], o_acc[:, t, ocol], ob2[:, 0:64], ALU.add)

    def rwkv_finish(l, t):
        o = o_sb[:]
        c.copy(o, o_acc[:, t, :])
        o3 = o.re("p (h v) -> p h v", h=4)
        msum = stat_mx[:, 0:4]
        ssum = stat_mx[:, 4:8]
        rstd = stat_mx[:, 8:12]
        c.reduce(msum, o3, ALU.add)
        c.ts(msum, msum, 1.0 / 64.0, ALU.mult)
        c.tt(o3, o3, msum.re("p (h o) -> p h o", o=1).bc([128, 4, 64]), ALU.subtract)
        sq = sq_sb[:]
        c.act(sq, o, AF.Square)
        c.reduce(ssum, sq.re("p (h v) -> p h v", h=4), ALU.add)
        c.ts(ssum, ssum, 1.0 / 64.0, ALU.mult)
        rstd_of(rstd, ssum, 2)
        c.tt(o3, o3, rstd.re("p (h o) -> p h o", o=1).bc([128, 4, 64]), ALU.mult)
        c.tt(o, o, rb_small[:, 128:384], ALU.mult)
        c.tt(o, o, rb_small[:, 384:640], ALU.add)
        c.tt(sq.re("p (h v) -> p h v", h=4), rw_vtok[:, t, :].re("p (h v) -> p h v", h=4),
             rw_bonus[:, t, :].re("p (h o) -> p h o", o=1).bc([128, 4, 64]), ALU.mult)
        c.tt(o, o, sq, ALU.add)
        c.tt(ytok[:, t, 512:768], o, rw_gtok[:, t, :], ALU.mult)

    def others(l):
        wp0, wp1 = take_piece(), take_piece()
        if mixers[0]:
            yield from hgrn(l, wp0, wp1)
        else:
            release(wp0)
            release(wp1)
        wp2, wp3 = take_piece(), take_piece()
        if mixers[1]:
            yield from gla(l, wp2, wp3)
        else:
            release(wp2)
            release(wp3)
        wp6, wp7 = take_piece(), take_piece()
        if mixers[3]:
            yield from ret(l, wp6, wp7)
        else:
            release(wp6)
            release(wp7)

    def to_hT(t):
        for hf in range(2):
            bank = trb.next()
            for k4 in range(4):
                k = hf * 4 + k4
                c.tr(bank[:, k4 * 128:(k4 + 1) * 128], htok[:, t, k * 128:(k + 1) * 128], K("ident"))
            c.copy(hT[:, hf * 4:(hf + 1) * 4, t * 128:(t + 1) * 128],
                   bank[:, 0:512].re("p (k n) -> p k n", k=4), eng="act")

    def layer_norm(l, which):
        for hf in range(2):
            c.dma("sp", P[4 + hf][:], ln_d[which + "_g"][l:l + 1, hf * 512:(hf + 1) * 512].partition_broadcast(128))
            c.dma("sp", P[6 + hf][:], ln_d[which + "_b"][l:l + 1, hf * 512:(hf + 1) * 512].partition_broadcast(128))
        for t in range(NT):
            z = htok[:, t, :]
            st6 = stat_ln[:, 0:12]
            for hf in range(2):
                zz = z[:, hf * 512:(hf + 1) * 512]
                dst = stat_ln[:, hf * 6:(hf + 1) * 6]
                c.op("dve", lambda zz=zz, dst=dst: nc.vector.bn_stats(dst.ap, zz.ap), reads=[zz], writes=[dst])
            mv = stat_ln[:, 12:14]
            c.op("dve", lambda: nc.vector.bn_aggr(mv.ap, st6.ap), reads=[st6], writes=[mv])
            rs = stat_ln[:, 14:15]
            rstd_of(rs, stat_ln[:, 13:14], 0)
            c.ts(z, z, stat_ln[:, 12:13], ALU.subtract, rs, ALU.mult)
            for hf in range(2):
                zz = z[:, hf * 512:(hf + 1) * 512]
                c.tt(zz, zz, P[4 + hf][:], ALU.mult)
                c.tt(zz, zz, P[6 + hf][:], ALU.add)
            to_hT(t)

    def residual_from_bank(t, hf, bank_v):
        z = htok[:, t, hf * 512:(hf + 1) * 512]
        c.stt(z, z, ALPHA, bank_v, ALU.mult, ALU.add)

    def gate_up(cols_list):
        ft = 0
        for (c0, n) in cols_list:
            wg = take_piece()
            wu = take_piece()
            for j in range(n // 128):
                gps = proj_F(wg, j * 128, 128)
                ups = proj_F(wu, j * 128, 128)
                sl = P[8 + ft % 3][:]
                c.act(sl, gps, AF.Silu)
                c.tt(mT[:, ft, :], sl, ups, ALU.mult)
                ft += 1
            release(wg)
            release(wu)
        return ft

    def down_proj(nft, row_groups, consume):
        for hf in range(2):
            accs = [accb.next() for _ in range(NT)]
            for (r0, nk) in row_groups:
                wd = take_piece()
                for kk_ in range(nk):
                    ft = r0 + kk_
                    for t in range(NT):
                        c.mm(accs[t][:, 0:512], mT[:, ft, t * 128:(t + 1) * 128], wd.v[:, kk_, :],
                             start=(ft == 0), stop=(ft == nft - 1))
                release(wd)
            for t in range(NT):
                consume(t, hf, accs[t][:, 0:512])

    def ffn_dense(l):
        gate_up(GU_COLS_D)
        down_proj(22, ((0, 8), (8, 8), (16, 6)), residual_from_bank)

    def moe(l):
        i = l // 2
        for t in range(NT):
            bank = trb.next()
            lg = bank[:, 0:NE]
            for k in range(8):
                c.mm(lg, hT[:, k, t * 128:(t + 1) * 128], mrs_bf[:, i, k, :], start=(k == 0), stop=(k == 7))
            lgs = stat_moe[:, 0:8]
            c.copy(lgs, lg)
            m8 = stat_moe[:, 8:16]
            c.op("dve", lambda: nc.vector.max(m8.ap, lgs.ap), reads=[lgs], writes=[m8])
            nm1 = stat_moe[:, 32:33]
            c.ts(nm1, m8[:, 0:1], -1.0, ALU.mult)
            ex = stat_moe[:, 16:24]
            c.act(ex, lgs, AF.Exp, bias=nm1)
            sel = stat_moe[:, 24:32]
            c.ts(sel, lgs, m8[:, 1:2], ALU.is_ge)
            c.tt(ex, ex, sel, ALU.mult)
            ssum = stat_moe[:, 33:34]
            c.reduce(ssum, ex, ALU.add)
            c.recip(ssum, ssum)
            c.ts(gates[:, t, :], ex, ssum, ALU.mult)
        c.memset(facc[:], 0.0)
        for e in range(NE):
            gate_up(GU_COLS_E)

            def cons(t, hf, bank_v, e=e):
                fa = facc[:, t, hf * 512:(hf + 1) * 512]
                c.stt(fa, bank_v, gates[:, t, e:e + 1], fa, ALU.mult, ALU.add)
            down_proj(11, ((0, 8), (8, 3)), cons)
        for t in range(NT):
            z = htok[:, t, :]
            c.stt(z, z, ALPHA, facc[:, t, :], ALU.mult, ALU.add)

    for l in range(L):
        for pt in range(2):
            for st in (st_hg, st_rt, st_gl):
                c.memset(st[pt][0][l][:], 0.0)
            c.memset(zst[l][pt][:], 0.0)
    c.memset(ytok[:], 0.0)

    for b in range(NBLK):
        for t in range(NT):
            r0 = b * TB + t * 128
            c.dma("sp", htok[:, t, :], x_d[r0:r0 + 128, :])
            to_hT(t)
        if mixers[3]:
            rope_tables(b)
        for l in range(L):
            load_layer_params(l)
            wp4, wp5, wp5b = take_piece(), take_piece(), take_piece()
            if mixers[2]:
                rwkv(l, b, wp4, wp5, wp5b)
            else:
                release(wp4)
                release(wp5)
                release(wp5b)
            og = others(l)
            og_live = [True]

            def og_step():
                if og_live[0]:
                    try:
                        next(og)
                    except StopIteration:
                        og_live[0] = False
            if mixers[2]:
                for t in range(NT):
                    live = [rwkv_core(l, t, h // 2, h % 2, h) for h in range(4)]
                    while live:
                        nxt = []
                        for g_ in live:
                            try:
                                next(g_)
                                nxt.append(g_)
                            except StopIteration:
                                pass
                        live = nxt
                        og_step()
                    rwkv_finish(l, t)
            while og_live[0]:
                og_step()
            for t in range(NT):
                for hf in range(2):
                    bank = trb.next()
                    bv = V(bank, bank.t[:, 0:256].bitcast(BF16))
                    for k4 in range(4):
                        k = hf * 4 + k4
                        c.tr(bv[:, k4 * 128:(k4 + 1) * 128], ytok[:, t, k * 128:(k + 1) * 128], ident_bf[:])
                    c.copy(yT[:, hf * 4:(hf + 1) * 4, t * 128:(t + 1) * 128],
                           bv.re("p (k n) -> p k n", k=4), eng="act")
            wo = [take_piece(), take_piece()]
            for t in range(NT):
                for hf in range(2):
                    bank = trb.next()
                    for k in range(8):
                        c.mm(bank[:, 0:512], yT[:, k, t * 128:(t + 1) * 128], wo[hf].v[:, k, :],
                             start=(k == 0), stop=(k == 7))
                    residual_from_bank(t, hf, bank[:, 0:512])
            release(wo[0])
            release(wo[1])
            layer_norm(l, "ln1")
            if ffn:
                if l % 2 == 0:
                    ffn_dense(l)
                else:
                    moe(l)
                layer_norm(l, "ln2")
        for t in range(NT):
            r0 = b * TB + t * 128
            c.dma("sp", V(out_tt, out_d[r0:r0 + 128, :]), htok[:, t, :])
    c.wait_all("sp", [out_tt])
    assert pst["taken"] == len(pieces), (pst, len(pieces))
    return nc, carr, c


_CACHE = {}

NAMES = ["w_in", "w_out", "ln1_g", "ln1_b", "ln2_g", "ln2_b", "hgrn_lb_logits", "hgrn_norm_g", "gla_gate_w2",
         "gla_gate_b", "gla_norm_g", "rwkv_mu", "rwkv_w0", "rwkv_w2", "rwkv_a0", "rwkv_a2", "rwkv_g2", "rwkv_k_k",
         "rwkv_k_a", "rwkv_r_k", "rwkv_lnx_g", "rwkv_lnx_b", "rwkv_v0", "rwkv_v1", "rwkv_v2", "ffn_w_gate",
         "ffn_w_up", "ffn_w_down", "moe_router", "moe_w_gate", "moe_w_up", "moe_w_down"]


def run(inputs, T, L=NL, TB=512, **kw):
    x = np.asarray(inputs["x"], dtype=np.float32)
    B = x.shape[0]
    key = (T, L, TB, tuple(sorted(kw.items())))
    if key not in _CACHE:
        _CACHE[key] = build(T, L, TB, **kw)
    nc, carr, c = _CACHE[key]
    shared = {}
    for n in NAMES:
        a = np.ascontiguousarray(np.asarray(inputs[n], dtype=np.float32))
        if n == "rwkv_r_k":
            a = a.reshape(NL, 256)
        shared[n] = a
    shared["consts"] = carr
    pos = np.asarray(inputs["positions"]).astype(np.int32)
    in_maps = []
    for b in range(B):
        m = dict(shared)
        m["x"] = np.ascontiguousarray(x[b, :T])
        m["positions"] = np.ascontiguousarray(pos[b:b + 1, :T])
        in_maps.append(m)
    res = run_bass_kernel_spmd(nc, in_maps, core_ids=list(range(B)))
    return np.stack([np.asarray(r["out"]) for r in res.results], axis=0)


def kernel(**inputs):
    x = np.asarray(inputs["x"])
    B, S, _ = x.shape
    out = run(inputs, S)
    return out.astype(x.dtype)
```

```python
import numpy as np
import ml_dtypes
import concourse.bass as bass
import concourse.mybir as mybir
from concourse.bass_utils import run_bass_kernel_spmd

F32 = mybir.dt.float32
BF16 = mybir.dt.bfloat16
I32 = mybir.dt.int32
AF = mybir.ActivationFunctionType
ALU = mybir.AluOpType
AX = mybir.AxisListType

D = 1024
NL = 4
INC = 3888
FFD = 2816
NE = 8
FFE = 1408
ALPHA = (2.0 * NL) ** 0.25
C = 64


class V:
    __slots__ = ("tt", "ap")

    def __init__(self, tt, ap):
        self.tt = tt
        self.ap = ap

    def __getitem__(self, k):
        return V(self.tt, self.ap[k])

    def re(self, s, **kw):
        return V(self.tt, self.ap.rearrange(s, **kw))

    def bc(self, shape):
        return V(self.tt, self.ap.to_broadcast(list(shape)))


class TT:
    __slots__ = ("t", "name", "w", "r", "al", "pe_row")

    def __init__(self, t, name):
        self.t = t
        self.name = name
        self.w = None
        self.r = []
        self.al = []
        self.pe_row = None

    def __getitem__(self, k):
        return V(self, self.t[k])


class Ctx:
    NDMA = 24

    def __init__(self, nc, same=True):
        self.nc = nc
        self.same = same
        self.engs = {"pe": nc.tensor, "dve": nc.vector, "act": nc.scalar, "pool": nc.gpsimd, "sp": nc.sync}
        self.sem = {}
        self.cnt = {}
        self.waited = {}
        self._cms = []
        for k in self.engs:
            cm = nc.semaphore("s_" + k)
            self.sem[k] = cm.__enter__()
            self._cms.append(cm)
            self.cnt[k] = 0
            self.waited[k] = {}
        self.dsem = []
        self.dcnt = []
        for i in range(self.NDMA):
            cm = nc.semaphore("d_%d" % i)
            self.dsem.append(cm.__enter__())
            self._cms.append(cm)
            self.dcnt.append(0)
        self.dnext = {"sp": 0, "pool": 0, "act": 0}
        self.drange = {"sp": (0, 14), "pool": (14, 22), "act": (22, 24)}
        self.ntile = 0
        self.ninst = 0

    def sb(self, shape, dt, name=None):
        self.ntile += 1
        name = name or "t%d" % self.ntile
        cm = self.nc.sbuf_tensor(name, list(shape), dt)
        t = cm.__enter__()
        self._cms.append(cm)
        return TT(t, name)

    def ps(self, shape, dt, name=None):
        self.ntile += 1
        name = name or "p%d" % self.ntile
        cm = self.nc.psum_tensor(name, list(shape), dt)
        t = cm.__enter__()
        self._cms.append(cm)
        return TT(t, name)

    def _semof(self, key):
        if key in self.sem:
            return self.sem[key]
        return self.dsem[int(key[1:])]

    def _deps(self, eng, reads, writes):
        need = {}

        def add(dep):
            if dep is None:
                return
            k, v = dep
            if need.get(k, 0) < v:
                need[k] = v
        rawonly = (self.same == "raw")
        need_raw = {}
        for t in reads:
            add(t.w)
            for a in t.al:
                add(a.w)
        if rawonly:
            need_raw = dict(need)
        for t in writes:
            add(t.w)
            for d in t.r:
                add(d)
            for a in t.al:
                add(a.w)
                for d in a.r:
                    add(d)
        if rawonly and eng in need:
            if eng in need_raw:
                need[eng] = need_raw[eng]
            else:
                del need[eng]
        h = self.engs[eng]
        for k, v in need.items():
            if k == eng and (not self.same or eng == "pe"):
                continue
            if self.waited[eng].get(k, 0) >= v:
                continue
            h.wait_ge(self._semof(k), v)
            self.waited[eng][k] = v
            self.ninst += 1

    def _mark(self, me, reads, writes):
        for t in reads:
            if len(t.r) > 64:
                mx = {}
                for (k, v) in t.r:
                    if mx.get(k, 0) < v:
                        mx[k] = v
                t.r = list(mx.items())
            t.r.append(me)
        for t in writes:
            t.w = me
            t.r = []

    def op(self, eng, fn, reads=(), writes=()):
        reads = [x.tt if isinstance(x, V) else x for x in reads if x is not None]
        writes = [x.tt if isinstance(x, V) else x for x in writes if x is not None]
        self._deps(eng, reads, writes)
        ins = fn()
        self.cnt[eng] += 1
        ins.then_inc(self.sem[eng], 1)
        self._mark((eng, self.cnt[eng]), reads, writes)
        self.ninst += 1
        return ins

    def dma(self, q, out, in_, **kw):
        reads = [in_.tt] if isinstance(in_, V) else []
        writes = [out.tt] if isinstance(out, V) else []
        lo, hi = self.drange[q]
        i = lo + self.dnext[q]
        self.dnext[q] = (self.dnext[q] + 1) % (hi - lo)
        key = "d%d" % i
        h = self.engs[q]
        if self.dcnt[i] > 0 and self.waited[q].get(key, 0) < self.dcnt[i]:
            h.wait_ge(self.dsem[i], self.dcnt[i])
            self.waited[q][key] = self.dcnt[i]
        self._deps(q, reads, writes)
        oa = out.ap if isinstance(out, V) else out
        ia = in_.ap if isinstance(in_, V) else in_
        ins = h.dma_start(out=oa, in_=ia, **kw)
        self.dcnt[i] += 16
        ins.then_inc(self.dsem[i], 16)
        self._mark((key, self.dcnt[i]), reads, writes)
        self.ninst += 1
        return ins

    def _pe_row_guard(self, out, lhsT):
        row = (int(lhsT.ap.start_partition()), int(lhsT.ap.partition_size()))
        t = out.tt
        if t.pe_row is not None and t.pe_row != row and t.w is not None and t.w[0] == "pe":
            v = t.w[1]
            if self.waited["pe"].get("pe", 0) < v:
                self.nc.tensor.wait_ge(self.sem["pe"], v)
                self.waited["pe"]["pe"] = v
                self.ninst += 1
        t.pe_row = row

    def mm(self, out, lhsT, rhs, start=True, stop=True, sg=False):
        nc = self.nc
        self._pe_row_guard(out, lhsT)
        return self.op("pe", lambda: nc.tensor.matmul(out.ap, lhsT=lhsT.ap, rhs=rhs.ap, start=start, stop=stop,
                                                      skip_group_check=sg),
                       reads=[lhsT, rhs], writes=[out])

    def tr(self, out, in_, ident):
        nc = self.nc
        self._pe_row_guard(out, in_)
        return self.op("pe", lambda: nc.tensor.transpose(out.ap, in_.ap, ident.ap), reads=[in_, ident], writes=[out])

    def act(self, out, in_, func, bias=None, scale=None, eng="act"):
        nc = self.nc
        kw = {}
        rd = [in_]
        if bias is not None:
            if isinstance(bias, V):
                kw["bias"] = bias.ap
                rd.append(bias)
            else:
                kw["bias"] = bias
        if scale is not None:
            if isinstance(scale, V):
                kw["scale"] = scale.ap
                rd.append(scale)
            else:
                kw["scale"] = scale
        if func == AF.Copy and isinstance(scale, V):
            func = AF.Identity
        return self.op("act", lambda: nc.scalar.activation(out.ap, in_.ap, func, **kw), reads=rd, writes=[out])

    def tt(self, out, in0, in1, op, eng="dve"):
        h = self.engs[eng]
        return self.op(eng, lambda: h.tensor_tensor(out.ap, in0.ap, in1.ap, op), reads=[in0, in1], writes=[out])

    def ts(self, out, in0, s1, op0, s2=None, op1=None, eng="dve"):
        h = self.engs[eng]
        rd = [in0]
        a1 = s1
        if isinstance(s1, V):
            rd.append(s1)
            a1 = s1.ap
        a2 = s2
        if isinstance(s2, V):
            rd.append(s2)
            a2 = s2.ap
        if op1 is None:
            return self.op(eng, lambda: h.tensor_scalar(out.ap, in0.ap, a1, None, op0), reads=rd, writes=[out])
        return self.op(eng, lambda: h.tensor_scalar(out.ap, in0.ap, a1, a2, op0, op1), reads=rd, writes=[out])

    def stt(self, out, in0, scalar, in1, op0, op1):
        nc = self.nc
        rd = [in0, in1]
        a = scalar
        if isinstance(scalar, V):
            rd.append(scalar)
            a = scalar.ap
        return self.op("dve", lambda: nc.vector.scalar_tensor_tensor(out.ap, in0.ap, a, in1.ap, op0, op1),
                       reads=rd, writes=[out])

    def copy(self, out, in_, eng="dve"):
        nc = self.nc
        if eng == "act":
            return self.op("act", lambda: nc.scalar.copy(out.ap, in_.ap), reads=[in_], writes=[out])
        h = self.engs[eng]
        return self.op(eng, lambda: h.tensor_copy(out.ap, in_.ap), reads=[in_], writes=[out])

    def memset(self, out, val, eng="dve"):
        h = self.engs[eng]
        return self.op(eng, lambda: h.memset(out.ap, val), reads=[], writes=[out])

    def scan(self, out, d0, d1, init, op0, op1):
        nc = self.nc
        rd = [d0, d1]
        a = init
        if isinstance(init, V):
            rd.append(init)
            a = init.ap
        return self.op("dve", lambda: nc.vector.tensor_tensor_scan(out.ap, d0.ap, d1.ap, a, op0, op1),
                       reads=rd, writes=[out])

    def reduce(self, out, in_, op, axis=AX.X):
        nc = self.nc
        return self.op("dve", lambda: nc.vector.tensor_reduce(out.ap, in_.ap, axis, op), reads=[in_], writes=[out])

    def recip(self, out, in_):
        nc = self.nc
        return self.op("dve", lambda: nc.vector.reciprocal(out.ap, in_.ap), reads=[in_], writes=[out])

    def wait_all(self, eng, tts):
        self._deps(eng, tts, ())


class Arena:
    def __init__(self, c, nbytes, name):
        self.tt = c.sb([128, nbytes // 4], F32, name)
        self.views = []
        self.nbytes = nbytes

    def view(self, off, shape, dt, name):
        esz = 4 if dt in (F32, I32) else 2
        n = esz
        for s in shape[1:]:
            n *= s
        assert off % 4 == 0 and n % 4 == 0 and off + n <= self.nbytes, (name, off, n, self.nbytes)
        ap = self.tt.t[:, off // 4:(off + n) // 4]
        if dt != F32:
            ap = ap.bitcast(dt)
        if len(shape) == 3:
            ap = ap.rearrange("p (a b) -> p a b", a=shape[1])
        elif len(shape) == 4:
            ap = ap.rearrange("p (a b c) -> p a b c", a=shape[1], b=shape[2])
        if shape[0] < 128:
            ap = ap[0:shape[0]]
        t = TT(ap, name)
        for (v, lo, hi) in self.views:
            if lo < off + n and off < hi:
                t.al.append(v)
                v.al.append(t)
        self.views.append((t, off, off + n))
        return t


class Ring:
    def __init__(self, tiles):
        self.tiles = tiles
        self.i = 0

    def next(self):
        t = self.tiles[self.i]
        self.i = (self.i + 1) % len(self.tiles)
        return t


def _consts(TB):
    p = np.arange(128)
    f = {}
    j = p[:, None]
    i = p[None, :]
    same = (j // C) == (i // C)
    f["mask_incl"] = (same & (j <= i)).astype(np.float32)
    f["mask_strict"] = (same & (j < i)).astype(np.float32)
    f["mask_strictT"] = (same & (j > i)).astype(np.float32)
    f["ident"] = np.eye(128, dtype=np.float32)
    f["blockones"] = same.astype(np.float32)
    f["identZ"] = ((p[:, None] % 64) == np.arange(64)[None, :]).astype(np.float32)
    hs = np.zeros((128, 2), np.float32)
    hs[:64, 0] = 1.0
    hs[64:, 1] = 1.0
    f["headsel"] = hs
    t = np.arange(TB)
    f["reset"] = np.broadcast_to((t % C != 0).astype(np.float32)[None, :], (128, TB)).copy()
    half = 32
    inv = (10000.0 ** (-np.arange(half, dtype=np.float32) / half)).astype(np.float32)
    d = p % 64
    f["invfreq"] = inv[d % 32][:, None].astype(np.float32)
    f["sinsign"] = np.where(d < 32, 1.0, -1.0)[:, None].astype(np.float32)
    lg = np.log(1.0 - 2.0 ** (-5.0 - np.arange(4, dtype=np.float64)))
    idx = np.arange(C, dtype=np.float64)
    req = np.zeros((128, 2, C), np.float32)
    rek = np.zeros((128, 2, C), np.float32)
    rdec = np.zeros((128, 2), np.float32)
    for tl in range(2):
        for pp in range(128):
            h = tl * 2 + pp // 64
            req[pp, tl] = np.exp((idx + 1.0) * lg[h])
            rek[pp, tl] = np.exp(-(idx + 1.0) * lg[h]) * (64.0 ** -0.5)
            rdec[pp, tl] = np.exp(C * lg[h])
    f["ret_eq"] = req.reshape(128, 2 * C)
    f["ret_ek"] = rek.reshape(128, 2 * C)
    f["ret_dec"] = rdec
    names = list(f.keys())
    offs = {}
    o = 0
    for n in names:
        offs[n] = (o, f[n].shape[1])
        o += f[n].shape[1]
    arr = np.concatenate([f[n] for n in names], axis=1).astype(np.float32)
    return arr, offs


def build(T, L=NL, TB=512, mixers=(1, 1, 1, 1), ffn=True, same="raw"):
    assert T % TB == 0 and TB % 128 == 0
    NT = TB // 128
    NBLK = T // TB
    NCH = TB // C
    nc = bass.Bass("TRN2", target_bir_lowering=False)
    c = Ctx(nc, same=same)
    carr, coff = _consts(TB)

    def din(name, shape, dt=F32):
        return nc.dram_tensor(name, list(shape), dt, kind="ExternalInput").ap()

    x_d = din("x", [T, D])
    pos_d = din("positions", [1, T], I32)
    w_in_d = din("w_in", [NL, D, INC])
    w_out_d = din("w_out", [NL, D, D])
    ln_d = {k: din(k, [NL, D]) for k in ("ln1_g", "ln1_b", "ln2_g", "ln2_b")}
    lb_d = din("hgrn_lb_logits", [NL, 256])
    hng_d = din("hgrn_norm_g", [NL, 64])
    gw2_d = din("gla_gate_w2", [NL, 16, 128])
    gb_d = din("gla_gate_b", [NL, 128])
    gng_d = din("gla_norm_g", [NL, 64])
    mu_d = din("rwkv_mu", [NL, 1056])
    w0_d = din("rwkv_w0", [NL, 256])
    w2_d = din("rwkv_w2", [NL, 64, 256])
    a0_d = din("rwkv_a0", [NL, 256])
    a2_d = din("rwkv_a2", [NL, 64, 256])
    g2_d = din("rwkv_g2", [NL, 160, 256])
    kk_d = din("rwkv_k_k", [NL, 256])
    ka_d = din("rwkv_k_a", [NL, 256])
    rk_d = din("rwkv_r_k", [NL, 256])
    lxg_d = din("rwkv_lnx_g", [NL, 256])
    lxb_d = din("rwkv_lnx_b", [NL, 256])
    v0_d = din("rwkv_v0", [NL - 1, 256])
    v1_d = din("rwkv_v1", [NL - 1, 256, 32])
    v2_d = din("rwkv_v2", [NL - 1, 32, 256])
    fg_d = din("ffn_w_gate", [2, D, FFD])
    fu_d = din("ffn_w_up", [2, D, FFD])
    fd_d = din("ffn_w_down", [2, FFD, D])
    mr_d = din("moe_router", [2, D, NE])
    mg_d = din("moe_w_gate", [2, NE, D, FFE])
    mu2_d = din("moe_w_up", [2, NE, D, FFE])
    md_d = din("moe_w_down", [2, NE, FFE, D])
    cst_d = din("consts", list(carr.shape))
    out_d = nc.dram_tensor("out", [T, D], F32, kind="ExternalOutput").ap()

    cst = c.sb([128, carr.shape[1]], F32, "cst")
    c.dma("sp", cst[:], cst_d)

    def K(name):
        o, n = coff[name]
        return cst[:, o:o + n]
    ident_bf = c.sb([128, 128], BF16, "identbf")
    c.copy(ident_bf[:], K("ident"))
    mask_bf = c.sb([128, 128], BF16, "maskbf")
    c.copy(mask_bf[:], K("mask_incl"))

    def pp_tile(dram, n, name, nl=NL):
        t = c.sb([128, nl, n], F32, name)
        for l in range(nl):
            for k in range(n):
                c.dma("sp", t[:, l, k:k + 1], dram[l:l + 1, k * 128:(k + 1) * 128].rearrange("o p -> p o"))
        return t

    lbl = pp_tile(lb_d, 2, "lbl")
    w0 = pp_tile(w0_d, 2, "w0")
    a0 = pp_tile(a0_d, 2, "a0")
    kkp = pp_tile(kk_d, 2, "kkp")
    kap = pp_tile(ka_d, 2, "kap")
    rkp = pp_tile(rk_d, 2, "rkp")
    v0p = pp_tile(v0_d, 2, "v0p", nl=NL - 1)
    mup = c.sb([128, NL, 9], F32, "mup")
    c.memset(mup[:], 0.0)
    for l in range(NL):
        for k in range(8):
            c.dma("sp", mup[:, l, k:k + 1], mu_d[l:l + 1, k * 128:(k + 1) * 128].rearrange("o p -> p o"))
        c.dma("sp", mup[0:32, l, 8:9], mu_d[l:l + 1, 1024:1056].rearrange("o p -> p o"))
    omu = c.sb([128, NL, 9], F32, "omu")
    c.ts(omu[:], mup[:], -1.0, ALU.mult, 1.0, ALU.add)
    gb2 = c.sb([64, NL, 2], F32, "gb2")
    for l in range(NL):
        for k in range(2):
            c.dma("sp", gb2[:, l, k:k + 1], gb_d[l:l + 1, k * 64:(k + 1) * 64].rearrange("o p -> p o"))
    ngb2 = c.sb([64, NL, 2], F32, "ngb2")
    c.ts(ngb2[:], gb2[:], -1.0, ALU.mult)
    nw0 = c.sb([128, NL, 2], F32, "nw0")
    c.ts(nw0[:], w0[:], -1.0, ALU.mult)
    lbe = c.sb([128, NL, 2], F32, "lbe")
    c.act(lbe[:], lbl[:], AF.Exp)
    lbs = c.sb([128, 2], F32, "lbs")
    c.tt(lbs[:], lbe[:, 0, :], lbe[:, 1, :], ALU.add)
    for l in range(2, NL):
        c.tt(lbs[:], lbs[:], lbe[:, l, :], ALU.add)
    c.recip(lbs[:], lbs[:])
    lb = c.sb([128, NL, 2], F32, "lb")
    c.memset(lb[:], 0.0)
    for l in range(1, NL):
        c.tt(lb[:, l, :], lbe[:, l, :], lbs[:], ALU.mult)
        c.tt(lb[:, l, :], lb[:, l, :], lb[:, l - 1, :], ALU.add)
    olb = c.sb([128, NL, 2], F32, "olb")
    c.ts(olb[:], lb[:], -1.0, ALU.mult, 1.0, ALU.add)
    nolb = c.sb([128, NL, 2], F32, "nolb")
    c.ts(nolb[:], olb[:], -1.0, ALU.mult)
    epsc = c.sb([128, 4], F32, "epsc")
    c.memset(epsc[:, 0:1], 1e-5)
    c.memset(epsc[:, 1:2], 1e-6)
    c.memset(epsc[:, 2:3], 64e-5)
    c.memset(epsc[:, 3:4], 1.0)
    mrs = c.sb([128, 2, 8, NE], F32, "mrs")
    for i in range(2):
        c.dma("sp", mrs[:, i], mr_d[i].rearrange("(k p) e -> p k e", p=128))
    mrs_bf = c.sb([128, 2, 8, NE], BF16, "mrsbf")
    c.copy(mrs_bf[:], mrs[:])

    rb_small = c.sb([128, 640], F32, "rbsmall")
    gw2 = c.sb([16, 128], F32, "gw2")
    w2s = c.sb([64, 256], F32, "w2s")
    a2s = c.sb([128, 256], F32, "a2s")
    g2a = c.sb([128, 256], F32, "g2a")
    g2b = c.sb([32, 256], F32, "g2b")
    v1s = c.sb([128, 2, 32], F32, "v1s")
    v2s = c.sb([32, 256], F32, "v2s")

    def load_layer_params(l):
        c.dma("sp", rb_small[:, 0:64], hng_d[l:l + 1, :].partition_broadcast(128))
        c.dma("sp", rb_small[:, 64:128], gng_d[l:l + 1, :].partition_broadcast(128))
        c.dma("sp", rb_small[:, 128:384], lxg_d[l:l + 1, :].partition_broadcast(128))
        c.dma("sp", rb_small[:, 384:640], lxb_d[l:l + 1, :].partition_broadcast(128))
        c.dma("sp", gw2[:], gw2_d[l])
        c.dma("sp", w2s[:], w2_d[l])
        c.dma("sp", a2s[64:128, :], a2_d[l])
        c.dma("sp", g2a[:], g2_d[l, 0:128, :])
        c.dma("sp", g2b[:], g2_d[l, 128:160, :])
        if l > 0:
            c.dma("sp", v1s[:], v1_d[l - 1].rearrange("(c p) n -> p c n", p=128))
            c.dma("sp", v2s[:], v2_d[l - 1])

    SLOTN = 8 * 512
    NSLOT = 4
    wslots = [c.sb([128, SLOTN], BF16, "wslot%d" % i) for i in range(NSLOT)]
    accb = Ring([c.ps([128, 512], F32, "acc%d" % i) for i in range(4)])
    trb = Ring([c.ps([128, 512], F32, "trb%d" % i) for i in range(4)])

    hT = c.sb([128, 8, TB], BF16, "hT")
    htok = c.sb([128, NT, D], F32, "htok")
    ytok = c.sb([128, NT, D], BF16, "ytok")
    P = [c.sb([128, TB], F32, "P%d" % i) for i in range(11)]
    R = [c.sb([128, TB], F32, "R%d" % i) for i in range(11)]
    qk = [c.sb([128, TB], BF16, "qk%d" % i) for i in range(4)]
    vt = c.sb([128, NT, 256], BF16, "vt")
    gt = c.sb([128, NT, 256], BF16, "gt")
    decs = [c.sb([128, NCH], F32, "dec%d" % i) for i in range(2)]
    dec_rt = [c.sb([128, NCH], F32, "decrt%d" % i) for i in range(2)]
    for i in range(2):
        c.copy(dec_rt[i][:], K("ret_dec")[:, i:i + 1].bc([128, NCH]))
    cosT = c.sb([128, TB], F32, "cosT")
    sinT = c.sb([128, TB], F32, "sinT")
    o_sb = c.sb([128, 256], F32, "o_sb")
    sq_sb = c.sb([128, 256], F32, "sq_sb")
    stat_ln = c.sb([128, 16], F32, "stat_ln")
    stat_mx = c.sb([128, 16], F32, "stat_mx")
    stat_moe = c.sb([128, 40], F32, "stat_moe")
    vfirst = c.sb([128, 2, TB], F32, "vfirst")
    rw_carry = c.sb([128, L, 9], F32, "rwcarry")
    rw_bonus = c.sb([128, NT, 4], F32, "rw_bonus")
    rw_dec = [c.sb([128, NCH], F32, "rwdec%d" % i) for i in range(2)]
    gates = c.sb([128, NT, NE], F32, "gates")

    def mk_state(W, name):
        s = [c.sb([128, W], F32, "%s_f%d" % (name, l)) for l in range(L)]
        sb_ = c.sb([128, W], BF16, "%s_b" % name)
        return s, sb_
    st_hg = [mk_state(128, "hg%d" % t) for t in range(2)]
    st_rt = [mk_state(128, "rt%d" % t) for t in range(2)]
    st_gl = [mk_state(128, "gl%d" % t) for t in range(2)]
    zst = [[c.sb([128, 64], F32, "z%d_%d" % (l, pt)) for pt in range(2)] for l in range(L)]

    RW_BYTES = 5 * 4 * TB + 1 * 4 * (TB + 1) + 2 * 4 * TB + 4 * 4 * TB + 16 * TB + NT * 1024 + NT * 512 + NT * 1024 \
        + 8 * 1024 + 8 * 1024 + 4 * 512 + 12 * 256 + 8 * 512
    FF_BYTES = 22 * TB * 2 + NT * D * 4 + 8 * TB * 2
    RW_BYTES = 5 * 4 * TB + 4 * (TB + 1) + 2 * 4 * TB + 4 * 2 * TB + 8 * TB + NT * 512 + 2 * 128 + NT * 512 + NT * 1024 \
        + 8 * 512 + 8 * 512 + 4 * 256 + 8 * 128 + 4 * 256 + 8 * 256
    ar_ = Arena(c, max(RW_BYTES, FF_BYTES) + 64, "arena")
    off = [0]

    def av(shape, dt, name):
        esz = 4 if dt == F32 else 2
        n = esz
        for s in shape[1:]:
            n *= s
        v = ar_.view(off[0], shape, dt, name)
        off[0] += n
        return v
    rw_sh = [av([128, TB], F32, "rwsh%d" % i) for i in range(5)]
    rw_raw = Ring([av([128, TB + 1], F32, "rwraw%d" % i) for i in range(1)])
    th_t = av([128, TB], F32, "th")
    lvs_t = av([128, TB], F32, "lvs")
    rw_kt = [av([128, TB], BF16, "rw_kt%d" % i) for i in range(2)]
    rw_bt = [av([128, TB], BF16, "rw_bt%d" % i) for i in range(2)]
    rw_ar = [av([128, 2, TB], BF16, "rw_ar%d" % i) for i in range(2)]
    rw_vtok = av([128, NT, 256], BF16, "rw_vtok")
    zbf = [av([128, 64], BF16, "zbf%d" % i) for i in range(2)]
    rw_gtok = av([128, NT, 256], BF16, "rw_gtok")
    o_acc = av([128, NT, 256], F32, "o_acc")
    a_ring = Ring([av([128, 256], BF16, "aring%d" % i) for i in range(8)])
    m_ring = Ring([av([128, 256], BF16, "mring%d" % i) for i in range(8)])
    tok_ring = Ring([av([128, 128], BF16, "tokr%d" % i) for i in range(4)])
    u_ring = Ring([av([128, 64], BF16, "ur%d" % i) for i in range(8)])
    g_ring = Ring([av([128, 64], F32, "gr%d" % i) for i in range(4)])
    TT_ring = Ring([av([128, 128], BF16, "TTr%d" % i) for i in range(8)])
    off[0] = 0
    mT = av([128, 22, TB], BF16, "mT")
    facc = av([128, NT, D], F32, "facc")
    yT = av([128, 8, TB], BF16, "yT")

    out_tt = TT(None, "out_dram")

    pieces = []
    pst = {"issued": 0, "taken": 0}
    free_slots = list(range(NSLOT))
    slot_of = {}

    class Piece:
        __slots__ = ("v", "slot")

    def plan_piece(dap, nk, ncols):
        assert nk * ncols <= SLOTN
        pieces.append((dap, nk, ncols))

    def pump():
        while free_slots and pst["issued"] < len(pieces):
            i = pst["issued"]
            dap, nk, ncols = pieces[i]
            s = free_slots.pop(0)
            dst = wslots[s][:, 0:nk * ncols].re("p (k c) -> p k c", k=nk)
            c.dma("pool", dst, dap)
            p_ = Piece()
            p_.v = dst
            p_.slot = s
            slot_of[i] = p_
            pst["issued"] += 1

    def take_piece():
        pump()
        i = pst["taken"]
        assert i in slot_of, ("weight ring deadlock", i)
        pst["taken"] += 1
        return slot_of.pop(i)

    def release(p_):
        free_slots.append(p_.slot)
        pump()

    def rows_piece(w2d, r0, nk, c0, ncols):
        return w2d[r0 * 128:(r0 + nk) * 128, c0:c0 + ncols].rearrange("(k p) c -> p k c", p=128)

    WIN_PIECES = [(1808, 512), (2320, 512), (2832, 32), (0, 512), (512, 512), (1024, 512), (1536, 272),
                  (2864, 512), (3376, 512)]
    GU_COLS_D = [(0, 512), (512, 512), (1024, 512), (1536, 512), (2048, 512), (2560, 256)]
    GU_COLS_E = [(0, 512), (512, 512), (1024, 384)]

    def plan_layer(l):
        for (c0, n) in WIN_PIECES:
            plan_piece(rows_piece(w_in_d[l], 0, 8, c0, n), 8, n)
        for hf in range(2):
            plan_piece(rows_piece(w_out_d[l], 0, 8, hf * 512, 512), 8, 512)
        if not ffn:
            return
        i = l // 2
        if l % 2 == 0:
            for (c0, n) in GU_COLS_D:
                plan_piece(rows_piece(fg_d[i], 0, 8, c0, n), 8, n)
                plan_piece(rows_piece(fu_d[i], 0, 8, c0, n), 8, n)
            for hf in range(2):
                for (r0, nk) in ((0, 8), (8, 8), (16, 6)):
                    plan_piece(rows_piece(fd_d[i], r0, nk, hf * 512, 512), nk, 512)
        else:
            for e in range(NE):
                for (c0, n) in GU_COLS_E:
                    plan_piece(rows_piece(mg_d[i, e], 0, 8, c0, n), 8, n)
                    plan_piece(rows_piece(mu2_d[i, e], 0, 8, c0, n), 8, n)
                for hf in range(2):
                    for (r0, nk) in ((0, 8), (8, 3)):
                        plan_piece(rows_piece(md_d[i, e], r0, nk, hf * 512, 512), nk, 512)

    for b in range(NBLK):
        for l in range(L):
            plan_layer(l)

    def proj_F(wp, cols, ncols, rhs=None):
        bank = trb.next()
        out = bank[0:ncols, 0:TB]
        for k in range(8):
            c.mm(out, wp.v[:, k, cols:cols + ncols], hT[:, k, :], start=(k == 0), stop=(k == 7))
        return out

    def proj_T(wp, cols, ncols, tile):
        bank = trb.next()
        out = bank[:, 0:ncols]
        for k in range(8):
            c.mm(out, hT[:, k, tile * 128:(tile + 1) * 128], wp.v[:, k, cols:cols + ncols],
                 start=(k == 0), stop=(k == 7))
        return out

    def cumdecay(g, sgn_scale, cum, eq, ek):
        c.scan(cum, K("reset")[:, 0:TB], g, 0.0, ALU.mult, ALU.add)
        c.act(eq, cum, AF.Exp, scale=sgn_scale)
        c.act(ek, cum, AF.Exp, scale=-sgn_scale)

    def chunk_end(v):
        return v.re("p (n c) -> p n c", c=C)[:, :, C - 1]

    def rstd_of(dst, src, eps_col):
        c.act(dst, src, AF.Sqrt, bias=epsc[:, eps_col:eps_col + 1])
        c.recip(dst, dst)

    sc_ring = Ring([c.sb([128, 128], BF16, "sc%d" % i) for i in range(4)])
    kt_ring = Ring([c.sb([128, 128], BF16, "kt%d" % i) for i in range(2)])
    md_ring = Ring([c.sb([128, 128], F32, "md%d" % i) for i in range(2)])

    def gla_tile(l, t, qT, kT, dcs, states, KD, nh_tile):
        tok = slice(t * 128, (t + 1) * 128)
        o_bank = accb.next()
        o_ps = o_bank[:, 0:256]
        first = [True]
        W = nh_tile * 64
        NP = nh_tile * KD
        for pt in range(len(qT)):
            S, Sb = states[pt][0][l][0:NP, :], states[pt][1][0:NP, :]
            if t == 0:
                c.copy(Sb, S, eng="act")
            ktp = trb.next()
            ktv = V(ktp, ktp.t[:, 0:NP // 2].bitcast(BF16))
            c.tr(ktv, kT[pt][:, tok], ident_bf[0:NP, 0:NP])
            ktok = kt_ring.next()[:, 0:NP]
            c.copy(ktok, ktv, eng="act")
            scs = []
            for hh in range(nh_tile):
                pr = slice(hh * KD, (hh + 1) * KD)
                sp_ = trb.next()
                c.mm(sp_[:, 0:128], kT[pt][pr, tok], qT[pt][pr, tok])
                sc = sc_ring.next()[:]
                c.tt(sc, sp_[:, 0:128], mask_bf[:], ALU.mult)
                scs.append(sc)
            yield
            for hh in range(nh_tile):
                hg = pt * nh_tile + hh
                c.mm(o_ps[:, hg * 64:(hg + 1) * 64], scs[hh], vt[:, t, hg * 64:(hg + 1) * 64],
                     start=first[0], stop=False, sg=True)
                first[0] = False
            for ch in range(2):
                cg = t * 2 + ch
                rows = slice(ch * 64, (ch + 1) * 64)
                ctok = slice(t * 128 + ch * 64, t * 128 + (ch + 1) * 64)
                for hh in range(nh_tile):
                    hg = pt * nh_tile + hh
                    pr = slice(hh * KD, (hh + 1) * KD)
                    c.mm(o_ps[rows, hg * 64:(hg + 1) * 64], qT[pt][pr, ctok], Sb[pr, hh * 64:(hh + 1) * 64],
                         start=False, stop=True, sg=True)
                mp = trb.next()
                c.mm(mp[0:NP, 0:W], ktok[rows, :], vt[rows, t, pt * W:(pt + 1) * W])
                md = md_ring.next()[0:NP, 0:W]
                c.act(md, mp[0:NP, 0:W], AF.Copy, scale=dcs[pt][:, cg:cg + 1])
                c.stt(S, S, dcs[pt][:, cg:cg + 1], md, ALU.mult, ALU.add)
                c.copy(Sb, S, eng="act")
                yield
        return o_ps

    def finish_simple(l, t, o_ps, mix_idx, kind, gam_off):
        o = o_sb[:]
        c.copy(o, o_ps, eng="act")
        o3 = o.re("p (h v) -> p h v", h=4)
        sq = sq_sb[:]
        msum = stat_mx[:, 0:4]
        ssum = stat_mx[:, 4:8]
        rstd = stat_mx[:, 8:12]
        if kind == "gn":
            c.reduce(msum, o3, ALU.add)
            c.ts(msum, msum, 1.0 / 64.0, ALU.mult)
            c.tt(o3, o3, msum.re("p (h o) -> p h o", o=1).bc([128, 4, 64]), ALU.subtract)
        c.act(sq, o, AF.Square)
        c.reduce(ssum, sq.re("p (h v) -> p h v", h=4), ALU.add)
        c.ts(ssum, ssum, 1.0 / 64.0, ALU.mult)
        rstd_of(rstd, ssum, 1 if kind == "rms" else 0)
        c.tt(o3, o3, rstd.re("p (h o) -> p h o", o=1).bc([128, 4, 64]), ALU.mult)
        if gam_off is not None:
            gm = rb_small[:, gam_off:gam_off + 64]
            c.tt(o3, o3, gm.re("p (o v) -> p o v", o=1).bc([128, 4, 64]), ALU.mult)
        c.tt(ytok[:, t, mix_idx * 256:(mix_idx + 1) * 256], o, gt[:, t, :], ALU.mult)

    def hgrn(l, wp0, wp1):
        for pt in range(2):
            qps = proj_F(wp0, pt * 128, 128)
            qs = P[0][:]
            c.act(qs, qps, AF.Silu)
            fps = proj_F(wp0, 256 + pt * 128, 128)
            s = P[1][:]
            c.act(s, fps, AF.Sigmoid)
            yield
            f = P[2][:]
            c.ts(f, s, olb[:, l, pt:pt + 1], ALU.mult, lb[:, l, pt:pt + 1], ALU.add)
            k = P[3][:]
            c.ts(k, s, nolb[:, l, pt:pt + 1], ALU.mult, olb[:, l, pt:pt + 1], ALU.add)
            g = P[4][:]
            c.act(g, f, AF.Ln)
            cum, eq, ek = P[5][:], P[6][:], P[7][:]
            cumdecay(g, 1.0, cum, eq, ek)
            c.tt(qk[0 + pt][:], qs, eq, ALU.mult)
            c.tt(qk[2 + pt][:], k, ek, ALU.mult)
            c.copy(decs[pt][:], chunk_end(eq))
            yield
        for t in range(NT):
            ps_ = proj_T(wp1, 0, 512, t)
            c.copy(vt[:, t, :], ps_[:, 0:256], eng="act")
            c.act(gt[:, t, :], ps_[:, 256:512], AF.Silu)
            yield
        release(wp0)
        release(wp1)
        for t in range(NT):
            o_ps = yield from gla_tile(l, t, [qk[0][:], qk[1][:]], [qk[2][:], qk[3][:]], [decs[0][:], decs[1][:]],
                                       st_hg, 64, 2)
            finish_simple(l, t, o_ps, 0, "rms", 0)
            yield

    def gla(l, wp2, wp3):
        gps = proj_F(wp3, 0, 16)
        glr = P[2]
        c.copy(glr[0:16, :], gps, eng="act")
        for pt in range(2):
            qps = proj_F(wp2, pt * 64, 64)
            qs = P[0][0:64, :]
            c.act(qs, qps, AF.Copy, scale=32.0 ** -0.5)
            kps = proj_F(wp2, 128 + pt * 64, 64)
            ks = P[1][0:64, :]
            c.copy(ks, kps, eng="act")
            yield
            bank = trb.next()
            zps = bank[0:64, 0:TB]
            c.mm(zps, gw2[:, pt * 64:(pt + 1) * 64], glr[0:16, :])
            e = P[3][0:64, :]
            c.act(e, zps, AF.Exp, bias=ngb2[:, l, pt:pt + 1], scale=-1.0)
            sp_ = P[4][0:64, :]
            c.act(sp_, e, AF.Ln, bias=epsc[0:64, 3:4])
            cum, eq, ek = P[5][0:64, :], P[6][0:64, :], P[7][0:64, :]
            c.scan(cum, K("reset")[0:64, 0:TB], sp_, 0.0, ALU.mult, ALU.add)
            c.act(eq, cum, AF.Exp, scale=-1.0 / 16.0)
            c.act(ek, cum, AF.Exp, scale=1.0 / 16.0)
            c.tt(qk[0 + pt][0:64, :], qs, eq, ALU.mult)
            c.tt(qk[2 + pt][0:64, :], ks, ek, ALU.mult)
            c.copy(decs[pt][0:64, :], chunk_end(eq))
            yield
        for t in range(NT):
            ps_ = proj_T(wp2, 256, 256, t)
            c.copy(vt[:, t, :], ps_, eng="act")
            ps2 = proj_T(wp3, 16, 256, t)
            c.act(gt[:, t, :], ps2, AF.Silu)
            yield
        release(wp2)
        release(wp3)
        for t in range(NT):
            o_ps = yield from gla_tile(l, t, [qk[0][0:64, :], qk[1][0:64, :]], [qk[2][0:64, :], qk[3][0:64, :]],
                                       [decs[0][0:64, :], decs[1][0:64, :]], st_gl, 32, 2)
            finish_simple(l, t, o_ps, 1, "rms", 64)
            yield

    def rope_tables(b):
        posi = V(P[0], P[0].t[:].bitcast(I32))
        c.dma("sp", posi, pos_d[0:1, b * TB:(b + 1) * TB].partition_broadcast(128))
        posf = P[5]
        c.copy(posf[:], posi)
        ang = P[1][:]
        c.ts(ang, posf[:], K("invfreq"), ALU.mult)
        kq = P[2][:]
        c.ts(kq, ang, float(1.0 / (2.0 * np.pi)), ALU.mult, 12582912.0, ALU.add)
        c.ts(kq, kq, -12582912.0, ALU.add)
        r = P[3][:]
        C1 = 6.28125
        C2 = float(2.0 * np.pi - 6.28125)
        c.stt(r, kq, -C1, ang, ALU.mult, ALU.add)
        c.stt(r, kq, -C2, r, ALU.mult, ALU.add)
        c.ts(r, r, 3.14159, ALU.min, -3.14159, ALU.max)
        c.act(sinT[:], r, AF.Sin)
        ab = P[4][:]
        c.ts(ab, r, -1.0, ALU.mult)
        c.tt(ab, ab, r, ALU.max)
        c.ts(ab, ab, -1.0, ALU.mult, float(np.pi / 2.0), ALU.add)
        c.act(cosT[:], ab, AF.Sin)
        c.ts(sinT[:], sinT[:], K("sinsign"), ALU.mult)

    def ret(l, wp6, wp7):
        for which in range(2):
            for pt in range(2):
                ps_ = proj_F(wp6, which * 256 + pt * 128, 128)
                xs = P[0][:]
                c.copy(xs, ps_, eng="act")
                a = P[1][:]
                c.tt(a, xs, cosT[:], ALU.mult)
                bsw = P[2][:]
                for hh in range(2):
                    lo = slice(hh * 64, hh * 64 + 32)
                    hi = slice(hh * 64 + 32, hh * 64 + 64)
                    c.tt(bsw[lo, :], xs[hi, :], sinT[hi, :], ALU.mult)
                    c.tt(bsw[hi, :], xs[lo, :], sinT[lo, :], ALU.mult)
                c.tt(a, a, bsw, ALU.add)
                tab = K("ret_eq" if which == 0 else "ret_ek")[:, pt * C:(pt + 1) * C]
                dst = qk[which * 2 + pt]
                c.tt(dst[:].re("p (n c) -> p n c", c=C), a.re("p (n c) -> p n c", c=C),
                     tab.re("p (o c) -> p o c", o=1).bc([128, NCH, C]), ALU.mult)
                yield
        for t in range(NT):
            ps_ = proj_T(wp7, 0, 512, t)
            c.copy(vt[:, t, :], ps_[:, 0:256], eng="act")
            c.act(gt[:, t, :], ps_[:, 256:512], AF.Silu)
            yield
        release(wp6)
        release(wp7)
        for t in range(NT):
            o_ps = yield from gla_tile(l, t, [qk[0][:], qk[1][:]], [qk[2][:], qk[3][:]],
                                       [dec_rt[0][:], dec_rt[1][:]], st_rt, 64, 2)
            finish_simple(l, t, o_ps, 3, "gn", None)
            yield

    def rw_shift(l, b, i, wp, c0, n, dst, tm):
        ps_ = proj_F(wp, c0, n)
        raw = rw_raw.next()
        if b == 0:
            c.memset(raw[0:n, 0:1], 0.0)
        else:
            c.copy(raw[0:n, 0:1], rw_carry[0:n, l, i:i + 1])
        c.copy(raw[0:n, 1:TB + 1], ps_, eng="act")
        c.copy(rw_carry[0:n, l, i:i + 1], raw[0:n, TB:TB + 1])
        c.act(tm[0:n, :], raw[0:n, 1:TB + 1], AF.Copy, scale=omu[0:n, l, i:i + 1])
        c.stt(dst[0:n, :], raw[0:n, 0:TB], mup[0:n, l, i:i + 1], tm[0:n, :], ALU.mult, ALU.add)

    def rwkv(l, b, wp4, wp5, wp5b, rel):
        vS = [rw_sh[0], rw_sh[1]]
        waS, rS, kS = rw_sh[2], rw_sh[3], rw_sh[4]
        rw_shift(l, b, 6, wp5, 256, 128, waS, R[2])
        g0S, g1S = R[0], R[1]
        rw_shift(l, b, 7, wp5, 384, 128, g0S, R[2])
        rw_shift(l, b, 8, wp5b, 0, 32, g1S, R[2])
        yield
        rw_shift(l, b, 4, wp5, 0, 128, vS[0], R[2])
        rw_shift(l, b, 5, wp5, 128, 128, vS[1], R[2])
        release(wp5)
        release(wp5b)
        rel["done"] = True
        yield
        c.act(th_t[0:64, :], waS[0:64, :], AF.Tanh)
        c.act(g0S[:], g0S[:], AF.Sigmoid)
        c.act(g1S[0:32, :], g1S[0:32, :], AF.Sigmoid)
        for t in range(NT):
            bank = trb.next()
            c.mm(bank[:, 0:256], g0S[:, t * 128:(t + 1) * 128], g2a[:], start=True, stop=False)
            c.mm(bank[:, 0:256], g1S[0:32, t * 128:(t + 1) * 128], g2b[:], start=False, stop=True)
            c.copy(rw_gtok[:, t, :], bank[:, 0:256], eng="act")
            yield
        if l > 0:
            bank = trb.next()
            lv = bank[0:32, 0:TB]
            for k in range(2):
                c.mm(lv, v1s[:, k, :], vS[k][:], start=(k == 0), stop=(k == 1))
            c.copy(lvs_t[0:32, :], lv, eng="act")
        for pt in range(2):
            if l == 0:
                c.copy(vfirst[:, pt, :], vS[pt][:])
            else:
                bank = trb.next()
                c.mm(bank[:, 0:TB], v2s[:, pt * 128:(pt + 1) * 128], lvs_t[0:32, :])
                sgm = R[2][:]
                c.act(sgm, bank[:, 0:TB], AF.Sigmoid, bias=v0p[:, l - 1, pt:pt + 1])
                dv = R[3][:]
                c.tt(dv, vfirst[:, pt, :], vS[pt][:], ALU.subtract)
                c.tt(dv, dv, sgm, ALU.mult)
                c.tt(vS[pt][:], vS[pt][:], dv, ALU.add)
                yield
            for t in range(NT):
                bank = trb.next()
                c.tr(bank[:, 0:128], vS[pt][:, t * 128:(t + 1) * 128], K("ident"))
                c.copy(rw_vtok[:, t, pt * 128:(pt + 1) * 128], bank[:, 0:128], eng="act")
            yield
        for pt in range(2):
            rw_shift(l, b, 0 + pt, wp4, pt * 128, 128, rS, R[0])
            rw_shift(l, b, 2 + pt, wp4, 256 + pt * 128, 128, kS, R[0])
            if pt == 1:
                release(wp4)
            bank = trb.next()
            c.mm(bank[:, 0:TB], w2s[:, pt * 128:(pt + 1) * 128], th_t[0:64, :])
            t1 = R[2][:]
            c.act(t1, bank[:, 0:TB], AF.Exp, bias=nw0[:, l, pt:pt + 1], scale=-1.0)
            c.act(t1, t1, AF.Ln, bias=epsc[:, 3:4])
            c.ts(t1, t1, -1.0, ALU.mult, -0.5, ALU.add)
            c.act(t1, t1, AF.Exp)
            g = R[3][:]
            c.ts(g, t1, -1.0, ALU.mult)
            cum, eq, ek = R[4][:], R[5][:], R[6][:]
            cumdecay(g, 1.0, cum, eq, ek)
            c.copy(rw_dec[pt][:], chunk_end(eq))
            yield
            bank = trb.next()
            c.mm(bank[:, 0:TB], a2s[64:128, pt * 128:(pt + 1) * 128], waS[64:128, :])
            ag = R[7][:]
            c.act(ag, bank[:, 0:TB], AF.Sigmoid, bias=a0[:, l, pt:pt + 1])
            kk = R[8][:]
            c.ts(kk, kS[:], kkp[:, l, pt:pt + 1], ALU.mult)
            nrm = R[9][:]
            c.act(nrm, kk, AF.Square)
            bank = trb.next()
            c.mm(bank[:, 0:TB], K("blockones"), nrm)
            c.act(nrm, bank[:, 0:TB], AF.Sqrt)
            c.ts(nrm, nrm, 1e-12, ALU.max)
            c.act(nrm, nrm, AF.Ln)
            c.act(nrm, nrm, AF.Exp, scale=-1.0)
            c.tt(kk, kk, nrm, ALU.mult)
            yield
            fk = R[9][:]
            c.ts(fk, ag, -1.0, ALU.add, kap[:, l, pt:pt + 1], ALU.mult)
            c.ts(fk, fk, 1.0, ALU.add)
            km = R[10][:]
            c.tt(km, kS[:], fk, ALU.mult)
            bo = R[2][:]
            c.stt(bo, rS[:], rkp[:, l, pt:pt + 1], km, ALU.mult, ALU.mult)
            for t in range(NT):
                bank = trb.next()
                c.mm(bank[:, 0:2], bo[:, t * 128:(t + 1) * 128], K("headsel"))
                c.copy(rw_bonus[:, t, pt * 2:pt * 2 + 2], bank[:, 0:2], eng="act")
            yield
            c.tt(rw_ar[pt][:, 1, :], rS[:], eq, ALU.mult)
            c.tt(rw_kt[pt][:], km, ek, ALU.mult)
            bb = R[9][:]
            c.tt(bb, kk, ag, ALU.mult)
            c.tt(rw_bt[pt][:], bb, ek, ALU.mult)
            yield
            ex = R[9][:]
            c.tt(ex, cum, g, ALU.subtract)
            c.act(ex, ex, AF.Exp)
            c.stt(rw_ar[pt][:, 0, :], kk, -1.0, ex, ALU.mult, ALU.mult)


    F32R = mybir.dt.float32r

    def R32(v):
        return V(v.tt, v.ap.bitcast(F32R))

    def rwkv_core(l, t, pt, hh, sidx):
        h = pt * 2 + hh
        ev = ("act", "dve") if sidx % 2 == 0 else ("dve", "act")
        tok = slice(t * 128, (t + 1) * 128)
        pr = slice(hh * 64, hh * 64 + 64)
        ar = rw_ar[pt][pr, :, tok]
        kt = rw_kt[pt][pr, tok]
        bt = rw_bt[pt][pr, tok]
        at = rw_ar[pt][pr, 0, tok]
        rt = rw_ar[pt][pr, 1, tok]
        Z = zst[l][pt]
        Zb = zbf[pt]
        if t == 0:
            c.copy(Zb[pr, :], Z[pr, :], eng=ev[1])
        vtk = rw_vtok[:, t, h * 64:(h + 1) * 64]
        ocol = slice(h * 64, (h + 1) * 64)
        b1 = trb.next()
        c.mm(b1[:, 0:256].re("p (a n) -> p a n", a=2), kt, ar)
        Ak = a_ring.next()[:]
        c.tt(Ak[:, 0:128], b1[:, 0:128], K("mask_strict"), ALU.mult)
        c.tt(Ak[:, 128:256], b1[:, 128:256], K("mask_incl"), ALU.mult)
        b2 = trb.next()
        c.mm(b2[:, 0:256].re("p (a n) -> p a n", a=2), bt, ar)
        Ab = a_ring.next()[:]
        c.tt(Ab[:, 0:128], b2[:, 0:128], K("mask_strict"), ALU.mult)
        c.tt(Ab[:, 128:256], b2[:, 128:256], K("mask_incl"), ALU.mult)
        b3 = trb.next()
        c.mm(b3[:, 0:128], at, bt)
        pq = m_ring.next()[:]
        Pm = pq[:, 0:128]
        c.tt(Pm, b3[:, 0:128], K("mask_strictT"), ALU.mult)
        Q = Ab[:, 0:128]
        Tc = TT_ring.next()[:]
        c.tt(Tc, Q, K("ident"), ALU.add)
        yield
        for i in range(1, 6):
            bpq = trb.next()
            c.mm(bpq[:, 0:128], Q, Pm)
            if i < 5:
                c.mm(bpq[:, 128:256], Pm, Q)
            pq = m_ring.next()[:]
            if i < 5:
                c.copy(pq, bpq[:, 0:256], eng=ev[0])
            else:
                c.copy(pq[:, 0:128], bpq[:, 0:128], eng=ev[0])
            Pm = pq[:, 0:128]
            Q = pq[:, 128:256]
            yield
            bt_ = trb.next()
            c.mm(bt_[:, 0:128], Pm, Tc)
            Tn = TT_ring.next()[:]
            c.tt(Tn, Tc, bt_[:, 0:128], ALU.add)
            Tc = Tn
            yield
        bk = trb.next()
        bkv = V(bk, bk.t[:, 0:64].bitcast(BF16))
        c.tr(bkv[:, 0:64], kt, ident_bf[pr, pr])
        c.tr(bkv[:, 64:128], bt, ident_bf[pr, pr])
        kb_tok = tok_ring.next()[:]
        c.copy(kb_tok, bkv, eng=ev[0])
        gb_ = trb.next()
        c.mm(gb_[:, 0:64], Ak[:, 0:128], vtk)
        utok = u_ring.next()[:]
        rhs_u = u_ring.next()[:]
        G_sb = g_ring.next()[:]
        c.copy(G_sb, gb_[:, 0:64], eng=ev[1])
        yield
        for ch in range(2):
            rows = slice(ch * 64, (ch + 1) * 64)
            cg = t * 2 + ch
            gz = trb.next()
            c.mm(gz[rows, 0:64], at[:, rows], Zb[pr, :])
            c.tt(rhs_u[rows, :], gz[rows, 0:64], G_sb[rows, :], ALU.add)
            ob = trb.next()
            c.mm(ob[rows, 0:64], rt[:, rows], Zb[pr, :])
            c.copy(o_acc[rows, t, ocol], ob[rows, 0:64], eng=ev[0])
            yield
            ub = trb.next()
            c.mm(ub[rows, 0:64], Tc[rows, rows], rhs_u[rows, :])
            c.copy(utok[rows, :], ub[rows, 0:64], eng=ev[0])
            yield
            zb = trb.next()
            c.mm(zb[pr, 0:64], K("identZ")[pr, :], Z[pr, :], start=True, stop=False, sg=True)
            c.mm(zb[pr, 0:64], kb_tok[rows, 0:64], vtk[rows, :], start=False, stop=False, sg=True)
            c.mm(zb[pr, 0:64], kb_tok[rows, 64:128], utok[rows, :], start=False, stop=True, sg=True)
            c.act(Z[pr, :], zb[pr, 0:64], AF.Copy, scale=rw_dec[pt][pr, cg:cg + 1])
            c.copy(Zb[pr, :], Z[pr, :], eng=ev[1])
            yield
        ob2 = trb.next()
        c.mm(ob2[:, 0:64], Ak[:, 128:256], vtk, start=True, stop=False)
        c.mm(ob2[:, 0:64], Ab[:, 128:256], utok, start=False, stop=True)
        c.tt(o_acc[:, t, ocol], o_acc[:, t, ocol], ob2[:, 0:64], ALU.add)

    def rwkv_finish(l, t):
        o = o_sb[:]
        c.copy(o, o_acc[:, t, :])
        o3 = o.re("p (h v) -> p h v", h=4)
        msum = stat_mx[:, 0:4]
        ssum = stat_mx[:, 4:8]
        rstd = stat_mx[:, 8:12]
        c.reduce(msum, o3, ALU.add)
        c.ts(msum, msum, 1.0 / 64.0, ALU.mult)
        c.tt(o3, o3, msum.re("p (h o) -> p h o", o=1).bc([128, 4, 64]), ALU.subtract)
        sq = sq_sb[:]
        c.act(sq, o, AF.Square)
        c.reduce(ssum, sq.re("p (h v) -> p h v", h=4), ALU.add)
        c.ts(ssum, ssum, 1.0 / 64.0, ALU.mult)
        rstd_of(rstd, ssum, 2)
        c.tt(o3, o3, rstd.re("p (h o) -> p h o", o=1).bc([128, 4, 64]), ALU.mult)
        c.tt(o, o, rb_small[:, 128:384], ALU.mult)
        c.tt(o, o, rb_small[:, 384:640], ALU.add)
        c.tt(sq.re("p (h v) -> p h v", h=4), rw_vtok[:, t, :].re("p (h v) -> p h v", h=4),
             rw_bonus[:, t, :].re("p (h o) -> p h o", o=1).bc([128, 4, 64]), ALU.mult)
        c.tt(o, o, sq, ALU.add)
        c.tt(ytok[:, t, 512:768], o, rw_gtok[:, t, :], ALU.mult)

    def others(l):
        wp0, wp1 = take_piece(), take_piece()
        if mixers[0]:
            yield from hgrn(l, wp0, wp1)
        else:
            release(wp0)
            release(wp1)
        wp2, wp3 = take_piece(), take_piece()
        if mixers[1]:
            yield from gla(l, wp2, wp3)
        else:
            release(wp2)
            release(wp3)
        wp6, wp7 = take_piece(), take_piece()
        if mixers[3]:
            yield from ret(l, wp6, wp7)
        else:
            release(wp6)
            release(wp7)

    def to_hT(t):
        for hf in range(2):
            bank = trb.next()
            for k4 in range(4):
                k = hf * 4 + k4
                c.tr(bank[:, k4 * 128:(k4 + 1) * 128], htok[:, t, k * 128:(k + 1) * 128], K("ident"))
            c.copy(hT[:, hf * 4:(hf + 1) * 4, t * 128:(t + 1) * 128],
                   bank[:, 0:512].re("p (k n) -> p k n", k=4), eng="act")

    def layer_norm(l, which):
        for hf in range(2):
            c.dma("sp", P[4 + hf][:], ln_d[which + "_g"][l:l + 1, hf * 512:(hf + 1) * 512].partition_broadcast(128))
            c.dma("sp", P[6 + hf][:], ln_d[which + "_b"][l:l + 1, hf * 512:(hf + 1) * 512].partition_broadcast(128))
        for t in range(NT):
            z = htok[:, t, :]
            st6 = stat_ln[:, 0:12]
            for hf in range(2):
                zz = z[:, hf * 512:(hf + 1) * 512]
                dst = stat_ln[:, hf * 6:(hf + 1) * 6]
                c.op("dve", lambda zz=zz, dst=dst: nc.vector.bn_stats(dst.ap, zz.ap), reads=[zz], writes=[dst])
            mv = stat_ln[:, 12:14]
            c.op("dve", lambda: nc.vector.bn_aggr(mv.ap, st6.ap), reads=[st6], writes=[mv])
            rs = stat_ln[:, 14:15]
            rstd_of(rs, stat_ln[:, 13:14], 0)
            c.ts(z, z, stat_ln[:, 12:13], ALU.subtract, rs, ALU.mult)
            for hf in range(2):
                zz = z[:, hf * 512:(hf + 1) * 512]
                c.tt(zz, zz, P[4 + hf][:], ALU.mult)
                c.tt(zz, zz, P[6 + hf][:], ALU.add)
            to_hT(t)

    def residual_from_bank(t, hf, bank_v):
        z = htok[:, t, hf * 512:(hf + 1) * 512]
        c.stt(z, z, ALPHA, bank_v, ALU.mult, ALU.add)

    def gate_up(cols_list):
        ft = 0
        for (c0, n) in cols_list:
            wg = take_piece()
            wu = take_piece()
            for j in range(n // 128):
                gps = proj_F(wg, j * 128, 128)
                ups = proj_F(wu, j * 128, 128)
                sl = P[8 + ft % 3][:]
                c.act(sl, gps, AF.Silu)
                c.tt(mT[:, ft, :], sl, ups, ALU.mult)
                ft += 1
            release(wg)
            release(wu)
        return ft

    def down_proj(nft, row_groups, consume):
        for hf in range(2):
            accs = [accb.next() for _ in range(NT)]
            for (r0, nk) in row_groups:
                wd = take_piece()
                for kk_ in range(nk):
                    ft = r0 + kk_
                    for t in range(NT):
                        c.mm(accs[t][:, 0:512], mT[:, ft, t * 128:(t + 1) * 128], wd.v[:, kk_, :],
                             start=(ft == 0), stop=(ft == nft - 1))
                release(wd)
            for t in range(NT):
                consume(t, hf, accs[t][:, 0:512])

    def ffn_dense(l):
        gate_up(GU_COLS_D)
        down_proj(22, ((0, 8), (8, 8), (16, 6)), residual_from_bank)

    def moe(l):
        i = l // 2
        for t in range(NT):
            bank = trb.next()
            lg = bank[:, 0:NE]
            for k in range(8):
                c.mm(lg, hT[:, k, t * 128:(t + 1) * 128], mrs_bf[:, i, k, :], start=(k == 0), stop=(k == 7))
            lgs = stat_moe[:, 0:8]
            c.copy(lgs, lg)
            m8 = stat_moe[:, 8:16]
            c.op("dve", lambda: nc.vector.max(m8.ap, lgs.ap), reads=[lgs], writes=[m8])
            nm1 = stat_moe[:, 32:33]
            c.ts(nm1, m8[:, 0:1], -1.0, ALU.mult)
            ex = stat_moe[:, 16:24]
            c.act(ex, lgs, AF.Exp, bias=nm1)
            sel = stat_moe[:, 24:32]
            c.ts(sel, lgs, m8[:, 1:2], ALU.is_ge)
            c.tt(ex, ex, sel, ALU.mult)
            ssum = stat_moe[:, 33:34]
            c.reduce(ssum, ex, ALU.add)
            c.recip(ssum, ssum)
            c.ts(gates[:, t, :], ex, ssum, ALU.mult)
        c.memset(facc[:], 0.0)
        for e in range(NE):
            gate_up(GU_COLS_E)

            def cons(t, hf, bank_v, e=e):
                fa = facc[:, t, hf * 512:(hf + 1) * 512]
                c.stt(fa, bank_v, gates[:, t, e:e + 1], fa, ALU.mult, ALU.add)
            down_proj(11, ((0, 8), (8, 3)), cons)
        for t in range(NT):
            z = htok[:, t, :]
            c.stt(z, z, ALPHA, facc[:, t, :], ALU.mult, ALU.add)

    for l in range(L):
        for pt in range(2):
            for st in (st_hg, st_rt, st_gl):
                c.memset(st[pt][0][l][:], 0.0)
            c.memset(zst[l][pt][:], 0.0)
    c.memset(ytok[:], 0.0)

    for b in range(NBLK):
        for t in range(NT):
            r0 = b * TB + t * 128
            c.dma("sp", htok[:, t, :], x_d[r0:r0 + 128, :])
            to_hT(t)
        if mixers[3]:
            rope_tables(b)
        for l in range(L):
            load_layer_params(l)
            wp4, wp5, wp5b = take_piece(), take_piece(), take_piece()
            rel = {"done": False}
            rp = None
            if mixers[2]:
                rp = rwkv(l, b, wp4, wp5, wp5b, rel)
                while not rel["done"]:
                    next(rp)
            else:
                release(wp4)
                release(wp5)
                release(wp5b)
            og = others(l)
            og_live = [True]

            def og_step():
                if og_live[0]:
                    try:
                        next(og)
                    except StopIteration:
                        og_live[0] = False
            if mixers[2]:
                rp_live = True
                while rp_live:
                    try:
                        next(rp)
                    except StopIteration:
                        rp_live = False
                    og_step()
                for t in range(NT):
                    live = [rwkv_core(l, t, h // 2, h % 2, h) for h in range(4)]
                    while live:
                        nxt = []
                        for g_ in live:
                            try:
                                next(g_)
                                nxt.append(g_)
                            except StopIteration:
                                pass
                        live = nxt
                        og_step()
                    rwkv_finish(l, t)
            while og_live[0]:
                og_step()
            for t in range(NT):
                for hf in range(2):
                    bank = trb.next()
                    bv = V(bank, bank.t[:, 0:256].bitcast(BF16))
                    for k4 in range(4):
                        k = hf * 4 + k4
                        c.tr(bv[:, k4 * 128:(k4 + 1) * 128], ytok[:, t, k * 128:(k + 1) * 128], ident_bf[:])
                    c.copy(yT[:, hf * 4:(hf + 1) * 4, t * 128:(t + 1) * 128],
                           bv.re("p (k n) -> p k n", k=4), eng="act")
            wo = [take_piece(), take_piece()]
            for t in range(NT):
                for hf in range(2):
                    bank = trb.next()
                    for k in range(8):
                        c.mm(bank[:, 0:512], yT[:, k, t * 128:(t + 1) * 128], wo[hf].v[:, k, :],
                             start=(k == 0), stop=(k == 7))
                    residual_from_bank(t, hf, bank[:, 0:512])
            release(wo[0])
            release(wo[1])
            layer_norm(l, "ln1")
            if ffn:
                if l % 2 == 0:
                    ffn_dense(l)
                else:
                    moe(l)
                layer_norm(l, "ln2")
        for t in range(NT):
            r0 = b * TB + t * 128
            c.dma("sp", V(out_tt, out_d[r0:r0 + 128, :]), htok[:, t, :])
    c.wait_all("sp", [out_tt])
    assert pst["taken"] == len(pieces), (pst, len(pieces))
    return nc, carr, c


_CACHE = {}

NAMES = ["w_in", "w_out", "ln1_g", "ln1_b", "ln2_g", "ln2_b", "hgrn_lb_logits", "hgrn_norm_g", "gla_gate_w2",
         "gla_gate_b", "gla_norm_g", "rwkv_mu", "rwkv_w0", "rwkv_w2", "rwkv_a0", "rwkv_a2", "rwkv_g2", "rwkv_k_k",
         "rwkv_k_a", "rwkv_r_k", "rwkv_lnx_g", "rwkv_lnx_b", "rwkv_v0", "rwkv_v1", "rwkv_v2", "ffn_w_gate",
         "ffn_w_up", "ffn_w_down", "moe_router", "moe_w_gate", "moe_w_up", "moe_w_down"]


def run(inputs, T, L=NL, TB=512, **kw):
    x = np.asarray(inputs["x"], dtype=np.float32)
    B = x.shape[0]
    key = (T, L, TB, tuple(sorted(kw.items())))
    if key not in _CACHE:
        _CACHE[key] = build(T, L, TB, **kw)
    nc, carr, c = _CACHE[key]
    shared = {}
    for n in NAMES:
        a = np.ascontiguousarray(np.asarray(inputs[n], dtype=np.float32))
        if n == "rwkv_r_k":
            a = a.reshape(NL, 256)
        shared[n] = a
    shared["consts"] = carr
    pos = np.asarray(inputs["positions"]).astype(np.int32)
    in_maps = []
    for b in range(B):
        m = dict(shared)
        m["x"] = np.ascontiguousarray(x[b, :T])
        m["positions"] = np.ascontiguousarray(pos[b:b + 1, :T])
        in_maps.append(m)
    res = run_bass_kernel_spmd(nc, in_maps, core_ids=list(range(B)))
    return np.stack([np.asarray(r["out"]) for r in res.results], axis=0)


def kernel(**inputs):
    x = np.asarray(inputs["x"])
    B, S, _ = x.shape
    out = run(inputs, S)
    return out.astype(x.dtype)
```

```python
import numpy as np
import ml_dtypes
import concourse.bass as bass
import concourse.mybir as mybir
from concourse.bass_utils import run_bass_kernel_spmd

F32 = mybir.dt.float32
BF16 = mybir.dt.bfloat16
I32 = mybir.dt.int32
AF = mybir.ActivationFunctionType
ALU = mybir.AluOpType
AX = mybir.AxisListType

D = 1024
NL = 4
INC = 3888
FFD = 2816
NE = 8
FFE = 1408
ALPHA = (2.0 * NL) ** 0.25
C = 64


class V:
    __slots__ = ("tt", "ap")

    def __init__(self, tt, ap):
        self.tt = tt
        self.ap = ap

    def __getitem__(self, k):
        return V(self.tt, self.ap[k])

    def re(self, s, **kw):
        return V(self.tt, self.ap.rearrange(s, **kw))

    def bc(self, shape):
        return V(self.tt, self.ap.to_broadcast(list(shape)))


class TT:
    __slots__ = ("t", "name", "w", "r", "al", "pe_row")

    def __init__(self, t, name):
        self.t = t
        self.name = name
        self.w = None
        self.r = []
        self.al = []
        self.pe_row = None

    def __getitem__(self, k):
        return V(self, self.t[k])


class Ctx:
    NDMA = 24

    def __init__(self, nc, same=True):
        self.nc = nc
        self.same = same
        self.engs = {"pe": nc.tensor, "dve": nc.vector, "act": nc.scalar, "pool": nc.gpsimd, "sp": nc.sync}
        self.sem = {}
        self.cnt = {}
        self.waited = {}
        self._cms = []
        for k in self.engs:
            cm = nc.semaphore("s_" + k)
            self.sem[k] = cm.__enter__()
            self._cms.append(cm)
            self.cnt[k] = 0
            self.waited[k] = {}
        self.dsem = []
        self.dcnt = []
        for i in range(self.NDMA):
            cm = nc.semaphore("d_%d" % i)
            self.dsem.append(cm.__enter__())
            self._cms.append(cm)
            self.dcnt.append(0)
        self.dnext = {"sp": 0, "pool": 0, "act": 0}
        self.drange = {"sp": (0, 14), "pool": (14, 22), "act": (22, 24)}
        self.ntile = 0
        self.ninst = 0

    def sb(self, shape, dt, name=None):
        self.ntile += 1
        name = name or "t%d" % self.ntile
        cm = self.nc.sbuf_tensor(name, list(shape), dt)
        t = cm.__enter__()
        self._cms.append(cm)
        return TT(t, name)

    def ps(self, shape, dt, name=None):
        self.ntile += 1
        name = name or "p%d" % self.ntile
        cm = self.nc.psum_tensor(name, list(shape), dt)
        t = cm.__enter__()
        self._cms.append(cm)
        return TT(t, name)

    def _semof(self, key):
        if key in self.sem:
            return self.sem[key]
        return self.dsem[int(key[1:])]

    def _deps(self, eng, reads, writes):
        need = {}

        def add(dep):
            if dep is None:
                return
            k, v = dep
            if need.get(k, 0) < v:
                need[k] = v
        rawonly = (self.same == "raw")
        need_raw = {}
        for t in reads:
            add(t.w)
            for a in t.al:
                add(a.w)
        if rawonly:
            need_raw = dict(need)
        for t in writes:
            add(t.w)
            for d in t.r:
                add(d)
            for a in t.al:
                add(a.w)
                for d in a.r:
                    add(d)
        if rawonly and eng in need:
            if eng in need_raw:
                need[eng] = need_raw[eng]
            else:
                del need[eng]
        h = self.engs[eng]
        for k, v in need.items():
            if k == eng and (not self.same or eng == "pe"):
                continue
            if self.waited[eng].get(k, 0) >= v:
                continue
            h.wait_ge(self._semof(k), v)
            self.waited[eng][k] = v
            self.ninst += 1

    def _mark(self, me, reads, writes):
        for t in reads:
            if len(t.r) > 64:
                mx = {}
                for (k, v) in t.r:
                    if mx.get(k, 0) < v:
                        mx[k] = v
                t.r = list(mx.items())
            t.r.append(me)
        for t in writes:
            t.w = me
            t.r = []

    def op(self, eng, fn, reads=(), writes=()):
        reads = [x.tt if isinstance(x, V) else x for x in reads if x is not None]
        writes = [x.tt if isinstance(x, V) else x for x in writes if x is not None]
        self._deps(eng, reads, writes)
        ins = fn()
        self.cnt[eng] += 1
        ins.then_inc(self.sem[eng], 1)
        self._mark((eng, self.cnt[eng]), reads, writes)
        self.ninst += 1
        return ins

    def dma(self, q, out, in_, **kw):
        reads = [in_.tt] if isinstance(in_, V) else []
        writes = [out.tt] if isinstance(out, V) else []
        lo, hi = self.drange[q]
        i = lo + self.dnext[q]
        self.dnext[q] = (self.dnext[q] + 1) % (hi - lo)
        key = "d%d" % i
        h = self.engs[q]
        if self.dcnt[i] > 0 and self.waited[q].get(key, 0) < self.dcnt[i]:
            h.wait_ge(self.dsem[i], self.dcnt[i])
            self.waited[q][key] = self.dcnt[i]
        self._deps(q, reads, writes)
        oa = out.ap if isinstance(out, V) else out
        ia = in_.ap if isinstance(in_, V) else in_
        ins = h.dma_start(out=oa, in_=ia, **kw)
        self.dcnt[i] += 16
        ins.then_inc(self.dsem[i], 16)
        self._mark((key, self.dcnt[i]), reads, writes)
        self.ninst += 1
        return ins

    def _pe_row_guard(self, out, lhsT):
        row = (int(lhsT.ap.start_partition()), int(lhsT.ap.partition_size()))
        t = out.tt
        if t.pe_row is not None and t.pe_row != row and t.w is not None and t.w[0] == "pe":
            v = t.w[1]
            if self.waited["pe"].get("pe", 0) < v:
                self.nc.tensor.wait_ge(self.sem["pe"], v)
                self.waited["pe"]["pe"] = v
                self.ninst += 1
        t.pe_row = row

    def mm(self, out, lhsT, rhs, start=True, stop=True, sg=False):
        nc = self.nc
        self._pe_row_guard(out, lhsT)
        return self.op("pe", lambda: nc.tensor.matmul(out.ap, lhsT=lhsT.ap, rhs=rhs.ap, start=start, stop=stop,
                                                      skip_group_check=sg),
                       reads=[lhsT, rhs], writes=[out])

    def tr(self, out, in_, ident):
        nc = self.nc
        self._pe_row_guard(out, in_)
        return self.op("pe", lambda: nc.tensor.transpose(out.ap, in_.ap, ident.ap), reads=[in_, ident], writes=[out])

    def act(self, out, in_, func, bias=None, scale=None, eng="act"):
        nc = self.nc
        kw = {}
        rd = [in_]
        if bias is not None:
            if isinstance(bias, V):
                kw["bias"] = bias.ap
                rd.append(bias)
            else:
                kw["bias"] = bias
        if scale is not None:
            if isinstance(scale, V):
                kw["scale"] = scale.ap
                rd.append(scale)
            else:
                kw["scale"] = scale
        if func == AF.Copy and isinstance(scale, V):
            func = AF.Identity
        return self.op("act", lambda: nc.scalar.activation(out.ap, in_.ap, func, **kw), reads=rd, writes=[out])

    def tt(self, out, in0, in1, op, eng="dve"):
        h = self.engs[eng]
        return self.op(eng, lambda: h.tensor_tensor(out.ap, in0.ap, in1.ap, op), reads=[in0, in1], writes=[out])

    def ts(self, out, in0, s1, op0, s2=None, op1=None, eng="dve"):
        h = self.engs[eng]
        rd = [in0]
        a1 = s1
        if isinstance(s1, V):
            rd.append(s1)
            a1 = s1.ap
        a2 = s2
        if isinstance(s2, V):
            rd.append(s2)
            a2 = s2.ap
        if op1 is None:
            return self.op(eng, lambda: h.tensor_scalar(out.ap, in0.ap, a1, None, op0), reads=rd, writes=[out])
        return self.op(eng, lambda: h.tensor_scalar(out.ap, in0.ap, a1, a2, op0, op1), reads=rd, writes=[out])

    def stt(self, out, in0, scalar, in1, op0, op1):
        nc = self.nc
        rd = [in0, in1]
        a = scalar
        if isinstance(scalar, V):
            rd.append(scalar)
            a = scalar.ap
        return self.op("dve", lambda: nc.vector.scalar_tensor_tensor(out.ap, in0.ap, a, in1.ap, op0, op1),
                       reads=rd, writes=[out])

    def copy(self, out, in_, eng="dve"):
        nc = self.nc
        if eng == "act":
            return self.op("act", lambda: nc.scalar.copy(out.ap, in_.ap), reads=[in_], writes=[out])
        h = self.engs[eng]
        return self.op(eng, lambda: h.tensor_copy(out.ap, in_.ap), reads=[in_], writes=[out])

    def memset(self, out, val, eng="dve"):
        h = self.engs[eng]
        return self.op(eng, lambda: h.memset(out.ap, val), reads=[], writes=[out])

    def scan(self, out, d0, d1, init, op0, op1):
        nc = self.nc
        rd = [d0, d1]
        a = init
        if isinstance(init, V):
            rd.append(init)
            a = init.ap
        return self.op("dve", lambda: nc.vector.tensor_tensor_scan(out.ap, d0.ap, d1.ap, a, op0, op1),
                       reads=rd, writes=[out])

    def reduce(self, out, in_, op, axis=AX.X):
        nc = self.nc
        return self.op("dve", lambda: nc.vector.tensor_reduce(out.ap, in_.ap, axis, op), reads=[in_], writes=[out])

    def recip(self, out, in_):
        nc = self.nc
        return self.op("dve", lambda: nc.vector.reciprocal(out.ap, in_.ap), reads=[in_], writes=[out])

    def wait_all(self, eng, tts):
        self._deps(eng, tts, ())


class Arena:
    def __init__(self, c, nbytes, name):
        self.tt = c.sb([128, nbytes // 4], F32, name)
        self.views = []
        self.nbytes = nbytes

    def view(self, off, shape, dt, name):
        esz = 4 if dt in (F32, I32) else 2
        n = esz
        for s in shape[1:]:
            n *= s
        assert off % 4 == 0 and n % 4 == 0 and off + n <= self.nbytes, (name, off, n, self.nbytes)
        ap = self.tt.t[:, off // 4:(off + n) // 4]
        if dt != F32:
            ap = ap.bitcast(dt)
        if len(shape) == 3:
            ap = ap.rearrange("p (a b) -> p a b", a=shape[1])
        elif len(shape) == 4:
            ap = ap.rearrange("p (a b c) -> p a b c", a=shape[1], b=shape[2])
        if shape[0] < 128:
            ap = ap[0:shape[0]]
        t = TT(ap, name)
        for (v, lo, hi) in self.views:
            if lo < off + n and off < hi:
                t.al.append(v)
                v.al.append(t)
        self.views.append((t, off, off + n))
        return t


class Ring:
    def __init__(self, tiles):
        self.tiles = tiles
        self.i = 0

    def next(self):
        t = self.tiles[self.i]
        self.i = (self.i + 1) % len(self.tiles)
        return t


def _consts(TB):
    p = np.arange(128)
    f = {}
    j = p[:, None]
    i = p[None, :]
    same = (j // C) == (i // C)
    f["mask_incl"] = (same & (j <= i)).astype(np.float32)
    f["mask_strict"] = (same & (j < i)).astype(np.float32)
    f["mask_strictT"] = (same & (j > i)).astype(np.float32)
    f["ident"] = np.eye(128, dtype=np.float32)
    f["blockones"] = same.astype(np.float32)
    f["identZ"] = ((p[:, None] % 64) == np.arange(64)[None, :]).astype(np.float32)
    hs = np.zeros((128, 2), np.float32)
    hs[:64, 0] = 1.0
    hs[64:, 1] = 1.0
    f["headsel"] = hs
    t = np.arange(TB)
    f["reset"] = np.broadcast_to((t % C != 0).astype(np.float32)[None, :], (128, TB)).copy()
    half = 32
    inv = (10000.0 ** (-np.arange(half, dtype=np.float32) / half)).astype(np.float32)
    d = p % 64
    f["invfreq"] = inv[d % 32][:, None].astype(np.float32)
    f["sinsign"] = np.where(d < 32, 1.0, -1.0)[:, None].astype(np.float32)
    lg = np.log(1.0 - 2.0 ** (-5.0 - np.arange(4, dtype=np.float64)))
    idx = np.arange(C, dtype=np.float64)
    req = np.zeros((128, 2, C), np.float32)
    rek = np.zeros((128, 2, C), np.float32)
    rdec = np.zeros((128, 2), np.float32)
    for tl in range(2):
        for pp in range(128):
            h = tl * 2 + pp // 64
            req[pp, tl] = np.exp((idx + 1.0) * lg[h])
            rek[pp, tl] = np.exp(-(idx + 1.0) * lg[h]) * (64.0 ** -0.5)
            rdec[pp, tl] = np.exp(C * lg[h])
    f["ret_eq"] = req.reshape(128, 2 * C)
    f["ret_ek"] = rek.reshape(128, 2 * C)
    f["ret_dec"] = rdec
    names = list(f.keys())
    offs = {}
    o = 0
    for n in names:
        offs[n] = (o, f[n].shape[1])
        o += f[n].shape[1]
    arr = np.concatenate([f[n] for n in names], axis=1).astype(np.float32)
    return arr, offs


def build(T, L=NL, TB=512, mixers=(1, 1, 1, 1), ffn=True, same="raw"):
    assert T % TB == 0 and TB % 128 == 0
    NT = TB // 128
    NBLK = T // TB
    NCH = TB // C
    nc = bass.Bass("TRN2", target_bir_lowering=False)
    c = Ctx(nc, same=same)
    carr, coff = _consts(TB)

    def din(name, shape, dt=F32):
        return nc.dram_tensor(name, list(shape), dt, kind="ExternalInput").ap()

    x_d = din("x", [T, D])
    pos_d = din("positions", [1, T], I32)
    w_in_d = din("w_in", [NL, D, INC])
    w_out_d = din("w_out", [NL, D, D])
    ln_d = {k: din(k, [NL, D]) for k in ("ln1_g", "ln1_b", "ln2_g", "ln2_b")}
    lb_d = din("hgrn_lb_logits", [NL, 256])
    hng_d = din("hgrn_norm_g", [NL, 64])
    gw2_d = din("gla_gate_w2", [NL, 16, 128])
    gb_d = din("gla_gate_b", [NL, 128])
    gng_d = din("gla_norm_g", [NL, 64])
    mu_d = din("rwkv_mu", [NL, 1056])
    w0_d = din("rwkv_w0", [NL, 256])
    w2_d = din("rwkv_w2", [NL, 64, 256])
    a0_d = din("rwkv_a0", [NL, 256])
    a2_d = din("rwkv_a2", [NL, 64, 256])
    g2_d = din("rwkv_g2", [NL, 160, 256])
    kk_d = din("rwkv_k_k", [NL, 256])
    ka_d = din("rwkv_k_a", [NL, 256])
    rk_d = din("rwkv_r_k", [NL, 256])
    lxg_d = din("rwkv_lnx_g", [NL, 256])
    lxb_d = din("rwkv_lnx_b", [NL, 256])
    v0_d = din("rwkv_v0", [NL - 1, 256])
    v1_d = din("rwkv_v1", [NL - 1, 256, 32])
    v2_d = din("rwkv_v2", [NL - 1, 32, 256])
    fg_d = din("ffn_w_gate", [2, D, FFD])
    fu_d = din("ffn_w_up", [2, D, FFD])
    fd_d = din("ffn_w_down", [2, FFD, D])
    mr_d = din("moe_router", [2, D, NE])
    mg_d = din("moe_w_gate", [2, NE, D, FFE])
    mu2_d = din("moe_w_up", [2, NE, D, FFE])
    md_d = din("moe_w_down", [2, NE, FFE, D])
    cst_d = din("consts", list(carr.shape))
    out_d = nc.dram_tensor("out", [T, D], F32, kind="ExternalOutput").ap()

    cst = c.sb([128, carr.shape[1]], F32, "cst")
    c.dma("sp", cst[:], cst_d)

    def K(name):
        o, n = coff[name]
        return cst[:, o:o + n]
    ident_bf = c.sb([128, 128], BF16, "identbf")
    c.copy(ident_bf[:], K("ident"))
    mask_bf = c.sb([128, 128], BF16, "maskbf")
    c.copy(mask_bf[:], K("mask_incl"))

    def pp_tile(dram, n, name, nl=NL):
        t = c.sb([128, nl, n], F32, name)
        for l in range(nl):
            for k in range(n):
                c.dma("sp", t[:, l, k:k + 1], dram[l:l + 1, k * 128:(k + 1) * 128].rearrange("o p -> p o"))
        return t

    lbl = pp_tile(lb_d, 2, "lbl")
    w0 = pp_tile(w0_d, 2, "w0")
    a0 = pp_tile(a0_d, 2, "a0")
    kkp = pp_tile(kk_d, 2, "kkp")
    kap = pp_tile(ka_d, 2, "kap")
    rkp = pp_tile(rk_d, 2, "rkp")
    v0p = pp_tile(v0_d, 2, "v0p", nl=NL - 1)
    mup = c.sb([128, NL, 9], F32, "mup")
    c.memset(mup[:], 0.0)
    for l in range(NL):
        for k in range(8):
            c.dma("sp", mup[:, l, k:k + 1], mu_d[l:l + 1, k * 128:(k + 1) * 128].rearrange("o p -> p o"))
        c.dma("sp", mup[0:32, l, 8:9], mu_d[l:l + 1, 1024:1056].rearrange("o p -> p o"))
    omu = c.sb([128, NL, 9], F32, "omu")
    c.ts(omu[:], mup[:], -1.0, ALU.mult, 1.0, ALU.add)
    gb2 = c.sb([64, NL, 2], F32, "gb2")
    for l in range(NL):
        for k in range(2):
            c.dma("sp", gb2[:, l, k:k + 1], gb_d[l:l + 1, k * 64:(k + 1) * 64].rearrange("o p -> p o"))
    ngb2 = c.sb([64, NL, 2], F32, "ngb2")
    c.ts(ngb2[:], gb2[:], -1.0, ALU.mult)
    nw0 = c.sb([128, NL, 2], F32, "nw0")
    c.ts(nw0[:], w0[:], -1.0, ALU.mult)
    lbe = c.sb([128, NL, 2], F32, "lbe")
    c.act(lbe[:], lbl[:], AF.Exp)
    lbs = c.sb([128, 2], F32, "lbs")
    c.tt(lbs[:], lbe[:, 0, :], lbe[:, 1, :], ALU.add)
    for l in range(2, NL):
        c.tt(lbs[:], lbs[:], lbe[:, l, :], ALU.add)
    c.recip(lbs[:], lbs[:])
    lb = c.sb([128, NL, 2], F32, "lb")
    c.memset(lb[:], 0.0)
    for l in range(1, NL):
        c.tt(lb[:, l, :], lbe[:, l, :], lbs[:], ALU.mult)
        c.tt(lb[:, l, :], lb[:, l, :], lb[:, l - 1, :], ALU.add)
    olb = c.sb([128, NL, 2], F32, "olb")
    c.ts(olb[:], lb[:], -1.0, ALU.mult, 1.0, ALU.add)
    nolb = c.sb([128, NL, 2], F32, "nolb")
    c.ts(nolb[:], olb[:], -1.0, ALU.mult)
    epsc = c.sb([128, 4], F32, "epsc")
    c.memset(epsc[:, 0:1], 1e-5)
    c.memset(epsc[:, 1:2], 1e-6)
    c.memset(epsc[:, 2:3], 64e-5)
    c.memset(epsc[:, 3:4], 1.0)
    mrs = c.sb([128, 2, 8, NE], F32, "mrs")
    for i in range(2):
        c.dma("sp", mrs[:, i], mr_d[i].rearrange("(k p) e -> p k e", p=128))
    mrs_bf = c.sb([128, 2, 8, NE], BF16, "mrsbf")
    c.copy(mrs_bf[:], mrs[:])

    rb_small = c.sb([128, 640], F32, "rbsmall")
    gw2 = c.sb([16, 128], F32, "gw2")
    w2s = c.sb([64, 256], F32, "w2s")
    a2s = c.sb([128, 256], F32, "a2s")
    g2a = c.sb([128, 256], F32, "g2a")
    g2b = c.sb([32, 256], F32, "g2b")
    v1s = c.sb([128, 2, 32], F32, "v1s")
    v2s = c.sb([32, 256], F32, "v2s")

    def load_layer_params(l):
        c.dma("sp", rb_small[:, 0:64], hng_d[l:l + 1, :].partition_broadcast(128))
        c.dma("sp", rb_small[:, 64:128], gng_d[l:l + 1, :].partition_broadcast(128))
        c.dma("sp", rb_small[:, 128:384], lxg_d[l:l + 1, :].partition_broadcast(128))
        c.dma("sp", rb_small[:, 384:640], lxb_d[l:l + 1, :].partition_broadcast(128))
        c.dma("sp", gw2[:], gw2_d[l])
        c.dma("sp", w2s[:], w2_d[l])
        c.dma("sp", a2s[64:128, :], a2_d[l])
        c.dma("sp", g2a[:], g2_d[l, 0:128, :])
        c.dma("sp", g2b[:], g2_d[l, 128:160, :])
        if l > 0:
            c.dma("sp", v1s[:], v1_d[l - 1].rearrange("(c p) n -> p c n", p=128))
            c.dma("sp", v2s[:], v2_d[l - 1])

    SLOTN = 8 * 512
    NSLOT = 4
    wslots = [c.sb([128, SLOTN], BF16, "wslot%d" % i) for i in range(NSLOT)]
    acc_banks = [c.ps([128, 512], F32, "acc%d" % i) for i in range(4)]
    tr_banks = [c.ps([128, 512], F32, "trb%d" % i) for i in range(4)]
    accb = Ring(list(acc_banks))
    trb = Ring(list(tr_banks))

    def psum_mode(mixer):
        if mixer:
            trb.tiles = tr_banks + acc_banks[1:4]
            accb.tiles = acc_banks[0:1]
        else:
            trb.tiles = list(tr_banks)
            accb.tiles = list(acc_banks)
        trb.i = 0
        accb.i = 0

    hT = c.sb([128, 8, TB], BF16, "hT")
    htok = c.sb([128, NT, D], F32, "htok")
    ytok = c.sb([128, NT, D], BF16, "ytok")
    P = [c.sb([128, TB], F32, "P%d" % i) for i in range(11)]
    R = [c.sb([128, TB], F32, "R%d" % i) for i in range(11)]
    qk = [c.sb([128, TB], BF16, "qk%d" % i) for i in range(4)]
    vt = c.sb([128, NT, 256], BF16, "vt")
    gt = c.sb([128, NT, 256], BF16, "gt")
    decs = [c.sb([128, NCH], F32, "dec%d" % i) for i in range(2)]
    dec_rt = [c.sb([128, NCH], F32, "decrt%d" % i) for i in range(2)]
    for i in range(2):
        c.copy(dec_rt[i][:], K("ret_dec")[:, i:i + 1].bc([128, NCH]))
    cosT = c.sb([128, TB], F32, "cosT")
    sinT = c.sb([128, TB], F32, "sinT")
    o_sb = c.sb([128, 256], F32, "o_sb")
    sq_sb = c.sb([128, 256], F32, "sq_sb")
    stat_ln = c.sb([128, 16], F32, "stat_ln")
    stat_mx = c.sb([128, 16], F32, "stat_mx")
    stat_moe = c.sb([128, 40], F32, "stat_moe")
    vfirst = c.sb([128, 2, TB], F32, "vfirst")
    rw_carry = c.sb([128, L, 9], F32, "rwcarry")
    rw_bonus = c.sb([128, NT, 4], F32, "rw_bonus")
    rw_dec = [c.sb([128, NCH], F32, "rwdec%d" % i) for i in range(2)]
    gates = c.sb([128, NT, NE], F32, "gates")

    def mk_state(W, name):
        s = [c.sb([128, W], F32, "%s_f%d" % (name, l)) for l in range(L)]
        sb_ = c.sb([128, W], BF16, "%s_b" % name)
        return s, sb_
    st_hg = [mk_state(128, "hg%d" % t) for t in range(2)]
    st_rt = [mk_state(128, "rt%d" % t) for t in range(2)]
    st_gl = [mk_state(128, "gl%d" % t) for t in range(2)]
    zst = [[c.sb([128, 64], F32, "z%d_%d" % (l, pt)) for pt in range(2)] for l in range(L)]

    RW_BYTES = 5 * 4 * TB + 1 * 4 * (TB + 1) + 2 * 4 * TB + 4 * 4 * TB + 16 * TB + NT * 1024 + NT * 512 + NT * 1024 \
        + 8 * 1024 + 8 * 1024 + 4 * 512 + 12 * 256 + 8 * 512
    FF_BYTES = 22 * TB * 2 + NT * D * 4 + 8 * TB * 2
    RW_BYTES = 5 * 4 * TB + 4 * (TB + 1) + 2 * 4 * TB + 4 * 2 * TB + 8 * TB + NT * 512 + 2 * 128 + NT * 512 + NT * 1024 \
        + 8 * 512 + 8 * 512 + 4 * 256 + 8 * 128 + 4 * 256 + 8 * 256
    ar_ = Arena(c, max(RW_BYTES, FF_BYTES) + 64, "arena")
    off = [0]

    def av(shape, dt, name):
        esz = 4 if dt == F32 else 2
        n = esz
        for s in shape[1:]:
            n *= s
        v = ar_.view(off[0], shape, dt, name)
        off[0] += n
        return v
    rw_sh = [av([128, TB], F32, "rwsh%d" % i) for i in range(5)]
    rw_raw = Ring([av([128, TB + 1], F32, "rwraw%d" % i) for i in range(1)])
    th_t = av([128, TB], F32, "th")
    lvs_t = av([128, TB], F32, "lvs")
    rw_kt = [av([128, TB], BF16, "rw_kt%d" % i) for i in range(2)]
    rw_bt = [av([128, TB], BF16, "rw_bt%d" % i) for i in range(2)]
    rw_ar = [av([128, 2, TB], BF16, "rw_ar%d" % i) for i in range(2)]
    rw_vtok = av([128, NT, 256], BF16, "rw_vtok")
    zbf = [av([128, 64], BF16, "zbf%d" % i) for i in range(2)]
    rw_gtok = av([128, NT, 256], BF16, "rw_gtok")
    o_acc = av([128, NT, 256], F32, "o_acc")
    a_ring = Ring([av([128, 256], BF16, "aring%d" % i) for i in range(8)])
    m_ring = Ring([av([128, 256], BF16, "mring%d" % i) for i in range(8)])
    tok_ring = Ring([av([128, 128], BF16, "tokr%d" % i) for i in range(4)])
    u_ring = Ring([av([128, 64], BF16, "ur%d" % i) for i in range(8)])
    g_ring = Ring([av([128, 64], F32, "gr%d" % i) for i in range(4)])
    TT_ring = Ring([av([128, 128], BF16, "TTr%d" % i) for i in range(8)])
    off[0] = 0
    mT = av([128, 22, TB], BF16, "mT")
    facc = av([128, NT, D], F32, "facc")
    yT = av([128, 8, TB], BF16, "yT")

    out_tt = TT(None, "out_dram")

    pieces = []
    pst = {"issued": 0, "taken": 0}
    free_slots = list(range(NSLOT))
    slot_of = {}

    class Piece:
        __slots__ = ("v", "slot")

    def plan_piece(dap, nk, ncols):
        assert nk * ncols <= SLOTN
        pieces.append((dap, nk, ncols))

    def pump():
        while free_slots and pst["issued"] < len(pieces):
            i = pst["issued"]
            dap, nk, ncols = pieces[i]
            s = free_slots.pop(0)
            dst = wslots[s][:, 0:nk * ncols].re("p (k c) -> p k c", k=nk)
            c.dma("pool", dst, dap)
            p_ = Piece()
            p_.v = dst
            p_.slot = s
            slot_of[i] = p_
            pst["issued"] += 1

    def take_piece():
        pump()
        i = pst["taken"]
        assert i in slot_of, ("weight ring deadlock", i)
        pst["taken"] += 1
        return slot_of.pop(i)

    def release(p_):
        free_slots.append(p_.slot)
        pump()

    def rows_piece(w2d, r0, nk, c0, ncols):
        return w2d[r0 * 128:(r0 + nk) * 128, c0:c0 + ncols].rearrange("(k p) c -> p k c", p=128)

    WIN_PIECES = [(1808, 512), (2320, 512), (2832, 32), (0, 512), (512, 512), (1024, 512), (1536, 272),
                  (2864, 512), (3376, 512)]
    GU_COLS_D = [(0, 512), (512, 512), (1024, 512), (1536, 512), (2048, 512), (2560, 256)]
    GU_COLS_E = [(0, 512), (512, 512), (1024, 384)]

    def plan_layer(l):
        for (c0, n) in WIN_PIECES:
            plan_piece(rows_piece(w_in_d[l], 0, 8, c0, n), 8, n)
        for hf in range(2):
            plan_piece(rows_piece(w_out_d[l], 0, 8, hf * 512, 512), 8, 512)
        if not ffn:
            return
        i = l // 2
        if l % 2 == 0:
            for (c0, n) in GU_COLS_D:
                plan_piece(rows_piece(fg_d[i], 0, 8, c0, n), 8, n)
                plan_piece(rows_piece(fu_d[i], 0, 8, c0, n), 8, n)
            for hf in range(2):
                for (r0, nk) in ((0, 8), (8, 8), (16, 6)):
                    plan_piece(rows_piece(fd_d[i], r0, nk, hf * 512, 512), nk, 512)
        else:
            for e in range(NE):
                for (c0, n) in GU_COLS_E:
                    plan_piece(rows_piece(mg_d[i, e], 0, 8, c0, n), 8, n)
                    plan_piece(rows_piece(mu2_d[i, e], 0, 8, c0, n), 8, n)
                for hf in range(2):
                    for (r0, nk) in ((0, 8), (8, 3)):
                        plan_piece(rows_piece(md_d[i, e], r0, nk, hf * 512, 512), nk, 512)

    for b in range(NBLK):
        for l in range(L):
            plan_layer(l)

    def proj_F(wp, cols, ncols, rhs=None):
        bank = trb.next()
        out = bank[0:ncols, 0:TB]
        for k in range(8):
            c.mm(out, wp.v[:, k, cols:cols + ncols], hT[:, k, :], start=(k == 0), stop=(k == 7))
        return out

    def proj_T(wp, cols, ncols, tile):
        bank = trb.next()
        out = bank[:, 0:ncols]
        for k in range(8):
            c.mm(out, hT[:, k, tile * 128:(tile + 1) * 128], wp.v[:, k, cols:cols + ncols],
                 start=(k == 0), stop=(k == 7))
        return out

    def cumdecay(g, sgn_scale, cum, eq, ek):
        c.scan(cum, K("reset")[:, 0:TB], g, 0.0, ALU.mult, ALU.add)
        c.act(eq, cum, AF.Exp, scale=sgn_scale)
        c.act(ek, cum, AF.Exp, scale=-sgn_scale)

    def chunk_end(v):
        return v.re("p (n c) -> p n c", c=C)[:, :, C - 1]

    def rstd_of(dst, src, eps_col):
        c.act(dst, src, AF.Sqrt, bias=epsc[:, eps_col:eps_col + 1])
        c.recip(dst, dst)

    sc_ring = Ring([c.sb([128, 128], BF16, "sc%d" % i) for i in range(4)])
    kt_ring = Ring([c.sb([128, 128], BF16, "kt%d" % i) for i in range(2)])
    md_ring = Ring([c.sb([128, 128], F32, "md%d" % i) for i in range(2)])

    def gla_tile(l, t, qT, kT, dcs, states, KD, nh_tile):
        tok = slice(t * 128, (t + 1) * 128)
        o_bank = accb.next()
        o_ps = o_bank[:, 0:256]
        first = [True]
        W = nh_tile * 64
        NP = nh_tile * KD
        for pt in range(len(qT)):
            S, Sb = states[pt][0][l][0:NP, :], states[pt][1][0:NP, :]
            if t == 0:
                c.copy(Sb, S, eng="act")
            ktp = trb.next()
            ktv = V(ktp, ktp.t[:, 0:NP // 2].bitcast(BF16))
            c.tr(ktv, kT[pt][:, tok], ident_bf[0:NP, 0:NP])
            ktok = kt_ring.next()[:, 0:NP]
            c.copy(ktok, ktv, eng="act")
            scs = []
            for hh in range(nh_tile):
                pr = slice(hh * KD, (hh + 1) * KD)
                sp_ = trb.next()
                c.mm(sp_[:, 0:128], kT[pt][pr, tok], qT[pt][pr, tok])
                sc = sc_ring.next()[:]
                c.tt(sc, sp_[:, 0:128], mask_bf[:], ALU.mult)
                scs.append(sc)
            yield
            for hh in range(nh_tile):
                hg = pt * nh_tile + hh
                c.mm(o_ps[:, hg * 64:(hg + 1) * 64], scs[hh], vt[:, t, hg * 64:(hg + 1) * 64],
                     start=first[0], stop=False, sg=True)
                first[0] = False
            for ch in range(2):
                cg = t * 2 + ch
                rows = slice(ch * 64, (ch + 1) * 64)
                ctok = slice(t * 128 + ch * 64, t * 128 + (ch + 1) * 64)
                for hh in range(nh_tile):
                    hg = pt * nh_tile + hh
                    pr = slice(hh * KD, (hh + 1) * KD)
                    c.mm(o_ps[rows, hg * 64:(hg + 1) * 64], qT[pt][pr, ctok], Sb[pr, hh * 64:(hh + 1) * 64],
                         start=False, stop=True, sg=True)
                mp = trb.next()
                c.mm(mp[0:NP, 0:W], ktok[rows, :], vt[rows, t, pt * W:(pt + 1) * W])
                md = md_ring.next()[0:NP, 0:W]
                c.act(md, mp[0:NP, 0:W], AF.Copy, scale=dcs[pt][:, cg:cg + 1])
                c.stt(S, S, dcs[pt][:, cg:cg + 1], md, ALU.mult, ALU.add)
                c.copy(Sb, S, eng="act")
                yield
        return o_ps

    def finish_simple(l, t, o_ps, mix_idx, kind, gam_off):
        o = o_sb[:]
        c.copy(o, o_ps, eng="act")
        o3 = o.re("p (h v) -> p h v", h=4)
        sq = sq_sb[:]
        msum = stat_mx[:, 0:4]
        ssum = stat_mx[:, 4:8]
        rstd = stat_mx[:, 8:12]
        if kind == "gn":
            c.reduce(msum, o3, ALU.add)
            c.ts(msum, msum, 1.0 / 64.0, ALU.mult)
            c.tt(o3, o3, msum.re("p (h o) -> p h o", o=1).bc([128, 4, 64]), ALU.subtract)
        c.act(sq, o, AF.Square)
        c.reduce(ssum, sq.re("p (h v) -> p h v", h=4), ALU.add)
        c.ts(ssum, ssum, 1.0 / 64.0, ALU.mult)
        rstd_of(rstd, ssum, 1 if kind == "rms" else 0)
        c.tt(o3, o3, rstd.re("p (h o) -> p h o", o=1).bc([128, 4, 64]), ALU.mult)
        if gam_off is not None:
            gm = rb_small[:, gam_off:gam_off + 64]
            c.tt(o3, o3, gm.re("p (o v) -> p o v", o=1).bc([128, 4, 64]), ALU.mult)
        c.tt(ytok[:, t, mix_idx * 256:(mix_idx + 1) * 256], o, gt[:, t, :], ALU.mult)

    def hgrn(l, wp0, wp1):
        for pt in range(2):
            qps = proj_F(wp0, pt * 128, 128)
            qs = P[0][:]
            c.act(qs, qps, AF.Silu)
            fps = proj_F(wp0, 256 + pt * 128, 128)
            s = P[1][:]
            c.act(s, fps, AF.Sigmoid)
            yield
            f = P[2][:]
            c.ts(f, s, olb[:, l, pt:pt + 1], ALU.mult, lb[:, l, pt:pt + 1], ALU.add)
            k = P[3][:]
            c.ts(k, s, nolb[:, l, pt:pt + 1], ALU.mult, olb[:, l, pt:pt + 1], ALU.add)
            g = P[4][:]
            c.act(g, f, AF.Ln)
            cum, eq, ek = P[5][:], P[6][:], P[7][:]
            cumdecay(g, 1.0, cum, eq, ek)
            c.tt(qk[0 + pt][:], qs, eq, ALU.mult)
            c.tt(qk[2 + pt][:], k, ek, ALU.mult)
            c.copy(decs[pt][:], chunk_end(eq))
            yield
        for t in range(NT):
            ps_ = proj_T(wp1, 0, 512, t)
            c.copy(vt[:, t, :], ps_[:, 0:256], eng="act")
            c.act(gt[:, t, :], ps_[:, 256:512], AF.Silu)
            yield
        release(wp0)
        release(wp1)
        for t in range(NT):
            o_ps = yield from gla_tile(l, t, [qk[0][:], qk[1][:]], [qk[2][:], qk[3][:]], [decs[0][:], decs[1][:]],
                                       st_hg, 64, 2)
            finish_simple(l, t, o_ps, 0, "rms", 0)
            yield

    def gla(l, wp2, wp3):
        gps = proj_F(wp3, 0, 16)
        glr = P[2]
        c.copy(glr[0:16, :], gps, eng="act")
        for pt in range(2):
            qps = proj_F(wp2, pt * 64, 64)
            qs = P[0][0:64, :]
            c.act(qs, qps, AF.Copy, scale=32.0 ** -0.5)
            kps = proj_F(wp2, 128 + pt * 64, 64)
            ks = P[1][0:64, :]
            c.copy(ks, kps, eng="act")
            yield
            bank = trb.next()
            zps = bank[0:64, 0:TB]
            c.mm(zps, gw2[:, pt * 64:(pt + 1) * 64], glr[0:16, :])
            e = P[3][0:64, :]
            c.act(e, zps, AF.Exp, bias=ngb2[:, l, pt:pt + 1], scale=-1.0)
            sp_ = P[4][0:64, :]
            c.act(sp_, e, AF.Ln, bias=epsc[0:64, 3:4])
            cum, eq, ek = P[5][0:64, :], P[6][0:64, :], P[7][0:64, :]
            c.scan(cum, K("reset")[0:64, 0:TB], sp_, 0.0, ALU.mult, ALU.add)
            c.act(eq, cum, AF.Exp, scale=-1.0 / 16.0)
            c.act(ek, cum, AF.Exp, scale=1.0 / 16.0)
            c.tt(qk[0 + pt][0:64, :], qs, eq, ALU.mult)
            c.tt(qk[2 + pt][0:64, :], ks, ek, ALU.mult)
            c.copy(decs[pt][0:64, :], chunk_end(eq))
            yield
        for t in range(NT):
            ps_ = proj_T(wp2, 256, 256, t)
            c.copy(vt[:, t, :], ps_, eng="act")
            ps2 = proj_T(wp3, 16, 256, t)
            c.act(gt[:, t, :], ps2, AF.Silu)
            yield
        release(wp2)
        release(wp3)
        for t in range(NT):
            o_ps = yield from gla_tile(l, t, [qk[0][0:64, :], qk[1][0:64, :]], [qk[2][0:64, :], qk[3][0:64, :]],
                                       [decs[0][0:64, :], decs[1][0:64, :]], st_gl, 32, 2)
            finish_simple(l, t, o_ps, 1, "rms", 64)
            yield

    def rope_tables(b):
        posi = V(P[0], P[0].t[:].bitcast(I32))
        c.dma("sp", posi, pos_d[0:1, b * TB:(b + 1) * TB].partition_broadcast(128))
        posf = P[5]
        c.copy(posf[:], posi)
        ang = P[1][:]
        c.ts(ang, posf[:], K("invfreq"), ALU.mult)
        kq = P[2][:]
        c.ts(kq, ang, float(1.0 / (2.0 * np.pi)), ALU.mult, 12582912.0, ALU.add)
        c.ts(kq, kq, -12582912.0, ALU.add)
        r = P[3][:]
        C1 = 6.28125
        C2 = float(2.0 * np.pi - 6.28125)
        c.stt(r, kq, -C1, ang, ALU.mult, ALU.add)
        c.stt(r, kq, -C2, r, ALU.mult, ALU.add)
        c.ts(r, r, 3.14159, ALU.min, -3.14159, ALU.max)
        c.act(sinT[:], r, AF.Sin)
        ab = P[4][:]
        c.ts(ab, r, -1.0, ALU.mult)
        c.tt(ab, ab, r, ALU.max)
        c.ts(ab, ab, -1.0, ALU.mult, float(np.pi / 2.0), ALU.add)
        c.act(cosT[:], ab, AF.Sin)
        c.ts(sinT[:], sinT[:], K("sinsign"), ALU.mult)

    def ret(l, wp6, wp7):
        for which in range(2):
            for pt in range(2):
                ps_ = proj_F(wp6, which * 256 + pt * 128, 128)
                xs = P[0][:]
                c.copy(xs, ps_, eng="act")
                a = P[1][:]
                c.tt(a, xs, cosT[:], ALU.mult)
                bsw = P[2][:]
                for hh in range(2):
                    lo = slice(hh * 64, hh * 64 + 32)
                    hi = slice(hh * 64 + 32, hh * 64 + 64)
                    c.tt(bsw[lo, :], xs[hi, :], sinT[hi, :], ALU.mult)
                    c.tt(bsw[hi, :], xs[lo, :], sinT[lo, :], ALU.mult)
                c.tt(a, a, bsw, ALU.add)
                tab = K("ret_eq" if which == 0 else "ret_ek")[:, pt * C:(pt + 1) * C]
                dst = qk[which * 2 + pt]
                c.tt(dst[:].re("p (n c) -> p n c", c=C), a.re("p (n c) -> p n c", c=C),
                     tab.re("p (o c) -> p o c", o=1).bc([128, NCH, C]), ALU.mult)
                yield
        for t in range(NT):
            ps_ = proj_T(wp7, 0, 512, t)
            c.copy(vt[:, t, :], ps_[:, 0:256], eng="act")
            c.act(gt[:, t, :], ps_[:, 256:512], AF.Silu)
            yield
        release(wp6)
        release(wp7)
        for t in range(NT):
            o_ps = yield from gla_tile(l, t, [qk[0][:], qk[1][:]], [qk[2][:], qk[3][:]],
                                       [dec_rt[0][:], dec_rt[1][:]], st_rt, 64, 2)
            finish_simple(l, t, o_ps, 3, "gn", None)
            yield

    def rw_shift(l, b, i, wp, c0, n, dst, tm):
        ps_ = proj_F(wp, c0, n)
        raw = rw_raw.next()
        if b == 0:
            c.memset(raw[0:n, 0:1], 0.0)
        else:
            c.copy(raw[0:n, 0:1], rw_carry[0:n, l, i:i + 1])
        c.copy(raw[0:n, 1:TB + 1], ps_, eng="act")
        c.copy(rw_carry[0:n, l, i:i + 1], raw[0:n, TB:TB + 1])
        c.act(tm[0:n, :], raw[0:n, 1:TB + 1], AF.Copy, scale=omu[0:n, l, i:i + 1])
        c.stt(dst[0:n, :], raw[0:n, 0:TB], mup[0:n, l, i:i + 1], tm[0:n, :], ALU.mult, ALU.add)

    def rwkv(l, b, wp4, wp5, wp5b, rel):
        vS = [rw_sh[0], rw_sh[1]]
        waS, rS, kS = rw_sh[2], rw_sh[3], rw_sh[4]
        rw_shift(l, b, 6, wp5, 256, 128, waS, R[2])
        g0S, g1S = R[0], R[1]
        rw_shift(l, b, 7, wp5, 384, 128, g0S, R[2])
        rw_shift(l, b, 8, wp5b, 0, 32, g1S, R[2])
        yield
        rw_shift(l, b, 4, wp5, 0, 128, vS[0], R[2])
        rw_shift(l, b, 5, wp5, 128, 128, vS[1], R[2])
        release(wp5)
        release(wp5b)
        rel["done"] = True
        yield
        c.act(th_t[0:64, :], waS[0:64, :], AF.Tanh)
        c.act(g0S[:], g0S[:], AF.Sigmoid)
        c.act(g1S[0:32, :], g1S[0:32, :], AF.Sigmoid)
        for t in range(NT):
            bank = trb.next()
            c.mm(bank[:, 0:256], g0S[:, t * 128:(t + 1) * 128], g2a[:], start=True, stop=False)
            c.mm(bank[:, 0:256], g1S[0:32, t * 128:(t + 1) * 128], g2b[:], start=False, stop=True)
            c.copy(rw_gtok[:, t, :], bank[:, 0:256], eng="act")
            yield
        if l > 0:
            bank = trb.next()
            lv = bank[0:32, 0:TB]
            for k in range(2):
                c.mm(lv, v1s[:, k, :], vS[k][:], start=(k == 0), stop=(k == 1))
            c.copy(lvs_t[0:32, :], lv, eng="act")
        for pt in range(2):
            if l == 0:
                c.copy(vfirst[:, pt, :], vS[pt][:])
            else:
                bank = trb.next()
                c.mm(bank[:, 0:TB], v2s[:, pt * 128:(pt + 1) * 128], lvs_t[0:32, :])
                sgm = R[2][:]
                c.act(sgm, bank[:, 0:TB], AF.Sigmoid, bias=v0p[:, l - 1, pt:pt + 1])
                dv = R[3][:]
                c.tt(dv, vfirst[:, pt, :], vS[pt][:], ALU.subtract)
                c.tt(dv, dv, sgm, ALU.mult)
                c.tt(vS[pt][:], vS[pt][:], dv, ALU.add)
                yield
            for t in range(NT):
                bank = trb.next()
                c.tr(bank[:, 0:128], vS[pt][:, t * 128:(t + 1) * 128], K("ident"))
                c.copy(rw_vtok[:, t, pt * 128:(pt + 1) * 128], bank[:, 0:128], eng="act")
            yield
        for pt in range(2):
            rw_shift(l, b, 0 + pt, wp4, pt * 128, 128, rS, R[0])
            rw_shift(l, b, 2 + pt, wp4, 256 + pt * 128, 128, kS, R[0])
            if pt == 1:
                release(wp4)
            bank = trb.next()
            c.mm(bank[:, 0:TB], w2s[:, pt * 128:(pt + 1) * 128], th_t[0:64, :])
            t1 = R[2][:]
            c.act(t1, bank[:, 0:TB], AF.Exp, bias=nw0[:, l, pt:pt + 1], scale=-1.0)
            c.act(t1, t1, AF.Ln, bias=epsc[:, 3:4])
            c.ts(t1, t1, -1.0, ALU.mult, -0.5, ALU.add)
            c.act(t1, t1, AF.Exp)
            g = R[3][:]
            c.ts(g, t1, -1.0, ALU.mult)
            cum, eq, ek = R[4][:], R[5][:], R[6][:]
            cumdecay(g, 1.0, cum, eq, ek)
            c.copy(rw_dec[pt][:], chunk_end(eq))
            yield
            bank = trb.next()
            c.mm(bank[:, 0:TB], a2s[64:128, pt * 128:(pt + 1) * 128], waS[64:128, :])
            ag = R[7][:]
            c.act(ag, bank[:, 0:TB], AF.Sigmoid, bias=a0[:, l, pt:pt + 1])
            kk = R[8][:]
            c.ts(kk, kS[:], kkp[:, l, pt:pt + 1], ALU.mult)
            nrm = R[9][:]
            c.act(nrm, kk, AF.Square)
            bank = trb.next()
            c.mm(bank[:, 0:TB], K("blockones"), nrm)
            c.act(nrm, bank[:, 0:TB], AF.Sqrt)
            c.ts(nrm, nrm, 1e-12, ALU.max)
            c.act(nrm, nrm, AF.Ln)
            c.act(nrm, nrm, AF.Exp, scale=-1.0)
            c.tt(kk, kk, nrm, ALU.mult)
            yield
            fk = R[9][:]
            c.ts(fk, ag, -1.0, ALU.add, kap[:, l, pt:pt + 1], ALU.mult)
            c.ts(fk, fk, 1.0, ALU.add)
            km = R[10][:]
            c.tt(km, kS[:], fk, ALU.mult)
            bo = R[2][:]
            c.stt(bo, rS[:], rkp[:, l, pt:pt + 1], km, ALU.mult, ALU.mult)
            for t in range(NT):
                bank = trb.next()
                c.mm(bank[:, 0:2], bo[:, t * 128:(t + 1) * 128], K("headsel"))
                c.copy(rw_bonus[:, t, pt * 2:pt * 2 + 2], bank[:, 0:2], eng="act")
            yield
            c.tt(rw_ar[pt][:, 1, :], rS[:], eq, ALU.mult)
            c.tt(rw_kt[pt][:], km, ek, ALU.mult)
            bb = R[9][:]
            c.tt(bb, kk, ag, ALU.mult)
            c.tt(rw_bt[pt][:], bb, ek, ALU.mult)
            yield
            ex = R[9][:]
            c.tt(ex, cum, g, ALU.subtract)
            c.act(ex, ex, AF.Exp)
            c.stt(rw_ar[pt][:, 0, :], kk, -1.0, ex, ALU.mult, ALU.mult)


    F32R = mybir.dt.float32r

    def R32(v):
        return V(v.tt, v.ap.bitcast(F32R))

    def rwkv_core(l, t, pt, hh, sidx):
        h = pt * 2 + hh
        ev = ("act", "dve") if sidx % 2 == 0 else ("dve", "act")
        tok = slice(t * 128, (t + 1) * 128)
        pr = slice(hh * 64, hh * 64 + 64)
        ar = rw_ar[pt][pr, :, tok]
        kt = rw_kt[pt][pr, tok]
        bt = rw_bt[pt][pr, tok]
        at = rw_ar[pt][pr, 0, tok]
        rt = rw_ar[pt][pr, 1, tok]
        Z = zst[l][pt]
        Zb = zbf[pt]
        if t == 0:
            c.copy(Zb[pr, :], Z[pr, :], eng=ev[1])
        vtk = rw_vtok[:, t, h * 64:(h + 1) * 64]
        ocol = slice(h * 64, (h + 1) * 64)
        b1 = trb.next()
        c.mm(b1[:, 0:256].re("p (a n) -> p a n", a=2), kt, ar)
        Ak = a_ring.next()[:]
        c.tt(Ak[:, 0:128], b1[:, 0:128], K("mask_strict"), ALU.mult)
        c.tt(Ak[:, 128:256], b1[:, 128:256], K("mask_incl"), ALU.mult)
        b2 = trb.next()
        c.mm(b2[:, 0:256].re("p (a n) -> p a n", a=2), bt, ar)
        Ab = a_ring.next()[:]
        c.tt(Ab[:, 0:128], b2[:, 0:128], K("mask_strict"), ALU.mult)
        c.tt(Ab[:, 128:256], b2[:, 128:256], K("mask_incl"), ALU.mult)
        b3 = trb.next()
        c.mm(b3[:, 0:128], at, bt)
        pq = m_ring.next()[:]
        Pm = pq[:, 0:128]
        c.tt(Pm, b3[:, 0:128], K("mask_strictT"), ALU.mult)
        Q = Ab[:, 0:128]
        Tc = TT_ring.next()[:]
        c.tt(Tc, Q, K("ident"), ALU.add)
        yield
        for i in range(1, 6):
            bpq = trb.next()
            c.mm(bpq[:, 0:128], Q, Pm)
            if i < 5:
                c.mm(bpq[:, 128:256], Pm, Q)
            pq = m_ring.next()[:]
            if i < 5:
                c.copy(pq, bpq[:, 0:256], eng=ev[0])
            else:
                c.copy(pq[:, 0:128], bpq[:, 0:128], eng=ev[0])
            Pm = pq[:, 0:128]
            Q = pq[:, 128:256]
            yield
            bt_ = trb.next()
            c.mm(bt_[:, 0:128], Pm, Tc)
            Tn = TT_ring.next()[:]
            c.tt(Tn, Tc, bt_[:, 0:128], ALU.add)
            Tc = Tn
            yield
        bk = trb.next()
        bkv = V(bk, bk.t[:, 0:64].bitcast(BF16))
        c.tr(bkv[:, 0:64], kt, ident_bf[pr, pr])
        c.tr(bkv[:, 64:128], bt, ident_bf[pr, pr])
        kb_tok = tok_ring.next()[:]
        c.copy(kb_tok, bkv, eng=ev[0])
        gb_ = trb.next()
        c.mm(gb_[:, 0:64], Ak[:, 0:128], vtk)
        utok = u_ring.next()[:]
        rhs_u = u_ring.next()[:]
        G_sb = g_ring.next()[:]
        c.copy(G_sb, gb_[:, 0:64], eng=ev[1])
        yield
        for ch in range(2):
            rows = slice(ch * 64, (ch + 1) * 64)
            cg = t * 2 + ch
            gz = trb.next()
            c.mm(gz[rows, 0:64], at[:, rows], Zb[pr, :])
            c.tt(rhs_u[rows, :], gz[rows, 0:64], G_sb[rows, :], ALU.add)
            ob = trb.next()
            c.mm(ob[rows, 0:64], rt[:, rows], Zb[pr, :])
            c.copy(o_acc[rows, t, ocol], ob[rows, 0:64], eng=ev[0])
            yield
            ub = trb.next()
            c.mm(ub[rows, 0:64], Tc[rows, rows], rhs_u[rows, :])
            c.copy(utok[rows, :], ub[rows, 0:64], eng=ev[0])
            yield
            zb = trb.next()
            c.mm(zb[pr, 0:64], K("identZ")[pr, :], Z[pr, :], start=True, stop=False, sg=True)
            c.mm(zb[pr, 0:64], kb_tok[rows, 0:64], vtk[rows, :], start=False, stop=False, sg=True)
            c.mm(zb[pr, 0:64], kb_tok[rows, 64:128], utok[rows, :], start=False, stop=True, sg=True)
            c.act(Z[pr, :], zb[pr, 0:64], AF.Copy, scale=rw_dec[pt][pr, cg:cg + 1])
            c.copy(Zb[pr, :], Z[pr, :], eng=ev[1])
            yield
        ob2 = trb.next()
        c.mm(ob2[:, 0:64], Ak[:, 128:256], vtk, start=True, stop=False)
        c.mm(ob2[:, 0:64], Ab[:, 128:256], utok, start=False, stop=True)
        c.tt(o_acc[:, t, ocol], o_acc[:, t, ocol], ob2[:, 0:64], ALU.add)

    def rwkv_finish(l, t):
        o = o_sb[:]
        c.copy(o, o_acc[:, t, :])
        o3 = o.re("p (h v) -> p h v", h=4)
        msum = stat_mx[:, 0:4]
        ssum = stat_mx[:, 4:8]
        rstd = stat_mx[:, 8:12]
        c.reduce(msum, o3, ALU.add)
        c.ts(msum, msum, 1.0 / 64.0, ALU.mult)
        c.tt(o3, o3, msum.re("p (h o) -> p h o", o=1).bc([128, 4, 64]), ALU.subtract)
        sq = sq_sb[:]
        c.act(sq, o, AF.Square)
        c.reduce(ssum, sq.re("p (h v) -> p h v", h=4), ALU.add)
        c.ts(ssum, ssum, 1.0 / 64.0, ALU.mult)
        rstd_of(rstd, ssum, 2)
        c.tt(o3, o3, rstd.re("p (h o) -> p h o", o=1).bc([128, 4, 64]), ALU.mult)
        c.tt(o, o, rb_small[:, 128:384], ALU.mult)
        c.tt(o, o, rb_small[:, 384:640], ALU.add)
        c.tt(sq.re("p (h v) -> p h v", h=4), rw_vtok[:, t, :].re("p (h v) -> p h v", h=4),
             rw_bonus[:, t, :].re("p (h o) -> p h o", o=1).bc([128, 4, 64]), ALU.mult)
        c.tt(o, o, sq, ALU.add)
        c.tt(ytok[:, t, 512:768], o, rw_gtok[:, t, :], ALU.mult)

    def others(l):
        wp0, wp1 = take_piece(), take_piece()
        if mixers[0]:
            yield from hgrn(l, wp0, wp1)
        else:
            release(wp0)
            release(wp1)
        wp2, wp3 = take_piece(), take_piece()
        if mixers[1]:
            yield from gla(l, wp2, wp3)
        else:
            release(wp2)
            release(wp3)
        wp6, wp7 = take_piece(), take_piece()
        if mixers[3]:
            yield from ret(l, wp6, wp7)
        else:
            release(wp6)
            release(wp7)

    def to_hT(t):
        for hf in range(2):
            bank = trb.next()
            for k4 in range(4):
                k = hf * 4 + k4
                c.tr(bank[:, k4 * 128:(k4 + 1) * 128], htok[:, t, k * 128:(k + 1) * 128], K("ident"))
            c.copy(hT[:, hf * 4:(hf + 1) * 4, t * 128:(t + 1) * 128],
                   bank[:, 0:512].re("p (k n) -> p k n", k=4), eng="act")

    def layer_norm(l, which):
        for hf in range(2):
            c.dma("sp", P[4 + hf][:], ln_d[which + "_g"][l:l + 1, hf * 512:(hf + 1) * 512].partition_broadcast(128))
            c.dma("sp", P[6 + hf][:], ln_d[which + "_b"][l:l + 1, hf * 512:(hf + 1) * 512].partition_broadcast(128))
        for t in range(NT):
            z = htok[:, t, :]
            st6 = stat_ln[:, 0:12]
            for hf in range(2):
                zz = z[:, hf * 512:(hf + 1) * 512]
                dst = stat_ln[:, hf * 6:(hf + 1) * 6]
                c.op("dve", lambda zz=zz, dst=dst: nc.vector.bn_stats(dst.ap, zz.ap), reads=[zz], writes=[dst])
            mv = stat_ln[:, 12:14]
            c.op("dve", lambda: nc.vector.bn_aggr(mv.ap, st6.ap), reads=[st6], writes=[mv])
            rs = stat_ln[:, 14:15]
            rstd_of(rs, stat_ln[:, 13:14], 0)
            c.ts(z, z, stat_ln[:, 12:13], ALU.subtract, rs, ALU.mult)
            for hf in range(2):
                zz = z[:, hf * 512:(hf + 1) * 512]
                c.tt(zz, zz, P[4 + hf][:], ALU.mult)
                c.tt(zz, zz, P[6 + hf][:], ALU.add)
            to_hT(t)

    def residual_from_bank(t, hf, bank_v):
        z = htok[:, t, hf * 512:(hf + 1) * 512]
        c.stt(z, z, ALPHA, bank_v, ALU.mult, ALU.add)

    def gate_up(cols_list):
        ft = 0
        for (c0, n) in cols_list:
            wg = take_piece()
            wu = take_piece()
            for j in range(n // 128):
                gps = proj_F(wg, j * 128, 128)
                ups = proj_F(wu, j * 128, 128)
                sl = P[8 + ft % 3][:]
                c.act(sl, gps, AF.Silu)
                c.tt(mT[:, ft, :], sl, ups, ALU.mult)
                ft += 1
            release(wg)
            release(wu)
        return ft

    def down_proj(nft, row_groups, consume):
        for hf in range(2):
            accs = [accb.next() for _ in range(NT)]
            for (r0, nk) in row_groups:
                wd = take_piece()
                for kk_ in range(nk):
                    ft = r0 + kk_
                    for t in range(NT):
                        c.mm(accs[t][:, 0:512], mT[:, ft, t * 128:(t + 1) * 128], wd.v[:, kk_, :],
                             start=(ft == 0), stop=(ft == nft - 1))
                release(wd)
            for t in range(NT):
                consume(t, hf, accs[t][:, 0:512])

    def ffn_dense(l):
        gate_up(GU_COLS_D)
        down_proj(22, ((0, 8), (8, 8), (16, 6)), residual_from_bank)

    def moe(l):
        i = l // 2
        for t in range(NT):
            bank = trb.next()
            lg = bank[:, 0:NE]
            for k in range(8):
                c.mm(lg, hT[:, k, t * 128:(t + 1) * 128], mrs_bf[:, i, k, :], start=(k == 0), stop=(k == 7))
            lgs = stat_moe[:, 0:8]
            c.copy(lgs, lg)
            m8 = stat_moe[:, 8:16]
            c.op("dve", lambda: nc.vector.max(m8.ap, lgs.ap), reads=[lgs], writes=[m8])
            nm1 = stat_moe[:, 32:33]
            c.ts(nm1, m8[:, 0:1], -1.0, ALU.mult)
            ex = stat_moe[:, 16:24]
            c.act(ex, lgs, AF.Exp, bias=nm1)
            sel = stat_moe[:, 24:32]
            c.ts(sel, lgs, m8[:, 1:2], ALU.is_ge)
            c.tt(ex, ex, sel, ALU.mult)
            ssum = stat_moe[:, 33:34]
            c.reduce(ssum, ex, ALU.add)
            c.recip(ssum, ssum)
            c.ts(gates[:, t, :], ex, ssum, ALU.mult)
        c.memset(facc[:], 0.0)
        for e in range(NE):
            gate_up(GU_COLS_E)

            def cons(t, hf, bank_v, e=e):
                fa = facc[:, t, hf * 512:(hf + 1) * 512]
                c.stt(fa, bank_v, gates[:, t, e:e + 1], fa, ALU.mult, ALU.add)
            down_proj(11, ((0, 8), (8, 3)), cons)
        for t in range(NT):
            z = htok[:, t, :]
            c.stt(z, z, ALPHA, facc[:, t, :], ALU.mult, ALU.add)

    for l in range(L):
        for pt in range(2):
            for st in (st_hg, st_rt, st_gl):
                c.memset(st[pt][0][l][:], 0.0)
            c.memset(zst[l][pt][:], 0.0)
    c.memset(ytok[:], 0.0)

    for b in range(NBLK):
        for t in range(NT):
            r0 = b * TB + t * 128
            c.dma("sp", htok[:, t, :], x_d[r0:r0 + 128, :])
            to_hT(t)
        if mixers[3]:
            rope_tables(b)
        for l in range(L):
            load_layer_params(l)
            psum_mode(True)
            wp4, wp5, wp5b = take_piece(), take_piece(), take_piece()
            rel = {"done": False}
            rp = None
            if mixers[2]:
                rp = rwkv(l, b, wp4, wp5, wp5b, rel)
                while not rel["done"]:
                    next(rp)
            else:
                release(wp4)
                release(wp5)
                release(wp5b)
            og = others(l)
            og_live = [True]

            def og_step():
                if og_live[0]:
                    try:
                        next(og)
                    except StopIteration:
                        og_live[0] = False
            if mixers[2]:
                rp_live = True
                while rp_live:
                    try:
                        next(rp)
                    except StopIteration:
                        rp_live = False
                    og_step()
                for t in range(NT):
                    live = [rwkv_core(l, t, h // 2, h % 2, h) for h in range(4)]
                    while live:
                        nxt = []
                        for g_ in live:
                            try:
                                next(g_)
                                nxt.append(g_)
                            except StopIteration:
                                pass
                        live = nxt
                        og_step()
                    rwkv_finish(l, t)
            while og_live[0]:
                og_step()
            psum_mode(False)
            for t in range(NT):
                for hf in range(2):
                    bank = trb.next()
                    bv = V(bank, bank.t[:, 0:256].bitcast(BF16))
                    for k4 in range(4):
                        k = hf * 4 + k4
                        c.tr(bv[:, k4 * 128:(k4 + 1) * 128], ytok[:, t, k * 128:(k + 1) * 128], ident_bf[:])
                    c.copy(yT[:, hf * 4:(hf + 1) * 4, t * 128:(t + 1) * 128],
                           bv.re("p (k n) -> p k n", k=4), eng="act")
            wo = [take_piece(), take_piece()]
            for t in range(NT):
                for hf in range(2):
                    bank = trb.next()
                    for k in range(8):
                        c.mm(bank[:, 0:512], yT[:, k, t * 128:(t + 1) * 128], wo[hf].v[:, k, :],
                             start=(k == 0), stop=(k == 7))
                    residual_from_bank(t, hf, bank[:, 0:512])
            release(wo[0])
            release(wo[1])
            layer_norm(l, "ln1")
            if ffn:
                if l % 2 == 0:
                    ffn_dense(l)
                else:
                    moe(l)
                layer_norm(l, "ln2")
        for t in range(NT):
            r0 = b * TB + t * 128
            c.dma("sp", V(out_tt, out_d[r0:r0 + 128, :]), htok[:, t, :])
    c.wait_all("sp", [out_tt])
    assert pst["taken"] == len(pieces), (pst, len(pieces))
    return nc, carr, c


_CACHE = {}

NAMES = ["w_in", "w_out", "ln1_g", "ln1_b", "ln2_g", "ln2_b", "hgrn_lb_logits", "hgrn_norm_g", "gla_gate_w2",
         "gla_gate_b", "gla_norm_g", "rwkv_mu", "rwkv_w0", "rwkv_w2", "rwkv_a0", "rwkv_a2", "rwkv_g2", "rwkv_k_k",
         "rwkv_k_a", "rwkv_r_k", "rwkv_lnx_g", "rwkv_lnx_b", "rwkv_v0", "rwkv_v1", "rwkv_v2", "ffn_w_gate",
         "ffn_w_up", "ffn_w_down", "moe_router", "moe_w_gate", "moe_w_up", "moe_w_down"]


def run(inputs, T, L=NL, TB=512, **kw):
    x = np.asarray(inputs["x"], dtype=np.float32)
    B = x.shape[0]
    key = (T, L, TB, tuple(sorted(kw.items())))
    if key not in _CACHE:
        _CACHE[key] = build(T, L, TB, **kw)
    nc, carr, c = _CACHE[key]
    shared = {}
    for n in NAMES:
        a = np.ascontiguousarray(np.asarray(inputs[n], dtype=np.float32))
        if n == "rwkv_r_k":
            a = a.reshape(NL, 256)
        shared[n] = a
    shared["consts"] = carr
    pos = np.asarray(inputs["positions"]).astype(np.int32)
    in_maps = []
    for b in range(B):
        m = dict(shared)
        m["x"] = np.ascontiguousarray(x[b, :T])
        m["positions"] = np.ascontiguousarray(pos[b:b + 1, :T])
        in_maps.append(m)
    res = run_bass_kernel_spmd(nc, in_maps, core_ids=list(range(B)))
    return np.stack([np.asarray(r["out"]) for r in res.results], axis=0)


def kernel(**inputs):
    x = np.asarray(inputs["x"])
    B, S, _ = x.shape
    out = run(inputs, S)
    return out.astype(x.dtype)
```

```python
import numpy as np
import ml_dtypes
import concourse.bass as bass
import concourse.mybir as mybir
from concourse.bass_utils import run_bass_kernel_spmd

F32 = mybir.dt.float32
BF16 = mybir.dt.bfloat16
I32 = mybir.dt.int32
AF = mybir.ActivationFunctionType
ALU = mybir.AluOpType
AX = mybir.AxisListType

D = 1024
NL = 4
INC = 3888
FFD = 2816
NE = 8
FFE = 1408
ALPHA = (2.0 * NL) ** 0.25
C = 64


class V:
    __slots__ = ("tt", "ap")

    def __init__(self, tt, ap):
        self.tt = tt
        self.ap = ap

    def __getitem__(self, k):
        return V(self.tt, self.ap[k])

    def re(self, s, **kw):
        return V(self.tt, self.ap.rearrange(s, **kw))

    def bc(self, shape):
        return V(self.tt, self.ap.to_broadcast(list(shape)))


class TT:
    __slots__ = ("t", "name", "w", "r", "al", "pe_row")

    def __init__(self, t, name):
        self.t = t
        self.name = name
        self.w = None
        self.r = []
        self.al = []
        self.pe_row = None

    def __getitem__(self, k):
        return V(self, self.t[k])


class Ctx:
    NDMA = 24

    def __init__(self, nc, same=True):
        self.nc = nc
        self.same = same
        self.engs = {"pe": nc.tensor, "dve": nc.vector, "act": nc.scalar, "pool": nc.gpsimd, "sp": nc.sync}
        self.sem = {}
        self.cnt = {}
        self.waited = {}
        self._cms = []
        for k in self.engs:
            cm = nc.semaphore("s_" + k)
            self.sem[k] = cm.__enter__()
            self._cms.append(cm)
            self.cnt[k] = 0
            self.waited[k] = {}
        self.dsem = []
        self.dcnt = []
        for i in range(self.NDMA):
            cm = nc.semaphore("d_%d" % i)
            self.dsem.append(cm.__enter__())
            self._cms.append(cm)
            self.dcnt.append(0)
        self.dnext = {"sp": 0, "pool": 0, "act": 0}
        self.drange = {"sp": (0, 14), "pool": (14, 22), "act": (22, 24)}
        self.ntile = 0
        self.ninst = 0

    def sb(self, shape, dt, name=None):
        self.ntile += 1
        name = name or "t%d" % self.ntile
        cm = self.nc.sbuf_tensor(name, list(shape), dt)
        t = cm.__enter__()
        self._cms.append(cm)
        return TT(t, name)

    def ps(self, shape, dt, name=None):
        self.ntile += 1
        name = name or "p%d" % self.ntile
        cm = self.nc.psum_tensor(name, list(shape), dt)
        t = cm.__enter__()
        self._cms.append(cm)
        return TT(t, name)

    def _semof(self, key):
        if key in self.sem:
            return self.sem[key]
        return self.dsem[int(key[1:])]

    def _deps(self, eng, reads, writes):
        need = {}

        def add(dep):
            if dep is None:
                return
            k, v = dep
            if need.get(k, 0) < v:
                need[k] = v
        rawonly = (self.same == "raw")
        need_raw = {}
        for t in reads:
            add(t.w)
            for a in t.al:
                add(a.w)
        if rawonly:
            need_raw = dict(need)
        for t in writes:
            add(t.w)
            for d in t.r:
                add(d)
            for a in t.al:
                add(a.w)
                for d in a.r:
                    add(d)
        if rawonly and eng in need:
            if eng in need_raw:
                need[eng] = need_raw[eng]
            else:
                del need[eng]
        h = self.engs[eng]
        for k, v in need.items():
            if k == eng and (not self.same or eng == "pe"):
                continue
            if self.waited[eng].get(k, 0) >= v:
                continue
            h.wait_ge(self._semof(k), v)
            self.waited[eng][k] = v
            self.ninst += 1

    def _mark(self, me, reads, writes):
        for t in reads:
            if len(t.r) > 64:
                mx = {}
                for (k, v) in t.r:
                    if mx.get(k, 0) < v:
                        mx[k] = v
                t.r = list(mx.items())
            t.r.append(me)
        for t in writes:
            t.w = me
            t.r = []

    def op(self, eng, fn, reads=(), writes=()):
        reads = [x.tt if isinstance(x, V) else x for x in reads if x is not None]
        writes = [x.tt if isinstance(x, V) else x for x in writes if x is not None]
        self._deps(eng, reads, writes)
        ins = fn()
        self.cnt[eng] += 1
        ins.then_inc(self.sem[eng], 1)
        self._mark((eng, self.cnt[eng]), reads, writes)
        self.ninst += 1
        return ins

    def dma(self, q, out, in_, **kw):
        reads = [in_.tt] if isinstance(in_, V) else []
        writes = [out.tt] if isinstance(out, V) else []
        lo, hi = self.drange[q]
        i = lo + self.dnext[q]
        self.dnext[q] = (self.dnext[q] + 1) % (hi - lo)
        key = "d%d" % i
        h = self.engs[q]
        if self.dcnt[i] > 0 and self.waited[q].get(key, 0) < self.dcnt[i]:
            h.wait_ge(self.dsem[i], self.dcnt[i])
            self.waited[q][key] = self.dcnt[i]
        self._deps(q, reads, writes)
        oa = out.ap if isinstance(out, V) else out
        ia = in_.ap if isinstance(in_, V) else in_
        ins = h.dma_start(out=oa, in_=ia, **kw)
        self.dcnt[i] += 16
        ins.then_inc(self.dsem[i], 16)
        self._mark((key, self.dcnt[i]), reads, writes)
        self.ninst += 1
        return ins

    def _pe_row_guard(self, out, lhsT):
        row = (int(lhsT.ap.start_partition()), int(lhsT.ap.partition_size()))
        t = out.tt
        if t.pe_row is not None and t.pe_row != row and t.w is not None and t.w[0] == "pe":
            v = t.w[1]
            if self.waited["pe"].get("pe", 0) < v:
                self.nc.tensor.wait_ge(self.sem["pe"], v)
                self.waited["pe"]["pe"] = v
                self.ninst += 1
        t.pe_row = row

    def mm(self, out, lhsT, rhs, start=True, stop=True, sg=False):
        nc = self.nc
        self._pe_row_guard(out, lhsT)
        return self.op("pe", lambda: nc.tensor.matmul(out.ap, lhsT=lhsT.ap, rhs=rhs.ap, start=start, stop=stop,
                                                      skip_group_check=sg),
                       reads=[lhsT, rhs], writes=[out])

    def tr(self, out, in_, ident):
        nc = self.nc
        self._pe_row_guard(out, in_)
        return self.op("pe", lambda: nc.tensor.transpose(out.ap, in_.ap, ident.ap), reads=[in_, ident], writes=[out])

    def act(self, out, in_, func, bias=None, scale=None, eng="act"):
        nc = self.nc
        kw = {}
        rd = [in_]
        if bias is not None:
            if isinstance(bias, V):
                kw["bias"] = bias.ap
                rd.append(bias)
            else:
                kw["bias"] = bias
        if scale is not None:
            if isinstance(scale, V):
                kw["scale"] = scale.ap
                rd.append(scale)
            else:
                kw["scale"] = scale
        if func == AF.Copy and isinstance(scale, V):
            func = AF.Identity
        return self.op("act", lambda: nc.scalar.activation(out.ap, in_.ap, func, **kw), reads=rd, writes=[out])

    def tt(self, out, in0, in1, op, eng="dve"):
        h = self.engs[eng]
        return self.op(eng, lambda: h.tensor_tensor(out.ap, in0.ap, in1.ap, op), reads=[in0, in1], writes=[out])

    def ts(self, out, in0, s1, op0, s2=None, op1=None, eng="dve"):
        h = self.engs[eng]
        rd = [in0]
        a1 = s1
        if isinstance(s1, V):
            rd.append(s1)
            a1 = s1.ap
        a2 = s2
        if isinstance(s2, V):
            rd.append(s2)
            a2 = s2.ap
        if op1 is None:
            return self.op(eng, lambda: h.tensor_scalar(out.ap, in0.ap, a1, None, op0), reads=rd, writes=[out])
        return self.op(eng, lambda: h.tensor_scalar(out.ap, in0.ap, a1, a2, op0, op1), reads=rd, writes=[out])

    def stt(self, out, in0, scalar, in1, op0, op1):
        nc = self.nc
        rd = [in0, in1]
        a = scalar
        if isinstance(scalar, V):
            rd.append(scalar)
            a = scalar.ap
        return self.op("dve", lambda: nc.vector.scalar_tensor_tensor(out.ap, in0.ap, a, in1.ap, op0, op1),
                       reads=rd, writes=[out])

    def copy(self, out, in_, eng="dve"):
        nc = self.nc
        if eng == "act":
            return self.op("act", lambda: nc.scalar.copy(out.ap, in_.ap), reads=[in_], writes=[out])
        h = self.engs[eng]
        return self.op(eng, lambda: h.tensor_copy(out.ap, in_.ap), reads=[in_], writes=[out])

    def memset(self, out, val, eng="dve"):
        h = self.engs[eng]
        return self.op(eng, lambda: h.memset(out.ap, val), reads=[], writes=[out])

    def scan(self, out, d0, d1, init, op0, op1):
        nc = self.nc
        rd = [d0, d1]
        a = init
        if isinstance(init, V):
            rd.append(init)
            a = init.ap
        return self.op("dve", lambda: nc.vector.tensor_tensor_scan(out.ap, d0.ap, d1.ap, a, op0, op1),
                       reads=rd, writes=[out])

    def reduce(self, out, in_, op, axis=AX.X):
        nc = self.nc
        return self.op("dve", lambda: nc.vector.tensor_reduce(out.ap, in_.ap, axis, op), reads=[in_], writes=[out])

    def recip(self, out, in_):
        nc = self.nc
        return self.op("dve", lambda: nc.vector.reciprocal(out.ap, in_.ap), reads=[in_], writes=[out])

    def wait_all(self, eng, tts):
        self._deps(eng, tts, ())


class Arena:
    def __init__(self, c, nbytes, name):
        self.tt = c.sb([128, nbytes // 4], F32, name)
        self.views = []
        self.nbytes = nbytes

    def view(self, off, shape, dt, name):
        esz = 4 if dt in (F32, I32) else 2
        n = esz
        for s in shape[1:]:
            n *= s
        assert off % 4 == 0 and n % 4 == 0 and off + n <= self.nbytes, (name, off, n, self.nbytes)
        ap = self.tt.t[:, off // 4:(off + n) // 4]
        if dt != F32:
            ap = ap.bitcast(dt)
        if len(shape) == 3:
            ap = ap.rearrange("p (a b) -> p a b", a=shape[1])
        elif len(shape) == 4:
            ap = ap.rearrange("p (a b c) -> p a b c", a=shape[1], b=shape[2])
        if shape[0] < 128:
            ap = ap[0:shape[0]]
        t = TT(ap, name)
        for (v, lo, hi) in self.views:
            if lo < off + n and off < hi:
                t.al.append(v)
                v.al.append(t)
        self.views.append((t, off, off + n))
        return t


class Ring:
    def __init__(self, tiles):
        self.tiles = tiles
        self.i = 0

    def next(self):
        t = self.tiles[self.i]
        self.i = (self.i + 1) % len(self.tiles)
        return t


def _consts(TB):
    p = np.arange(128)
    f = {}
    j = p[:, None]
    i = p[None, :]
    same = (j // C) == (i // C)
    f["mask_incl"] = (same & (j <= i)).astype(np.float32)
    f["mask_strict"] = (same & (j < i)).astype(np.float32)
    f["mask_strictT"] = (same & (j > i)).astype(np.float32)
    f["ident"] = np.eye(128, dtype=np.float32)
    f["blockones"] = same.astype(np.float32)
    f["identZ"] = ((p[:, None] % 64) == np.arange(64)[None, :]).astype(np.float32)
    hs = np.zeros((128, 2), np.float32)
    hs[:64, 0] = 1.0
    hs[64:, 1] = 1.0
    f["headsel"] = hs
    t = np.arange(TB)
    f["reset"] = np.broadcast_to((t % C != 0).astype(np.float32)[None, :], (128, TB)).copy()
    half = 32
    inv = (10000.0 ** (-np.arange(half, dtype=np.float32) / half)).astype(np.float32)
    d = p % 64
    f["invfreq"] = inv[d % 32][:, None].astype(np.float32)
    f["sinsign"] = np.where(d < 32, 1.0, -1.0)[:, None].astype(np.float32)
    lg = np.log(1.0 - 2.0 ** (-5.0 - np.arange(4, dtype=np.float64)))
    idx = np.arange(C, dtype=np.float64)
    req = np.zeros((128, 2, C), np.float32)
    rek = np.zeros((128, 2, C), np.float32)
    rdec = np.zeros((128, 2), np.float32)
    for tl in range(2):
        for pp in range(128):
            h = tl * 2 + pp // 64
            req[pp, tl] = np.exp((idx + 1.0) * lg[h])
            rek[pp, tl] = np.exp(-(idx + 1.0) * lg[h]) * (64.0 ** -0.5)
            rdec[pp, tl] = np.exp(C * lg[h])
    f["ret_eq"] = req.reshape(128, 2 * C)
    f["ret_ek"] = rek.reshape(128, 2 * C)
    f["ret_dec"] = rdec
    names = list(f.keys())
    offs = {}
    o = 0
    for n in names:
        offs[n] = (o, f[n].shape[1])
        o += f[n].shape[1]
    arr = np.concatenate([f[n] for n in names], axis=1).astype(np.float32)
    return arr, offs


def build(T, L=NL, TB=512, mixers=(1, 1, 1, 1), ffn=True, same="raw"):
    assert T % TB == 0 and TB % 128 == 0
    NT = TB // 128
    NBLK = T // TB
    NCH = TB // C
    nc = bass.Bass("TRN2", target_bir_lowering=False)
    c = Ctx(nc, same=same)
    carr, coff = _consts(TB)

    def din(name, shape, dt=F32):
        return nc.dram_tensor(name, list(shape), dt, kind="ExternalInput").ap()

    x_d = din("x", [T, D])
    pos_d = din("positions", [1, T], I32)
    w_in_d = din("w_in", [NL, D, INC])
    w_out_d = din("w_out", [NL, D, D])
    ln_d = {k: din(k, [NL, D]) for k in ("ln1_g", "ln1_b", "ln2_g", "ln2_b")}
    lb_d = din("hgrn_lb_logits", [NL, 256])
    hng_d = din("hgrn_norm_g", [NL, 64])
    gw2_d = din("gla_gate_w2", [NL, 16, 128])
    gb_d = din("gla_gate_b", [NL, 128])
    gng_d = din("gla_norm_g", [NL, 64])
    mu_d = din("rwkv_mu", [NL, 1056])
    w0_d = din("rwkv_w0", [NL, 256])
    w2_d = din("rwkv_w2", [NL, 64, 256])
    a0_d = din("rwkv_a0", [NL, 256])
    a2_d = din("rwkv_a2", [NL, 64, 256])
    g2_d = din("rwkv_g2", [NL, 160, 256])
    kk_d = din("rwkv_k_k", [NL, 256])
    ka_d = din("rwkv_k_a", [NL, 256])
    rk_d = din("rwkv_r_k", [NL, 256])
    lxg_d = din("rwkv_lnx_g", [NL, 256])
    lxb_d = din("rwkv_lnx_b", [NL, 256])
    v0_d = din("rwkv_v0", [NL - 1, 256])
    v1_d = din("rwkv_v1", [NL - 1, 256, 32])
    v2_d = din("rwkv_v2", [NL - 1, 32, 256])
    fg_d = din("ffn_w_gate", [2, D, FFD])
    fu_d = din("ffn_w_up", [2, D, FFD])
    fd_d = din("ffn_w_down", [2, FFD, D])
    mr_d = din("moe_router", [2, D, NE])
    mg_d = din("moe_w_gate", [2, NE, D, FFE])
    mu2_d = din("moe_w_up", [2, NE, D, FFE])
    md_d = din("moe_w_down", [2, NE, FFE, D])
    cst_d = din("consts", list(carr.shape))
    out_d = nc.dram_tensor("out", [T, D], F32, kind="ExternalOutput").ap()

    cst = c.sb([128, carr.shape[1]], F32, "cst")
    c.dma("sp", cst[:], cst_d)

    def K(name):
        o, n = coff[name]
        return cst[:, o:o + n]
    ident_bf = c.sb([128, 128], BF16, "identbf")
    c.copy(ident_bf[:], K("ident"))
    mask_bf = c.sb([128, 128], BF16, "maskbf")
    c.copy(mask_bf[:], K("mask_incl"))

    def pp_tile(dram, n, name, nl=NL):
        t = c.sb([128, nl, n], F32, name)
        for l in range(nl):
            for k in range(n):
                c.dma("sp", t[:, l, k:k + 1], dram[l:l + 1, k * 128:(k + 1) * 128].rearrange("o p -> p o"))
        return t

    lbl = pp_tile(lb_d, 2, "lbl")
    w0 = pp_tile(w0_d, 2, "w0")
    a0 = pp_tile(a0_d, 2, "a0")
    kkp = pp_tile(kk_d, 2, "kkp")
    kap = pp_tile(ka_d, 2, "kap")
    rkp = pp_tile(rk_d, 2, "rkp")
    v0p = pp_tile(v0_d, 2, "v0p", nl=NL - 1)
    mup = c.sb([128, NL, 9], F32, "mup")
    c.memset(mup[:], 0.0)
    for l in range(NL):
        for k in range(8):
            c.dma("sp", mup[:, l, k:k + 1], mu_d[l:l + 1, k * 128:(k + 1) * 128].rearrange("o p -> p o"))
        c.dma("sp", mup[0:32, l, 8:9], mu_d[l:l + 1, 1024:1056].rearrange("o p -> p o"))
    omu = c.sb([128, NL, 9], F32, "omu")
    c.ts(omu[:], mup[:], -1.0, ALU.mult, 1.0, ALU.add)
    gb2 = c.sb([64, NL, 2], F32, "gb2")
    for l in range(NL):
        for k in range(2):
            c.dma("sp", gb2[:, l, k:k + 1], gb_d[l:l + 1, k * 64:(k + 1) * 64].rearrange("o p -> p o"))
    ngb2 = c.sb([64, NL, 2], F32, "ngb2")
    c.ts(ngb2[:], gb2[:], -1.0, ALU.mult)
    nw0 = c.sb([128, NL, 2], F32, "nw0")
    c.ts(nw0[:], w0[:], -1.0, ALU.mult)
    lbe = c.sb([128, NL, 2], F32, "lbe")
    c.act(lbe[:], lbl[:], AF.Exp)
    lbs = c.sb([128, 2], F32, "lbs")
    c.tt(lbs[:], lbe[:, 0, :], lbe[:, 1, :], ALU.add)
    for l in range(2, NL):
        c.tt(lbs[:], lbs[:], lbe[:, l, :], ALU.add)
    c.recip(lbs[:], lbs[:])
    lb = c.sb([128, NL, 2], F32, "lb")
    c.memset(lb[:], 0.0)
    for l in range(1, NL):
        c.tt(lb[:, l, :], lbe[:, l, :], lbs[:], ALU.mult)
        c.tt(lb[:, l, :], lb[:, l, :], lb[:, l - 1, :], ALU.add)
    olb = c.sb([128, NL, 2], F32, "olb")
    c.ts(olb[:], lb[:], -1.0, ALU.mult, 1.0, ALU.add)
    nolb = c.sb([128, NL, 2], F32, "nolb")
    c.ts(nolb[:], olb[:], -1.0, ALU.mult)
    epsc = c.sb([128, 4], F32, "epsc")
    c.memset(epsc[:, 0:1], 1e-5)
    c.memset(epsc[:, 1:2], 1e-6)
    c.memset(epsc[:, 2:3], 64e-5)
    c.memset(epsc[:, 3:4], 1.0)
    mrs = c.sb([128, 2, 8, NE], F32, "mrs")
    for i in range(2):
        c.dma("sp", mrs[:, i], mr_d[i].rearrange("(k p) e -> p k e", p=128))
    mrs_bf = c.sb([128, 2, 8, NE], BF16, "mrsbf")
    c.copy(mrs_bf[:], mrs[:])

    rb_small = c.sb([128, 640], F32, "rbsmall")
    gw2 = c.sb([16, 128], F32, "gw2")
    w2s = c.sb([64, 256], F32, "w2s")
    a2s = c.sb([128, 256], F32, "a2s")
    g2a = c.sb([128, 256], F32, "g2a")
    g2b = c.sb([32, 256], F32, "g2b")
    v1s = c.sb([128, 2, 32], F32, "v1s")
    v2s = c.sb([32, 256], F32, "v2s")

    def load_layer_params(l):
        c.dma("sp", rb_small[:, 0:64], hng_d[l:l + 1, :].partition_broadcast(128))
        c.dma("sp", rb_small[:, 64:128], gng_d[l:l + 1, :].partition_broadcast(128))
        c.dma("sp", rb_small[:, 128:384], lxg_d[l:l + 1, :].partition_broadcast(128))
        c.dma("sp", rb_small[:, 384:640], lxb_d[l:l + 1, :].partition_broadcast(128))
        c.dma("sp", gw2[:], gw2_d[l])
        c.dma("sp", w2s[:], w2_d[l])
        c.dma("sp", a2s[64:128, :], a2_d[l])
        c.dma("sp", g2a[:], g2_d[l, 0:128, :])
        c.dma("sp", g2b[:], g2_d[l, 128:160, :])
        if l > 0:
            c.dma("sp", v1s[:], v1_d[l - 1].rearrange("(c p) n -> p c n", p=128))
            c.dma("sp", v2s[:], v2_d[l - 1])

    SLOTN = 8 * 512
    NSLOT = 4
    wslots = [c.sb([128, SLOTN], BF16, "wslot%d" % i) for i in range(NSLOT)]
    acc_banks = [c.ps([128, 512], F32, "acc%d" % i) for i in range(4)]
    tr_banks = [c.ps([128, 512], F32, "trb%d" % i) for i in range(4)]
    accb = Ring(list(acc_banks))
    trb = Ring(list(tr_banks))

    def psum_mode(mixer):
        if mixer:
            trb.tiles = tr_banks + acc_banks[1:4]
            accb.tiles = acc_banks[0:1]
        else:
            trb.tiles = list(tr_banks)
            accb.tiles = list(acc_banks)
        trb.i = 0
        accb.i = 0

    hT = c.sb([128, 8, TB], BF16, "hT")
    htok = c.sb([128, NT, D], F32, "htok")
    ytok = c.sb([128, NT, D], BF16, "ytok")
    P = [c.sb([128, TB], F32, "P%d" % i) for i in range(11)]
    R = [c.sb([128, TB], F32, "R%d" % i) for i in range(11)]
    qk = [c.sb([128, TB], BF16, "qk%d" % i) for i in range(4)]
    vt = c.sb([128, NT, 256], BF16, "vt")
    gt = c.sb([128, NT, 256], BF16, "gt")
    decs = [c.sb([128, NCH], F32, "dec%d" % i) for i in range(2)]
    dec_rt = [c.sb([128, NCH], F32, "decrt%d" % i) for i in range(2)]
    for i in range(2):
        c.copy(dec_rt[i][:], K("ret_dec")[:, i:i + 1].bc([128, NCH]))
    cosT = c.sb([128, TB], F32, "cosT")
    sinT = c.sb([128, TB], F32, "sinT")
    o_sb = c.sb([128, 256], F32, "o_sb")
    sq_sb = c.sb([128, 256], F32, "sq_sb")
    stat_ln = c.sb([128, 16], F32, "stat_ln")
    stat_mx = c.sb([128, 16], F32, "stat_mx")
    stat_moe = c.sb([128, 40], F32, "stat_moe")
    vfirst = c.sb([128, 2, TB], F32, "vfirst")
    rw_carry = c.sb([128, L, 9], F32, "rwcarry")
    rw_bonus = c.sb([128, NT, 4], F32, "rw_bonus")
    rw_dec = [c.sb([128, NCH], F32, "rwdec%d" % i) for i in range(2)]
    gates = c.sb([128, NT, NE], F32, "gates")

    def mk_state(W, name):
        s = [c.sb([128, W], F32, "%s_f%d" % (name, l)) for l in range(L)]
        sb_ = c.sb([128, W], BF16, "%s_b" % name)
        return s, sb_
    st_hg = [mk_state(128, "hg%d" % t) for t in range(2)]
    st_rt = [mk_state(128, "rt%d" % t) for t in range(2)]
    st_gl = [mk_state(128, "gl%d" % t) for t in range(2)]
    zst = [[c.sb([128, 64], F32, "z%d_%d" % (l, pt)) for pt in range(2)] for l in range(L)]

    RW_BYTES = 5 * 4 * TB + 1 * 4 * (TB + 1) + 2 * 4 * TB + 4 * 4 * TB + 16 * TB + NT * 1024 + NT * 512 + NT * 1024 \
        + 8 * 1024 + 8 * 1024 + 4 * 512 + 12 * 256 + 8 * 512
    FF_BYTES = 22 * TB * 2 + NT * D * 4 + 8 * TB * 2
    RW_BYTES = 5 * 4 * TB + 4 * (TB + 1) + 2 * 4 * TB + 4 * 2 * TB + 8 * TB + NT * 512 + 2 * 128 + NT * 512 + NT * 1024 \
        + 8 * 512 + 8 * 512 + 4 * 256 + 8 * 128 + 4 * 256 + 8 * 256
    ar_ = Arena(c, max(RW_BYTES, FF_BYTES) + 64, "arena")
    off = [0]

    def av(shape, dt, name):
        esz = 4 if dt == F32 else 2
        n = esz
        for s in shape[1:]:
            n *= s
        v = ar_.view(off[0], shape, dt, name)
        off[0] += n
        return v
    rw_sh = [av([128, TB], F32, "rwsh%d" % i) for i in range(5)]
    rw_raw = Ring([av([128, TB + 1], F32, "rwraw%d" % i) for i in range(1)])
    th_t = av([128, TB], F32, "th")
    lvs_t = av([128, TB], F32, "lvs")
    rw_kt = [av([128, TB], BF16, "rw_kt%d" % i) for i in range(2)]
    rw_bt = [av([128, TB], BF16, "rw_bt%d" % i) for i in range(2)]
    rw_ar = [av([128, 2, TB], BF16, "rw_ar%d" % i) for i in range(2)]
    rw_vtok = av([128, NT, 256], BF16, "rw_vtok")
    zbf = [av([128, 64], BF16, "zbf%d" % i) for i in range(2)]
    rw_gtok = av([128, NT, 256], BF16, "rw_gtok")
    o_acc = av([128, NT, 256], F32, "o_acc")
    a_ring = Ring([av([128, 256], BF16, "aring%d" % i) for i in range(8)])
    m_ring = Ring([av([128, 256], BF16, "mring%d" % i) for i in range(8)])
    tok_ring = Ring([av([128, 128], BF16, "tokr%d" % i) for i in range(4)])
    u_ring = Ring([av([128, 64], BF16, "ur%d" % i) for i in range(8)])
    g_ring = Ring([av([128, 64], F32, "gr%d" % i) for i in range(4)])
    TT_ring = Ring([av([128, 128], BF16, "TTr%d" % i) for i in range(8)])
    off[0] = 0
    mT = av([128, 22, TB], BF16, "mT")
    facc = av([128, NT, D], F32, "facc")
    yT = av([128, 8, TB], BF16, "yT")

    out_tt = TT(None, "out_dram")

    pieces = []
    pst = {"issued": 0, "taken": 0}
    free_slots = list(range(NSLOT))
    slot_of = {}

    class Piece:
        __slots__ = ("v", "slot")

    def plan_piece(dap, nk, ncols):
        assert nk * ncols <= SLOTN
        pieces.append((dap, nk, ncols))

    def pump():
        while free_slots and pst["issued"] < len(pieces):
            i = pst["issued"]
            dap, nk, ncols = pieces[i]
            s = free_slots.pop(0)
            dst = wslots[s][:, 0:nk * ncols].re("p (k c) -> p k c", k=nk)
            c.dma("pool", dst, dap)
            p_ = Piece()
            p_.v = dst
            p_.slot = s
            slot_of[i] = p_
            pst["issued"] += 1

    def take_piece():
        pump()
        i = pst["taken"]
        assert i in slot_of, ("weight ring deadlock", i)
        pst["taken"] += 1
        return slot_of.pop(i)

    def release(p_):
        free_slots.append(p_.slot)
        pump()

    def rows_piece(w2d, r0, nk, c0, ncols):
        return w2d[r0 * 128:(r0 + nk) * 128, c0:c0 + ncols].rearrange("(k p) c -> p k c", p=128)

    WIN_PIECES = [(1808, 512), (2320, 512), (2832, 32), (0, 512), (512, 512), (1024, 512), (1536, 272),
                  (2864, 512), (3376, 512)]
    GU_COLS_D = [(0, 512), (512, 512), (1024, 512), (1536, 512), (2048, 512), (2560, 256)]
    GU_COLS_E = [(0, 512), (512, 512), (1024, 384)]

    def plan_layer(l):
        for (c0, n) in WIN_PIECES:
            plan_piece(rows_piece(w_in_d[l], 0, 8, c0, n), 8, n)
        for hf in range(2):
            plan_piece(rows_piece(w_out_d[l], 0, 8, hf * 512, 512), 8, 512)
        if not ffn:
            return
        i = l // 2
        if l % 2 == 0:
            for (c0, n) in GU_COLS_D:
                plan_piece(rows_piece(fg_d[i], 0, 8, c0, n), 8, n)
                plan_piece(rows_piece(fu_d[i], 0, 8, c0, n), 8, n)
            for hf in range(2):
                for (r0, nk) in ((0, 8), (8, 8), (16, 6)):
                    plan_piece(rows_piece(fd_d[i], r0, nk, hf * 512, 512), nk, 512)
        else:
            for e in range(NE):
                for (c0, n) in GU_COLS_E:
                    plan_piece(rows_piece(mg_d[i, e], 0, 8, c0, n), 8, n)
                    plan_piece(rows_piece(mu2_d[i, e], 0, 8, c0, n), 8, n)
                for hf in range(2):
                    for (r0, nk) in ((0, 8), (8, 3)):
                        plan_piece(rows_piece(md_d[i, e], r0, nk, hf * 512, 512), nk, 512)

    for b in range(NBLK):
        for l in range(L):
            plan_layer(l)

    def proj_F(wp, cols, ncols, rhs=None):
        bank = trb.next()
        out = bank[0:ncols, 0:TB]
        for k in range(8):
            c.mm(out, wp.v[:, k, cols:cols + ncols], hT[:, k, :], start=(k == 0), stop=(k == 7))
        return out

    def proj_T(wp, cols, ncols, tile):
        bank = trb.next()
        out = bank[:, 0:ncols]
        for k in range(8):
            c.mm(out, hT[:, k, tile * 128:(tile + 1) * 128], wp.v[:, k, cols:cols + ncols],
                 start=(k == 0), stop=(k == 7))
        return out

    def cumdecay(g, sgn_scale, cum, eq, ek):
        c.scan(cum, K("reset")[:, 0:TB], g, 0.0, ALU.mult, ALU.add)
        c.act(eq, cum, AF.Exp, scale=sgn_scale)
        c.act(ek, cum, AF.Exp, scale=-sgn_scale)

    def chunk_end(v):
        return v.re("p (n c) -> p n c", c=C)[:, :, C - 1]

    def rstd_of(dst, src, eps_col):
        c.act(dst, src, AF.Sqrt, bias=epsc[:, eps_col:eps_col + 1])
        c.recip(dst, dst)

    sc_ring = Ring([c.sb([128, 128], BF16, "sc%d" % i) for i in range(4)])
    kt_ring = Ring([c.sb([128, 128], BF16, "kt%d" % i) for i in range(2)])
    md_ring = Ring([c.sb([128, 128], F32, "md%d" % i) for i in range(2)])

    def gla_tile(l, t, qT, kT, dcs, states, KD, nh_tile):
        tok = slice(t * 128, (t + 1) * 128)
        o_bank = accb.next()
        o_ps = o_bank[:, 0:256]
        first = [True]
        W = nh_tile * 64
        NP = nh_tile * KD
        for pt in range(len(qT)):
            S, Sb = states[pt][0][l][0:NP, :], states[pt][1][0:NP, :]
            if t == 0:
                c.copy(Sb, S, eng="act")
            ktp = trb.next()
            ktv = V(ktp, ktp.t[:, 0:NP // 2].bitcast(BF16))
            c.tr(ktv, kT[pt][:, tok], ident_bf[0:NP, 0:NP])
            ktok = kt_ring.next()[:, 0:NP]
            c.copy(ktok, ktv, eng="act")
            scs = []
            for hh in range(nh_tile):
                pr = slice(hh * KD, (hh + 1) * KD)
                sp_ = trb.next()
                c.mm(sp_[:, 0:128], kT[pt][pr, tok], qT[pt][pr, tok])
                sc = sc_ring.next()[:]
                c.tt(sc, sp_[:, 0:128], mask_bf[:], ALU.mult)
                scs.append(sc)
            yield
            for hh in range(nh_tile):
                hg = pt * nh_tile + hh
                c.mm(o_ps[:, hg * 64:(hg + 1) * 64], scs[hh], vt[:, t, hg * 64:(hg + 1) * 64],
                     start=first[0], stop=False, sg=True)
                first[0] = False
            for ch in range(2):
                cg = t * 2 + ch
                rows = slice(ch * 64, (ch + 1) * 64)
                ctok = slice(t * 128 + ch * 64, t * 128 + (ch + 1) * 64)
                for hh in range(nh_tile):
                    hg = pt * nh_tile + hh
                    pr = slice(hh * KD, (hh + 1) * KD)
                    c.mm(o_ps[rows, hg * 64:(hg + 1) * 64], qT[pt][pr, ctok], Sb[pr, hh * 64:(hh + 1) * 64],
                         start=False, stop=True, sg=True)
                mp = trb.next()
                c.mm(mp[0:NP, 0:W], ktok[rows, :], vt[rows, t, pt * W:(pt + 1) * W])
                md = md_ring.next()[0:NP, 0:W]
                c.act(md, mp[0:NP, 0:W], AF.Copy, scale=dcs[pt][:, cg:cg + 1])
                c.stt(S, S, dcs[pt][:, cg:cg + 1], md, ALU.mult, ALU.add)
                c.copy(Sb, S, eng="act")
                yield
        return o_ps

    def finish_simple(l, t, o_ps, mix_idx, kind, gam_off):
        o = o_sb[:]
        c.copy(o, o_ps, eng="act")
        o3 = o.re("p (h v) -> p h v", h=4)
        sq = sq_sb[:]
        msum = stat_mx[:, 0:4]
        ssum = stat_mx[:, 4:8]
        rstd = stat_mx[:, 8:12]
        if kind == "gn":
            c.reduce(msum, o3, ALU.add)
            c.ts(msum, msum, 1.0 / 64.0, ALU.mult)
            c.tt(o3, o3, msum.re("p (h o) -> p h o", o=1).bc([128, 4, 64]), ALU.subtract)
        c.act(sq, o, AF.Square)
        c.reduce(ssum, sq.re("p (h v) -> p h v", h=4), ALU.add)
        c.ts(ssum, ssum, 1.0 / 64.0, ALU.mult)
        rstd_of(rstd, ssum, 1 if kind == "rms" else 0)
        c.tt(o3, o3, rstd.re("p (h o) -> p h o", o=1).bc([128, 4, 64]), ALU.mult)
        if gam_off is not None:
            gm = rb_small[:, gam_off:gam_off + 64]
            c.tt(o3, o3, gm.re("p (o v) -> p o v", o=1).bc([128, 4, 64]), ALU.mult)
        c.tt(ytok[:, t, mix_idx * 256:(mix_idx + 1) * 256], o, gt[:, t, :], ALU.mult)

    def hgrn(l, wp0, wp1):
        for pt in range(2):
            qps = proj_F(wp0, pt * 128, 128)
            qs = P[0][:]
            c.act(qs, qps, AF.Silu)
            fps = proj_F(wp0, 256 + pt * 128, 128)
            s = P[1][:]
            c.act(s, fps, AF.Sigmoid)
            yield
            f = P[2][:]
            c.ts(f, s, olb[:, l, pt:pt + 1], ALU.mult, lb[:, l, pt:pt + 1], ALU.add)
            k = P[3][:]
            c.ts(k, s, nolb[:, l, pt:pt + 1], ALU.mult, olb[:, l, pt:pt + 1], ALU.add)
            g = P[4][:]
            c.act(g, f, AF.Ln)
            cum, eq, ek = P[5][:], P[6][:], P[7][:]
            cumdecay(g, 1.0, cum, eq, ek)
            c.tt(qk[0 + pt][:], qs, eq, ALU.mult)
            c.tt(qk[2 + pt][:], k, ek, ALU.mult)
            c.copy(decs[pt][:], chunk_end(eq))
            yield
        for t in range(NT):
            ps_ = proj_T(wp1, 0, 512, t)
            c.copy(vt[:, t, :], ps_[:, 0:256], eng="act")
            c.act(gt[:, t, :], ps_[:, 256:512], AF.Silu)
            yield
        release(wp0)
        release(wp1)
        for t in range(NT):
            o_ps = yield from gla_tile(l, t, [qk[0][:], qk[1][:]], [qk[2][:], qk[3][:]], [decs[0][:], decs[1][:]],
                                       st_hg, 64, 2)
            finish_simple(l, t, o_ps, 0, "rms", 0)
            yield

    def gla(l, wp2, wp3):
        gps = proj_F(wp3, 0, 16)
        glr = P[2]
        c.copy(glr[0:16, :], gps, eng="act")
        for pt in range(2):
            qps = proj_F(wp2, pt * 64, 64)
            qs = P[0][0:64, :]
            c.act(qs, qps, AF.Copy, scale=32.0 ** -0.5)
            kps = proj_F(wp2, 128 + pt * 64, 64)
            ks = P[1][0:64, :]
            c.copy(ks, kps, eng="act")
            yield
            bank = trb.next()
            zps = bank[0:64, 0:TB]
            c.mm(zps, gw2[:, pt * 64:(pt + 1) * 64], glr[0:16, :])
            e = P[3][0:64, :]
            c.act(e, zps, AF.Exp, bias=ngb2[:, l, pt:pt + 1], scale=-1.0)
            sp_ = P[4][0:64, :]
            c.act(sp_, e, AF.Ln, bias=epsc[0:64, 3:4])
            cum, eq, ek = P[5][0:64, :], P[6][0:64, :], P[7][0:64, :]
            c.scan(cum, K("reset")[0:64, 0:TB], sp_, 0.0, ALU.mult, ALU.add)
            c.act(eq, cum, AF.Exp, scale=-1.0 / 16.0)
            c.act(ek, cum, AF.Exp, scale=1.0 / 16.0)
            c.tt(qk[0 + pt][0:64, :], qs, eq, ALU.mult)
            c.tt(qk[2 + pt][0:64, :], ks, ek, ALU.mult)
            c.copy(decs[pt][0:64, :], chunk_end(eq))
            yield
        for t in range(NT):
            ps_ = proj_T(wp2, 256, 256, t)
            c.copy(vt[:, t, :], ps_, eng="act")
            ps2 = proj_T(wp3, 16, 256, t)
            c.act(gt[:, t, :], ps2, AF.Silu)
            yield
        release(wp2)
        release(wp3)
        for t in range(NT):
            o_ps = yield from gla_tile(l, t, [qk[0][0:64, :], qk[1][0:64, :]], [qk[2][0:64, :], qk[3][0:64, :]],
                                       [decs[0][0:64, :], decs[1][0:64, :]], st_gl, 32, 2)
            finish_simple(l, t, o_ps, 1, "rms", 64)
            yield

    def rope_tables(b):
        posi = V(P[0], P[0].t[:].bitcast(I32))
        c.dma("sp", posi, pos_d[0:1, b * TB:(b + 1) * TB].partition_broadcast(128))
        posf = P[5]
        c.copy(posf[:], posi)
        ang = P[1][:]
        c.ts(ang, posf[:], K("invfreq"), ALU.mult)
        kq = P[2][:]
        c.ts(kq, ang, float(1.0 / (2.0 * np.pi)), ALU.mult, 12582912.0, ALU.add)
        c.ts(kq, kq, -12582912.0, ALU.add)
        r = P[3][:]
        C1 = 6.28125
        C2 = float(2.0 * np.pi - 6.28125)
        c.stt(r, kq, -C1, ang, ALU.mult, ALU.add)
        c.stt(r, kq, -C2, r, ALU.mult, ALU.add)
        c.ts(r, r, 3.14159, ALU.min, -3.14159, ALU.max)
        c.act(sinT[:], r, AF.Sin)
        ab = P[4][:]
        c.ts(ab, r, -1.0, ALU.mult)
        c.tt(ab, ab, r, ALU.max)
        c.ts(ab, ab, -1.0, ALU.mult, float(np.pi / 2.0), ALU.add)
        c.act(cosT[:], ab, AF.Sin)
        c.ts(sinT[:], sinT[:], K("sinsign"), ALU.mult)

    def ret(l, wp6, wp7):
        for which in range(2):
            for pt in range(2):
                ps_ = proj_F(wp6, which * 256 + pt * 128, 128)
                xs = P[0][:]
                c.copy(xs, ps_, eng="act")
                a = P[1][:]
                c.tt(a, xs, cosT[:], ALU.mult)
                bsw = P[2][:]
                for hh in range(2):
                    lo = slice(hh * 64, hh * 64 + 32)
                    hi = slice(hh * 64 + 32, hh * 64 + 64)
                    c.tt(bsw[lo, :], xs[hi, :], sinT[hi, :], ALU.mult)
                    c.tt(bsw[hi, :], xs[lo, :], sinT[lo, :], ALU.mult)
                c.tt(a, a, bsw, ALU.add)
                tab = K("ret_eq" if which == 0 else "ret_ek")[:, pt * C:(pt + 1) * C]
                dst = qk[which * 2 + pt]
                c.tt(dst[:].re("p (n c) -> p n c", c=C), a.re("p (n c) -> p n c", c=C),
                     tab.re("p (o c) -> p o c", o=1).bc([128, NCH, C]), ALU.mult)
                yield
        for t in range(NT):
            ps_ = proj_T(wp7, 0, 512, t)
            c.copy(vt[:, t, :], ps_[:, 0:256], eng="act")
            c.act(gt[:, t, :], ps_[:, 256:512], AF.Silu)
            yield
        release(wp6)
        release(wp7)
        for t in range(NT):
            o_ps = yield from gla_tile(l, t, [qk[0][:], qk[1][:]], [qk[2][:], qk[3][:]],
                                       [dec_rt[0][:], dec_rt[1][:]], st_rt, 64, 2)
            finish_simple(l, t, o_ps, 3, "gn", None)
            yield

    def rw_shift(l, b, i, wp, c0, n, dst, tm):
        ps_ = proj_F(wp, c0, n)
        raw = rw_raw.next()
        if b == 0:
            c.memset(raw[0:n, 0:1], 0.0)
        else:
            c.copy(raw[0:n, 0:1], rw_carry[0:n, l, i:i + 1])
        c.copy(raw[0:n, 1:TB + 1], ps_, eng="act")
        c.copy(rw_carry[0:n, l, i:i + 1], raw[0:n, TB:TB + 1])
        c.act(tm[0:n, :], raw[0:n, 1:TB + 1], AF.Copy, scale=omu[0:n, l, i:i + 1])
        c.stt(dst[0:n, :], raw[0:n, 0:TB], mup[0:n, l, i:i + 1], tm[0:n, :], ALU.mult, ALU.add)

    def rwkv(l, b, wp4, wp5, wp5b, rel):
        vS = [rw_sh[0], rw_sh[1]]
        waS, rS, kS = rw_sh[2], rw_sh[3], rw_sh[4]
        rw_shift(l, b, 6, wp5, 256, 128, waS, R[2])
        g0S, g1S = R[0], R[1]
        rw_shift(l, b, 7, wp5, 384, 128, g0S, R[2])
        rw_shift(l, b, 8, wp5b, 0, 32, g1S, R[2])
        yield
        rw_shift(l, b, 4, wp5, 0, 128, vS[0], R[2])
        rw_shift(l, b, 5, wp5, 128, 128, vS[1], R[2])
        release(wp5)
        release(wp5b)
        rel["done"] = True
        yield
        c.act(th_t[0:64, :], waS[0:64, :], AF.Tanh)
        c.act(g0S[:], g0S[:], AF.Sigmoid)
        c.act(g1S[0:32, :], g1S[0:32, :], AF.Sigmoid)
        for t in range(NT):
            bank = trb.next()
            c.mm(bank[:, 0:256], g0S[:, t * 128:(t + 1) * 128], g2a[:], start=True, stop=False)
            c.mm(bank[:, 0:256], g1S[0:32, t * 128:(t + 1) * 128], g2b[:], start=False, stop=True)
            c.copy(rw_gtok[:, t, :], bank[:, 0:256], eng="act")
            yield
        if l > 0:
            bank = trb.next()
            lv = bank[0:32, 0:TB]
            for k in range(2):
                c.mm(lv, v1s[:, k, :], vS[k][:], start=(k == 0), stop=(k == 1))
            c.copy(lvs_t[0:32, :], lv, eng="act")
        for pt in range(2):
            if l == 0:
                c.copy(vfirst[:, pt, :], vS[pt][:])
            else:
                bank = trb.next()
                c.mm(bank[:, 0:TB], v2s[:, pt * 128:(pt + 1) * 128], lvs_t[0:32, :])
                sgm = R[2][:]
                c.act(sgm, bank[:, 0:TB], AF.Sigmoid, bias=v0p[:, l - 1, pt:pt + 1])
                dv = R[3][:]
                c.tt(dv, vfirst[:, pt, :], vS[pt][:], ALU.subtract)
                c.tt(dv, dv, sgm, ALU.mult)
                c.tt(vS[pt][:], vS[pt][:], dv, ALU.add)
                yield
            for t in range(NT):
                bank = trb.next()
                c.tr(bank[:, 0:128], vS[pt][:, t * 128:(t + 1) * 128], K("ident"))
                c.copy(rw_vtok[:, t, pt * 128:(pt + 1) * 128], bank[:, 0:128], eng="act")
            yield
        for pt in range(2):
            rw_shift(l, b, 0 + pt, wp4, pt * 128, 128, rS, R[0])
            rw_shift(l, b, 2 + pt, wp4, 256 + pt * 128, 128, kS, R[0])
            if pt == 1:
                release(wp4)
            bank = trb.next()
            c.mm(bank[:, 0:TB], w2s[:, pt * 128:(pt + 1) * 128], th_t[0:64, :])
            t1 = R[2][:]
            c.act(t1, bank[:, 0:TB], AF.Exp, bias=nw0[:, l, pt:pt + 1], scale=-1.0)
            c.act(t1, t1, AF.Ln, bias=epsc[:, 3:4])
            c.ts(t1, t1, -1.0, ALU.mult, -0.5, ALU.add)
            c.act(t1, t1, AF.Exp)
            g = R[3][:]
            c.ts(g, t1, -1.0, ALU.mult)
            cum, eq, ek = R[4][:], R[5][:], R[6][:]
            cumdecay(g, 1.0, cum, eq, ek)
            c.copy(rw_dec[pt][:], chunk_end(eq))
            yield
            bank = trb.next()
            c.mm(bank[:, 0:TB], a2s[64:128, pt * 128:(pt + 1) * 128], waS[64:128, :])
            ag = R[7][:]
            c.act(ag, bank[:, 0:TB], AF.Sigmoid, bias=a0[:, l, pt:pt + 1])
            kk = R[8][:]
            c.ts(kk, kS[:], kkp[:, l, pt:pt + 1], ALU.mult)
            nrm = R[9][:]
            c.act(nrm, kk, AF.Square)
            bank = trb.next()
            c.mm(bank[:, 0:TB], K("blockones"), nrm)
            c.act(nrm, bank[:, 0:TB], AF.Sqrt)
            c.ts(nrm, nrm, 1e-12, ALU.max)
            c.act(nrm, nrm, AF.Ln)
            c.act(nrm, nrm, AF.Exp, scale=-1.0)
            c.tt(kk, kk, nrm, ALU.mult)
            yield
            fk = R[9][:]
            c.ts(fk, ag, -1.0, ALU.add, kap[:, l, pt:pt + 1], ALU.mult)
            c.ts(fk, fk, 1.0, ALU.add)
            km = R[10][:]
            c.tt(km, kS[:], fk, ALU.mult)
            bo = R[2][:]
            c.stt(bo, rS[:], rkp[:, l, pt:pt + 1], km, ALU.mult, ALU.mult)
            for t in range(NT):
                bank = trb.next()
                c.mm(bank[:, 0:2], bo[:, t * 128:(t + 1) * 128], K("headsel"))
                c.copy(rw_bonus[:, t, pt * 2:pt * 2 + 2], bank[:, 0:2], eng="act")
            yield
            c.tt(rw_ar[pt][:, 1, :], rS[:], eq, ALU.mult)
            c.tt(rw_kt[pt][:], km, ek, ALU.mult)
            bb = R[9][:]
            c.tt(bb, kk, ag, ALU.mult)
            c.tt(rw_bt[pt][:], bb, ek, ALU.mult)
            yield
            ex = R[9][:]
            c.tt(ex, cum, g, ALU.subtract)
            c.act(ex, ex, AF.Exp)
            c.stt(rw_ar[pt][:, 0, :], kk, -1.0, ex, ALU.mult, ALU.mult)


    F32R = mybir.dt.float32r

    def R32(v):
        return V(v.tt, v.ap.bitcast(F32R))

    def rwkv_core(l, t, pt, hh, sidx):
        h = pt * 2 + hh
        ev = ("act", "dve") if sidx % 2 == 0 else ("dve", "act")
        tok = slice(t * 128, (t + 1) * 128)
        pr = slice(hh * 64, hh * 64 + 64)
        ar = rw_ar[pt][pr, :, tok]
        kt = rw_kt[pt][pr, tok]
        bt = rw_bt[pt][pr, tok]
        at = rw_ar[pt][pr, 0, tok]
        rt = rw_ar[pt][pr, 1, tok]
        Z = zst[l][pt]
        Zb = zbf[pt]
        if t == 0:
            c.copy(Zb[pr, :], Z[pr, :], eng=ev[1])
        vtk = rw_vtok[:, t, h * 64:(h + 1) * 64]
        ocol = slice(h * 64, (h + 1) * 64)
        b1 = trb.next()
        c.mm(b1[:, 0:256].re("p (a n) -> p a n", a=2), kt, ar)
        Ak = a_ring.next()[:]
        c.tt(Ak[:, 0:128], b1[:, 0:128], K("mask_strict"), ALU.mult)
        c.tt(Ak[:, 128:256], b1[:, 128:256], K("mask_incl"), ALU.mult)
        b2 = trb.next()
        c.mm(b2[:, 0:256].re("p (a n) -> p a n", a=2), bt, ar)
        Ab = a_ring.next()[:]
        c.tt(Ab[:, 0:128], b2[:, 0:128], K("mask_strict"), ALU.mult)
        c.tt(Ab[:, 128:256], b2[:, 128:256], K("mask_incl"), ALU.mult)
        b3 = trb.next()
        c.mm(b3[:, 0:128], at, bt)
        pq = m_ring.next()[:]
        Pm = pq[:, 0:128]
        c.tt(Pm, b3[:, 0:128], K("mask_strictT"), ALU.mult)
        Q = Ab[:, 0:128]
        Tc = TT_ring.next()[:]
        c.tt(Tc, Q, K("ident"), ALU.add)
        yield
        for i in range(1, 6):
            bpq = trb.next()
            c.mm(bpq[:, 0:128], Q, Pm)
            if i < 5:
                c.mm(bpq[:, 128:256], Pm, Q)
            pq = m_ring.next()[:]
            if i < 5:
                c.copy(pq, bpq[:, 0:256], eng=ev[0])
            else:
                c.copy(pq[:, 0:128], bpq[:, 0:128], eng=ev[0])
            Pm = pq[:, 0:128]
            Q = pq[:, 128:256]
            yield
            bt_ = trb.next()
            c.mm(bt_[:, 0:128], Pm, Tc)
            Tn = TT_ring.next()[:]
            c.tt(Tn, Tc, bt_[:, 0:128], ALU.add)
            Tc = Tn
            yield
        bk = trb.next()
        bkv = V(bk, bk.t[:, 0:64].bitcast(BF16))
        c.tr(bkv[:, 0:64], kt, ident_bf[pr, pr])
        c.tr(bkv[:, 64:128], bt, ident_bf[pr, pr])
        kb_tok = tok_ring.next()[:]
        c.copy(kb_tok, bkv, eng=ev[0])
        gb_ = trb.next()
        c.mm(gb_[:, 0:64], Ak[:, 0:128], vtk)
        utok = u_ring.next()[:]
        rhs_u = u_ring.next()[:]
        G_sb = g_ring.next()[:]
        c.copy(G_sb, gb_[:, 0:64], eng=ev[1])
        yield
        for ch in range(2):
            rows = slice(ch * 64, (ch + 1) * 64)
            cg = t * 2 + ch
            gz = trb.next()
            c.mm(gz[rows, 0:64], at[:, rows], Zb[pr, :])
            c.tt(rhs_u[rows, :], gz[rows, 0:64], G_sb[rows, :], ALU.add)
            ob = trb.next()
            c.mm(ob[rows, 0:64], rt[:, rows], Zb[pr, :])
            c.copy(o_acc[rows, t, ocol], ob[rows, 0:64], eng=ev[0])
            yield
            ub = trb.next()
            c.mm(ub[rows, 0:64], Tc[rows, rows], rhs_u[rows, :])
            c.copy(utok[rows, :], ub[rows, 0:64], eng=ev[0])
            yield
            zb = trb.next()
            c.mm(zb[pr, 0:64], K("identZ")[pr, :], Z[pr, :], start=True, stop=False, sg=True)
            c.mm(zb[pr, 0:64], kb_tok[rows, 0:64], vtk[rows, :], start=False, stop=False, sg=True)
            c.mm(zb[pr, 0:64], kb_tok[rows, 64:128], utok[rows, :], start=False, stop=True, sg=True)
            c.act(Z[pr, :], zb[pr, 0:64], AF.Copy, scale=rw_dec[pt][pr, cg:cg + 1])
            c.copy(Zb[pr, :], Z[pr, :], eng=ev[1])
            yield
        ob2 = trb.next()
        c.mm(ob2[:, 0:64], Ak[:, 128:256], vtk, start=True, stop=False)
        c.mm(ob2[:, 0:64], Ab[:, 128:256], utok, start=False, stop=True)
        c.tt(o_acc[:, t, ocol], o_acc[:, t, ocol], ob2[:, 0:64], ALU.add)

    def rwkv_finish(l, t):
        o = o_sb[:]
        c.copy(o, o_acc[:, t, :])
        o3 = o.re("p (h v) -> p h v", h=4)
        msum = stat_mx[:, 0:4]
        ssum = stat_mx[:, 4:8]
        rstd = stat_mx[:, 8:12]
        c.reduce(msum, o3, ALU.add)
        c.ts(msum, msum, 1.0 / 64.0, ALU.mult)
        c.tt(o3, o3, msum.re("p (h o) -> p h o", o=1).bc([128, 4, 64]), ALU.subtract)
        sq = sq_sb[:]
        c.act(sq, o, AF.Square)
        c.reduce(ssum, sq.re("p (h v) -> p h v", h=4), ALU.add)
        c.ts(ssum, ssum, 1.0 / 64.0, ALU.mult)
        rstd_of(rstd, ssum, 2)
        c.tt(o3, o3, rstd.re("p (h o) -> p h o", o=1).bc([128, 4, 64]), ALU.mult)
        c.tt(o, o, rb_small[:, 128:384], ALU.mult)
        c.tt(o, o, rb_small[:, 384:640], ALU.add)
        c.tt(sq.re("p (h v) -> p h v", h=4), rw_vtok[:, t, :].re("p (h v) -> p h v", h=4),
             rw_bonus[:, t, :].re("p (h o) -> p h o", o=1).bc([128, 4, 64]), ALU.mult)
        c.tt(o, o, sq, ALU.add)
        c.tt(ytok[:, t, 512:768], o, rw_gtok[:, t, :], ALU.mult)

    def others(l):
        wp0, wp1 = take_piece(), take_piece()
        if mixers[0]:
            yield from hgrn(l, wp0, wp1)
        else:
            release(wp0)
            release(wp1)
        wp2, wp3 = take_piece(), take_piece()
        if mixers[1]:
            yield from gla(l, wp2, wp3)
        else:
            release(wp2)
            release(wp3)
        wp6, wp7 = take_piece(), take_piece()
        if mixers[3]:
            yield from ret(l, wp6, wp7)
        else:
            release(wp6)
            release(wp7)

    def to_hT(t):
        for hf in range(2):
            bank = trb.next()
            for k4 in range(4):
                k = hf * 4 + k4
                c.tr(bank[:, k4 * 128:(k4 + 1) * 128], htok[:, t, k * 128:(k + 1) * 128], K("ident"))
            c.copy(hT[:, hf * 4:(hf + 1) * 4, t * 128:(t + 1) * 128],
                   bank[:, 0:512].re("p (k n) -> p k n", k=4), eng="act")

    def layer_norm(l, which):
        for hf in range(2):
            c.dma("sp", P[4 + hf][:], ln_d[which + "_g"][l:l + 1, hf * 512:(hf + 1) * 512].partition_broadcast(128))
            c.dma("sp", P[6 + hf][:], ln_d[which + "_b"][l:l + 1, hf * 512:(hf + 1) * 512].partition_broadcast(128))
        for t in range(NT):
            z = htok[:, t, :]
            st6 = stat_ln[:, 0:12]
            for hf in range(2):
                zz = z[:, hf * 512:(hf + 1) * 512]
                dst = stat_ln[:, hf * 6:(hf + 1) * 6]
                c.op("dve", lambda zz=zz, dst=dst: nc.vector.bn_stats(dst.ap, zz.ap), reads=[zz], writes=[dst])
            mv = stat_ln[:, 12:14]
            c.op("dve", lambda: nc.vector.bn_aggr(mv.ap, st6.ap), reads=[st6], writes=[mv])
            rs = stat_ln[:, 14:15]
            rstd_of(rs, stat_ln[:, 13:14], 0)
            c.ts(z, z, stat_ln[:, 12:13], ALU.subtract, rs, ALU.mult)
            for hf in range(2):
                zz = z[:, hf * 512:(hf + 1) * 512]
                c.tt(zz, zz, P[4 + hf][:], ALU.mult)
                c.tt(zz, zz, P[6 + hf][:], ALU.add)
            to_hT(t)

    def residual_from_bank(t, hf, bank_v):
        z = htok[:, t, hf * 512:(hf + 1) * 512]
        c.stt(z, z, ALPHA, bank_v, ALU.mult, ALU.add)

    def gate_up(cols_list):
        ft = 0
        for (c0, n) in cols_list:
            wg = take_piece()
            wu = take_piece()
            for j in range(n // 128):
                gps = proj_F(wg, j * 128, 128)
                ups = proj_F(wu, j * 128, 128)
                sl = P[8 + ft % 3][:]
                c.act(sl, gps, AF.Silu)
                c.tt(mT[:, ft, :], sl, ups, ALU.mult)
                ft += 1
            release(wg)
            release(wu)
        return ft

    def down_proj(nft, row_groups, consume):
        for hf in range(2):
            accs = [accb.next() for _ in range(NT)]
            for gi, (r0, nk) in enumerate(row_groups):
                wd = take_piece()
                if gi < len(row_groups) - 1:
                    for kk_ in range(nk):
                        ft = r0 + kk_
                        for t in range(NT):
                            c.mm(accs[t][:, 0:512], mT[:, ft, t * 128:(t + 1) * 128], wd.v[:, kk_, :],
                                 start=(ft == 0), stop=(ft == nft - 1))
                else:
                    for t in range(NT):
                        for kk_ in range(nk):
                            ft = r0 + kk_
                            c.mm(accs[t][:, 0:512], mT[:, ft, t * 128:(t + 1) * 128], wd.v[:, kk_, :],
                                 start=(ft == 0), stop=(ft == nft - 1))
                        consume(t, hf, accs[t][:, 0:512])
                release(wd)

    def ffn_dense(l):
        gate_up(GU_COLS_D)
        down_proj(22, ((0, 8), (8, 8), (16, 6)), residual_from_bank)

    def moe(l):
        i = l // 2
        for t in range(NT):
            bank = trb.next()
            lg = bank[:, 0:NE]
            for k in range(8):
                c.mm(lg, hT[:, k, t * 128:(t + 1) * 128], mrs_bf[:, i, k, :], start=(k == 0), stop=(k == 7))
            lgs = stat_moe[:, 0:8]
            c.copy(lgs, lg)
            m8 = stat_moe[:, 8:16]
            c.op("dve", lambda: nc.vector.max(m8.ap, lgs.ap), reads=[lgs], writes=[m8])
            nm1 = stat_moe[:, 32:33]
            c.ts(nm1, m8[:, 0:1], -1.0, ALU.mult)
            ex = stat_moe[:, 16:24]
            c.act(ex, lgs, AF.Exp, bias=nm1)
            sel = stat_moe[:, 24:32]
            c.ts(sel, lgs, m8[:, 1:2], ALU.is_ge)
            c.tt(ex, ex, sel, ALU.mult)
            ssum = stat_moe[:, 33:34]
            c.reduce(ssum, ex, ALU.add)
            c.recip(ssum, ssum)
            c.ts(gates[:, t, :], ex, ssum, ALU.mult)
        c.memset(facc[:], 0.0)
        for e in range(NE):
            gate_up(GU_COLS_E)

            def cons(t, hf, bank_v, e=e):
                fa = facc[:, t, hf * 512:(hf + 1) * 512]
                c.stt(fa, bank_v, gates[:, t, e:e + 1], fa, ALU.mult, ALU.add)
            down_proj(11, ((0, 8), (8, 3)), cons)
        for t in range(NT):
            z = htok[:, t, :]
            c.stt(z, z, ALPHA, facc[:, t, :], ALU.mult, ALU.add)

    for l in range(L):
        for pt in range(2):
            for st in (st_hg, st_rt, st_gl):
                c.memset(st[pt][0][l][:], 0.0)
            c.memset(zst[l][pt][:], 0.0)
    c.memset(ytok[:], 0.0)

    for b in range(NBLK):
        for t in range(NT):
            r0 = b * TB + t * 128
            c.dma("sp", htok[:, t, :], x_d[r0:r0 + 128, :])
            to_hT(t)
        if mixers[3]:
            rope_tables(b)
        for l in range(L):
            load_layer_params(l)
            psum_mode(True)
            wp4, wp5, wp5b = take_piece(), take_piece(), take_piece()
            rel = {"done": False}
            rp = None
            if mixers[2]:
                rp = rwkv(l, b, wp4, wp5, wp5b, rel)
                while not rel["done"]:
                    next(rp)
            else:
                release(wp4)
                release(wp5)
                release(wp5b)
            og = others(l)
            og_live = [True]

            def og_step():
                if og_live[0]:
                    try:
                        next(og)
                    except StopIteration:
                        og_live[0] = False
            if mixers[2]:
                rp_live = True
                while rp_live:
                    try:
                        next(rp)
                    except StopIteration:
                        rp_live = False
                    og_step()
                for t in range(NT):
                    live = [rwkv_core(l, t, h // 2, h % 2, h) for h in range(4)]
                    while live:
                        nxt = []
                        for g_ in live:
                            try:
                                next(g_)
                                nxt.append(g_)
                            except StopIteration:
                                pass
                        live = nxt
                        og_step()
                    rwkv_finish(l, t)
            while og_live[0]:
                og_step()
            psum_mode(False)
            for t in range(NT):
                for hf in range(2):
                    bank = trb.next()
                    bv = V(bank, bank.t[:, 0:256].bitcast(BF16))
                    for k4 in range(4):
                        k = hf * 4 + k4
                        c.tr(bv[:, k4 * 128:(k4 + 1) * 128], ytok[:, t, k * 128:(k + 1) * 128], ident_bf[:])
                    c.copy(yT[:, hf * 4:(hf + 1) * 4, t * 128:(t + 1) * 128],
                           bv.re("p (k n) -> p k n", k=4), eng="act")
            wo = [take_piece(), take_piece()]
            for t in range(NT):
                for hf in range(2):
                    bank = trb.next()
                    for k in range(8):
                        c.mm(bank[:, 0:512], yT[:, k, t * 128:(t + 1) * 128], wo[hf].v[:, k, :],
                             start=(k == 0), stop=(k == 7))
                    residual_from_bank(t, hf, bank[:, 0:512])
            release(wo[0])
            release(wo[1])
            layer_norm(l, "ln1")
            if ffn:
                if l % 2 == 0:
                    ffn_dense(l)
                else:
                    moe(l)
                layer_norm(l, "ln2")
        for t in range(NT):
            r0 = b * TB + t * 128
            c.dma("sp", V(out_tt, out_d[r0:r0 + 128, :]), htok[:, t, :])
    c.wait_all("sp", [out_tt])
    assert pst["taken"] == len(pieces), (pst, len(pieces))
    return nc, carr, c


_CACHE = {}

NAMES = ["w_in", "w_out", "ln1_g", "ln1_b", "ln2_g", "ln2_b", "hgrn_lb_logits", "hgrn_norm_g", "gla_gate_w2",
         "gla_gate_b", "gla_norm_g", "rwkv_mu", "rwkv_w0", "rwkv_w2", "rwkv_a0", "rwkv_a2", "rwkv_g2", "rwkv_k_k",
         "rwkv_k_a", "rwkv_r_k", "rwkv_lnx_g", "rwkv_lnx_b", "rwkv_v0", "rwkv_v1", "rwkv_v2", "ffn_w_gate",
         "ffn_w_up", "ffn_w_down", "moe_router", "moe_w_gate", "moe_w_up", "moe_w_down"]


def run(inputs, T, L=NL, TB=512, **kw):
    x = np.asarray(inputs["x"], dtype=np.float32)
    B = x.shape[0]
    key = (T, L, TB, tuple(sorted(kw.items())))
    if key not in _CACHE:
        _CACHE[key] = build(T, L, TB, **kw)
    nc, carr, c = _CACHE[key]
    shared = {}
    for n in NAMES:
        a = np.ascontiguousarray(np.asarray(inputs[n], dtype=np.float32))
        if n == "rwkv_r_k":
            a = a.reshape(NL, 256)
        shared[n] = a
    shared["consts"] = carr
    pos = np.asarray(inputs["positions"]).astype(np.int32)
    in_maps = []
    for b in range(B):
        m = dict(shared)
        m["x"] = np.ascontiguousarray(x[b, :T])
        m["positions"] = np.ascontiguousarray(pos[b:b + 1, :T])
        in_maps.append(m)
    res = run_bass_kernel_spmd(nc, in_maps, core_ids=list(range(B)))
    return np.stack([np.asarray(r["out"]) for r in res.results], axis=0)


def kernel(**inputs):
    x = np.asarray(inputs["x"])
    B, S, _ = x.shape
    out = run(inputs, S)
    return out.astype(x.dtype)
```
